# Optimizing a Trainium2 kernel written in Bass

```python
import math
import jax, jax.numpy as jnp
from jax import lax
import numpy as np

D_MODEL = 1024
BATCH = 16
SEQ = 2048
DEPTH = 2

CTX_LEN = 256
GRID_W = 64
EPS = 1e-6

ATT_HEADS = 8
ATT_HEAD_DIM = 64
ATT_V_DIM = 2 * ATT_HEAD_DIM
ATT_QK_WIDTH = ATT_HEADS * 2 * ATT_HEAD_DIM
ATT_WIDTH = ATT_HEADS * ATT_V_DIM
Q_BLOCK = 128
ROPE_BASE = 10000.0

REC_HEADS = 8
REC_KEY_DIM = 128
REC_VAL_DIM = D_MODEL // REC_HEADS
REC_KEY_WIDTH = REC_HEADS * REC_KEY_DIM
REC_WIDTH = REC_HEADS * REC_VAL_DIM
CHUNK = 64

SPLITS = (ATT_QK_WIDTH, ATT_QK_WIDTH, ATT_WIDTH, REC_KEY_WIDTH, REC_KEY_WIDTH, REC_KEY_WIDTH, REC_WIDTH, REC_WIDTH, D_MODEL, D_MODEL)
N_IN = sum(SPLITS)

N_EXPERTS = 16
N_GROUPS = 4
EXPERTS_PER_GROUP = N_EXPERTS // N_GROUPS
TOP_K = 2
D_EXPERT = 1024

kernel_name = 'hybrid_diffattn_hgrn2_groupmoe_dit'


def rmsnorm(x, g):
    xf = x.astype(jnp.float32)
    y = xf * lax.rsqrt(jnp.mean(xf * xf, axis=-1, keepdims=True) + EPS)
    return (y * g.astype(jnp.float32)).astype(x.dtype)


def modulate(x, shift, scale):
    return x * (1 + scale) + shift


def project(h, w):
    outs, start = [], 0
    for width in SPLITS:
        outs.append(h @ w[:, start:start + width])
        start += width
    return outs


def axial_angles(n_tok):
    rows = n_tok // GRID_W
    row = jnp.repeat(jnp.arange(rows, dtype=jnp.float32), GRID_W)
    col = jnp.tile(jnp.arange(GRID_W, dtype=jnp.float32), rows)
    n_freq = ATT_HEAD_DIM // 4
    inv = ROPE_BASE ** (-jnp.arange(n_freq, dtype=jnp.float32) / n_freq)
    return row[:, None] * inv, col[:, None] * inv


def _rotate(x, ang):
    x1, x2 = jnp.split(x, 2, axis=-1)
    cos, sin = jnp.cos(ang), jnp.sin(ang)
    return jnp.concatenate([x1 * cos - x2 * sin, x1 * sin + x2 * cos], axis=-1)


def axial_rope(x, ang_r, ang_c):
    xf = x.astype(jnp.float32)
    xr, xc = jnp.split(xf, 2, axis=-1)
    ar, ac = ang_r[:, None, None, :], ang_c[:, None, None, :]
    return jnp.concatenate([_rotate(xr, ar), _rotate(xc, ac)], axis=-1).astype(x.dtype)


def diff_attend(q, k, v, lam):
    s = jnp.einsum('bhqmd,bhkmd->bhmqk', q.astype(jnp.float32), k.astype(jnp.float32)) * (ATT_HEAD_DIM ** -0.5)
    p = jax.nn.softmax(s, axis=-1)
    w = p[:, :, 0] - lam * p[:, :, 1]
    return jnp.einsum('bhqk,bhkv->bhqv', w, v.astype(jnp.float32))


def diff_attention_latent(q, k, v, lam):
    b, h, n_tok = q.shape[:3]
    nb = n_tok // Q_BLOCK
    qb = q.reshape(b, h, nb, Q_BLOCK, 2, ATT_HEAD_DIM).transpose(2, 0, 1, 3, 4, 5)
    ob = lax.map(lambda blk: diff_attend(blk, k, v, lam), qb)
    return ob.transpose(1, 2, 0, 3, 4).reshape(b, h, n_tok, ATT_V_DIM)


def gla_chunk_scan(q, k, v, log_f, s0):
    b, h, n_tok, _ = q.shape
    dv = v.shape[-1]
    n_chunks = n_tok // CHUNK

    def chunks(a):
        return a.reshape(b, h, n_chunks, CHUNK, a.shape[-1]).transpose(2, 0, 1, 3, 4)

    lower = jnp.tril(jnp.ones((CHUNK, CHUNK), dtype=bool))[:, :, None]

    def step(state, inp):
        qc, kc, vc, gc = inp
        g_cum = jnp.cumsum(gc, axis=2)
        inter = jnp.einsum('bhtd,bhde->bhte', qc * jnp.exp(g_cum), state)
        rel = g_cum[:, :, :, None, :] - g_cum[:, :, None, :, :]
        decay = jnp.exp(jnp.where(lower, rel, -jnp.inf))
        scores = jnp.einsum('bhtd,bhsd,bhtsd->bhts', qc, kc, decay)
        intra = jnp.einsum('bhts,bhse->bhte', scores, vc)
        g_last = g_cum[:, :, -1:, :]
        new_state = (jnp.exp(g_last[:, :, 0, :, None]) * state
                     + jnp.einsum('bhsd,bhse->bhde', kc * jnp.exp(g_last - g_cum), vc))
        return new_state, inter + intra

    s_fin, out = lax.scan(step, s0, (chunks(q), chunks(k), chunks(v), chunks(log_f)))
    return out.transpose(1, 2, 0, 3, 4).reshape(b, h, n_tok, dv), s_fin


def hgrn2_forget(z, lb):
    log_f = jnp.logaddexp(jnp.log(lb), jnp.log1p(-lb) + jax.nn.log_sigmoid(z))
    k = (1.0 - lb) * jax.nn.sigmoid(-z)
    return k, log_f


def bidir_hgrn2(q, k_f, k_b, v, log_f_f, log_f_b, s0_f, s0_b):
    o_f, s_f = gla_chunk_scan(q, k_f, v, log_f_f, s0_f)
    o_b, s_b = gla_chunk_scan(jnp.flip(q, 2), jnp.flip(k_b, 2), jnp.flip(v, 2), jnp.flip(log_f_b, 2), s0_b)
    return o_f + jnp.flip(o_b, 2), s_f, s_b


def merge_branches(o_att, o_rec, rec_gate, gate_att, gate_rec, lam_init, g_subln, g_rec, w_br_a, w_br_r, w_out, dt):
    b, _, n_tok, _ = o_att.shape
    a = rmsnorm(o_att.transpose(0, 2, 1, 3), g_subln) * (1.0 - lam_init)
    a = a.reshape(b, n_tok, ATT_WIDTH).astype(dt)
    r = rmsnorm(o_rec.transpose(0, 2, 1, 3), g_rec).reshape(b, n_tok, REC_WIDTH)
    r = (r * jax.nn.silu(rec_gate.astype(jnp.float32))).astype(dt)
    mixed = jax.nn.sigmoid(gate_att) * (a @ w_br_a) + jax.nn.sigmoid(gate_rec) * (r @ w_br_r)
    return mixed @ w_out


def token_mixers(h, hc, w_in, lam, lam_init, g_subln, lb, g_rec, w_br_a, w_br_r, w_out, ang_r, ang_c, need_ctx):
    b, n_lat, _ = h.shape
    n_ctx = hc.shape[1]
    dt = h.dtype
    aq, ak, av, rq, rff, rfb, ri, rg, ga, gr = project(h, w_in)
    aqc, akc, avc, rqc, rffc, rfbc, ric, rgc, gac, grc = project(hc, w_in)

    def att_qk(a, n):
        return a.reshape(b, n, ATT_HEADS, 2, ATT_HEAD_DIM)

    def att_v(a, n):
        return a.reshape(b, n, ATT_HEADS, ATT_V_DIM).transpose(0, 2, 1, 3)

    q_l = axial_rope(att_qk(aq, n_lat), ang_r, ang_c).transpose(0, 2, 1, 3, 4)
    k_l = axial_rope(att_qk(ak, n_lat), ang_r, ang_c).transpose(0, 2, 1, 3, 4)
    q_c = att_qk(aqc, n_ctx).transpose(0, 2, 1, 3, 4)
    k_c = att_qk(akc, n_ctx).transpose(0, 2, 1, 3, 4)
    v_l, v_c = att_v(av, n_lat), att_v(avc, n_ctx)
    k_all = jnp.concatenate([k_c, k_l], axis=2)
    v_all = jnp.concatenate([v_c, v_l], axis=2)
    o_att = diff_attention_latent(q_l, k_all, v_all, lam)

    def rec(a, n, dim):
        return a.reshape(b, n, REC_HEADS, dim).transpose(0, 2, 1, 3).astype(jnp.float32)

    lb = lb.reshape(REC_HEADS, 1, REC_KEY_DIM)
    kcf, gcf = hgrn2_forget(rec(rffc, n_ctx, REC_KEY_DIM), lb)
    kcb, gcb = hgrn2_forget(rec(rfbc, n_ctx, REC_KEY_DIM), lb)
    s0 = jnp.zeros((b, REC_HEADS, REC_KEY_DIM, REC_VAL_DIM), jnp.float32)
    o_rec_c, s_f, s_b = bidir_hgrn2(rec(rqc, n_ctx, REC_KEY_DIM), kcf, kcb, rec(ric, n_ctx, REC_VAL_DIM), gcf, gcb, s0, s0)
    klf, glf = hgrn2_forget(rec(rff, n_lat, REC_KEY_DIM), lb)
    klb, glb = hgrn2_forget(rec(rfb, n_lat, REC_KEY_DIM), lb)
    o_rec, _, _ = bidir_hgrn2(rec(rq, n_lat, REC_KEY_DIM), klf, klb, rec(ri, n_lat, REC_VAL_DIM), glf, glb, s_f, s_b)

    y = merge_branches(o_att, o_rec, rg, ga, gr, lam_init, g_subln, g_rec, w_br_a, w_br_r, w_out, dt)
    yc = None
    if need_ctx:
        o_att_c = diff_attend(q_c, k_c, v_c, lam)
        yc = merge_branches(o_att_c, o_rec_c, rgc, gac, grc, lam_init, g_subln, g_rec, w_br_a, w_br_r, w_out, dt)
    return y, yc


def moe_ffn(h, w_router, b_router, w_gate, w_up, w_down):
    scores = jax.nn.sigmoid(h.astype(jnp.float32) @ w_router.astype(jnp.float32))
    biased = (scores + b_router.astype(jnp.float32)).reshape(-1, N_GROUPS, EXPERTS_PER_GROUP)
    group_score = jnp.sum(lax.top_k(biased, TOP_K)[0], axis=-1)
    group = jnp.argmax(group_score, axis=-1)
    in_group = jnp.einsum('tge,tg->te', biased, jax.nn.one_hot(group, N_GROUPS, dtype=jnp.float32))
    _, local = lax.top_k(in_group, TOP_K)
    expert = group[:, None] * EXPERTS_PER_GROUP + local
    w = jnp.take_along_axis(scores, expert, axis=1)
    w = w / jnp.sum(w, axis=-1, keepdims=True)
    gates = jnp.einsum('tk,tke->te', w, jax.nn.one_hot(expert, N_EXPERTS, dtype=jnp.float32))
    y = jnp.zeros(h.shape, jnp.float32)
    for e in range(N_EXPERTS):
        hid = jax.nn.silu(h @ w_gate[e]) * (h @ w_up[e])
        y = y + gates[:, e:e + 1] * (hid @ w_down[e]).astype(jnp.float32)
    return y.astype(h.dtype)


def setup_inputs(seed: int = 0) -> dict:
    key = jax.random.key(seed)
    ks = jax.random.split(key, 25)
    D = D_MODEL

    def nrm(k, shape, s):
        return jax.random.normal(k, shape, jnp.float32) * s

    return {
        'x': nrm(ks[0], (BATCH, SEQ, D), 1.0),
        'c': nrm(ks[1], (BATCH, D), 1.0),
        'ctx': nrm(ks[2], (BATCH, CTX_LEN, D), 1.0),
        'c_ctx': nrm(ks[3], (D,), 1.0),
        'w_mod': nrm(ks[4], (DEPTH, D, 6 * D), 0.5 * D ** -0.5),
        'b_mod': nrm(ks[5], (DEPTH, 6 * D), 0.01),
        'g_norm1': 1.0 + nrm(ks[6], (DEPTH, D), 0.02),
        'g_norm2': 1.0 + nrm(ks[7], (DEPTH, D), 0.02),
        'w_in': nrm(ks[8], (DEPTH, D, N_IN), D ** -0.5),
        'lambda_q1': nrm(ks[9], (DEPTH, ATT_HEAD_DIM), 0.1),
        'lambda_k1': nrm(ks[10], (DEPTH, ATT_HEAD_DIM), 0.1),
        'lambda_q2': nrm(ks[11], (DEPTH, ATT_HEAD_DIM), 0.1),
        'lambda_k2': nrm(ks[12], (DEPTH, ATT_HEAD_DIM), 0.1),
        'g_subln': 1.0 + nrm(ks[13], (DEPTH, ATT_V_DIM), 0.02),
        'lb_logits': nrm(ks[14], (DEPTH, REC_KEY_WIDTH), 0.5),
        'g_rec_norm': 1.0 + nrm(ks[15], (DEPTH, REC_VAL_DIM), 0.02),
        'w_br_attn': nrm(ks[16], (DEPTH, ATT_WIDTH, D), ATT_WIDTH ** -0.5),
        'w_br_rec': nrm(ks[17], (DEPTH, REC_WIDTH, D), REC_WIDTH ** -0.5),
        'w_out': nrm(ks[18], (DEPTH, D, D), D ** -0.5),
        'w_router': nrm(ks[19], (D, N_EXPERTS), D ** -0.5),
        'b_router': nrm(ks[20], (N_EXPERTS,), 0.01),
        'w_gate': nrm(ks[21], (DEPTH, N_EXPERTS, D, D_EXPERT), D ** -0.5),
        'w_up': nrm(ks[22], (DEPTH, N_EXPERTS, D, D_EXPERT), D ** -0.5),
        'w_down': nrm(ks[23], (DEPTH, N_EXPERTS, D_EXPERT, D), D_EXPERT ** -0.5),
        'g_final': 1.0 + nrm(ks[24], (D,), 0.02),
    }


def reference(x, c, ctx, c_ctx, w_mod, b_mod, g_norm1, g_norm2, w_in, lambda_q1, lambda_k1, lambda_q2, lambda_k2,
              g_subln, lb_logits, g_rec_norm, w_br_attn, w_br_rec, w_out, w_router, b_router, w_gate, w_up, w_down, g_final):
    b, n_lat, d = x.shape
    ang_r, ang_c = axial_angles(n_lat)
    lbs = jnp.cumsum(jax.nn.softmax(lb_logits.astype(jnp.float32), axis=0), axis=0)
    lbs = lbs - lbs[:1]
    xc = ctx
    for l in range(DEPTH):
        last = l == DEPTH - 1
        lam_init = 0.8 - 0.6 * math.exp(-0.3 * l)
        lam = (jnp.exp(jnp.sum(lambda_q1[l].astype(jnp.float32) * lambda_k1[l].astype(jnp.float32)))
               - jnp.exp(jnp.sum(lambda_q2[l].astype(jnp.float32) * lambda_k2[l].astype(jnp.float32))) + lam_init)
        mod = jax.nn.silu(c) @ w_mod[l] + b_mod[l]
        sh1, sc1, gt1, sh2, sc2, gt2 = [m[:, None, :] for m in jnp.split(mod, 6, axis=-1)]
        mod_c = jax.nn.silu(c_ctx) @ w_mod[l] + b_mod[l]
        sh1c, sc1c, gt1c, sh2c, sc2c, gt2c = jnp.split(mod_c, 6, axis=-1)

        h = modulate(rmsnorm(x, g_norm1[l]), sh1, sc1)
        hc = modulate(rmsnorm(xc, g_norm1[l]), sh1c, sc1c)
        y, yc = token_mixers(h, hc, w_in[l], lam, lam_init, g_subln[l], lbs[l], g_rec_norm[l],
                             w_br_attn[l], w_br_rec[l], w_out[l], ang_r, ang_c, not last)
        x = x + gt1 * y
        h2 = modulate(rmsnorm(x, g_norm2[l]), sh2, sc2)
        if not last:
            xc = xc + gt1c * yc
            h2c = modulate(rmsnorm(xc, g_norm2[l]), sh2c, sc2c)
            n_ctx_tok = b * xc.shape[1]
            tokens = jnp.concatenate([h2c.reshape(-1, d), h2.reshape(-1, d)], axis=0)
            out = moe_ffn(tokens, w_router, b_router, w_gate[l], w_up[l], w_down[l])
            xc = xc + gt2c * out[:n_ctx_tok].reshape(xc.shape)
            x = x + gt2 * out[n_ctx_tok:].reshape(x.shape)
        else:
            x = x + gt2 * moe_ffn(h2.reshape(-1, d), w_router, b_router, w_gate[l], w_up[l], w_down[l]).reshape(x.shape)
    return rmsnorm(x, g_final)
```

```python
import contextlib
import math
import os
import sys
import numpy as np
import ml_dtypes
import concourse.bass as bass
import concourse.mybir as mybir
from concourse.bass_utils import run_bass_kernel_spmd

F32 = mybir.dt.float32
BF16 = mybir.dt.bfloat16
I32 = mybir.dt.int32
AF = mybir.ActivationFunctionType
ALU = mybir.AluOpType
AX = mybir.AxisListType

DEBUG_ANNOTATE = bool(os.environ.get('K_ANNOTATE'))
DMA_SEM_POOL = int(os.environ.get('K_DSEMS', '1000'))
POOL_TO_DVE = os.environ.get('K_POOLDVE', '0') == '1'
D = 1024
KC = 8
N_IN = 10240
EPS = 1e-6
NEXP = 16
CHUNK = 32
NPT = 128 // CHUNK
GRID_W = 64
OFF_AQ, OFF_AK, OFF_AV, OFF_RQ, OFF_RFF, OFF_RFB, OFF_RI, OFF_RG, OFF_GA, OFF_GR = [1024 * i for i in range(10)]


class Buf:
    __slots__ = ("name", "w", "r", "psum")

    def __init__(self, name="", psum=False):
        self.name = name
        self.w = None
        self.r = {}
        self.psum = psum


class DmaSem:
    __slots__ = ("idx", "h", "total")

    def __init__(self, idx, h):
        self.idx = idx
        self.h = h
        self.total = 0


class _Rec:
    def __init__(self):
        self.calls = []

    def __getattr__(self, name):
        def m(*a, **k):
            self.calls.append((name, a, k))
            return self
        return m


class Sched:
    ENG = ("pe", "act", "dve", "pool", "sp")

    def __init__(self, nc, stack):
        self.nc = nc
        self.stack = stack
        self.semh = []
        self.semidx = {}
        self.cnt = {}
        self.ops = {}
        self.known = {}
        for e in self.ENG:
            self.semidx[e] = self._new_sem("s_" + e)
            self.cnt[e] = 0
            self.ops[e] = []
            self.known[e] = {}
        self.n_wait = 0
        self.n_ins = 0
        self.dead = False

    def _new_sem(self, name):
        h = self.stack.enter_context(self.nc.semaphore(name))
        self.semh.append(h)
        return len(self.semh) - 1

    def begin_iter(self):
        if not hasattr(self, "_ipool"):
            self._ipool = []
        self._iidx = 0

    def dma_sem(self, name=None):
        if not hasattr(self, "_dpool"):
            self._dpool = []
            self._dnext = 0
        if getattr(self, "_iidx", None) is not None:
            if self._iidx < len(self._ipool):
                ds = self._ipool[self._iidx]
            else:
                idx = self._new_sem("d%d" % len(self.semh))
                ds = DmaSem(idx, self.semh[idx])
                self._ipool.append(ds)
                self._dpool.append(ds)
            self._iidx += 1
            return ds
        if len(self._dpool) < DMA_SEM_POOL:
            idx = self._new_sem("d%d" % len(self.semh))
            ds = DmaSem(idx, self.semh[idx])
            self._dpool.append(ds)
            return ds
        ds = self._dpool[self._dnext % len(self._dpool)]
        self._dnext += 1
        return ds

    @property
    def _dsems(self):
        return getattr(self, "_dpool", [])

    def _collect(self, eng, reads, writes, extra=()):
        deps = {}
        own_ = self.semidx[eng]
        for b in reads:
            t = b.w
            if t is not None and deps.get(t[0], 0) < t[1]:
                deps[t[0]] = t[1]
            if b.psum:
                for si, v in b.r.items():
                    if si != own_ and deps.get(si, 0) < v:
                        deps[si] = v
        for b in writes:
            t = b.w
            if t is not None and deps.get(t[0], 0) < t[1]:
                deps[t[0]] = t[1]
            for si, v in b.r.items():
                if deps.get(si, 0) < v:
                    deps[si] = v
        for t in extra:
            if t is not None and deps.get(t[0], 0) < t[1]:
                deps[t[0]] = t[1]
        own = self.semidx[eng]
        kn = self.known[eng]
        waits = []
        for si, v in deps.items():
            if eng == "pe" and si == own:
                continue
            if kn.get(si, 0) >= v:
                continue
            kn[si] = v
            waits.append((si, v))
        self.n_wait += len(waits)
        return waits

    def _mark(self, tok, reads, writes):
        si, v = tok
        for b in reads:
            if b.r.get(si, 0) < v:
                b.r[si] = v
        for b in writes:
            b.w = tok
            b.r = {}

    def op(self, eng, fn, reads=(), writes=(), extra=()):
        if self.dead:
            return (0, 0)
        if eng == "pool" and POOL_TO_DVE:
            eng = "dve"
        rec = _Rec()
        fn(rec)
        calls = rec.calls
        lineno = sys._getframe(1).f_lineno

        def fn(e, calls=calls, lineno=lineno):
            ins = None
            for name, a, k in calls:
                ins = getattr(e, name)(*a, **k)
                if DEBUG_ANNOTATE:
                    ins.annotate("L%d" % lineno)
            return ins
        waits = self._collect(eng, reads, writes, extra)
        self.cnt[eng] += 1
        tok = (self.semidx[eng], self.cnt[eng])
        self.ops[eng].append((waits, fn, tok[0]))
        self._mark(tok, reads, writes)
        self.n_ins += 1
        return tok

    def dma(self, queue, dsem, out, in_, reads=(), writes=(), extra=()):
        if self.dead:
            return (0, 0)
        if dsem.total > 0:
            extra = tuple(extra) + ((dsem.idx, dsem.total),)
        waits = self._collect(queue, reads, writes, extra)
        dsem.total += 16
        tok = (dsem.idx, dsem.total)
        h = dsem.h

        def fn(e, out=out, in_=in_, h=h):
            e.dma_start(out=out, in_=in_).then_inc(h, 16)
            return None

        self.ops[queue].append((waits, fn, None))
        self._mark(tok, reads, writes)
        self.n_ins += 1
        return tok

    def wait_only(self, eng, toks):
        waits = self._collect(eng, (), (), toks)
        self.ops[eng].append((waits, None, None))

    def final_barrier(self, eng="sp"):
        toks = [(self.semidx[e], self.cnt[e]) for e in self.ENG if self.cnt[e] > 0 and e != eng]
        for ds in self._dsems:
            if ds.total > 0:
                toks.append((ds.idx, ds.total))
        self.wait_only(eng, toks)

    def emit(self):
        nc = self.nc
        semh = self.semh
        if os.environ.get("K_SEMCLR", "1") == "1":
            for h in semh:
                nc.sync.sem_clear(h)
            nc.all_engine_barrier()
        with nc.Block() as block:
            def mk(name):
                ops = self.ops[name]

                def body(e):
                    for waits, fn, si in ops:
                        for (wi, v) in waits:
                            e.wait_ge(semh[wi], v)
                        if fn is None:
                            continue
                        ins = fn(e)
                        if si is not None:
                            ins.then_inc(semh[si], 1)
                return body

            block.tensor(mk("pe"))
            block.scalar(mk("act"))
            block.vector(mk("dve"))
            block.gpsimd(mk("pool"))
            block.sync(mk("sp"))


class Arena:
    def __init__(self, ap, words):
        self.ap = ap
        self.words = words
        self.top = 0
        self.hist = []
        self.peak = 0

    def mark(self):
        return self.top

    def reset(self, m):
        self.top = m

    def alloc(self, words, nbuf=1, name=""):
        words = (words + 1) // 2 * 2
        st, en = self.top, self.top + words
        assert en <= self.words, "SBUF arena overflow %s %d" % (name, en)
        self.top = en
        self.peak = max(self.peak, en)
        bufs = [Buf(name) for _ in range(nbuf)]
        inherit = {}
        keep = []
        for (a, b, bl) in self.hist:
            if a < en and st < b:
                for ob in bl:
                    if ob.w is not None and inherit.get(ob.w[0], 0) < ob.w[1]:
                        inherit[ob.w[0]] = ob.w[1]
                    for si, v in ob.r.items():
                        if inherit.get(si, 0) < v:
                            inherit[si] = v
                if not (st <= a and b <= en):
                    keep.append((a, b, bl))
            else:
                keep.append((a, b, bl))
        self.hist = keep
        for nb in bufs:
            nb.r = dict(inherit)
        self.hist.append((st, en, bufs))
        return self.ap[:, st:en], bufs


def host_constants(C, S):
    T = C + S
    ident = np.eye(128, dtype=np.float32)
    inv = (10000.0 ** (-np.arange(16, dtype=np.float32) / 16.0)).astype(np.float32)
    t = np.arange(S)
    row = (t // GRID_W).astype(np.float32)
    col = (t % GRID_W).astype(np.float32)
    cos = np.ones((128, T), np.float32)
    sin = np.zeros((128, T), np.float32)
    swap = np.zeros((128, 128), np.float32)
    for p in range(128):
        j = p % 64
        base = row if j < 32 else col
        ang = (base * inv[j % 16]).astype(np.float32)
        first = (j % 32) < 16
        cos[p, C:] = np.cos(ang)
        sin[p, C:] = (-np.sin(ang)) if first else np.sin(ang)
        sp = p + 16 if first else p - 16
        swap[sp, p] = 1.0
    s_i = np.arange(128)[:, None]
    t_i = np.arange(128)[None, :]
    same = (s_i // CHUNK) == (t_i // CHUNK)
    maskf = (same & (s_i <= t_i)).astype(np.float32)
    maskb = (same & (s_i >= t_i)).astype(np.float32)
    scanm = np.ones((128, T), np.float32)
    scanm[:, ::CHUNK] = 0.0
    cmask = np.zeros((128, NPT), np.float32)
    for p in range(128):
        cmask[p, p // CHUNK] = 1.0
    onehot = np.zeros((16, 16, 128), np.float32)
    for e in range(16):
        onehot[e, e, :] = 1.0
    return dict(c_ident=ident, c_cos=cos, c_sin=sin, c_swap=swap, c_maskf=maskf, c_maskb=maskb,
                c_scanm=scanm, c_onehot=onehot.reshape(16, 16 * 128), c_cmask=cmask)


def build_program(NB, S, C, L, debug=(), stop=None):
    T = C + S
    NT = T // 128
    NCT = C // 128
    NCH = T // CHUNK
    TG = [(0, C, True)]
    gsz = min(512, S)
    for g in range(S // gsz):
        TG.append((C + g * gsz, gsz, False))
    MGN = 256 if S >= 256 else S
    TGM = []
    for st_ in range(0, C, min(MGN, C)):
        TGM.append((st_, min(MGN, C), True))
    for st_ in range(C, T, MGN):
        TGM.append((st_, MGN, False))

    nc = bass.Bass("TRN2", target_bir_lowering=False)

    def din(name, shape, dt=F32):
        return nc.dram_tensor(name, list(shape), dt, kind="ExternalInput").ap()

    x_in = din("x", [NB, S, D])
    ctx_in = din("ctx", [NB, C, D])
    cvec = din("cvec", [4, D])
    w_mod = din("w_mod", [L, D, 6 * D])
    b_mod = din("b_mod", [L, 6 * D])
    g_norm1 = din("g_norm1", [L, D])
    g_norm2 = din("g_norm2", [L, D])
    w_in = din("w_in", [L, D, N_IN])
    lam_q1 = din("lambda_q1", [L, 64])
    lam_k1 = din("lambda_k1", [L, 64])
    lam_q2 = din("lambda_q2", [L, 64])
    lam_k2 = din("lambda_k2", [L, 64])
    g_subln = din("g_subln", [L, 128])
    lb_logits = din("lb_logits", [L, D])
    g_rec = din("g_rec_norm", [L, 128])
    w_bra = din("w_br_attn", [L, D, D])
    w_brr = din("w_br_rec", [L, D, D])
    w_out = din("w_out", [L, D, D])
    w_router = din("w_router", [D, NEXP])
    b_router = din("b_router", [1, NEXP])
    w_gate = din("w_gate", [L, NEXP, D, D])
    w_up = din("w_up", [L, NEXP, D, D])
    w_down = din("w_down", [L, NEXP, D, D])
    g_final = din("g_final", [1, D])
    c_ident = din("c_ident", [128, 128])
    c_cos = din("c_cos", [128, T])
    c_sin = din("c_sin", [128, T])
    c_swap = din("c_swap", [128, 128])
    c_maskf = din("c_maskf", [128, 128])
    c_maskb = din("c_maskb", [128, 128])
    c_scanm = din("c_scanm", [128, T])
    c_onehot = din("c_onehot", [16, 16 * 128])
    c_cmask = din("c_cmask", [128, NPT])

    out = nc.dram_tensor("out", [NB, S, D], F32, kind="ExternalOutput").ap()
    xT = nc.dram_tensor("xT_scr", [NB, 128, KC, T], F32, kind="Internal").ap()
    aT = nc.dram_tensor("aT_scr", [128, KC, T], BF16, kind="Internal").ap()
    rT = nc.dram_tensor("rT_scr", [128, KC, T], BF16, kind="Internal").ap()
    dbg_out = {}

    st = contextlib.ExitStack()
    with st:
        s = Sched(nc, st)
        AW = int(os.environ.get("K_AW", 52224))
        arena_t = st.enter_context(nc.sbuf_tensor("arena", [128, AW], F32))
        A = Arena(arena_t, AW)
        psum = [st.enter_context(nc.psum_tensor("ps%d" % i, [128, 512], F32)) for i in range(8)]
        psb = [Buf("ps%d" % i, psum=True) for i in range(8)]
        rr = {"a": 0, "b": 0}

        def ps_a():
            i = rr["a"] % 4
            rr["a"] += 1
            return psum[i], psb[i]

        def ps_b():
            i = 4 + rr["b"] % 2
            rr["b"] += 1
            return psum[i], psb[i]

        def bf(ap):
            return ap.bitcast(BF16)

        dbg_sem = []
        dbg_ds = [None]

        def chk(name):
            if stop == name:
                s.dead = True

        def _dbg_ds():
            if dbg_ds[0] is None:
                dbg_ds[0] = s.dma_sem()
            return dbg_ds[0]

        def dbg(name, ap, bufs):
            if name not in debug:
                return
            shp = list(ap.shape)
            o = nc.dram_tensor("dbg_" + name, shp, ap.dtype, kind="ExternalOutput").ap()
            dbg_out[name] = o
            ds = _dbg_ds()
            tok = s.dma("sp", ds, o, ap, reads=bufs)
            dbg_sem.append(tok)

        def dscr(name, dram_ap, dram_bufs):
            if name not in debug:
                return
            o = nc.dram_tensor("dbg_" + name, list(dram_ap.shape), dram_ap.dtype, kind="ExternalOutput").ap()
            ds = _dbg_ds()
            tok = s.dma("sp", ds, o, dram_ap, reads=dram_bufs)
            dbg_sem.append(tok)

        ident_ap, (ident_b,) = A.alloc(128, 1, "ident")
        identb_w, (identb_b,) = A.alloc(64, 1, "identb")
        identb = bf(identb_w)
        ones_ap, (ones_b,) = A.alloc(128, 1, "ones")
        onesb_w, (onesb_b,) = A.alloc(64, 1, "onesb")
        onesb = bf(onesb_w)
        swap_w, (swap_b,) = A.alloc(64, 1, "swap")
        swapm = bf(swap_w)
        maskf_w, (maskf_b,) = A.alloc(64, 1, "maskf")
        maskb_w, (maskb_b,) = A.alloc(64, 1, "maskb")
        maskf = bf(maskf_w)
        maskb = bf(maskb_w)
        NCOL = NB + 1
        modv_ap, (modv_b,) = A.alloc(L * 48 * NCOL, 1, "modv")
        modv = modv_ap.rearrange("p (l c n) -> p l c n", l=L, c=48)
        a1_ap, (a1_b,) = A.alloc(L * KC * NCOL, 1, "a1")
        a1 = a1_ap.rearrange("p (l c n) -> p l c n", l=L, c=KC)
        a2_ap, (a2_b,) = A.alloc(L * KC * NCOL, 1, "a2")
        a2 = a2_ap.rearrange("p (l c n) -> p l c n", l=L, c=KC)
        small_ap, (small_b,) = A.alloc(256, 1, "small")
        lbv_ap, (lbv_b,) = A.alloc(L * 8 * 3, 1, "lbv")
        lbv = lbv_ap.rearrange("p (l h k) -> p l h k", l=L, h=8)
        lamv_ap, (lamv_b,) = A.alloc(L * 4, 1, "lamv")
        lamv = lamv_ap.rearrange("p (l k) -> p l k", l=L)
        brt_ap, (brt_b,) = A.alloc(16, 1, "brt")
        wr_w, (wr_b,) = A.alloc(KC * 16 // 2, 1, "wr")
        wrt = bf(wr_w).rearrange("p (c e) -> p c e", c=KC)
        eps_ap, (eps_b,) = A.alloc(2, 1, "eps")
        cmask_ap, (cmask_b,) = A.alloc(NPT, 1, "cmask")

        cs = [s.dma_sem() for _ in range(4)]
        s.dma("sp", cs[0], ident_ap, c_ident, writes=[ident_b])
        s.dma("sp", cs[0], cmask_ap, c_cmask, writes=[cmask_b])
        hm_ap, (hm_b,) = A.alloc(2, 1, "hmask")
        s.op("dve", lambda e: e.tensor_tensor(hm_ap[:, 0:1], cmask_ap[:, 0:1], cmask_ap[:, 1:2], ALU.add), reads=[cmask_b], writes=[hm_b])
        s.op("dve", lambda e: e.tensor_tensor(hm_ap[:, 1:2], cmask_ap[:, 2:3], cmask_ap[:, 3:4], ALU.add), reads=[cmask_b], writes=[hm_b])
        cs2 = [s.dma_sem() for _ in range(5)]
        s.dma("pool", cs2[0], swapm, c_swap, writes=[swap_b])
        s.dma("pool", cs2[1], maskf, c_maskf, writes=[maskf_b])
        s.dma("pool", cs2[2], maskb, c_maskb, writes=[maskb_b])
        s.dma("pool", cs2[4], wrt, w_router.rearrange("(c p) e -> p c e", p=128), writes=[wr_b])
        s.op("pool", lambda e: e.memset(ones_ap, 1.0), writes=[ones_b])
        s.op("pool", lambda e: e.memset(lamv_ap, 0.0), writes=[lamv_b])
        s.op("pool", lambda e: e.memset(onesb, 1.0), writes=[onesb_b])
        s.op("pool", lambda e: e.memset(eps_ap, EPS), writes=[eps_b])
        s.op("act", lambda e: e.copy(identb, ident_ap), reads=[ident_b], writes=[identb_b])

        chk("c0")
        m0 = A.mark()
        stA_ap, (stA_b,) = A.alloc(128, 1, "stA")
        stB_ap, (stB_b,) = A.alloc(128, 1, "stB")
        cv_ap, (cv_b,) = A.alloc(D, 1, "cv")
        scT_ap, (scT_b,) = A.alloc(KC * NCOL, 1, "scT")
        scT = scT_ap.rearrange("p (c n) -> p c n", c=KC)
        lamst_ap, (lamst_b,) = A.alloc(4 * L * 64 + 2 * L * 128, 1, "lamst")
        p0s = [s.dma_sem() for _ in range(4)]
        s.dma("sp", p0s[0], stA_ap[0:L * 48, :], b_mod.rearrange("l (c p) -> (l c) p", p=128), writes=[stA_b])
        s.dma("sp", p0s[1], stB_ap[0:L * 8, :], g_norm1.rearrange("l (c p) -> (l c) p", p=128), writes=[stB_b])
        s.dma("sp", p0s[1], stB_ap[16:16 + L * 8, :], g_norm2.rearrange("l (c p) -> (l c) p", p=128), writes=[stB_b])
        s.dma("sp", p0s[1], stB_ap[32:40, :], g_final.rearrange("o (c p) -> (o c) p", p=128), writes=[stB_b])
        s.dma("sp", p0s[1], stB_ap[40:40 + L * 8, :], lb_logits.rearrange("l (c p) -> (l c) p", p=128), writes=[stB_b])
        s.dma("sp", p0s[2], cv_ap[0:4, :], cvec, writes=[cv_b])
        lam_srcs = [lam_q1, lam_k1, lam_q2, lam_k2]
        for i, src in enumerate(lam_srcs):
            s.dma("sp", p0s[3], lamst_ap[:, i * L * 64:(i + 1) * L * 64],
                  src.rearrange("l d -> (l d)").partition_broadcast(128), writes=[lamst_b])
        s.dma("sp", p0s[3], brt_ap, b_router.rearrange("o e -> (o e)").partition_broadcast(128), writes=[brt_b])
        pst, pstb = ps_a()
        s.op("pe", lambda e: e.transpose(pst[:, 0:96], stA_ap[0:96, :], ident_ap[0:96, 0:96]),
             reads=[stA_b, ident_b], writes=[pstb])
        s.op("dve", lambda e: e.tensor_copy(small_ap[:, 0:96], pst[:, 0:96]), reads=[pstb], writes=[small_b])
        pst2, pstb2 = ps_a()
        s.op("pe", lambda e: e.transpose(pst2[:, 0:56], stB_ap[0:56, :], ident_ap[0:56, 0:56]),
             reads=[stB_b, ident_b], writes=[pstb2])
        s.op("dve", lambda e: e.tensor_copy(small_ap[:, 96:152], pst2[:, 0:56]), reads=[pstb2], writes=[small_b])
        chk("p0a")
        g1v = small_ap[:, 96:112].rearrange("p (l c) -> p l c", l=2)
        g2v = small_ap[:, 112:128].rearrange("p (l c) -> p l c", l=2)
        gfv = small_ap[:, 128:136]
        lbl = small_ap[:, 136:152].rearrange("p (l c) -> p l c", l=2)
        bmv = small_ap[:, 0:96].rearrange("p (l c) -> p l c", l=2)
        for l in range(L):
            s.dma("sp", p0s[3], lamv[:, l, 1:2], g_subln[l:l + 1, :].rearrange("o p -> p o"), writes=[lamv_b])
            s.dma("sp", p0s[3], lamv[:, l, 2:3], g_rec[l:l + 1, :].rearrange("o p -> p o"), writes=[lamv_b])
        lt_ap, (lt_b,) = A.alloc(L * 64 * 2 + 8 * L, 1, "lamtmp")
        for l in range(L):
            lam_init = 0.8 - 0.6 * math.exp(-0.3 * l)
            for j in range(2):
                qv = lamst_ap[:, (2 * j) * L * 64 + l * 64:(2 * j) * L * 64 + (l + 1) * 64]
                kv = lamst_ap[:, (2 * j + 1) * L * 64 + l * 64:(2 * j + 1) * L * 64 + (l + 1) * 64]
                pr = lt_ap[:, (l * 2 + j) * 64:(l * 2 + j + 1) * 64]
                sm = lt_ap[:, L * 128 + l * 4 + j:L * 128 + l * 4 + j + 1]
                s.op("dve", lambda e, pr=pr, qv=qv, kv=kv: e.tensor_tensor(pr, qv, kv, ALU.mult), reads=[lamst_b], writes=[lt_b])
                s.op("dve", lambda e, pr=pr, sm=sm: e.tensor_reduce(sm, pr, AX.X, ALU.add), reads=[lt_b], writes=[lt_b])
                s.op("act", lambda e, sm=sm: e.activation(out=sm, in_=sm, func=AF.Exp), reads=[lt_b], writes=[lt_b])
            e1 = lt_ap[:, L * 128 + l * 4:L * 128 + l * 4 + 1]
            e2 = lt_ap[:, L * 128 + l * 4 + 1:L * 128 + l * 4 + 2]
            s.op("dve", lambda e, l=l, e1=e1, e2=e2, li=lam_init: e.scalar_tensor_tensor(
                lamv[:, l, 0:1], e2, -li, e1, ALU.add, ALU.subtract), reads=[lt_b], writes=[lamv_b])
            s.op("dve", lambda e, l=l, li=lam_init: e.tensor_scalar(lamv[:, l, 1:2], lamv[:, l, 1:2], 1.0 - li, None, ALU.mult),
                 reads=[lamv_b], writes=[lamv_b])
        chk("p0b")
        lbe_ap, (lbe_b,) = A.alloc(L * 8 + 16, 1, "lbe")
        lbe = lbe_ap[:, 0:L * 8].rearrange("p (l c) -> p l c", l=L)
        lsum = lbe_ap[:, L * 8:L * 8 + 8]
        s.op("act", lambda e: e.activation(out=lbe_ap[:, 0:L * 8], in_=small_ap[:, 136:136 + L * 8], func=AF.Exp), reads=[small_b], writes=[lbe_b])
        s.op("dve", lambda e: e.tensor_copy(lsum, lbe[:, 0, :]), reads=[lbe_b], writes=[lbe_b])
        for l in range(1, L):
            s.op("dve", lambda e, l=l: e.tensor_tensor(lsum, lsum, lbe[:, l, :], ALU.add), reads=[lbe_b], writes=[lbe_b])
        s.op("dve", lambda e: e.reciprocal(lsum, lsum), reads=[lbe_b], writes=[lbe_b])
        s.op("pool", lambda e: e.memset(lbv[:, 0, :, 0], 0.0), writes=[lbv_b])
        for l in range(1, L):
            s.op("dve", lambda e, l=l: e.tensor_tensor(lbe[:, l, :], lbe[:, l, :], lsum, ALU.mult), reads=[lbe_b], writes=[lbe_b])
            s.op("dve", lambda e, l=l: e.tensor_tensor(lbv[:, l, :, 0], lbv[:, l - 1, :, 0], lbe[:, l, :], ALU.add),
                 reads=[lbe_b, lbv_b], writes=[lbv_b])
        for l in range(L):
            s.op("dve", lambda e, l=l: e.tensor_scalar(lbv[:, l, :, 1], lbv[:, l, :, 0], -1.0, 1.0, ALU.mult, ALU.add),
                 reads=[lbv_b], writes=[lbv_b])
            s.op("dve", lambda e, l=l: e.tensor_scalar(lbv[:, l, :, 2], lbv[:, l, :, 0], 1.0, -1.0, ALU.mult, ALU.add),
                 reads=[lbv_b], writes=[lbv_b])
        chk("p0c")
        s.op("act", lambda e: e.activation(out=cv_ap[0:NCOL, :], in_=cv_ap[0:NCOL, :], func=AF.Silu), reads=[cv_b], writes=[cv_b])
        pst3, pstb3 = ps_a()

        def f_ct(e):
            ins = None
            for c in range(KC):
                ins = e.transpose(pst3[:, c * NCOL:(c + 1) * NCOL], cv_ap[0:NCOL, c * 128:(c + 1) * 128], ident_ap[0:NCOL, 0:NCOL])
            return ins
        s.op("pe", f_ct, reads=[cv_b, ident_b], writes=[pstb3])
        s.op("dve", lambda e: e.tensor_copy(scT_ap, pst3[:, 0:KC * NCOL]), reads=[pstb3], writes=[scT_b])
        chk("p0d")
        wm_slots = []
        for i in range(2):
            ap_, (b_,) = A.alloc(KC * 512, 1, "wmod%d" % i)
            wm_slots.append((ap_.rearrange("p (c n) -> p c n", c=KC), b_, s.dma_sem()))
        gi = 0
        for l in range(L):
            pm, pmb = ps_a()
            for g in range(12):
                wt, wb, wsem = wm_slots[gi % 2]
                gi += 1
                s.dma("sp", wsem, wt, w_mod[l].rearrange("(c p) n -> p c n", p=128)[:, :, g * 512:(g + 1) * 512], writes=[wb])

                def f_mod(e, wt=wt, g=g, pm=pm):
                    ins = None
                    for j in range(4):
                        oc = g * 4 + j
                        for c in range(KC):
                            ins = e.matmul(pm[:, oc * NCOL:(oc + 1) * NCOL], wt[:, c, j * 128:(j + 1) * 128], scT[:, c, :],
                                           start=(c == 0), stop=(c == KC - 1))
                    return ins
                s.op("pe", f_mod, reads=[wb, scT_b], writes=[pmb])
            if l == 0:
                chk("p0e")
            s.op("dve", lambda e, l=l, pm=pm: e.tensor_tensor(
                modv[:, l], pm[:, 0:48 * NCOL].rearrange("p (c n) -> p c n", c=48),
                bmv[:, l, :].unsqueeze(2).to_broadcast([128, 48, NCOL]), ALU.add),
                reads=[pmb, small_b], writes=[modv_b])
            if l == 0:
                chk("p0f")
            for (dst, gv, off) in ((a1, g1v, 8), (a2, g2v, 32)):
                for col_ in range(NCOL):
                    s.op("dve", lambda e, l=l, dst=dst, gv=gv, off=off, col_=col_: e.scalar_tensor_tensor(
                        dst[:, l, :, col_], modv[:, l, off:off + 8, col_], 1.0, gv[:, l, :], ALU.add, ALU.mult),
                        reads=[modv_b, small_b], writes=[a1_b if dst is a1 else a2_b])
        dbg("modv", modv_ap, [modv_b])
        dbg("a1", a1_ap, [a1_b])
        dbg("lamv", lamv_ap, [lamv_b])
        dbg("lbv", lbv_ap, [lbv_b])
        A.reset(m0)
        chk("p0")

        xT_b = [[Buf("xT%d_%d" % (b, g)) for g in range(T // 128)] for b in range(NB)]
        aT_b = [Buf("aT%d" % h) for h in range(8)]
        rT_b = [Buf("rT%d" % h) for h in range(8)]

        def tiles_of(t0, n):
            return range(t0 // 128, (t0 + n) // 128)

        def act_rstd(dst, src_ps, inv_n, rd, wr):
            s.op("act", lambda e: e.activation(out=dst, in_=src_ps, func=AF.Ln, bias=eps_ap[:, 0:1], scale=inv_n), reads=rd + [eps_b], writes=wr)
            s.op("act", lambda e: e.activation(out=dst, in_=dst, func=AF.Exp, scale=-0.5), reads=wr, writes=wr)

        def norm_mod(xg, xg_b, n, hdst, hdst_b, av, sv, av_b):
            mk = A.mark()
            sq_ap, (sq_b,) = A.alloc(KC * n, 1, "sq")
            sq = sq_ap.rearrange("p (c n) -> p c n", c=KC)
            rs_ap, (rs_b,) = A.alloc(n, 1, "rstd")
            s.op("act", lambda e: e.activation(out=sq, in_=xg, func=AF.Square), reads=[xg_b], writes=[sq_b])
            pss, pssb = ps_a()

            def f(e):
                ins = None
                for c in range(KC):
                    ins = e.matmul(pss[:, 0:n], ones_ap, sq[:, c, :], start=(c == 0), stop=(c == KC - 1))
                return ins
            s.op("pe", f, reads=[sq_b, ones_b], writes=[pssb])
            act_rstd(rs_ap, pss[:, 0:n], 1.0 / D, [pssb], [rs_b])
            for c in range(KC):
                tmp = sq[:, c, :]
                s.op("dve", lambda e, c=c, tmp=tmp: e.scalar_tensor_tensor(tmp, xg[:, c, :], av[:, c:c + 1], rs_ap, ALU.mult, ALU.mult),
                     reads=[xg_b, rs_b, av_b], writes=[sq_b])
                s.op("act", lambda e, c=c, tmp=tmp: e.activation(out=hdst[:, c, :], in_=tmp, func=AF.Identity, bias=sv[:, c:c + 1], scale=1.0),
                     reads=[sq_b, av_b], writes=[hdst_b])
            A.reset(mk)

        def load_w(dst, dst_b, sem, src):
            return s.dma("pool", sem, dst, src, writes=[dst_b])

        m1 = A.mark()
        xin_slots = []
        for i in range(2):
            ap_, (b_,) = A.alloc(D, 1, "xin%d" % i)
            xo_, (ob_,) = A.alloc(D, 1, "xo%d" % i)
            xin_slots.append((ap_, b_, s.dma_sem(), xo_, ob_, s.dma_sem()))
        k = 0
        for b in range(NB):
            for ti in range(NT):
                ap_, b_, sm_, xo_, ob_, osm_ = xin_slots[k % 2]
                k += 1
                src = ctx_in[b, ti * 128:(ti + 1) * 128, :] if ti < NCT else x_in[b, (ti - NCT) * 128:(ti - NCT + 1) * 128, :]
                s.dma("sp", sm_, ap_, src, writes=[b_])
                for half in range(2):
                    pt, ptb = ps_a()

                    def f(e, ap_=ap_, pt=pt, half=half):
                        ins = None
                        for j in range(4):
                            c = half * 4 + j
                            ins = e.transpose(pt[:, j * 128:(j + 1) * 128], ap_[:, c * 128:(c + 1) * 128], ident_ap)
                        return ins
                    s.op("pe", f, reads=[b_, ident_b], writes=[ptb])
                    eng = "act" if half == 0 else "dve"
                    if eng == "act":
                        s.op("act", lambda e, xo_=xo_, pt=pt, half=half: e.copy(xo_[:, half * 512:(half + 1) * 512], pt[:, :]), reads=[ptb], writes=[ob_])
                    else:
                        s.op("dve", lambda e, xo_=xo_, pt=pt, half=half: e.tensor_copy(xo_[:, half * 512:(half + 1) * 512], pt[:, :]), reads=[ptb], writes=[ob_])
                s.dma("sp", osm_, xT[b, :, :, ti * 128:(ti + 1) * 128], xo_.rearrange("p (c t) -> p c t", c=KC), reads=[ob_], writes=[xT_b[b][ti]])
        A.reset(m1)
        chk("x0")

        out_toks = []
        for b in range(NB):
            for l in range(L):
                last = (l == L - 1)
                s.begin_iter()
                mL = A.mark()
                h_w, h_bufs = A.alloc(KC * T // 2, len(TG), "h")
                hT = bf(h_w).rearrange("p (c t) -> p c t", c=KC)
                hb_of = {}
                for gi_, (t0, n, isc) in enumerate(TG):
                    for ti in tiles_of(t0, n):
                        hb_of[ti] = h_bufs[gi_]

                def hbufs(t0, n):
                    return list({id(hb_of[ti]): hb_of[ti] for ti in tiles_of(t0, n)}.values())

                mk = A.mark()
                xg_slots = []
                for i in range(2):
                    ap_, (b_,) = A.alloc(KC * 512, 1, "xg%d" % i)
                    xg_slots.append((ap_, b_, s.dma_sem()))
                for gi_, (t0, n, isc) in enumerate(TG):
                    ap_, b_, sm_ = xg_slots[gi_ % 2]
                    xg = ap_[:, 0:KC * n].rearrange("p (c n) -> p c n", c=KC)
                    s.dma("sp", sm_, xg, xT[b, :, :, t0:t0 + n], reads=[xT_b[b][ti] for ti in tiles_of(t0, n)], writes=[b_])
                    col = NB if isc else b
                    norm_mod(xg, b_, n, hT[:, :, t0:t0 + n], h_bufs[gi_], a1[:, l, :, col], modv[:, l, 0:8, col], a1_b)
                A.reset(mk)
                if b == 0 and l == 0:
                    dbg("h", h_w, h_bufs)
                chk("n1")

                mk = A.mark()
                cos_ap, (cos_b,) = A.alloc(T, 1, "cos")
                sin_ap, (sin_b,) = A.alloc(T, 1, "sin")
                s.dma("sp", cs[1], cos_ap, c_cos, writes=[cos_b])
                s.dma("sp", cs[2], sin_ap, c_sin, writes=[sin_b])
                wq_w, (wq_b,) = A.alloc(KC * 128 // 2, 1, "wq")
                wk_w, (wk_b,) = A.alloc(KC * 128 // 2, 1, "wk")
                wv_w, (wv_b,) = A.alloc(KC * 128 // 2, 1, "wv")
                wq = bf(wq_w).rearrange("p (c n) -> p c n", c=KC)
                wk = bf(wk_w).rearrange("p (c n) -> p c n", c=KC)
                wv = bf(wv_w).rearrange("p (c n) -> p c n", c=KC)
                wsem = [s.dma_sem() for _ in range(3)]
                qr_w, (qr_b,) = A.alloc(T // 2, 1, "qr")
                qr1_w, (qr1_b,) = A.alloc(T // 2, 1, "qr1")
                kr_w, (kr_b,) = A.alloc(T // 2, 1, "kr")
                qr = bf(qr_w)
                qr1 = bf(qr1_w)
                kr = bf(kr_w)
                v_w, (v_b,) = A.alloc(NT * 128 // 2, 1, "v")
                vv = bf(v_w).rearrange("p (t d) -> p t d", t=NT)
                qb_slots = []
                for i in range(2):
                    ap_, (b_,) = A.alloc(256, 1, "qb%d" % i)
                    t1_, (t1b_,) = A.alloc(512, 1, "t1_%d" % i)
                    t2_, (t2b_,) = A.alloc(512, 1, "t2_%d" % i)
                    qb_slots.append((bf(ap_), b_, t1_, t1b_, t2_, t2b_))
                pT_slots = []
                for i in range(6):
                    ap_, (b_,) = A.alloc(256, 1, "pT%d" % i)
                    pT_slots.append((bf(ap_), b_))
                acc_slots = []
                for i in range(2):
                    ap_, (b_,) = A.alloc(512, 1, "acc%d" % i)
                    acc_slots.append((ap_, b_))
                rc_slots = []
                for i in range(2):
                    ap_, (b_,) = A.alloc(512, 1, "rc%d" % i)
                    rc_slots.append((ap_, b_))
                o_ap, (o_b,) = A.alloc(512, 1, "o")
                osq_ap, (osq_b,) = A.alloc(512, 1, "osq")
                ors_ap, (ors_b,) = A.alloc(512, 1, "ors")
                ao_slots = []
                for i in range(2):
                    ap_, (b_,) = A.alloc(256, 1, "ao%d" % i)
                    ao_slots.append((bf(ap_), b_, s.dma_sem()))
                qk_i = 0
                pt_i = 0
                ao_i = 0
                for hd in range(8):
                    wsrc = w_in[l].rearrange("(c p) n -> p c n", p=128)
                    load_w(wq, wq_b, wsem[0], wsrc[:, :, OFF_AQ + hd * 128:OFF_AQ + (hd + 1) * 128])
                    load_w(wk, wk_b, wsem[1], wsrc[:, :, OFF_AK + hd * 128:OFF_AK + (hd + 1) * 128])
                    load_w(wv, wv_b, wsem[2], wsrc[:, :, OFF_AV + hd * 128:OFF_AV + (hd + 1) * 128])
                    chk("a1")
                    for gi_, (t0, n, isc) in enumerate(TG):
                        for which in range(2):
                            if which == 0 and isc and last:
                                continue
                            wmat, wmb = (wq, wq_b) if which == 0 else (wk, wk_b)
                            dst, dst_b = (qr, qr_b) if which == 0 else (kr, kr_b)
                            qb_, qbb_, t1_, t1b_, t2_, t2b_ = qb_slots[qk_i % 2]
                            qk_i += 1
                            pq, pqb = ps_a()

                            def f(e, pq=pq, wmat=wmat, t0=t0, n=n):
                                ins = None
                                for c in range(KC):
                                    ins = e.matmul(pq[:, 0:n], wmat[:, c, :], hT[:, c, t0:t0 + n], start=(c == 0), stop=(c == KC - 1))
                                return ins
                            s.op("pe", f, reads=[wmb, h_bufs[gi_]], writes=[pqb])
                            s.op("act", lambda e, qb_=qb_, pq=pq, n=n: e.copy(qb_[:, 0:n], pq[:, 0:n]), reads=[pqb], writes=[qbb_])
                            chk("a2")
                            chk("i%d_a2" % qk_i)
                            psw, pswb = ps_a()
                            s.op("pe", lambda e, psw=psw, qb_=qb_, n=n: e.matmul(psw[:, 0:n], swapm, qb_[:, 0:n], start=True, stop=True),
                                 reads=[qbb_, swap_b], writes=[pswb])
                            chk("a3")
                            chk("i%d_a3" % qk_i)
                            s.op("dve", lambda e, t1_=t1_, pq=pq, t0=t0, n=n: e.tensor_tensor(t1_[:, 0:n], pq[:, 0:n], cos_ap[:, t0:t0 + n], ALU.mult),
                                 reads=[pqb, cos_b], writes=[t1b_])
                            s.op("dve", lambda e, t2_=t2_, psw=psw, t0=t0, n=n: e.tensor_tensor(t2_[:, 0:n], psw[:, 0:n], sin_ap[:, t0:t0 + n], ALU.mult),
                                 reads=[pswb, sin_b], writes=[t2b_])
                            chk("a4")
                            chk("i%d_a4" % qk_i)
                            if which == 1:
                                s.op("pool", lambda e, dst=dst, t1_=t1_, t2_=t2_, t0=t0, n=n: e.tensor_tensor(dst[:, t0:t0 + n], t1_[:, 0:n], t2_[:, 0:n], ALU.add),
                                     reads=[t1b_, t2b_], writes=[dst_b])
                            else:
                                s.op("pool", lambda e, t1_=t1_, t2_=t2_, n=n: e.tensor_tensor(t1_[:, 0:n], t1_[:, 0:n], t2_[:, 0:n], ALU.add),
                                     reads=[t1b_, t2b_], writes=[t1b_])
                                s.op("dve", lambda e, t1_=t1_, t0=t0, n=n: e.tensor_scalar(qr[:, t0:t0 + n], t1_[:, 0:n], hm_ap[:, 0:1], None, ALU.mult),
                                     reads=[t1b_, hm_b], writes=[qr_b])
                                s.op("dve", lambda e, t1_=t1_, t0=t0, n=n: e.tensor_scalar(qr1[:, t0:t0 + n], t1_[:, 0:n], hm_ap[:, 1:2], None, ALU.mult),
                                     reads=[t1b_, hm_b], writes=[qr1_b])
                            chk("a5")
                            chk("qk%d" % qk_i)
                    chk("att_a")
                    for t4 in range(0, NT, 4):
                        pv, pvb = ps_a()
                        nt4 = min(4, NT - t4)

                        def f(e, pv=pv, t4=t4, nt4=nt4):
                            ins = None
                            for j in range(nt4):
                                ti = t4 + j
                                for c in range(KC):
                                    ins = e.matmul(pv[:, j * 128:(j + 1) * 128], hT[:, c, ti * 128:(ti + 1) * 128], wv[:, c, :],
                                                   start=(c == 0), stop=(c == KC - 1))
                            return ins
                        s.op("pe", f, reads=[wv_b] + hbufs(t4 * 128, nt4 * 128), writes=[pvb])
                        s.op("act", lambda e, pv=pv, t4=t4, nt4=nt4: e.copy(
                            vv[:, t4:t4 + nt4, :], pv[:, 0:nt4 * 128].rearrange("p (t d) -> p t d", t=nt4)), reads=[pvb], writes=[v_b])
                    if b == 0 and l == 0 and hd == 0:
                        dbg("qr", qr_w, [qr_b])
                        dbg("kr", kr_w, [kr_b])
                        dbg("v", v_w, [v_b])
                    chk("att_b")
                    for gi_, (t0, n, isc) in enumerate(TG):
                        if isc and last:
                            continue
                        kts = range(0, NCT) if isc else range(0, NT)
                        nk = len(kts)
                        po = [psum[6], psum[7]]
                        pob = [psb[6], psb[7]]
                        for ki, kt in enumerate(kts):
                            for m in range(2):
                                pS, pSb = ps_a()
                                qm, qmb = (qr, qr_b) if m == 0 else (qr1, qr1_b)
                                s.op("pe", lambda e, pS=pS, kt=kt, qm=qm, t0=t0, n=n: e.matmul(
                                    pS[:, 0:n], kr[:, kt * 128:(kt + 1) * 128], qm[:, t0:t0 + n],
                                    start=True, stop=True), reads=[kr_b, qmb], writes=[pSb])
                                pT_, pTb_ = pT_slots[pt_i % 6]
                                pt_i += 1
                                s.op("act", lambda e, pT_=pT_, pS=pS, n=n: e.activation(out=pT_[:, 0:n], in_=pS[:, 0:n], func=AF.Exp, scale=0.125),
                                     reads=[pSb], writes=[pTb_])
                                acc_, accb_ = acc_slots[m]
                                if ki == 0:
                                    s.op("dve", lambda e, acc_=acc_, pT_=pT_, n=n: e.tensor_copy(acc_[:, 0:n], pT_[:, 0:n]), reads=[pTb_], writes=[accb_])
                                else:
                                    s.op("dve", lambda e, acc_=acc_, pT_=pT_, n=n: e.tensor_tensor(acc_[:, 0:n], acc_[:, 0:n], pT_[:, 0:n], ALU.add),
                                         reads=[pTb_, accb_], writes=[accb_])
                                s.op("pe", lambda e, m=m, kt=kt, pT_=pT_, n=n, ki=ki, nk=nk: e.matmul(
                                    po[m][:, 0:n], vv[:, kt, :], pT_[:, 0:n], start=(ki == 0), stop=(ki == nk - 1)),
                                    reads=[v_b, pTb_], writes=[pob[m]])
                        chk("att_c")
                        for m in range(2):
                            acc_, accb_ = acc_slots[m]
                            rc_, rcb_ = rc_slots[m]
                            pS, pSb = ps_a()
                            s.op("pe", lambda e, pS=pS, acc_=acc_, n=n: e.matmul(pS[:, 0:n], ones_ap, acc_[:, 0:n], start=True, stop=True),
                                 reads=[accb_, ones_b], writes=[pSb])
                            s.op("act", lambda e, rc_=rc_, pS=pS, n=n: e.activation(out=rc_[:, 0:n], in_=pS[:, 0:n], func=AF.Ln), reads=[pSb], writes=[rcb_])
                            s.op("act", lambda e, rc_=rc_, n=n: e.activation(out=rc_[:, 0:n], in_=rc_[:, 0:n], func=AF.Exp, scale=-1.0), reads=[rcb_], writes=[rcb_])
                            s.op("dve", lambda e, rc_=rc_, m=m, n=n: e.tensor_tensor(rc_[:, 0:n], po[m][:, 0:n], rc_[:, 0:n], ALU.mult),
                                 reads=[pob[m], rcb_], writes=[rcb_])
                        s.op("dve", lambda e, n=n: e.scalar_tensor_tensor(o_ap[:, 0:n], rc_slots[1][0][:, 0:n], lamv[:, l, 0:1], rc_slots[0][0][:, 0:n], ALU.mult, ALU.add),
                             reads=[rc_slots[0][1], rc_slots[1][1], lamv_b], writes=[o_b])
                        s.op("act", lambda e, n=n: e.activation(out=osq_ap[:, 0:n], in_=o_ap[:, 0:n], func=AF.Square), reads=[o_b], writes=[osq_b])
                        pS, pSb = ps_a()
                        s.op("pe", lambda e, pS=pS, n=n: e.matmul(pS[:, 0:n], ones_ap, osq_ap[:, 0:n], start=True, stop=True), reads=[osq_b, ones_b], writes=[pSb])
                        act_rstd(ors_ap[:, 0:n], pS[:, 0:n], 1.0 / 128, [pSb], [ors_b])
                        ao_, aob_, aosem_ = ao_slots[ao_i % 2]
                        ao_i += 1
                        s.op("dve", lambda e, ao_=ao_, n=n: e.scalar_tensor_tensor(ao_[:, 0:n], o_ap[:, 0:n], lamv[:, l, 1:2], ors_ap[:, 0:n], ALU.mult, ALU.mult),
                             reads=[o_b, ors_b, lamv_b], writes=[aob_])
                        chk("att_d")
                        s.dma("sp", aosem_, aT[:, hd, t0:t0 + n], ao_[:, 0:n], reads=[aob_], writes=[aT_b[hd]])
                        chk("att_e")
                A.reset(mk)
                if b == 0 and l == 0:
                    dscr("aT", aT, aT_b)
                chk("att")
                chk("L%d_att" % l)

                mk = A.mark()
                wnames = ["rq", "rff", "rfb", "ri", "rg"]
                woffs = [OFF_RQ, OFF_RFF, OFF_RFB, OFF_RI, OFF_RG]
                wr_ = {}
                for nm in wnames:
                    w_, (wb_,) = A.alloc(KC * 128 // 2, 1, "w" + nm)
                    wr_[nm] = (bf(w_).rearrange("p (c n) -> p c n", c=KC), wb_, s.dma_sem())
                scanm_w, (scanm_b,) = A.alloc(T // 2, 1, "scanm")
                scanm_ap = bf(scanm_w)
                s.dma("pool", cs2[3], scanm_ap, c_scanm, writes=[scanm_b])
                W1, (W1b,) = A.alloc(T, 1, "W1")
                W2, (W2b,) = A.alloc(T, 1, "W2")
                qf_w, (qf_b,) = A.alloc(T // 2, 1, "qf")
                kk_w, (kk_b,) = A.alloc(T // 2, 1, "kk")
                eg_w, (eg_b,) = A.alloc(T // 2, 1, "eg")
                en_w, (en_b,) = A.alloc(T // 2, 1, "en")
                sg_w, (sg_b,) = A.alloc(T // 2, 1, "sg")
                qf, kk, eg, en, sg = bf(qf_w), bf(kk_w), bf(eg_w), bf(en_w), bf(sg_w)
                vr_w, (vr_b,) = A.alloc(NT * 128 // 2, 1, "vr")
                vr = bf(vr_w).rearrange("p (t d) -> p t d", t=NT)
                vblk_w, (vblk_b,) = A.alloc(NT * NPT * 128 // 2, 1, "vblk")
                vblk = bf(vblk_w).rearrange("p (t c d) -> p t c d", t=NT, c=NPT)
                dirs = {}
                for dname in ("f", "b"):
                    QT_w, (QT_b,) = A.alloc(T // 2, 1, "QT" + dname)
                    KT_w, (KT_b,) = A.alloc(T // 2, 1, "KT" + dname)
                    KH_w, (KH_b,) = A.alloc(T // 2, 1, "KH" + dname)
                    egl_ap, (egl_b,) = A.alloc(NCH, 1, "egl" + dname)
                    st_w, st_bufs = A.alloc((NCH + 1) * 128 // 2, NCH + 1, "st" + dname)
                    S32, (S32b,) = A.alloc(128, 1, "S32" + dname)
                    dirs[dname] = dict(QT=bf(QT_w), QT_b=QT_b, KT=bf(KT_w), KT_b=KT_b, KH=bf(KH_w), KH_b=KH_b, egl=egl_ap, egl_b=egl_b,
                                       st=bf(st_w).rearrange("p (c d) -> p c d", c=NCH + 1), st_bufs=st_bufs, S32=S32, S32b=S32b)
                khT_slots = []
                for i in range(4):
                    ap_, (b_,) = A.alloc(64, 1, "khT%d" % i)
                    khT_slots.append((bf(ap_), b_))
                am_slots = []
                for i in range(4):
                    ap_, (b_,) = A.alloc(64, 1, "am%d" % i)
                    am_slots.append((bf(ap_), b_))
                osq2_ap, (osq2_b,) = A.alloc(512, 1, "osq2")
                ors2_ap, (ors2_b,) = A.alloc(512, 1, "ors2")
                ro_slots = []
                for i in range(2):
                    ap_, (b_,) = A.alloc(256, 1, "ro%d" % i)
                    ro_slots.append((bf(ap_), b_, s.dma_sem()))
                kh_i = 0
                am_i = 0
                ro_i = 0
                ctx_ch = list(range(0, C // CHUNK))
                lat_ch = list(range(C // CHUNK, NCH))
                order = {"f": ctx_ch + lat_ch, "b": ctx_ch[::-1] + lat_ch[::-1]}
                for hd in range(8):
                    wsrc = w_in[l].rearrange("(c p) n -> p c n", p=128)
                    for nm, off in zip(wnames, woffs):
                        load_w(wr_[nm][0], wr_[nm][1], wr_[nm][2], wsrc[:, :, off + hd * 128:off + (hd + 1) * 128])
                    lb_s = lbv[:, l, hd, 0:1]
                    oml_s = lbv[:, l, hd, 1:2]
                    noml_s = lbv[:, l, hd, 2:3]
                    for gi_, (t0, n, isc) in enumerate(TG):
                        pq, pqb = ps_a()

                        def f(e, pq=pq, t0=t0, n=n):
                            ins = None
                            for c in range(KC):
                                ins = e.matmul(pq[:, 0:n], wr_["rq"][0][:, c, :], hT[:, c, t0:t0 + n], start=(c == 0), stop=(c == KC - 1))
                            return ins
                        s.op("pe", f, reads=[wr_["rq"][1], h_bufs[gi_]], writes=[pqb])
                        s.op("act", lambda e, pq=pq, t0=t0, n=n: e.copy(qf[:, t0:t0 + n], pq[:, 0:n]), reads=[pqb], writes=[qf_b])
                        pg, pgb = ps_a()

                        def f(e, pg=pg, t0=t0, n=n):
                            ins = None
                            for c in range(KC):
                                ins = e.matmul(pg[:, 0:n], wr_["rg"][0][:, c, :], hT[:, c, t0:t0 + n], start=(c == 0), stop=(c == KC - 1))
                            return ins
                        s.op("pe", f, reads=[wr_["rg"][1], h_bufs[gi_]], writes=[pgb])
                        s.op("act", lambda e, pg=pg, t0=t0, n=n: e.activation(out=sg[:, t0:t0 + n], in_=pg[:, 0:n], func=AF.Silu), reads=[pgb], writes=[sg_b])
                    for t4 in range(0, NT, 4):
                        pv, pvb = ps_a()
                        nt4 = min(4, NT - t4)

                        def f(e, pv=pv, t4=t4, nt4=nt4):
                            ins = None
                            for j in range(nt4):
                                ti = t4 + j
                                for c in range(KC):
                                    ins = e.matmul(pv[:, j * 128:(j + 1) * 128], hT[:, c, ti * 128:(ti + 1) * 128], wr_["ri"][0][:, c, :],
                                                   start=(c == 0), stop=(c == KC - 1))
                            return ins
                        s.op("pe", f, reads=[wr_["ri"][1]] + hbufs(t4 * 128, nt4 * 128), writes=[pvb])
                        s.op("act", lambda e, pv=pv, t4=t4, nt4=nt4: e.copy(
                            vr[:, t4:t4 + nt4, :], pv[:, 0:nt4 * 128].rearrange("p (t d) -> p t d", t=nt4)), reads=[pvb], writes=[vr_b])
                        for cc in range(NPT):
                            s.op("dve", lambda e, pv=pv, t4=t4, nt4=nt4, cc=cc: e.tensor_scalar(
                                vblk[:, t4:t4 + nt4, cc, :], pv[:, 0:nt4 * 128].rearrange("p (t d) -> p t d", t=nt4), cmask_ap[:, cc:cc + 1], None, ALU.mult),
                                reads=[pvb, cmask_b], writes=[vblk_b])
                    for dname in ("f", "b"):
                        dd = dirs[dname]
                        wz = wr_["rff"] if dname == "f" else wr_["rfb"]
                        for gi_, (t0, n, isc) in enumerate(TG):
                            pz, pzb = ps_a()

                            def f(e, pz=pz, t0=t0, n=n, wz=wz):
                                ins = None
                                for c in range(KC):
                                    ins = e.matmul(pz[:, 0:n], wz[0][:, c, :], hT[:, c, t0:t0 + n], start=(c == 0), stop=(c == KC - 1))
                                return ins
                            s.op("pe", f, reads=[wz[1], h_bufs[gi_]], writes=[pzb])
                            s.op("act", lambda e, pz=pz, t0=t0, n=n: e.activation(out=W1[:, t0:t0 + n], in_=pz[:, 0:n], func=AF.Sigmoid), reads=[pzb], writes=[W1b])
                        s.op("dve", lambda e: e.tensor_scalar(W2, W1, oml_s, lb_s, ALU.mult, ALU.add), reads=[W1b, lbv_b], writes=[W2b])
                        s.op("dve", lambda e: e.tensor_scalar(kk, W1, noml_s, oml_s, ALU.mult, ALU.add), reads=[W1b, lbv_b], writes=[kk_b])
                        s.op("act", lambda e: e.activation(out=W1, in_=W2, func=AF.Ln), reads=[W2b], writes=[W1b])
                        s.op("dve", lambda e: e.tensor_tensor_scan(W2, scanm_ap, W1, 0.0, ALU.mult, ALU.add), reads=[W1b, scanm_b], writes=[W2b])
                        W13 = W1.rearrange("p (c t) -> p c t", t=CHUNK)
                        W23 = W2.rearrange("p (c t) -> p c t", t=CHUNK)
                        if dname == "f":
                            G, Gb, G3 = W2, W2b, W23
                            last_col = CHUNK - 1
                            X, Xb = W1, W1b
                        else:
                            s.op("dve", lambda e: e.tensor_tensor(W1, W1, W2, ALU.subtract), reads=[W1b, W2b], writes=[W1b])
                            s.op("dve", lambda e: e.tensor_tensor(W13, W13, W23[:, :, CHUNK - 1:CHUNK].to_broadcast([128, NCH, CHUNK]), ALU.add),
                                 reads=[W1b, W2b], writes=[W1b])
                            G, Gb, G3 = W1, W1b, W13
                            last_col = 0
                            X, Xb = W2, W2b
                        s.op("act", lambda e, G=G: e.activation(out=eg, in_=G, func=AF.Exp), reads=[Gb], writes=[eg_b])
                        s.op("act", lambda e, G=G: e.activation(out=en, in_=G, func=AF.Exp, scale=-1.0), reads=[Gb], writes=[en_b])
                        s.op("act", lambda e, G3=G3, last_col=last_col, dd=dd: e.activation(out=dd["egl"], in_=G3[:, :, last_col], func=AF.Exp), reads=[Gb], writes=[dd["egl_b"]])
                        s.op("dve", lambda e, dd=dd: e.tensor_tensor(dd["QT"], qf, eg, ALU.mult), reads=[qf_b, eg_b], writes=[dd["QT_b"]])
                        s.op("pool", lambda e, dd=dd: e.tensor_tensor(dd["KT"], kk, en, ALU.mult), reads=[kk_b, en_b], writes=[dd["KT_b"]])
                        X3 = X.rearrange("p (c t) -> p c t", t=CHUNK)
                        s.op("dve", lambda e, dd=dd, X3=X3: e.tensor_tensor(
                            X3, dd["KT"].rearrange("p (c t) -> p c t", t=CHUNK), dd["egl"].unsqueeze(2).to_broadcast([128, NCH, CHUNK]), ALU.mult),
                            reads=[dd["KT_b"], dd["egl_b"], Xb], writes=[Xb])
                        s.op("pool", lambda e, dd=dd, X=X: e.tensor_copy(dd["KH"], X), reads=[Xb], writes=[dd["KH_b"]])
                        if b == 0 and l == 0 and hd == 0:
                            dbg("QT" + dname, dd["QT"].bitcast(F32), [dd["QT_b"]])
                            dbg("KT" + dname, dd["KT"].bitcast(F32), [dd["KT_b"]])
                            dbg("KH" + dname, dd["KH"].bitcast(F32), [dd["KH_b"]])
                            dbg("egl" + dname, dd["egl"], [dd["egl_b"]])
                    for dname in ("f", "b"):
                        dd = dirs[dname]
                        ordr = order[dname]
                        s.op("pool", lambda e, dd=dd: e.memset(dd["S32"], 0.0), writes=[dd["S32b"]])
                        c0 = ordr[0]
                        s.op("pool", lambda e, dd=dd, c0=c0: e.memset(dd["st"][:, c0, :], 0.0), writes=[dd["st_bufs"][c0]])
                        tile_order = []
                        for cch in ordr:
                            if cch // NPT not in tile_order:
                                tile_order.append(cch // NPT)
                        pos = 0
                        for ti in tile_order:
                            khT_, khTb_ = khT_slots[kh_i % 4]
                            kh_i += 1
                            ptr, ptrb = ps_b()
                            ptr_bf = ptr[:, 0:64].bitcast(BF16)
                            s.op("pe", lambda e, ptr_bf=ptr_bf, dd=dd, ti=ti: e.transpose(ptr_bf, dd["KH"][:, ti * 128:(ti + 1) * 128], identb),
                                 reads=[dd["KH_b"], identb_b], writes=[ptrb])
                            s.op("act", lambda e, khT_=khT_, ptr_bf=ptr_bf: e.copy(khT_, ptr_bf), reads=[ptrb], writes=[khTb_])
                            pu, pub = ps_b()

                            s.op("pe", lambda e, pu=pu, khT_=khT_, ti=ti: e.matmul(
                                pu[:, 0:NPT * 128], khT_, vblk[:, ti].rearrange("p c d -> p (c d)"), start=True, stop=True),
                                reads=[khTb_, vblk_b], writes=[pub])
                            chunks_here = [cch for cch in ordr if cch // NPT == ti]
                            for cch in chunks_here:
                                j = cch % NPT
                                nxt = ordr[pos + 1] if pos + 1 < len(ordr) else NCH
                                pos += 1
                                s.op("dve", lambda e, dd=dd, cch=cch, j=j, pu=pu: e.scalar_tensor_tensor(
                                    dd["S32"], dd["S32"], dd["egl"][:, cch:cch + 1], pu[:, j * 128:(j + 1) * 128], ALU.mult, ALU.add),
                                    reads=[dd["S32b"], dd["egl_b"], pub], writes=[dd["S32b"]])
                                s.op("pool", lambda e, dd=dd, nxt=nxt: e.tensor_copy(dd["st"][:, nxt, :], dd["S32"]), reads=[dd["S32b"]], writes=[dd["st_bufs"][nxt]])
                    for t4 in range(0, NT, 4):
                        nt4 = min(4, NT - t4)
                        po_, pob_ = psum[6 + (t4 // 4) % 2], psb[6 + (t4 // 4) % 2]
                        for jt in range(nt4):
                            ti = t4 + jt
                            ams = []
                            for dname in ("f", "b"):
                                dd = dirs[dname]
                                pa_, pab_ = ps_b()
                                s.op("pe", lambda e, pa_=pa_, dd=dd, ti=ti: e.matmul(
                                    pa_[:, 0:128], dd["KT"][:, ti * 128:(ti + 1) * 128], dd["QT"][:, ti * 128:(ti + 1) * 128], start=True, stop=True),
                                    reads=[dd["KT_b"], dd["QT_b"]], writes=[pab_])
                                am_, amb_ = am_slots[am_i % 4]
                                am_i += 1
                                mk_, mkb_ = (maskf, maskf_b) if dname == "f" else (maskb, maskb_b)
                                s.op("dve", lambda e, am_=am_, pa_=pa_, mk_=mk_: e.tensor_tensor(am_, pa_[:, 0:128], mk_, ALU.mult), reads=[pab_, mkb_], writes=[amb_])
                                ams.append((am_, amb_))

                            def f(e, po_=po_, jt=jt, ti=ti, ams=ams):
                                ins = None
                                first = True
                                for di, dname in enumerate(("f", "b")):
                                    dd = dirs[dname]
                                    ins = e.matmul(po_[:, jt * 128:(jt + 1) * 128], vr[:, ti, :], ams[di][0], start=first, stop=False)
                                    first = False
                                    for j in range(NPT):
                                        cch = ti * NPT + j
                                        ins = e.matmul(po_[:, jt * 128 + j * CHUNK:jt * 128 + (j + 1) * CHUNK], dd["st"][:, cch, :],
                                                       dd["QT"][:, cch * CHUNK:(cch + 1) * CHUNK], start=False, stop=(di == 1 and j == NPT - 1))
                                return ins
                            rds = [vr_b, ams[0][1], ams[1][1]]
                            for dname in ("f", "b"):
                                dd = dirs[dname]
                                rds += [dd["QT_b"]] + [dd["st_bufs"][ti * NPT + j] for j in range(NPT)]
                            s.op("pe", f, reads=rds, writes=[pob_])
                        n = nt4 * 128
                        t0 = t4 * 128
                        s.op("act", lambda e, po_=po_, n=n: e.activation(out=osq2_ap[:, 0:n], in_=po_[:, 0:n], func=AF.Square), reads=[pob_], writes=[osq2_b])
                        pS, pSb = ps_a()
                        s.op("pe", lambda e, pS=pS, n=n: e.matmul(pS[:, 0:n], ones_ap, osq2_ap[:, 0:n], start=True, stop=True), reads=[osq2_b, ones_b], writes=[pSb])
                        act_rstd(ors2_ap[:, 0:n], pS[:, 0:n], 1.0 / 128, [pSb], [ors2_b])
                        s.op("dve", lambda e, po_=po_, n=n: e.scalar_tensor_tensor(osq2_ap[:, 0:n], po_[:, 0:n], lamv[:, l, 2:3], ors2_ap[:, 0:n], ALU.mult, ALU.mult),
                             reads=[pob_, ors2_b, lamv_b], writes=[osq2_b])
                        ro_, rob_, rosem_ = ro_slots[ro_i % 2]
                        ro_i += 1
                        s.op("pool", lambda e, ro_=ro_, n=n, t0=t0: e.tensor_tensor(ro_[:, 0:n], osq2_ap[:, 0:n], sg[:, t0:t0 + n], ALU.mult),
                             reads=[osq2_b, sg_b], writes=[rob_])
                        s.dma("sp", rosem_, rT[:, hd, t0:t0 + n], ro_[:, 0:n], reads=[rob_], writes=[rT_b[hd]])
                A.reset(mk)
                if b == 0 and l == 0:
                    dscr("rT", rT, rT_b)
                chk("rec")
                chk("L%d_rec" % l)

                mk = A.mark()
                wm = {}
                for nm in ("ga", "gr", "ba", "br", "wo"):
                    w_, (wb_,) = A.alloc(KC * D // 2, 1, "wm" + nm)
                    wm[nm] = (bf(w_).rearrange("p (c n) -> p c n", c=KC), wb_, s.dma_sem())
                wsrc = w_in[l].rearrange("(c p) n -> p c n", p=128)
                load_w(wm["ga"][0], wm["ga"][1], wm["ga"][2], wsrc[:, :, OFF_GA:OFF_GA + D])
                load_w(wm["gr"][0], wm["gr"][1], wm["gr"][2], wsrc[:, :, OFF_GR:OFF_GR + D])
                load_w(wm["ba"][0], wm["ba"][1], wm["ba"][2], w_bra[l].rearrange("(c p) n -> p c n", p=128))
                load_w(wm["br"][0], wm["br"][1], wm["br"][2], w_brr[l].rearrange("(c p) n -> p c n", p=128))
                load_w(wm["wo"][0], wm["wo"][1], wm["wo"][2], w_out[l].rearrange("(c p) n -> p c n", p=128))
                ar_slots = []
                for i in range(2):
                    a_, (ab_,) = A.alloc(KC * MGN // 2, 1, "ag%d" % i)
                    r_, (rb_,) = A.alloc(KC * MGN // 2, 1, "rg%d" % i)
                    x_, (xb_,) = A.alloc(KC * MGN, 1, "xm%d" % i)
                    ar_slots.append((bf(a_).rearrange("p (c n) -> p c n", c=KC), ab_, s.dma_sem(),
                                     bf(r_).rearrange("p (c n) -> p c n", c=KC), rb_, s.dma_sem(),
                                     x_.rearrange("p (c n) -> p c n", c=KC), xb_, s.dma_sem(), s.dma_sem()))
                mx_w, (mx_b,) = A.alloc(KC * MGN // 2, 1, "mixed")
                mixed = bf(mx_w).rearrange("p (c n) -> p c n", c=KC)
                sg_slots = []
                for i in range(2):
                    s1_, (s1b_,) = A.alloc(MGN, 1, "sga%d" % i)
                    s2_, (s2b_,) = A.alloc(MGN, 1, "sgr%d" % i)
                    sg_slots.append((s1_, s1b_, s2_, s2b_))
                sgi = 0
                for gi_, (t0, n, isc) in enumerate(TGM):
                    if isc and last:
                        continue
                    col = NB if isc else b
                    a_, ab_, asem_, r_, rb_, rsem_, x_, xb_, xsem_, xosem_ = ar_slots[gi_ % 2]
                    s.dma("sp", asem_, a_[:, :, 0:n], aT[:, :, t0:t0 + n], reads=aT_b, writes=[ab_])
                    s.dma("sp", rsem_, r_[:, :, 0:n], rT[:, :, t0:t0 + n], reads=rT_b, writes=[rb_])
                    s.dma("sp", xsem_, x_[:, :, 0:n], xT[b, :, :, t0:t0 + n], reads=[xT_b[b][ti] for ti in tiles_of(t0, n)], writes=[xb_])
                    hbs = hbufs(t0, n)
                    for oc in range(KC):
                        s1_, s1b_, s2_, s2b_ = sg_slots[sgi % 2]
                        sgi += 1
                        pgs = []
                        for (wnm, src, srcb) in (("ga", hT[:, :, t0:t0 + n], hbs), ("gr", hT[:, :, t0:t0 + n], hbs), ("ba", a_[:, :, 0:n], [ab_]), ("br", r_[:, :, 0:n], [rb_])):
                            pg, pgb = ps_a()

                            def f(e, pg=pg, wnm=wnm, src=src, oc=oc, n=n):
                                ins = None
                                for c in range(KC):
                                    ins = e.matmul(pg[:, 0:n], wm[wnm][0][:, c, oc * 128:(oc + 1) * 128], src[:, c, :], start=(c == 0), stop=(c == KC - 1))
                                return ins
                            s.op("pe", f, reads=[wm[wnm][1]] + list(srcb), writes=[pgb])
                            pgs.append((pg, pgb))
                        s.op("act", lambda e, s1_=s1_, pg=pgs[0][0], n=n: e.activation(out=s1_[:, 0:n], in_=pg[:, 0:n], func=AF.Sigmoid), reads=[pgs[0][1]], writes=[s1b_])
                        s.op("act", lambda e, s2_=s2_, pg=pgs[1][0], n=n: e.activation(out=s2_[:, 0:n], in_=pg[:, 0:n], func=AF.Sigmoid), reads=[pgs[1][1]], writes=[s2b_])
                        s.op("dve", lambda e, s1_=s1_, pg=pgs[2][0], n=n: e.tensor_tensor(s1_[:, 0:n], pg[:, 0:n], s1_[:, 0:n], ALU.mult), reads=[pgs[2][1], s1b_], writes=[s1b_])
                        s.op("dve", lambda e, s2_=s2_, pg=pgs[3][0], n=n: e.tensor_tensor(s2_[:, 0:n], pg[:, 0:n], s2_[:, 0:n], ALU.mult), reads=[pgs[3][1], s2b_], writes=[s2b_])
                        s.op("pool", lambda e, s1_=s1_, s2_=s2_, oc=oc, n=n: e.tensor_tensor(mixed[:, oc, 0:n], s1_[:, 0:n], s2_[:, 0:n], ALU.add),
                             reads=[s1b_, s2b_], writes=[mx_b])
                    for oc in range(KC):
                        py, pyb = ps_a()

                        def f(e, py=py, oc=oc, n=n):
                            ins = None
                            for c in range(KC):
                                ins = e.matmul(py[:, 0:n], wm["wo"][0][:, c, oc * 128:(oc + 1) * 128], mixed[:, c, 0:n], start=(c == 0), stop=(c == KC - 1))
                            return ins
                        s.op("pe", f, reads=[wm["wo"][1], mx_b], writes=[pyb])
                        s.op("dve", lambda e, py=py, oc=oc, n=n, x_=x_, col=col: e.scalar_tensor_tensor(
                            x_[:, oc, 0:n], py[:, 0:n], modv[:, l, 16 + oc, col:col + 1], x_[:, oc, 0:n], ALU.mult, ALU.add),
                            reads=[pyb, xb_, modv_b], writes=[xb_])
                    s.dma("sp", xosem_, xT[b, :, :, t0:t0 + n], x_[:, :, 0:n], reads=[xb_], writes=[xT_b[b][ti] for ti in tiles_of(t0, n)])
                    norm_mod(x_[:, :, 0:n], xb_, n, hT[:, :, t0:t0 + n], hbs[0], a2[:, l, :, col], modv[:, l, 24:32, col], a2_b)
                    for ob in hbs[1:]:
                        ob.w = hbs[0].w
                        ob.r = {}
                A.reset(mk)
                if b == 0 and l == 0:
                    dbg("h2", h_w, h_bufs)
                    dscr("x1", xT[0], xT_b[0])
                chk("merge")
                chk("L%d_merge" % l)

                mk = A.mark()
                onehot_w, (onehot_b,) = A.alloc(16 * 128 // 2, 1, "onehot")
                onehot = bf(onehot_w)
                s.dma("pool", cs2[3], onehot[0:16, :], c_onehot, writes=[onehot_b])
                xr_ap, xr_bufs = A.alloc(KC * T, len(TG), "xres")
                xres = xr_ap.rearrange("p (c t) -> p c t", c=KC)
                xrsem = [s.dma_sem() for _ in TG]
                tg_moe = [(gi_, t0, n, isc) for gi_, (t0, n, isc) in enumerate(TG) if not (isc and last)]
                for (gi_, t0, n, isc) in tg_moe:
                    s.dma("sp", xrsem[gi_], xres[:, :, t0:t0 + n], xT[b, :, :, t0:t0 + n], reads=[xT_b[b][ti] for ti in tiles_of(t0, n)], writes=[xr_bufs[gi_]])
                tiles_moe = [ti for ti in range(NT) if not (last and ti < NCT)]
                ntm = len(tiles_moe)
                sc_ap, (sc_b,) = A.alloc(NT * 16, 1, "scores")
                bi_ap, (bi_b,) = A.alloc(NT * 16, 1, "biased")
                t_ap, (t_b,) = A.alloc(NT * 16, 1, "rt_tmp")
                g_ap, (g_b,) = A.alloc(NT * 16, 1, "gates")
                m1_ap, (m1_b,) = A.alloc(NT * 4, 1, "max1")
                m2_ap, (m2_b,) = A.alloc(NT * 4, 1, "max2")
                gs_ap, (gs_b,) = A.alloc(NT * 4, 1, "gsel")
                gm_ap, (gm_b,) = A.alloc(NT, 1, "gmax")
                gT_w, (gT_b,) = A.alloc(T // 2, 1, "gT")
                gT = bf(gT_w)
                gb_w, gb_bufs = A.alloc(T // 2, len(TG), "gbs")
                gbs = bf(gb_w)
                prt, prtb = ps_a()

                def f(e):
                    ins = None
                    for ti in tiles_moe:
                        for c in range(KC):
                            ins = e.matmul(prt[:, ti * 16:(ti + 1) * 16], hT[:, c, ti * 128:(ti + 1) * 128], wrt[:, c, :], start=(c == 0), stop=(c == KC - 1))
                    return ins
                s.op("pe", f, reads=[wr_b] + h_bufs, writes=[prtb])
                lo, hi = tiles_moe[0], tiles_moe[-1] + 1

                def v3(ap):
                    return ap[:, lo * 16:hi * 16].rearrange("p (t e) -> p t e", e=16)

                def v4(ap):
                    return ap[:, lo * 16:hi * 16].rearrange("p (t e) -> p t e", e=4)

                def v2(ap):
                    return ap[:, lo * 4:hi * 4]
                s.op("act", lambda e: e.activation(out=sc_ap[:, lo * 16:hi * 16], in_=prt[:, lo * 16:hi * 16], func=AF.Sigmoid), reads=[prtb], writes=[sc_b])
                s.op("dve", lambda e: e.tensor_tensor(v3(bi_ap), v3(sc_ap), brt_ap.unsqueeze(1).to_broadcast([128, ntm, 16]), ALU.add), reads=[sc_b, brt_b], writes=[bi_b])
                s.op("dve", lambda e: e.tensor_reduce(v2(m1_ap), v4(bi_ap), AX.X, ALU.max), reads=[bi_b], writes=[m1_b])
                s.op("dve", lambda e: e.tensor_tensor(v4(t_ap), v4(bi_ap), v2(m1_ap).unsqueeze(2).to_broadcast([128, ntm * 4, 4]), ALU.is_ge), reads=[bi_b, m1_b], writes=[t_b])
                s.op("dve", lambda e: e.scalar_tensor_tensor(t_ap[:, lo * 16:hi * 16], t_ap[:, lo * 16:hi * 16], -1e9, bi_ap[:, lo * 16:hi * 16], ALU.mult, ALU.add), reads=[t_b, bi_b], writes=[t_b])
                s.op("dve", lambda e: e.tensor_reduce(v2(m2_ap), v4(t_ap), AX.X, ALU.max), reads=[t_b], writes=[m2_b])
                s.op("dve", lambda e: e.tensor_tensor(v2(gs_ap), v2(m1_ap), v2(m2_ap), ALU.add), reads=[m1_b, m2_b], writes=[gs_b])
                s.op("dve", lambda e: e.tensor_reduce(gm_ap[:, lo:hi], v2(gs_ap).rearrange("p (t g) -> p t g", g=4), AX.X, ALU.max), reads=[gs_b], writes=[gm_b])
                s.op("dve", lambda e: e.tensor_tensor(v2(gs_ap).rearrange("p (t g) -> p t g", g=4), v2(gs_ap).rearrange("p (t g) -> p t g", g=4),
                                                      gm_ap[:, lo:hi].unsqueeze(2).to_broadcast([128, ntm, 4]), ALU.is_ge), reads=[gs_b, gm_b], writes=[gs_b])
                s.op("dve", lambda e: e.tensor_tensor(v4(t_ap), v4(bi_ap), v2(m2_ap).unsqueeze(2).to_broadcast([128, ntm * 4, 4]), ALU.is_ge), reads=[bi_b, m2_b], writes=[t_b])
                s.op("dve", lambda e: e.tensor_tensor(v4(t_ap), v4(t_ap), v2(gs_ap).unsqueeze(2).to_broadcast([128, ntm * 4, 4]), ALU.mult), reads=[t_b, gs_b], writes=[t_b])
                s.op("dve", lambda e: e.tensor_tensor(t_ap[:, lo * 16:hi * 16], t_ap[:, lo * 16:hi * 16], sc_ap[:, lo * 16:hi * 16], ALU.mult), reads=[t_b, sc_b], writes=[t_b])
                s.op("dve", lambda e: e.tensor_reduce(gm_ap[:, lo:hi], v3(t_ap), AX.X, ALU.add), reads=[t_b], writes=[gm_b])
                s.op("dve", lambda e: e.reciprocal(gm_ap[:, lo:hi], gm_ap[:, lo:hi]), reads=[gm_b], writes=[gm_b])
                s.op("dve", lambda e: e.tensor_tensor(v3(g_ap), v3(t_ap), gm_ap[:, lo:hi].unsqueeze(2).to_broadcast([128, ntm, 16]), ALU.mult), reads=[t_b, gm_b], writes=[g_b])
                if b == 0 and l == 0:
                    dbg("gates", g_ap, [g_b])
                for t4 in range(lo, hi, 4):
                    nt4 = min(4, hi - t4)
                    pgt, pgtb = ps_a()

                    def f(e, pgt=pgt, t4=t4, nt4=nt4):
                        ins = None
                        for j in range(nt4):
                            ins = e.transpose(pgt[0:16, j * 128:(j + 1) * 128], g_ap[:, (t4 + j) * 16:(t4 + j + 1) * 16], ident_ap)
                        return ins
                    s.op("pe", f, reads=[g_b, ident_b], writes=[pgtb])
                    s.op("act", lambda e, pgt=pgt, t4=t4, nt4=nt4: e.copy(gT[0:16, t4 * 128:(t4 + nt4) * 128], pgt[0:16, 0:nt4 * 128]), reads=[pgtb], writes=[gT_b])
                chk("L%d_moe_r" % l)
                m_exp = A.mark()
                wu_slots = []
                for i in range(2):
                    g_, (gb_,) = A.alloc(KC * 512 // 2, 1, "wg%d" % i)
                    u_, (ub_,) = A.alloc(KC * 512 // 2, 1, "wu%d" % i)
                    d_, (db_,) = A.alloc(4 * D // 2, 1, "wd%d" % i)
                    wu_slots.append((bf(g_).rearrange("p (c n) -> p c n", c=KC), gb_, s.dma_sem(),
                                     bf(u_).rearrange("p (c n) -> p c n", c=KC), ub_, s.dma_sem(),
                                     bf(d_).rearrange("p (c n) -> p c n", c=4), db_, s.dma_sem()))
                hid_w, (hid_b,) = A.alloc(4 * 512 // 2, 1, "hid")
                hid = bf(hid_w).rearrange("p (c n) -> p c n", c=4)
                sl_slots = []
                for i in range(2):
                    a_, (ab_,) = A.alloc(512, 1, "silu%d" % i)
                    sl_slots.append((a_, ab_))
                sli = 0
                ui = 0
                for ex in range(NEXP):
                    for (gi_, t0, n, isc) in tg_moe:
                        pgb_, pgbb_ = ps_a()
                        s.op("pe", lambda e, pgb_=pgb_, ex=ex, t0=t0, n=n: e.matmul(pgb_[:, 0:n], onehot[0:16, ex * 128:(ex + 1) * 128], gT[0:16, t0:t0 + n], start=True, stop=True),
                             reads=[onehot_b, gT_b], writes=[pgbb_])
                        s.op("act", lambda e, pgb_=pgb_, t0=t0, n=n: e.copy(gbs[:, t0:t0 + n], pgb_[:, 0:n]), reads=[pgbb_], writes=[gb_bufs[gi_]])
                    for fh in range(2):
                        wg_, wgb_, wgs_, wu_, wub_, wus_, wd_, wdb_, wds_ = wu_slots[ui % 2]
                        ui += 1
                        load_w(wg_, wgb_, wgs_, w_gate[l, ex].rearrange("(c p) n -> p c n", p=128)[:, :, fh * 512:(fh + 1) * 512])
                        load_w(wu_, wub_, wus_, w_up[l, ex].rearrange("(c p) n -> p c n", p=128)[:, :, fh * 512:(fh + 1) * 512])
                        load_w(wd_, wdb_, wds_, w_down[l, ex, fh * 512:(fh + 1) * 512, :].rearrange("(c p) n -> p c n", p=128))
                        for (gi_, t0, n, isc) in tg_moe:
                            col = NB if isc else b
                            for fc in range(4):
                                pg, pgb = ps_a()
                                pu, pub = ps_a()

                                def f(e, pg=pg, wg_=wg_, fc=fc, t0=t0, n=n):
                                    ins = None
                                    for c in range(KC):
                                        ins = e.matmul(pg[:, 0:n], wg_[:, c, fc * 128:(fc + 1) * 128], hT[:, c, t0:t0 + n], start=(c == 0), stop=(c == KC - 1))
                                    return ins
                                s.op("pe", f, reads=[wgb_, h_bufs[gi_]], writes=[pgb])

                                def f(e, pu=pu, wu_=wu_, fc=fc, t0=t0, n=n):
                                    ins = None
                                    for c in range(KC):
                                        ins = e.matmul(pu[:, 0:n], wu_[:, c, fc * 128:(fc + 1) * 128], hT[:, c, t0:t0 + n], start=(c == 0), stop=(c == KC - 1))
                                    return ins
                                s.op("pe", f, reads=[wub_, h_bufs[gi_]], writes=[pub])
                                sl_, slb_ = sl_slots[sli % 2]
                                sli += 1
                                s.op("act", lambda e, sl_=sl_, pg=pg, n=n: e.activation(out=sl_[:, 0:n], in_=pg[:, 0:n], func=AF.Silu), reads=[pgb], writes=[slb_])
                                s.op("dve", lambda e, sl_=sl_, pu=pu, n=n: e.tensor_tensor(sl_[:, 0:n], pu[:, 0:n], sl_[:, 0:n], ALU.mult), reads=[pub, slb_], writes=[slb_])
                                s.op("pool", lambda e, sl_=sl_, fc=fc, t0=t0, n=n: e.tensor_tensor(hid[:, fc, 0:n], sl_[:, 0:n], gbs[:, t0:t0 + n], ALU.mult),
                                     reads=[slb_, gb_bufs[gi_]], writes=[hid_b])
                            for oc in range(KC):
                                pd, pdb = ps_b()

                                def f(e, pd=pd, wd_=wd_, oc=oc, n=n):
                                    ins = None
                                    for c in range(4):
                                        ins = e.matmul(pd[:, 0:n], wd_[:, c, oc * 128:(oc + 1) * 128], hid[:, c, 0:n], start=(c == 0), stop=(c == 3))
                                    return ins
                                s.op("pe", f, reads=[wdb_, hid_b], writes=[pdb])
                                s.op("dve", lambda e, pd=pd, oc=oc, t0=t0, n=n, col=col: e.scalar_tensor_tensor(
                                    xres[:, oc, t0:t0 + n], pd[:, 0:n], modv[:, l, 40 + oc, col:col + 1], xres[:, oc, t0:t0 + n], ALU.mult, ALU.add),
                                    reads=[pdb, xr_bufs[gi_], modv_b], writes=[xr_bufs[gi_]])
                chk("L%d_moe_e" % l)
                if not last:
                    xwsem = [s.dma_sem() for _ in TG]
                    for (gi_, t0, n, isc) in tg_moe:
                        s.dma("sp", xwsem[gi_], xT[b, :, :, t0:t0 + n], xres[:, :, t0:t0 + n], reads=[xr_bufs[gi_]], writes=[xT_b[b][ti] for ti in tiles_of(t0, n)])
                    if b == 0 and l == 0:
                        dscr("x2", xT[0], xT_b[0])
                else:
                    A.reset(m_exp)
                    fin_slots = []
                    for i in range(2):
                        y_, (yb_,) = A.alloc(KC * 512, 1, "yfin%d" % i)
                        fin_slots.append((y_.rearrange("p (c n) -> p c n", c=KC), yb_))
                    rsf_ap, (rsf_b,) = A.alloc(512, 1, "rsf")
                    ot_slots = []
                    for i in range(2):
                        o_, (ob_,) = A.alloc(D, 1, "ot%d" % i)
                        ot_slots.append((o_, ob_, s.dma_sem()))
                    oti = 0
                    for fi, (gi_, t0, n, isc) in enumerate(tg_moe):
                        y_, yb_ = fin_slots[fi % 2]
                        yf = y_[:, :, 0:n]
                        s.op("act", lambda e, yf=yf, t0=t0, n=n: e.activation(out=yf, in_=xres[:, :, t0:t0 + n], func=AF.Square), reads=[xr_bufs[gi_]], writes=[yb_])
                        pss, pssb = ps_a()

                        def f(e, pss=pss, yf=yf, n=n):
                            ins = None
                            for c in range(KC):
                                ins = e.matmul(pss[:, 0:n], ones_ap, yf[:, c, :], start=(c == 0), stop=(c == KC - 1))
                            return ins
                        s.op("pe", f, reads=[yb_, ones_b], writes=[pssb])
                        act_rstd(rsf_ap[:, 0:n], pss[:, 0:n], 1.0 / D, [pssb], [rsf_b])
                        chk("fin_a")
                        for c in range(KC):
                            s.op("dve", lambda e, yf=yf, c=c, t0=t0, n=n: e.scalar_tensor_tensor(yf[:, c, :], xres[:, c, t0:t0 + n], gfv[:, c:c + 1], rsf_ap[:, 0:n], ALU.mult, ALU.mult),
                                 reads=[xr_bufs[gi_], rsf_b, small_b, yb_], writes=[yb_])
                        chk("fin_b")
                        for tj in range(n // 128):
                            o_, ob_, osem_ = ot_slots[oti % 2]
                            oti += 1
                            for half in range(2):
                                pt, ptb = ps_a()

                                def f(e, pt=pt, yf=yf, tj=tj, half=half):
                                    ins = None
                                    for j in range(4):
                                        c = half * 4 + j
                                        ins = e.transpose(pt[:, j * 128:(j + 1) * 128], yf[:, c, tj * 128:(tj + 1) * 128], ident_ap)
                                    return ins
                                s.op("pe", f, reads=[yb_, ident_b], writes=[ptb])
                                if half == 0:
                                    s.op("act", lambda e, o_=o_, pt=pt: e.copy(o_[:, 0:512], pt[:, :]), reads=[ptb], writes=[ob_])
                                else:
                                    s.op("dve", lambda e, o_=o_, pt=pt: e.tensor_copy(o_[:, 512:1024], pt[:, :]), reads=[ptb], writes=[ob_])
                            chk("fin_c")
                            tl = t0 - C + tj * 128
                            out_toks.append(s.dma("sp", osem_, out[b, tl:tl + 128, :], o_, reads=[ob_]))
                chk("L%d_moe" % l)
                A.reset(mk)
                A.reset(mL)

        s.dead = False
        s.wait_only("sp", out_toks + dbg_sem)
        s.final_barrier("sp")
        s.emit()
        print("program: ins=%d waits=%d sems=%d arena_peak=%d words" % (s.n_ins, s.n_wait, len(s.semh), A.peak))
    return nc


_CACHE = {}


def _get_program(NB, S, C, L, debug=()):
    stop = os.environ.get("K_STOP") or None
    key = (NB, S, C, L, tuple(debug), stop)
    if key not in _CACHE:
        _CACHE[key] = build_program(NB, S, C, L, debug, stop)
    return _CACHE[key]


def run(inputs, n_cores=8, debug=()):
    x = np.asarray(inputs["x"], np.float32)
    ctx = np.asarray(inputs["ctx"], np.float32)
    c = np.asarray(inputs["c"], np.float32)
    c_ctx = np.asarray(inputs["c_ctx"], np.float32)
    B, S, _ = x.shape
    C = ctx.shape[1]
    L = inputs["w_mod"].shape[0]
    NB = B // n_cores
    nc = _get_program(NB, S, C, L, debug)
    consts = host_constants(C, S)
    shared = {}
    for k in ("w_mod", "b_mod", "g_norm1", "g_norm2", "w_in", "lambda_q1", "lambda_k1", "lambda_q2", "lambda_k2", "g_subln",
              "lb_logits", "g_rec_norm", "w_br_attn", "w_br_rec", "w_out", "w_router", "w_gate", "w_up", "w_down"):
        shared[k] = np.ascontiguousarray(np.asarray(inputs[k], np.float32))
    shared["b_router"] = np.ascontiguousarray(np.asarray(inputs["b_router"], np.float32).reshape(1, NEXP))
    shared["g_final"] = np.ascontiguousarray(np.asarray(inputs["g_final"], np.float32).reshape(1, D))
    shared.update(consts)
    in_maps = []
    for i in range(n_cores):
        m = dict(shared)
        m["x"] = np.ascontiguousarray(x[i * NB:(i + 1) * NB])
        m["ctx"] = np.ascontiguousarray(ctx[i * NB:(i + 1) * NB])
        cv4 = np.zeros((4, D), np.float32)
        cv4[0:NB] = c[i * NB:(i + 1) * NB]
        cv4[NB] = c_ctx
        m["cvec"] = cv4
        in_maps.append(m)
    res = run_bass_kernel_spmd(nc, in_maps, core_ids=list(range(n_cores)))
    outs = np.concatenate([np.asarray(r["out"]) for r in res.results], axis=0)
    return outs, res


def kernel(**inputs):
    out, _ = run(inputs, n_cores=8)
    return out.astype(np.float32)
```

```python
import contextlib
import math
import os
import sys
import numpy as np
import ml_dtypes
import concourse.bass as bass
import concourse.mybir as mybir
from concourse.bass_utils import run_bass_kernel_spmd

F32 = mybir.dt.float32
BF16 = mybir.dt.bfloat16
I32 = mybir.dt.int32
AF = mybir.ActivationFunctionType
ALU = mybir.AluOpType
AX = mybir.AxisListType

DEBUG_ANNOTATE = bool(os.environ.get('K_ANNOTATE'))
DMA_SEM_POOL = int(os.environ.get('K_DSEMS', '1000'))
POOL_TO_DVE = os.environ.get('K_POOLDVE', '0') == '1'
D = 1024
KC = 8
N_IN = 10240
EPS = 1e-6
NEXP = 16
CHUNK = 32
NPT = 128 // CHUNK
GRID_W = 64
OFF_AQ, OFF_AK, OFF_AV, OFF_RQ, OFF_RFF, OFF_RFB, OFF_RI, OFF_RG, OFF_GA, OFF_GR = [1024 * i for i in range(10)]


class Buf:
    __slots__ = ("name", "w", "r", "psum")

    def __init__(self, name="", psum=False):
        self.name = name
        self.w = None
        self.r = {}
        self.psum = psum


class DmaSem:
    __slots__ = ("idx", "h", "total")

    def __init__(self, idx, h):
        self.idx = idx
        self.h = h
        self.total = 0


class _Rec:
    def __init__(self):
        self.calls = []

    def __getattr__(self, name):
        def m(*a, **k):
            self.calls.append((name, a, k))
            return self
        return m


class Sched:
    ENG = ("pe", "act", "dve", "pool", "sp")

    def __init__(self, nc, stack):
        self.nc = nc
        self.stack = stack
        self.semh = []
        self.semidx = {}
        self.cnt = {}
        self.ops = {}
        self.known = {}
        for e in self.ENG:
            self.semidx[e] = self._new_sem("s_" + e)
            self.cnt[e] = 0
            self.ops[e] = []
            self.known[e] = {}
        self.n_wait = 0
        self.n_ins = 0
        self.dead = False

    def _new_sem(self, name):
        h = self.stack.enter_context(self.nc.semaphore(name))
        self.semh.append(h)
        return len(self.semh) - 1

    def begin_iter(self):
        if not hasattr(self, "_ipool"):
            self._ipool = []
        self._iidx = 0

    def dma_sem(self, name=None):
        if not hasattr(self, "_dpool"):
            self._dpool = []
            self._dnext = 0
        if getattr(self, "_iidx", None) is not None:
            if self._iidx < len(self._ipool):
                ds = self._ipool[self._iidx]
            else:
                idx = self._new_sem("d%d" % len(self.semh))
                ds = DmaSem(idx, self.semh[idx])
                self._ipool.append(ds)
                self._dpool.append(ds)
            self._iidx += 1
            return ds
        if len(self._dpool) < DMA_SEM_POOL:
            idx = self._new_sem("d%d" % len(self.semh))
            ds = DmaSem(idx, self.semh[idx])
            self._dpool.append(ds)
            return ds
        ds = self._dpool[self._dnext % len(self._dpool)]
        self._dnext += 1
        return ds

    @property
    def _dsems(self):
        return getattr(self, "_dpool", [])

    def _collect(self, eng, reads, writes, extra=()):
        deps = {}
        own_ = self.semidx[eng]
        for b in reads:
            t = b.w
            if t is not None and deps.get(t[0], 0) < t[1]:
                deps[t[0]] = t[1]
            if b.psum:
                for si, v in b.r.items():
                    if si != own_ and deps.get(si, 0) < v:
                        deps[si] = v
        for b in writes:
            t = b.w
            if t is not None and deps.get(t[0], 0) < t[1]:
                deps[t[0]] = t[1]
            for si, v in b.r.items():
                if deps.get(si, 0) < v:
                    deps[si] = v
        for t in extra:
            if t is not None and deps.get(t[0], 0) < t[1]:
                deps[t[0]] = t[1]
        own = self.semidx[eng]
        kn = self.known[eng]
        waits = []
        for si, v in deps.items():
            if eng == "pe" and si == own:
                continue
            if kn.get(si, 0) >= v:
                continue
            kn[si] = v
            waits.append((si, v))
        self.n_wait += len(waits)
        return waits

    def _mark(self, tok, reads, writes):
        si, v = tok
        for b in reads:
            if b.r.get(si, 0) < v:
                b.r[si] = v
        for b in writes:
            b.w = tok
            b.r = {}

    def op(self, eng, fn, reads=(), writes=(), extra=()):
        if self.dead:
            return (0, 0)
        if eng == "pool" and POOL_TO_DVE:
            eng = "dve"
        rec = _Rec()
        fn(rec)
        calls = rec.calls
        lineno = sys._getframe(1).f_lineno

        def fn(e, calls=calls, lineno=lineno):
            ins = None
            for name, a, k in calls:
                ins = getattr(e, name)(*a, **k)
                if DEBUG_ANNOTATE:
                    ins.annotate("L%d" % lineno)
            return ins
        waits = self._collect(eng, reads, writes, extra)
        self.cnt[eng] += 1
        tok = (self.semidx[eng], self.cnt[eng])
        self.ops[eng].append((waits, fn, tok[0]))
        self._mark(tok, reads, writes)
        self.n_ins += 1
        return tok

    def dma(self, queue, dsem, out, in_, reads=(), writes=(), extra=()):
        if self.dead:
            return (0, 0)
        if dsem.total > 0:
            extra = tuple(extra) + ((dsem.idx, dsem.total),)
        waits = self._collect(queue, reads, writes, extra)
        dsem.total += 16
        tok = (dsem.idx, dsem.total)
        h = dsem.h

        def fn(e, out=out, in_=in_, h=h):
            e.dma_start(out=out, in_=in_).then_inc(h, 16)
            return None

        self.ops[queue].append((waits, fn, None))
        self._mark(tok, reads, writes)
        self.n_ins += 1
        return tok

    def wait_only(self, eng, toks):
        waits = self._collect(eng, (), (), toks)
        self.ops[eng].append((waits, None, None))

    def final_barrier(self, eng="sp"):
        toks = [(self.semidx[e], self.cnt[e]) for e in self.ENG if self.cnt[e] > 0 and e != eng]
        for ds in self._dsems:
            if ds.total > 0:
                toks.append((ds.idx, ds.total))
        self.wait_only(eng, toks)

    def emit(self):
        nc = self.nc
        semh = self.semh
        if os.environ.get("K_SEMCLR", "1") == "1":
            for h in semh:
                nc.sync.sem_clear(h)
            nc.all_engine_barrier()
        with nc.Block() as block:
            def mk(name):
                ops = self.ops[name]

                def body(e):
                    for waits, fn, si in ops:
                        for (wi, v) in waits:
                            e.wait_ge(semh[wi], v)
                        if fn is None:
                            continue
                        ins = fn(e)
                        if si is not None:
                            ins.then_inc(semh[si], 1)
                return body

            block.tensor(mk("pe"))
            block.scalar(mk("act"))
            block.vector(mk("dve"))
            block.gpsimd(mk("pool"))
            block.sync(mk("sp"))


class Arena:
    def __init__(self, ap, words):
        self.ap = ap
        self.words = words
        self.top = 0
        self.hist = []
        self.peak = 0

    def mark(self):
        return self.top

    def reset(self, m):
        self.top = m

    def alloc(self, words, nbuf=1, name=""):
        words = (words + 1) // 2 * 2
        st, en = self.top, self.top + words
        assert en <= self.words, "SBUF arena overflow %s %d" % (name, en)
        self.top = en
        self.peak = max(self.peak, en)
        bufs = [Buf(name) for _ in range(nbuf)]
        inherit = {}
        keep = []
        for (a, b, bl) in self.hist:
            if a < en and st < b:
                for ob in bl:
                    if ob.w is not None and inherit.get(ob.w[0], 0) < ob.w[1]:
                        inherit[ob.w[0]] = ob.w[1]
                    for si, v in ob.r.items():
                        if inherit.get(si, 0) < v:
                            inherit[si] = v
                if not (st <= a and b <= en):
                    keep.append((a, b, bl))
            else:
                keep.append((a, b, bl))
        self.hist = keep
        for nb in bufs:
            nb.r = dict(inherit)
        self.hist.append((st, en, bufs))
        return self.ap[:, st:en], bufs


def host_constants(C, S):
    T = C + S
    ident = np.eye(128, dtype=np.float32)
    inv = (10000.0 ** (-np.arange(16, dtype=np.float32) / 16.0)).astype(np.float32)
    t = np.arange(S)
    row = (t // GRID_W).astype(np.float32)
    col = (t % GRID_W).astype(np.float32)
    cos = np.ones((128, T), np.float32)
    sin = np.zeros((128, T), np.float32)
    swap = np.zeros((128, 128), np.float32)
    for p in range(128):
        j = p % 64
        base = row if j < 32 else col
        ang = (base * inv[j % 16]).astype(np.float32)
        first = (j % 32) < 16
        cos[p, C:] = np.cos(ang)
        sin[p, C:] = (-np.sin(ang)) if first else np.sin(ang)
        sp = p + 16 if first else p - 16
        swap[sp, p] = 1.0
    s_i = np.arange(128)[:, None]
    t_i = np.arange(128)[None, :]
    same = (s_i // CHUNK) == (t_i // CHUNK)
    maskf = (same & (s_i <= t_i)).astype(np.float32)
    maskb = (same & (s_i >= t_i)).astype(np.float32)
    scanm = np.ones((128, T), np.float32)
    scanm[:, ::CHUNK] = 0.0
    cmask = np.zeros((128, NPT), np.float32)
    for p in range(128):
        cmask[p, p // CHUNK] = 1.0
    onehot = np.zeros((16, 16, 128), np.float32)
    for e in range(16):
        onehot[e, e, :] = 1.0
    return dict(c_ident=ident, c_cos=cos, c_sin=sin, c_swap=swap, c_maskf=maskf, c_maskb=maskb,
                c_scanm=scanm, c_onehot=onehot.reshape(16, 16 * 128), c_cmask=cmask)


def build_program(NB, S, C, L, debug=(), stop=None):
    T = C + S
    NT = T // 128
    NCT = C // 128
    NCH = T // CHUNK
    TG = [(0, C, True)]
    gsz = min(512, S)
    for g in range(S // gsz):
        TG.append((C + g * gsz, gsz, False))
    MGN = 256 if S >= 256 else S
    TGM = []
    for st_ in range(0, C, min(MGN, C)):
        TGM.append((st_, min(MGN, C), True))
    for st_ in range(C, T, MGN):
        TGM.append((st_, MGN, False))

    nc = bass.Bass("TRN2", target_bir_lowering=False)

    def din(name, shape, dt=F32):
        return nc.dram_tensor(name, list(shape), dt, kind="ExternalInput").ap()

    x_in = din("x", [NB, S, D])
    ctx_in = din("ctx", [NB, C, D])
    cvec = din("cvec", [4, D])
    w_mod = din("w_mod", [L, D, 6 * D])
    b_mod = din("b_mod", [L, 6 * D])
    g_norm1 = din("g_norm1", [L, D])
    g_norm2 = din("g_norm2", [L, D])
    w_in = din("w_in", [L, D, N_IN])
    lam_q1 = din("lambda_q1", [L, 64])
    lam_k1 = din("lambda_k1", [L, 64])
    lam_q2 = din("lambda_q2", [L, 64])
    lam_k2 = din("lambda_k2", [L, 64])
    g_subln = din("g_subln", [L, 128])
    lb_logits = din("lb_logits", [L, D])
    g_rec = din("g_rec_norm", [L, 128])
    w_bra = din("w_br_attn", [L, D, D])
    w_brr = din("w_br_rec", [L, D, D])
    w_out = din("w_out", [L, D, D])
    w_router = din("w_router", [D, NEXP])
    b_router = din("b_router", [1, NEXP])
    w_gate = din("w_gate", [L, NEXP, D, D])
    w_up = din("w_up", [L, NEXP, D, D])
    w_down = din("w_down", [L, NEXP, D, D])
    g_final = din("g_final", [1, D])
    c_ident = din("c_ident", [128, 128])
    c_cos = din("c_cos", [128, T])
    c_sin = din("c_sin", [128, T])
    c_swap = din("c_swap", [128, 128])
    c_maskf = din("c_maskf", [128, 128])
    c_maskb = din("c_maskb", [128, 128])
    c_scanm = din("c_scanm", [128, T])
    c_onehot = din("c_onehot", [16, 16 * 128])
    c_cmask = din("c_cmask", [128, NPT])

    out = nc.dram_tensor("out", [NB, S, D], F32, kind="ExternalOutput").ap()
    xT = nc.dram_tensor("xT_scr", [NB, 128, KC, T], F32, kind="Internal").ap()
    aT = nc.dram_tensor("aT_scr", [128, KC, T], BF16, kind="Internal").ap()
    rT = nc.dram_tensor("rT_scr", [128, KC, T], BF16, kind="Internal").ap()
    dbg_out = {}

    st = contextlib.ExitStack()
    with st:
        s = Sched(nc, st)
        AW = int(os.environ.get("K_AW", 52224))
        arena_t = st.enter_context(nc.sbuf_tensor("arena", [128, AW], F32))
        A = Arena(arena_t, AW)
        psum = [st.enter_context(nc.psum_tensor("ps%d" % i, [128, 512], F32)) for i in range(8)]
        psb = [Buf("ps%d" % i, psum=True) for i in range(8)]
        rr = {"a": 0, "b": 0, "bn": 2}

        def ps_a():
            i = rr["a"] % 4
            rr["a"] += 1
            return psum[i], psb[i]

        def ps_b():
            i = 4 + rr["b"] % rr["bn"]
            rr["b"] += 1
            return psum[i], psb[i]

        def bf(ap):
            return ap.bitcast(BF16)

        dbg_sem = []
        dbg_ds = [None]

        def chk(name):
            if stop == name:
                s.dead = True

        def _dbg_ds():
            if dbg_ds[0] is None:
                dbg_ds[0] = s.dma_sem()
            return dbg_ds[0]

        def dbg(name, ap, bufs):
            if name not in debug:
                return
            shp = list(ap.shape)
            o = nc.dram_tensor("dbg_" + name, shp, ap.dtype, kind="ExternalOutput").ap()
            dbg_out[name] = o
            ds = _dbg_ds()
            tok = s.dma("sp", ds, o, ap, reads=bufs)
            dbg_sem.append(tok)

        def dscr(name, dram_ap, dram_bufs):
            if name not in debug:
                return
            o = nc.dram_tensor("dbg_" + name, list(dram_ap.shape), dram_ap.dtype, kind="ExternalOutput").ap()
            ds = _dbg_ds()
            tok = s.dma("sp", ds, o, dram_ap, reads=dram_bufs)
            dbg_sem.append(tok)

        ident_ap, (ident_b,) = A.alloc(128, 1, "ident")
        identb_w, (identb_b,) = A.alloc(64, 1, "identb")
        identb = bf(identb_w)
        ones_ap, (ones_b,) = A.alloc(128, 1, "ones")
        onesb_w, (onesb_b,) = A.alloc(64, 1, "onesb")
        onesb = bf(onesb_w)
        swap_w, (swap_b,) = A.alloc(64, 1, "swap")
        swapm = bf(swap_w)
        maskf_w, (maskf_b,) = A.alloc(64, 1, "maskf")
        maskb_w, (maskb_b,) = A.alloc(64, 1, "maskb")
        maskf = bf(maskf_w)
        maskb = bf(maskb_w)
        NCOL = NB + 1
        modv_ap, (modv_b,) = A.alloc(L * 48 * NCOL, 1, "modv")
        modv = modv_ap.rearrange("p (l c n) -> p l c n", l=L, c=48)
        a1_ap, (a1_b,) = A.alloc(L * KC * NCOL, 1, "a1")
        a1 = a1_ap.rearrange("p (l c n) -> p l c n", l=L, c=KC)
        a2_ap, (a2_b,) = A.alloc(L * KC * NCOL, 1, "a2")
        a2 = a2_ap.rearrange("p (l c n) -> p l c n", l=L, c=KC)
        small_ap, (small_b,) = A.alloc(256, 1, "small")
        lbv_ap, (lbv_b,) = A.alloc(L * 8 * 3, 1, "lbv")
        lbv = lbv_ap.rearrange("p (l h k) -> p l h k", l=L, h=8)
        lamv_ap, (lamv_b,) = A.alloc(L * 4, 1, "lamv")
        lamv = lamv_ap.rearrange("p (l k) -> p l k", l=L)
        brt_ap, (brt_b,) = A.alloc(16, 1, "brt")
        wr_w, (wr_b,) = A.alloc(KC * 16 // 2, 1, "wr")
        wrt = bf(wr_w).rearrange("p (c e) -> p c e", c=KC)
        eps_ap, (eps_b,) = A.alloc(2, 1, "eps")
        cmask_ap, (cmask_b,) = A.alloc(NPT, 1, "cmask")

        cs = [s.dma_sem() for _ in range(4)]
        s.dma("sp", cs[0], ident_ap, c_ident, writes=[ident_b])
        s.dma("sp", cs[0], cmask_ap, c_cmask, writes=[cmask_b])
        hm_ap, (hm_b,) = A.alloc(2, 1, "hmask")
        s.op("dve", lambda e: e.tensor_tensor(hm_ap[:, 0:1], cmask_ap[:, 0:1], cmask_ap[:, 1:2], ALU.add), reads=[cmask_b], writes=[hm_b])
        s.op("dve", lambda e: e.tensor_tensor(hm_ap[:, 1:2], cmask_ap[:, 2:3], cmask_ap[:, 3:4], ALU.add), reads=[cmask_b], writes=[hm_b])
        cs2 = [s.dma_sem() for _ in range(5)]
        s.dma("pool", cs2[0], swapm, c_swap, writes=[swap_b])
        s.dma("pool", cs2[1], maskf, c_maskf, writes=[maskf_b])
        s.dma("pool", cs2[2], maskb, c_maskb, writes=[maskb_b])
        s.dma("pool", cs2[4], wrt, w_router.rearrange("(c p) e -> p c e", p=128), writes=[wr_b])
        s.op("pool", lambda e: e.memset(ones_ap, 1.0), writes=[ones_b])
        s.op("pool", lambda e: e.memset(lamv_ap, 0.0), writes=[lamv_b])
        s.op("pool", lambda e: e.memset(onesb, 1.0), writes=[onesb_b])
        s.op("pool", lambda e: e.memset(eps_ap, EPS), writes=[eps_b])
        s.op("act", lambda e: e.copy(identb, ident_ap), reads=[ident_b], writes=[identb_b])

        chk("c0")
        m0 = A.mark()
        stA_ap, (stA_b,) = A.alloc(128, 1, "stA")
        stB_ap, (stB_b,) = A.alloc(128, 1, "stB")
        cv_ap, (cv_b,) = A.alloc(D, 1, "cv")
        scT_ap, (scT_b,) = A.alloc(KC * NCOL, 1, "scT")
        scT = scT_ap.rearrange("p (c n) -> p c n", c=KC)
        lamst_ap, (lamst_b,) = A.alloc(4 * L * 64 + 2 * L * 128, 1, "lamst")
        p0s = [s.dma_sem() for _ in range(4)]
        s.dma("sp", p0s[0], stA_ap[0:L * 48, :], b_mod.rearrange("l (c p) -> (l c) p", p=128), writes=[stA_b])
        s.dma("sp", p0s[1], stB_ap[0:L * 8, :], g_norm1.rearrange("l (c p) -> (l c) p", p=128), writes=[stB_b])
        s.dma("sp", p0s[1], stB_ap[16:16 + L * 8, :], g_norm2.rearrange("l (c p) -> (l c) p", p=128), writes=[stB_b])
        s.dma("sp", p0s[1], stB_ap[32:40, :], g_final.rearrange("o (c p) -> (o c) p", p=128), writes=[stB_b])
        s.dma("sp", p0s[1], stB_ap[40:40 + L * 8, :], lb_logits.rearrange("l (c p) -> (l c) p", p=128), writes=[stB_b])
        s.dma("sp", p0s[2], cv_ap[0:4, :], cvec, writes=[cv_b])
        lam_srcs = [lam_q1, lam_k1, lam_q2, lam_k2]
        for i, src in enumerate(lam_srcs):
            s.dma("sp", p0s[3], lamst_ap[:, i * L * 64:(i + 1) * L * 64],
                  src.rearrange("l d -> (l d)").partition_broadcast(128), writes=[lamst_b])
        s.dma("sp", p0s[3], brt_ap, b_router.rearrange("o e -> (o e)").partition_broadcast(128), writes=[brt_b])
        pst, pstb = ps_a()
        s.op("pe", lambda e: e.transpose(pst[:, 0:96], stA_ap[0:96, :], ident_ap[0:96, 0:96]),
             reads=[stA_b, ident_b], writes=[pstb])
        s.op("dve", lambda e: e.tensor_copy(small_ap[:, 0:96], pst[:, 0:96]), reads=[pstb], writes=[small_b])
        pst2, pstb2 = ps_a()
        s.op("pe", lambda e: e.transpose(pst2[:, 0:56], stB_ap[0:56, :], ident_ap[0:56, 0:56]),
             reads=[stB_b, ident_b], writes=[pstb2])
        s.op("dve", lambda e: e.tensor_copy(small_ap[:, 96:152], pst2[:, 0:56]), reads=[pstb2], writes=[small_b])
        chk("p0a")
        g1v = small_ap[:, 96:112].rearrange("p (l c) -> p l c", l=2)
        g2v = small_ap[:, 112:128].rearrange("p (l c) -> p l c", l=2)
        gfv = small_ap[:, 128:136]
        lbl = small_ap[:, 136:152].rearrange("p (l c) -> p l c", l=2)
        bmv = small_ap[:, 0:96].rearrange("p (l c) -> p l c", l=2)
        for l in range(L):
            s.dma("sp", p0s[3], lamv[:, l, 1:2], g_subln[l:l + 1, :].rearrange("o p -> p o"), writes=[lamv_b])
            s.dma("sp", p0s[3], lamv[:, l, 2:3], g_rec[l:l + 1, :].rearrange("o p -> p o"), writes=[lamv_b])
        lt_ap, (lt_b,) = A.alloc(L * 64 * 2 + 8 * L, 1, "lamtmp")
        for l in range(L):
            lam_init = 0.8 - 0.6 * math.exp(-0.3 * l)
            for j in range(2):
                qv = lamst_ap[:, (2 * j) * L * 64 + l * 64:(2 * j) * L * 64 + (l + 1) * 64]
                kv = lamst_ap[:, (2 * j + 1) * L * 64 + l * 64:(2 * j + 1) * L * 64 + (l + 1) * 64]
                pr = lt_ap[:, (l * 2 + j) * 64:(l * 2 + j + 1) * 64]
                sm = lt_ap[:, L * 128 + l * 4 + j:L * 128 + l * 4 + j + 1]
                s.op("dve", lambda e, pr=pr, qv=qv, kv=kv: e.tensor_tensor(pr, qv, kv, ALU.mult), reads=[lamst_b], writes=[lt_b])
                s.op("dve", lambda e, pr=pr, sm=sm: e.tensor_reduce(sm, pr, AX.X, ALU.add), reads=[lt_b], writes=[lt_b])
                s.op("act", lambda e, sm=sm: e.activation(out=sm, in_=sm, func=AF.Exp), reads=[lt_b], writes=[lt_b])
            e1 = lt_ap[:, L * 128 + l * 4:L * 128 + l * 4 + 1]
            e2 = lt_ap[:, L * 128 + l * 4 + 1:L * 128 + l * 4 + 2]
            s.op("dve", lambda e, l=l, e1=e1, e2=e2, li=lam_init: e.scalar_tensor_tensor(
                lamv[:, l, 0:1], e2, -li, e1, ALU.add, ALU.subtract), reads=[lt_b], writes=[lamv_b])
            s.op("dve", lambda e, l=l, li=lam_init: e.tensor_scalar(lamv[:, l, 1:2], lamv[:, l, 1:2], 1.0 - li, None, ALU.mult),
                 reads=[lamv_b], writes=[lamv_b])
        chk("p0b")
        lbe_ap, (lbe_b,) = A.alloc(L * 8 + 16, 1, "lbe")
        lbe = lbe_ap[:, 0:L * 8].rearrange("p (l c) -> p l c", l=L)
        lsum = lbe_ap[:, L * 8:L * 8 + 8]
        s.op("act", lambda e: e.activation(out=lbe_ap[:, 0:L * 8], in_=small_ap[:, 136:136 + L * 8], func=AF.Exp), reads=[small_b], writes=[lbe_b])
        s.op("dve", lambda e: e.tensor_copy(lsum, lbe[:, 0, :]), reads=[lbe_b], writes=[lbe_b])
        for l in range(1, L):
            s.op("dve", lambda e, l=l: e.tensor_tensor(lsum, lsum, lbe[:, l, :], ALU.add), reads=[lbe_b], writes=[lbe_b])
        s.op("dve", lambda e: e.reciprocal(lsum, lsum), reads=[lbe_b], writes=[lbe_b])
        s.op("pool", lambda e: e.memset(lbv[:, 0, :, 0], 0.0), writes=[lbv_b])
        for l in range(1, L):
            s.op("dve", lambda e, l=l: e.tensor_tensor(lbe[:, l, :], lbe[:, l, :], lsum, ALU.mult), reads=[lbe_b], writes=[lbe_b])
            s.op("dve", lambda e, l=l: e.tensor_tensor(lbv[:, l, :, 0], lbv[:, l - 1, :, 0], lbe[:, l, :], ALU.add),
                 reads=[lbe_b, lbv_b], writes=[lbv_b])
        for l in range(L):
            s.op("dve", lambda e, l=l: e.tensor_scalar(lbv[:, l, :, 1], lbv[:, l, :, 0], -1.0, 1.0, ALU.mult, ALU.add),
                 reads=[lbv_b], writes=[lbv_b])
            s.op("dve", lambda e, l=l: e.tensor_scalar(lbv[:, l, :, 2], lbv[:, l, :, 0], 1.0, -1.0, ALU.mult, ALU.add),
                 reads=[lbv_b], writes=[lbv_b])
        chk("p0c")
        s.op("act", lambda e: e.activation(out=cv_ap[0:NCOL, :], in_=cv_ap[0:NCOL, :], func=AF.Silu), reads=[cv_b], writes=[cv_b])
        pst3, pstb3 = ps_a()

        def f_ct(e):
            ins = None
            for c in range(KC):
                ins = e.transpose(pst3[:, c * NCOL:(c + 1) * NCOL], cv_ap[0:NCOL, c * 128:(c + 1) * 128], ident_ap[0:NCOL, 0:NCOL])
            return ins
        s.op("pe", f_ct, reads=[cv_b, ident_b], writes=[pstb3])
        s.op("dve", lambda e: e.tensor_copy(scT_ap, pst3[:, 0:KC * NCOL]), reads=[pstb3], writes=[scT_b])
        chk("p0d")
        wm_slots = []
        for i in range(2):
            ap_, (b_,) = A.alloc(KC * 512, 1, "wmod%d" % i)
            wm_slots.append((ap_.rearrange("p (c n) -> p c n", c=KC), b_, s.dma_sem()))
        gi = 0
        for l in range(L):
            pm, pmb = ps_a()
            for g in range(12):
                wt, wb, wsem = wm_slots[gi % 2]
                gi += 1
                s.dma("sp", wsem, wt, w_mod[l].rearrange("(c p) n -> p c n", p=128)[:, :, g * 512:(g + 1) * 512], writes=[wb])

                def f_mod(e, wt=wt, g=g, pm=pm):
                    ins = None
                    for j in range(4):
                        oc = g * 4 + j
                        for c in range(KC):
                            ins = e.matmul(pm[:, oc * NCOL:(oc + 1) * NCOL], wt[:, c, j * 128:(j + 1) * 128], scT[:, c, :],
                                           start=(c == 0), stop=(c == KC - 1))
                    return ins
                s.op("pe", f_mod, reads=[wb, scT_b], writes=[pmb])
            if l == 0:
                chk("p0e")
            s.op("dve", lambda e, l=l, pm=pm: e.tensor_tensor(
                modv[:, l], pm[:, 0:48 * NCOL].rearrange("p (c n) -> p c n", c=48),
                bmv[:, l, :].unsqueeze(2).to_broadcast([128, 48, NCOL]), ALU.add),
                reads=[pmb, small_b], writes=[modv_b])
            if l == 0:
                chk("p0f")
            for (dst, gv, off) in ((a1, g1v, 8), (a2, g2v, 32)):
                for col_ in range(NCOL):
                    s.op("dve", lambda e, l=l, dst=dst, gv=gv, off=off, col_=col_: e.scalar_tensor_tensor(
                        dst[:, l, :, col_], modv[:, l, off:off + 8, col_], 1.0, gv[:, l, :], ALU.add, ALU.mult),
                        reads=[modv_b, small_b], writes=[a1_b if dst is a1 else a2_b])
        dbg("modv", modv_ap, [modv_b])
        dbg("a1", a1_ap, [a1_b])
        dbg("lamv", lamv_ap, [lamv_b])
        dbg("lbv", lbv_ap, [lbv_b])
        A.reset(m0)
        chk("p0")

        xT_b = [[Buf("xT%d_%d" % (b, g)) for g in range(T // 128)] for b in range(NB)]
        aT_b = [Buf("aT%d" % h) for h in range(8)]
        rT_b = [Buf("rT%d" % h) for h in range(8)]

        def tiles_of(t0, n):
            return range(t0 // 128, (t0 + n) // 128)

        def act_rstd(dst, src_ps, inv_n, rd, wr):
            s.op("act", lambda e: e.activation(out=dst, in_=src_ps, func=AF.Ln, bias=eps_ap[:, 0:1], scale=inv_n), reads=rd + [eps_b], writes=wr)
            s.op("act", lambda e: e.activation(out=dst, in_=dst, func=AF.Exp, scale=-0.5), reads=wr, writes=wr)

        def norm_mod(xg, xg_b, n, hdst, hdst_b, av, sv, av_b):
            mk = A.mark()
            sq_ap, (sq_b,) = A.alloc(KC * n, 1, "sq")
            sq = sq_ap.rearrange("p (c n) -> p c n", c=KC)
            rs_ap, (rs_b,) = A.alloc(n, 1, "rstd")
            s.op("act", lambda e: e.activation(out=sq, in_=xg, func=AF.Square), reads=[xg_b], writes=[sq_b])
            pss, pssb = ps_a()

            def f(e):
                ins = None
                for c in range(KC):
                    ins = e.matmul(pss[:, 0:n], ones_ap, sq[:, c, :], start=(c == 0), stop=(c == KC - 1))
                return ins
            s.op("pe", f, reads=[sq_b, ones_b], writes=[pssb])
            act_rstd(rs_ap, pss[:, 0:n], 1.0 / D, [pssb], [rs_b])
            for c in range(KC):
                tmp = sq[:, c, :]
                s.op("dve", lambda e, c=c, tmp=tmp: e.scalar_tensor_tensor(tmp, xg[:, c, :], av[:, c:c + 1], rs_ap, ALU.mult, ALU.mult),
                     reads=[xg_b, rs_b, av_b], writes=[sq_b])
                s.op("act", lambda e, c=c, tmp=tmp: e.activation(out=hdst[:, c, :], in_=tmp, func=AF.Identity, bias=sv[:, c:c + 1], scale=1.0),
                     reads=[sq_b, av_b], writes=[hdst_b])
            A.reset(mk)

        def load_w(dst, dst_b, sem, src):
            return s.dma("pool", sem, dst, src, writes=[dst_b])

        m1 = A.mark()
        xin_slots = []
        for i in range(2):
            ap_, (b_,) = A.alloc(D, 1, "xin%d" % i)
            xo_, (ob_,) = A.alloc(D, 1, "xo%d" % i)
            xin_slots.append((ap_, b_, s.dma_sem(), xo_, ob_, s.dma_sem()))
        k = 0
        for b in range(NB):
            for ti in range(NT):
                ap_, b_, sm_, xo_, ob_, osm_ = xin_slots[k % 2]
                k += 1
                src = ctx_in[b, ti * 128:(ti + 1) * 128, :] if ti < NCT else x_in[b, (ti - NCT) * 128:(ti - NCT + 1) * 128, :]
                s.dma("sp", sm_, ap_, src, writes=[b_])
                for half in range(2):
                    pt, ptb = ps_a()

                    def f(e, ap_=ap_, pt=pt, half=half):
                        ins = None
                        for j in range(4):
                            c = half * 4 + j
                            ins = e.transpose(pt[:, j * 128:(j + 1) * 128], ap_[:, c * 128:(c + 1) * 128], ident_ap)
                        return ins
                    s.op("pe", f, reads=[b_, ident_b], writes=[ptb])
                    eng = "act" if half == 0 else "dve"
                    if eng == "act":
                        s.op("act", lambda e, xo_=xo_, pt=pt, half=half: e.copy(xo_[:, half * 512:(half + 1) * 512], pt[:, :]), reads=[ptb], writes=[ob_])
                    else:
                        s.op("dve", lambda e, xo_=xo_, pt=pt, half=half: e.tensor_copy(xo_[:, half * 512:(half + 1) * 512], pt[:, :]), reads=[ptb], writes=[ob_])
                s.dma("sp", osm_, xT[b, :, :, ti * 128:(ti + 1) * 128], xo_.rearrange("p (c t) -> p c t", c=KC), reads=[ob_], writes=[xT_b[b][ti]])
        A.reset(m1)
        chk("x0")

        out_toks = []
        for b in range(NB):
            for l in range(L):
                last = (l == L - 1)
                s.begin_iter()
                mL = A.mark()
                h_w, h_bufs = A.alloc(KC * T // 2, len(TG), "h")
                hT = bf(h_w).rearrange("p (c t) -> p c t", c=KC)
                hb_of = {}
                for gi_, (t0, n, isc) in enumerate(TG):
                    for ti in tiles_of(t0, n):
                        hb_of[ti] = h_bufs[gi_]

                def hbufs(t0, n):
                    return list({id(hb_of[ti]): hb_of[ti] for ti in tiles_of(t0, n)}.values())

                mk = A.mark()
                xg_slots = []
                for i in range(2):
                    ap_, (b_,) = A.alloc(KC * 512, 1, "xg%d" % i)
                    xg_slots.append((ap_, b_, s.dma_sem()))
                for gi_, (t0, n, isc) in enumerate(TG):
                    ap_, b_, sm_ = xg_slots[gi_ % 2]
                    xg = ap_[:, 0:KC * n].rearrange("p (c n) -> p c n", c=KC)
                    s.dma("sp", sm_, xg, xT[b, :, :, t0:t0 + n], reads=[xT_b[b][ti] for ti in tiles_of(t0, n)], writes=[b_])
                    col = NB if isc else b
                    norm_mod(xg, b_, n, hT[:, :, t0:t0 + n], h_bufs[gi_], a1[:, l, :, col], modv[:, l, 0:8, col], a1_b)
                A.reset(mk)
                if b == 0 and l == 0:
                    dbg("h", h_w, h_bufs)
                chk("n1")

                mk = A.mark()
                cos_ap, (cos_b,) = A.alloc(T, 1, "cos")
                sin_ap, (sin_b,) = A.alloc(T, 1, "sin")
                s.dma("sp", cs[1], cos_ap, c_cos, writes=[cos_b])
                s.dma("sp", cs[2], sin_ap, c_sin, writes=[sin_b])
                wq_w, (wq_b,) = A.alloc(KC * 128 // 2, 1, "wq")
                wk_w, (wk_b,) = A.alloc(KC * 128 // 2, 1, "wk")
                wv_w, (wv_b,) = A.alloc(KC * 128 // 2, 1, "wv")
                wq = bf(wq_w).rearrange("p (c n) -> p c n", c=KC)
                wk = bf(wk_w).rearrange("p (c n) -> p c n", c=KC)
                wv = bf(wv_w).rearrange("p (c n) -> p c n", c=KC)
                wsem = [s.dma_sem() for _ in range(3)]
                qr_w, (qr_b,) = A.alloc(T // 2, 1, "qr")
                qr1_w, (qr1_b,) = A.alloc(T // 2, 1, "qr1")
                kr_w, (kr_b,) = A.alloc(T // 2, 1, "kr")
                qr = bf(qr_w)
                qr1 = bf(qr1_w)
                kr = bf(kr_w)
                v_w, (v_b,) = A.alloc(NT * 128 // 2, 1, "v")
                vv = bf(v_w).rearrange("p (t d) -> p t d", t=NT)
                qb_slots = []
                for i in range(2):
                    ap_, (b_,) = A.alloc(256, 1, "qb%d" % i)
                    t1_, (t1b_,) = A.alloc(512, 1, "t1_%d" % i)
                    t2_, (t2b_,) = A.alloc(512, 1, "t2_%d" % i)
                    qb_slots.append((bf(ap_), b_, t1_, t1b_, t2_, t2b_))
                pT_slots = []
                for i in range(6):
                    ap_, (b_,) = A.alloc(256, 1, "pT%d" % i)
                    pT_slots.append((bf(ap_), b_))
                acc_slots = []
                for i in range(2):
                    ap_, (b_,) = A.alloc(512, 1, "acc%d" % i)
                    acc_slots.append((ap_, b_))
                rc_slots = []
                for i in range(2):
                    ap_, (b_,) = A.alloc(512, 1, "rc%d" % i)
                    rc_slots.append((ap_, b_))
                o_ap, (o_b,) = A.alloc(512, 1, "o")
                osq_ap, (osq_b,) = A.alloc(512, 1, "osq")
                ors_ap, (ors_b,) = A.alloc(512, 1, "ors")
                ao_slots = []
                for i in range(2):
                    ap_, (b_,) = A.alloc(256, 1, "ao%d" % i)
                    ao_slots.append((bf(ap_), b_, s.dma_sem()))
                qk_i = 0
                pt_i = 0
                ao_i = 0
                for hd in range(8):
                    wsrc = w_in[l].rearrange("(c p) n -> p c n", p=128)
                    load_w(wq, wq_b, wsem[0], wsrc[:, :, OFF_AQ + hd * 128:OFF_AQ + (hd + 1) * 128])
                    load_w(wk, wk_b, wsem[1], wsrc[:, :, OFF_AK + hd * 128:OFF_AK + (hd + 1) * 128])
                    load_w(wv, wv_b, wsem[2], wsrc[:, :, OFF_AV + hd * 128:OFF_AV + (hd + 1) * 128])
                    chk("a1")
                    for gi_, (t0, n, isc) in enumerate(TG):
                        for which in range(2):
                            if which == 0 and isc and last:
                                continue
                            wmat, wmb = (wq, wq_b) if which == 0 else (wk, wk_b)
                            dst, dst_b = (qr, qr_b) if which == 0 else (kr, kr_b)
                            qb_, qbb_, t1_, t1b_, t2_, t2b_ = qb_slots[qk_i % 2]
                            qk_i += 1
                            pq, pqb = ps_a()

                            def f(e, pq=pq, wmat=wmat, t0=t0, n=n):
                                ins = None
                                for c in range(KC):
                                    ins = e.matmul(pq[:, 0:n], wmat[:, c, :], hT[:, c, t0:t0 + n], start=(c == 0), stop=(c == KC - 1))
                                return ins
                            s.op("pe", f, reads=[wmb, h_bufs[gi_]], writes=[pqb])
                            s.op("act", lambda e, qb_=qb_, pq=pq, n=n: e.copy(qb_[:, 0:n], pq[:, 0:n]), reads=[pqb], writes=[qbb_])
                            chk("a2")
                            chk("i%d_a2" % qk_i)
                            psw, pswb = ps_a()
                            s.op("pe", lambda e, psw=psw, qb_=qb_, n=n: e.matmul(psw[:, 0:n], swapm, qb_[:, 0:n], start=True, stop=True),
                                 reads=[qbb_, swap_b], writes=[pswb])
                            chk("a3")
                            chk("i%d_a3" % qk_i)
                            s.op("dve", lambda e, t1_=t1_, pq=pq, t0=t0, n=n: e.tensor_tensor(t1_[:, 0:n], pq[:, 0:n], cos_ap[:, t0:t0 + n], ALU.mult),
                                 reads=[pqb, cos_b], writes=[t1b_])
                            s.op("dve", lambda e, t2_=t2_, psw=psw, t0=t0, n=n: e.tensor_tensor(t2_[:, 0:n], psw[:, 0:n], sin_ap[:, t0:t0 + n], ALU.mult),
                                 reads=[pswb, sin_b], writes=[t2b_])
                            chk("a4")
                            chk("i%d_a4" % qk_i)
                            if which == 1:
                                s.op("pool", lambda e, dst=dst, t1_=t1_, t2_=t2_, t0=t0, n=n: e.tensor_tensor(dst[:, t0:t0 + n], t1_[:, 0:n], t2_[:, 0:n], ALU.add),
                                     reads=[t1b_, t2b_], writes=[dst_b])
                            else:
                                s.op("pool", lambda e, t1_=t1_, t2_=t2_, n=n: e.tensor_tensor(t1_[:, 0:n], t1_[:, 0:n], t2_[:, 0:n], ALU.add),
                                     reads=[t1b_, t2b_], writes=[t1b_])
                                s.op("dve", lambda e, t1_=t1_, t0=t0, n=n: e.tensor_scalar(qr[:, t0:t0 + n], t1_[:, 0:n], hm_ap[:, 0:1], None, ALU.mult),
                                     reads=[t1b_, hm_b], writes=[qr_b])
                                s.op("dve", lambda e, t1_=t1_, t0=t0, n=n: e.tensor_scalar(qr1[:, t0:t0 + n], t1_[:, 0:n], hm_ap[:, 1:2], None, ALU.mult),
                                     reads=[t1b_, hm_b], writes=[qr1_b])
                            chk("a5")
                            chk("qk%d" % qk_i)
                    chk("att_a")
                    for t4 in range(0, NT, 4):
                        pv, pvb = ps_a()
                        nt4 = min(4, NT - t4)

                        def f(e, pv=pv, t4=t4, nt4=nt4):
                            ins = None
                            for j in range(nt4):
                                ti = t4 + j
                                for c in range(KC):
                                    ins = e.matmul(pv[:, j * 128:(j + 1) * 128], hT[:, c, ti * 128:(ti + 1) * 128], wv[:, c, :],
                                                   start=(c == 0), stop=(c == KC - 1))
                            return ins
                        s.op("pe", f, reads=[wv_b] + hbufs(t4 * 128, nt4 * 128), writes=[pvb])
                        s.op("act", lambda e, pv=pv, t4=t4, nt4=nt4: e.copy(
                            vv[:, t4:t4 + nt4, :], pv[:, 0:nt4 * 128].rearrange("p (t d) -> p t d", t=nt4)), reads=[pvb], writes=[v_b])
                    if b == 0 and l == 0 and hd == 0:
                        dbg("qr", qr_w, [qr_b])
                        dbg("kr", kr_w, [kr_b])
                        dbg("v", v_w, [v_b])
                    chk("att_b")
                    for gi_, (t0, n, isc) in enumerate(TG):
                        if isc and last:
                            continue
                        kts = range(0, NCT) if isc else range(0, NT)
                        nk = len(kts)
                        po = [psum[6], psum[7]]
                        pob = [psb[6], psb[7]]
                        for ki, kt in enumerate(kts):
                            for m in range(2):
                                pS, pSb = ps_a()
                                qm, qmb = (qr, qr_b) if m == 0 else (qr1, qr1_b)
                                s.op("pe", lambda e, pS=pS, kt=kt, qm=qm, t0=t0, n=n: e.matmul(
                                    pS[:, 0:n], kr[:, kt * 128:(kt + 1) * 128], qm[:, t0:t0 + n],
                                    start=True, stop=True), reads=[kr_b, qmb], writes=[pSb])
                                pT_, pTb_ = pT_slots[pt_i % 6]
                                pt_i += 1
                                s.op("act", lambda e, pT_=pT_, pS=pS, n=n: e.activation(out=pT_[:, 0:n], in_=pS[:, 0:n], func=AF.Exp, scale=0.125),
                                     reads=[pSb], writes=[pTb_])
                                acc_, accb_ = acc_slots[m]
                                if ki == 0:
                                    s.op("dve", lambda e, acc_=acc_, pT_=pT_, n=n: e.tensor_copy(acc_[:, 0:n], pT_[:, 0:n]), reads=[pTb_], writes=[accb_])
                                else:
                                    s.op("dve", lambda e, acc_=acc_, pT_=pT_, n=n: e.tensor_tensor(acc_[:, 0:n], acc_[:, 0:n], pT_[:, 0:n], ALU.add),
                                         reads=[pTb_, accb_], writes=[accb_])
                                s.op("pe", lambda e, m=m, kt=kt, pT_=pT_, n=n, ki=ki, nk=nk: e.matmul(
                                    po[m][:, 0:n], vv[:, kt, :], pT_[:, 0:n], start=(ki == 0), stop=(ki == nk - 1)),
                                    reads=[v_b, pTb_], writes=[pob[m]])
                        chk("att_c")
                        for m in range(2):
                            acc_, accb_ = acc_slots[m]
                            rc_, rcb_ = rc_slots[m]
                            pS, pSb = ps_a()
                            s.op("pe", lambda e, pS=pS, acc_=acc_, n=n: e.matmul(pS[:, 0:n], ones_ap, acc_[:, 0:n], start=True, stop=True),
                                 reads=[accb_, ones_b], writes=[pSb])
                            s.op("act", lambda e, rc_=rc_, pS=pS, n=n: e.activation(out=rc_[:, 0:n], in_=pS[:, 0:n], func=AF.Ln), reads=[pSb], writes=[rcb_])
                            s.op("act", lambda e, rc_=rc_, n=n: e.activation(out=rc_[:, 0:n], in_=rc_[:, 0:n], func=AF.Exp, scale=-1.0), reads=[rcb_], writes=[rcb_])
                            s.op("dve", lambda e, rc_=rc_, m=m, n=n: e.tensor_tensor(rc_[:, 0:n], po[m][:, 0:n], rc_[:, 0:n], ALU.mult),
                                 reads=[pob[m], rcb_], writes=[rcb_])
                        s.op("dve", lambda e, n=n: e.scalar_tensor_tensor(o_ap[:, 0:n], rc_slots[1][0][:, 0:n], lamv[:, l, 0:1], rc_slots[0][0][:, 0:n], ALU.mult, ALU.add),
                             reads=[rc_slots[0][1], rc_slots[1][1], lamv_b], writes=[o_b])
                        s.op("act", lambda e, n=n: e.activation(out=osq_ap[:, 0:n], in_=o_ap[:, 0:n], func=AF.Square), reads=[o_b], writes=[osq_b])
                        pS, pSb = ps_a()
                        s.op("pe", lambda e, pS=pS, n=n: e.matmul(pS[:, 0:n], ones_ap, osq_ap[:, 0:n], start=True, stop=True), reads=[osq_b, ones_b], writes=[pSb])
                        act_rstd(ors_ap[:, 0:n], pS[:, 0:n], 1.0 / 128, [pSb], [ors_b])
                        ao_, aob_, aosem_ = ao_slots[ao_i % 2]
                        ao_i += 1
                        s.op("dve", lambda e, ao_=ao_, n=n: e.scalar_tensor_tensor(ao_[:, 0:n], o_ap[:, 0:n], lamv[:, l, 1:2], ors_ap[:, 0:n], ALU.mult, ALU.mult),
                             reads=[o_b, ors_b, lamv_b], writes=[aob_])
                        chk("att_d")
                        s.dma("sp", aosem_, aT[:, hd, t0:t0 + n], ao_[:, 0:n], reads=[aob_], writes=[aT_b[hd]])
                        chk("att_e")
                A.reset(mk)
                if b == 0 and l == 0:
                    dscr("aT", aT, aT_b)
                chk("att")
                chk("L%d_att" % l)

                mk = A.mark()
                wnames = ["rq", "rff", "rfb", "ri", "rg"]
                woffs = [OFF_RQ, OFF_RFF, OFF_RFB, OFF_RI, OFF_RG]
                wr_ = {}
                for nm in wnames:
                    w_, (wb_,) = A.alloc(KC * 128 // 2, 1, "w" + nm)
                    wr_[nm] = (bf(w_).rearrange("p (c n) -> p c n", c=KC), wb_, s.dma_sem())
                scanm_w, (scanm_b,) = A.alloc(T // 2, 1, "scanm")
                scanm_ap = bf(scanm_w)
                s.dma("pool", cs2[3], scanm_ap, c_scanm, writes=[scanm_b])
                W1, (W1b,) = A.alloc(T, 1, "W1")
                W2, (W2b,) = A.alloc(T, 1, "W2")
                qf_w, (qf_b,) = A.alloc(T // 2, 1, "qf")
                kk_w, (kk_b,) = A.alloc(T // 2, 1, "kk")
                eg_w, (eg_b,) = A.alloc(T // 2, 1, "eg")
                en_w, (en_b,) = A.alloc(T // 2, 1, "en")
                sg_w, (sg_b,) = A.alloc(T // 2, 1, "sg")
                qf, kk, eg, en, sg = bf(qf_w), bf(kk_w), bf(eg_w), bf(en_w), bf(sg_w)
                vr_w, (vr_b,) = A.alloc(NT * 128 // 2, 1, "vr")
                vr = bf(vr_w).rearrange("p (t d) -> p t d", t=NT)
                vblk_w, (vblk_b,) = A.alloc(NT * NPT * 128 // 2, 1, "vblk")
                vblk = bf(vblk_w).rearrange("p (t c d) -> p t c d", t=NT, c=NPT)
                dirs = {}
                for dname in ("f", "b"):
                    QT_w, (QT_b,) = A.alloc(T // 2, 1, "QT" + dname)
                    KT_w, (KT_b,) = A.alloc(T // 2, 1, "KT" + dname)
                    KH_w, (KH_b,) = A.alloc(T // 2, 1, "KH" + dname)
                    egl_ap, (egl_b,) = A.alloc(NCH, 1, "egl" + dname)
                    st_w, st_bufs = A.alloc((NCH + 1) * 128 // 2, NCH + 1, "st" + dname)
                    S32w, S32bufs = A.alloc(256, 2, "S32" + dname)
                    S32 = [S32w[:, 0:128], S32w[:, 128:256]]
                    S32b = S32bufs
                    dirs[dname] = dict(QT=bf(QT_w), QT_b=QT_b, KT=bf(KT_w), KT_b=KT_b, KH=bf(KH_w), KH_b=KH_b, egl=egl_ap, egl_b=egl_b,
                                       st=bf(st_w).rearrange("p (c d) -> p c d", c=NCH + 1), st_bufs=st_bufs, S32=S32, S32b=S32b)
                khT_slots = []
                for i in range(4):
                    ap_, (b_,) = A.alloc(64, 1, "khT%d" % i)
                    khT_slots.append((bf(ap_), b_))
                am_slots = []
                for i in range(4):
                    ap_, (b_,) = A.alloc(64, 1, "am%d" % i)
                    am_slots.append((bf(ap_), b_))
                osq2_ap, (osq2_b,) = A.alloc(512, 1, "osq2")
                ors2_ap, (ors2_b,) = A.alloc(512, 1, "ors2")
                ro_slots = []
                for i in range(2):
                    ap_, (b_,) = A.alloc(256, 1, "ro%d" % i)
                    ro_slots.append((bf(ap_), b_, s.dma_sem()))
                kh_i = 0
                am_i = 0
                ro_i = 0
                ctx_ch = list(range(0, C // CHUNK))
                lat_ch = list(range(C // CHUNK, NCH))
                order = {"f": ctx_ch + lat_ch, "b": ctx_ch[::-1] + lat_ch[::-1]}
                for hd in range(8):
                    wsrc = w_in[l].rearrange("(c p) n -> p c n", p=128)
                    for nm, off in zip(wnames, woffs):
                        load_w(wr_[nm][0], wr_[nm][1], wr_[nm][2], wsrc[:, :, off + hd * 128:off + (hd + 1) * 128])
                    lb_s = lbv[:, l, hd, 0:1]
                    oml_s = lbv[:, l, hd, 1:2]
                    noml_s = lbv[:, l, hd, 2:3]
                    for gi_, (t0, n, isc) in enumerate(TG):
                        pq, pqb = ps_a()

                        def f(e, pq=pq, t0=t0, n=n):
                            ins = None
                            for c in range(KC):
                                ins = e.matmul(pq[:, 0:n], wr_["rq"][0][:, c, :], hT[:, c, t0:t0 + n], start=(c == 0), stop=(c == KC - 1))
                            return ins
                        s.op("pe", f, reads=[wr_["rq"][1], h_bufs[gi_]], writes=[pqb])
                        s.op("act", lambda e, pq=pq, t0=t0, n=n: e.copy(qf[:, t0:t0 + n], pq[:, 0:n]), reads=[pqb], writes=[qf_b])
                        pg, pgb = ps_a()

                        def f(e, pg=pg, t0=t0, n=n):
                            ins = None
                            for c in range(KC):
                                ins = e.matmul(pg[:, 0:n], wr_["rg"][0][:, c, :], hT[:, c, t0:t0 + n], start=(c == 0), stop=(c == KC - 1))
                            return ins
                        s.op("pe", f, reads=[wr_["rg"][1], h_bufs[gi_]], writes=[pgb])
                        s.op("act", lambda e, pg=pg, t0=t0, n=n: e.activation(out=sg[:, t0:t0 + n], in_=pg[:, 0:n], func=AF.Silu), reads=[pgb], writes=[sg_b])
                    for t4 in range(0, NT, 4):
                        pv, pvb = ps_a()
                        nt4 = min(4, NT - t4)

                        def f(e, pv=pv, t4=t4, nt4=nt4):
                            ins = None
                            for j in range(nt4):
                                ti = t4 + j
                                for c in range(KC):
                                    ins = e.matmul(pv[:, j * 128:(j + 1) * 128], hT[:, c, ti * 128:(ti + 1) * 128], wr_["ri"][0][:, c, :],
                                                   start=(c == 0), stop=(c == KC - 1))
                            return ins
                        s.op("pe", f, reads=[wr_["ri"][1]] + hbufs(t4 * 128, nt4 * 128), writes=[pvb])
                        s.op("act", lambda e, pv=pv, t4=t4, nt4=nt4: e.copy(
                            vr[:, t4:t4 + nt4, :], pv[:, 0:nt4 * 128].rearrange("p (t d) -> p t d", t=nt4)), reads=[pvb], writes=[vr_b])
                        for cc in range(NPT):
                            s.op("dve", lambda e, pv=pv, t4=t4, nt4=nt4, cc=cc: e.tensor_scalar(
                                vblk[:, t4:t4 + nt4, cc, :], pv[:, 0:nt4 * 128].rearrange("p (t d) -> p t d", t=nt4), cmask_ap[:, cc:cc + 1], None, ALU.mult),
                                reads=[pvb, cmask_b], writes=[vblk_b])
                    for dname in ("f", "b"):
                        dd = dirs[dname]
                        wz = wr_["rff"] if dname == "f" else wr_["rfb"]
                        for gi_, (t0, n, isc) in enumerate(TG):
                            pz, pzb = ps_a()

                            def f(e, pz=pz, t0=t0, n=n, wz=wz):
                                ins = None
                                for c in range(KC):
                                    ins = e.matmul(pz[:, 0:n], wz[0][:, c, :], hT[:, c, t0:t0 + n], start=(c == 0), stop=(c == KC - 1))
                                return ins
                            s.op("pe", f, reads=[wz[1], h_bufs[gi_]], writes=[pzb])
                            s.op("act", lambda e, pz=pz, t0=t0, n=n: e.activation(out=W1[:, t0:t0 + n], in_=pz[:, 0:n], func=AF.Sigmoid), reads=[pzb], writes=[W1b])
                        s.op("dve", lambda e: e.tensor_scalar(W2, W1, oml_s, lb_s, ALU.mult, ALU.add), reads=[W1b, lbv_b], writes=[W2b])
                        s.op("dve", lambda e: e.tensor_scalar(kk, W1, noml_s, oml_s, ALU.mult, ALU.add), reads=[W1b, lbv_b], writes=[kk_b])
                        s.op("act", lambda e: e.activation(out=W1, in_=W2, func=AF.Ln), reads=[W2b], writes=[W1b])
                        s.op("dve", lambda e: e.tensor_tensor_scan(W2, scanm_ap, W1, 0.0, ALU.mult, ALU.add), reads=[W1b, scanm_b], writes=[W2b])
                        W13 = W1.rearrange("p (c t) -> p c t", t=CHUNK)
                        W23 = W2.rearrange("p (c t) -> p c t", t=CHUNK)
                        if dname == "f":
                            G, Gb, G3 = W2, W2b, W23
                            last_col = CHUNK - 1
                            X, Xb = W1, W1b
                        else:
                            s.op("dve", lambda e: e.tensor_tensor(W1, W1, W2, ALU.subtract), reads=[W1b, W2b], writes=[W1b])
                            s.op("dve", lambda e: e.tensor_tensor(W13, W13, W23[:, :, CHUNK - 1:CHUNK].to_broadcast([128, NCH, CHUNK]), ALU.add),
                                 reads=[W1b, W2b], writes=[W1b])
                            G, Gb, G3 = W1, W1b, W13
                            last_col = 0
                            X, Xb = W2, W2b
                        s.op("act", lambda e, G=G: e.activation(out=eg, in_=G, func=AF.Exp), reads=[Gb], writes=[eg_b])
                        s.op("act", lambda e, G=G: e.activation(out=en, in_=G, func=AF.Exp, scale=-1.0), reads=[Gb], writes=[en_b])
                        s.op("act", lambda e, G3=G3, last_col=last_col, dd=dd: e.activation(out=dd["egl"], in_=G3[:, :, last_col], func=AF.Exp), reads=[Gb], writes=[dd["egl_b"]])
                        s.op("dve", lambda e, dd=dd: e.tensor_tensor(dd["QT"], qf, eg, ALU.mult), reads=[qf_b, eg_b], writes=[dd["QT_b"]])
                        s.op("pool", lambda e, dd=dd: e.tensor_tensor(dd["KT"], kk, en, ALU.mult), reads=[kk_b, en_b], writes=[dd["KT_b"]])
                        X3 = X.rearrange("p (c t) -> p c t", t=CHUNK)
                        s.op("dve", lambda e, dd=dd, X3=X3: e.tensor_tensor(
                            X3, dd["KT"].rearrange("p (c t) -> p c t", t=CHUNK), dd["egl"].unsqueeze(2).to_broadcast([128, NCH, CHUNK]), ALU.mult),
                            reads=[dd["KT_b"], dd["egl_b"], Xb], writes=[Xb])
                        s.op("pool", lambda e, dd=dd, X=X: e.tensor_copy(dd["KH"], X), reads=[Xb], writes=[dd["KH_b"]])
                        if b == 0 and l == 0 and hd == 0:
                            dbg("QT" + dname, dd["QT"].bitcast(F32), [dd["QT_b"]])
                            dbg("KT" + dname, dd["KT"].bitcast(F32), [dd["KT_b"]])
                            dbg("KH" + dname, dd["KH"].bitcast(F32), [dd["KH_b"]])
                            dbg("egl" + dname, dd["egl"], [dd["egl_b"]])
                    for dname in ("f", "b"):
                        dd = dirs[dname]
                        ordr = order[dname]
                        s.op("pool", lambda e, dd=dd: e.memset(dd["S32"][0], 0.0), writes=[dd["S32b"][0]])
                        kstep = 0
                        c0 = ordr[0]
                        s.op("pool", lambda e, dd=dd, c0=c0: e.memset(dd["st"][:, c0, :], 0.0), writes=[dd["st_bufs"][c0]])
                        tile_order = []
                        for cch in ordr:
                            if cch // NPT not in tile_order:
                                tile_order.append(cch // NPT)
                        pos = 0
                        for ti in tile_order:
                            khT_, khTb_ = khT_slots[kh_i % 4]
                            kh_i += 1
                            ptr, ptrb = ps_b()
                            ptr_bf = ptr[:, 0:64].bitcast(BF16)
                            s.op("pe", lambda e, ptr_bf=ptr_bf, dd=dd, ti=ti: e.transpose(ptr_bf, dd["KH"][:, ti * 128:(ti + 1) * 128], identb),
                                 reads=[dd["KH_b"], identb_b], writes=[ptrb])
                            s.op("act", lambda e, khT_=khT_, ptr_bf=ptr_bf: e.copy(khT_, ptr_bf), reads=[ptrb], writes=[khTb_])
                            pu, pub = ps_b()

                            s.op("pe", lambda e, pu=pu, khT_=khT_, ti=ti: e.matmul(
                                pu[:, 0:NPT * 128], khT_, vblk[:, ti].rearrange("p c d -> p (c d)"), start=True, stop=True),
                                reads=[khTb_, vblk_b], writes=[pub])
                            chunks_here = [cch for cch in ordr if cch // NPT == ti]
                            for cch in chunks_here:
                                j = cch % NPT
                                nxt = ordr[pos + 1] if pos + 1 < len(ordr) else NCH
                                pos += 1
                                sa, sab = dd["S32"][kstep % 2], dd["S32b"][kstep % 2]
                                sn, snb = dd["S32"][(kstep + 1) % 2], dd["S32b"][(kstep + 1) % 2]
                                kstep += 1
                                s.op("dve", lambda e, dd=dd, cch=cch, j=j, pu=pu: e.scalar_tensor_tensor(
                                    sn, sa, dd["egl"][:, cch:cch + 1], pu[:, j * 128:(j + 1) * 128], ALU.mult, ALU.add),
                                    reads=[sab, dd["egl_b"], pub], writes=[snb])
                                s.op("pool", lambda e, dd=dd, nxt=nxt: e.tensor_copy(dd["st"][:, nxt, :], sn), reads=[snb], writes=[dd["st_bufs"][nxt]])
                    for t4 in range(0, NT, 4):
                        nt4 = min(4, NT - t4)
                        po_, pob_ = psum[6 + (t4 // 4) % 2], psb[6 + (t4 // 4) % 2]
                        for jt in range(nt4):
                            ti = t4 + jt
                            ams = []
                            for dname in ("f", "b"):
                                dd = dirs[dname]
                                pa_, pab_ = ps_b()
                                s.op("pe", lambda e, pa_=pa_, dd=dd, ti=ti: e.matmul(
                                    pa_[:, 0:128], dd["KT"][:, ti * 128:(ti + 1) * 128], dd["QT"][:, ti * 128:(ti + 1) * 128], start=True, stop=True),
                                    reads=[dd["KT_b"], dd["QT_b"]], writes=[pab_])
                                am_, amb_ = am_slots[am_i % 4]
                                am_i += 1
                                mk_, mkb_ = (maskf, maskf_b) if dname == "f" else (maskb, maskb_b)
                                s.op("dve", lambda e, am_=am_, pa_=pa_, mk_=mk_: e.tensor_tensor(am_, pa_[:, 0:128], mk_, ALU.mult), reads=[pab_, mkb_], writes=[amb_])
                                ams.append((am_, amb_))

                            def f(e, po_=po_, jt=jt, ti=ti, ams=ams):
                                ins = None
                                first = True
                                for di, dname in enumerate(("f", "b")):
                                    dd = dirs[dname]
                                    ins = e.matmul(po_[:, jt * 128:(jt + 1) * 128], vr[:, ti, :], ams[di][0], start=first, stop=False)
                                    first = False
                                    for j in range(NPT):
                                        cch = ti * NPT + j
                                        ins = e.matmul(po_[:, jt * 128 + j * CHUNK:jt * 128 + (j + 1) * CHUNK], dd["st"][:, cch, :],
                                                       dd["QT"][:, cch * CHUNK:(cch + 1) * CHUNK], start=False, stop=(di == 1 and j == NPT - 1))
                                return ins
                            rds = [vr_b, ams[0][1], ams[1][1]]
                            for dname in ("f", "b"):
                                dd = dirs[dname]
                                rds += [dd["QT_b"]] + [dd["st_bufs"][ti * NPT + j] for j in range(NPT)]
                            s.op("pe", f, reads=rds, writes=[pob_])
                        n = nt4 * 128
                        t0 = t4 * 128
                        s.op("act", lambda e, po_=po_, n=n: e.activation(out=osq2_ap[:, 0:n], in_=po_[:, 0:n], func=AF.Square), reads=[pob_], writes=[osq2_b])
                        pS, pSb = ps_a()
                        s.op("pe", lambda e, pS=pS, n=n: e.matmul(pS[:, 0:n], ones_ap, osq2_ap[:, 0:n], start=True, stop=True), reads=[osq2_b, ones_b], writes=[pSb])
                        act_rstd(ors2_ap[:, 0:n], pS[:, 0:n], 1.0 / 128, [pSb], [ors2_b])
                        s.op("dve", lambda e, po_=po_, n=n: e.scalar_tensor_tensor(osq2_ap[:, 0:n], po_[:, 0:n], lamv[:, l, 2:3], ors2_ap[:, 0:n], ALU.mult, ALU.mult),
                             reads=[pob_, ors2_b, lamv_b], writes=[osq2_b])
                        ro_, rob_, rosem_ = ro_slots[ro_i % 2]
                        ro_i += 1
                        s.op("pool", lambda e, ro_=ro_, n=n, t0=t0: e.tensor_tensor(ro_[:, 0:n], osq2_ap[:, 0:n], sg[:, t0:t0 + n], ALU.mult),
                             reads=[osq2_b, sg_b], writes=[rob_])
                        s.dma("sp", rosem_, rT[:, hd, t0:t0 + n], ro_[:, 0:n], reads=[rob_], writes=[rT_b[hd]])
                A.reset(mk)
                if b == 0 and l == 0:
                    dscr("rT", rT, rT_b)
                chk("rec")
                chk("L%d_rec" % l)

                mk = A.mark()
                wm = {}
                for nm in ("ga", "gr", "ba", "br", "wo"):
                    w_, (wb_,) = A.alloc(KC * D // 2, 1, "wm" + nm)
                    wm[nm] = (bf(w_).rearrange("p (c n) -> p c n", c=KC), wb_, s.dma_sem())
                wsrc = w_in[l].rearrange("(c p) n -> p c n", p=128)
                load_w(wm["ga"][0], wm["ga"][1], wm["ga"][2], wsrc[:, :, OFF_GA:OFF_GA + D])
                load_w(wm["gr"][0], wm["gr"][1], wm["gr"][2], wsrc[:, :, OFF_GR:OFF_GR + D])
                load_w(wm["ba"][0], wm["ba"][1], wm["ba"][2], w_bra[l].rearrange("(c p) n -> p c n", p=128))
                load_w(wm["br"][0], wm["br"][1], wm["br"][2], w_brr[l].rearrange("(c p) n -> p c n", p=128))
                load_w(wm["wo"][0], wm["wo"][1], wm["wo"][2], w_out[l].rearrange("(c p) n -> p c n", p=128))
                ar_slots = []
                for i in range(2):
                    a_, (ab_,) = A.alloc(KC * MGN // 2, 1, "ag%d" % i)
                    r_, (rb_,) = A.alloc(KC * MGN // 2, 1, "rg%d" % i)
                    x_, (xb_,) = A.alloc(KC * MGN, 1, "xm%d" % i)
                    ar_slots.append((bf(a_).rearrange("p (c n) -> p c n", c=KC), ab_, s.dma_sem(),
                                     bf(r_).rearrange("p (c n) -> p c n", c=KC), rb_, s.dma_sem(),
                                     x_.rearrange("p (c n) -> p c n", c=KC), xb_, s.dma_sem(), s.dma_sem()))
                mx_w, (mx_b,) = A.alloc(KC * MGN // 2, 1, "mixed")
                mixed = bf(mx_w).rearrange("p (c n) -> p c n", c=KC)
                sg_slots = []
                for i in range(2):
                    s1_, (s1b_,) = A.alloc(MGN, 1, "sga%d" % i)
                    s2_, (s2b_,) = A.alloc(MGN, 1, "sgr%d" % i)
                    sg_slots.append((s1_, s1b_, s2_, s2b_))
                sgi = 0
                for gi_, (t0, n, isc) in enumerate(TGM):
                    if isc and last:
                        continue
                    col = NB if isc else b
                    a_, ab_, asem_, r_, rb_, rsem_, x_, xb_, xsem_, xosem_ = ar_slots[gi_ % 2]
                    s.dma("sp", asem_, a_[:, :, 0:n], aT[:, :, t0:t0 + n], reads=aT_b, writes=[ab_])
                    s.dma("sp", rsem_, r_[:, :, 0:n], rT[:, :, t0:t0 + n], reads=rT_b, writes=[rb_])
                    s.dma("sp", xsem_, x_[:, :, 0:n], xT[b, :, :, t0:t0 + n], reads=[xT_b[b][ti] for ti in tiles_of(t0, n)], writes=[xb_])
                    hbs = hbufs(t0, n)
                    for oc in range(KC):
                        s1_, s1b_, s2_, s2b_ = sg_slots[sgi % 2]
                        sgi += 1
                        pgs = []
                        for (wnm, src, srcb) in (("ga", hT[:, :, t0:t0 + n], hbs), ("gr", hT[:, :, t0:t0 + n], hbs), ("ba", a_[:, :, 0:n], [ab_]), ("br", r_[:, :, 0:n], [rb_])):
                            pg, pgb = ps_a()

                            def f(e, pg=pg, wnm=wnm, src=src, oc=oc, n=n):
                                ins = None
                                for c in range(KC):
                                    ins = e.matmul(pg[:, 0:n], wm[wnm][0][:, c, oc * 128:(oc + 1) * 128], src[:, c, :], start=(c == 0), stop=(c == KC - 1))
                                return ins
                            s.op("pe", f, reads=[wm[wnm][1]] + list(srcb), writes=[pgb])
                            pgs.append((pg, pgb))
                        s.op("act", lambda e, s1_=s1_, pg=pgs[0][0], n=n: e.activation(out=s1_[:, 0:n], in_=pg[:, 0:n], func=AF.Sigmoid), reads=[pgs[0][1]], writes=[s1b_])
                        s.op("act", lambda e, s2_=s2_, pg=pgs[1][0], n=n: e.activation(out=s2_[:, 0:n], in_=pg[:, 0:n], func=AF.Sigmoid), reads=[pgs[1][1]], writes=[s2b_])
                        s.op("dve", lambda e, s1_=s1_, pg=pgs[2][0], n=n: e.tensor_tensor(s1_[:, 0:n], pg[:, 0:n], s1_[:, 0:n], ALU.mult), reads=[pgs[2][1], s1b_], writes=[s1b_])
                        s.op("dve", lambda e, s2_=s2_, pg=pgs[3][0], n=n: e.tensor_tensor(s2_[:, 0:n], pg[:, 0:n], s2_[:, 0:n], ALU.mult), reads=[pgs[3][1], s2b_], writes=[s2b_])
                        s.op("pool", lambda e, s1_=s1_, s2_=s2_, oc=oc, n=n: e.tensor_tensor(mixed[:, oc, 0:n], s1_[:, 0:n], s2_[:, 0:n], ALU.add),
                             reads=[s1b_, s2b_], writes=[mx_b])
                    for oc in range(KC):
                        py, pyb = ps_a()

                        def f(e, py=py, oc=oc, n=n):
                            ins = None
                            for c in range(KC):
                                ins = e.matmul(py[:, 0:n], wm["wo"][0][:, c, oc * 128:(oc + 1) * 128], mixed[:, c, 0:n], start=(c == 0), stop=(c == KC - 1))
                            return ins
                        s.op("pe", f, reads=[wm["wo"][1], mx_b], writes=[pyb])
                        s.op("dve", lambda e, py=py, oc=oc, n=n, x_=x_, col=col: e.scalar_tensor_tensor(
                            x_[:, oc, 0:n], py[:, 0:n], modv[:, l, 16 + oc, col:col + 1], x_[:, oc, 0:n], ALU.mult, ALU.add),
                            reads=[pyb, xb_, modv_b], writes=[xb_])
                    s.dma("sp", xosem_, xT[b, :, :, t0:t0 + n], x_[:, :, 0:n], reads=[xb_], writes=[xT_b[b][ti] for ti in tiles_of(t0, n)])
                    norm_mod(x_[:, :, 0:n], xb_, n, hT[:, :, t0:t0 + n], hbs[0], a2[:, l, :, col], modv[:, l, 24:32, col], a2_b)
                    for ob in hbs[1:]:
                        ob.w = hbs[0].w
                        ob.r = {}
                A.reset(mk)
                if b == 0 and l == 0:
                    dbg("h2", h_w, h_bufs)
                    dscr("x1", xT[0], xT_b[0])
                chk("merge")
                chk("L%d_merge" % l)

                mk = A.mark()
                onehot_w, (onehot_b,) = A.alloc(16 * 128 // 2, 1, "onehot")
                onehot = bf(onehot_w)
                s.dma("pool", cs2[3], onehot[0:16, :], c_onehot, writes=[onehot_b])
                xr_ap, xr_bufs = A.alloc(KC * T, len(TG), "xres")
                xres = xr_ap.rearrange("p (c t) -> p c t", c=KC)
                xrsem = [s.dma_sem() for _ in TG]
                tg_moe = [(gi_, t0, n, isc) for gi_, (t0, n, isc) in enumerate(TG) if not (isc and last)]
                for (gi_, t0, n, isc) in tg_moe:
                    s.dma("sp", xrsem[gi_], xres[:, :, t0:t0 + n], xT[b, :, :, t0:t0 + n], reads=[xT_b[b][ti] for ti in tiles_of(t0, n)], writes=[xr_bufs[gi_]])
                tiles_moe = [ti for ti in range(NT) if not (last and ti < NCT)]
                ntm = len(tiles_moe)
                sc_ap, (sc_b,) = A.alloc(NT * 16, 1, "scores")
                bi_ap, (bi_b,) = A.alloc(NT * 16, 1, "biased")
                t_ap, (t_b,) = A.alloc(NT * 16, 1, "rt_tmp")
                g_ap, (g_b,) = A.alloc(NT * 16, 1, "gates")
                m1_ap, (m1_b,) = A.alloc(NT * 4, 1, "max1")
                m2_ap, (m2_b,) = A.alloc(NT * 4, 1, "max2")
                gs_ap, (gs_b,) = A.alloc(NT * 4, 1, "gsel")
                gm_ap, (gm_b,) = A.alloc(NT, 1, "gmax")
                gT_w, (gT_b,) = A.alloc(T // 2, 1, "gT")
                gT = bf(gT_w)
                gb_w, gb_bufs = A.alloc(T // 2, len(TG), "gbs")
                gbs = bf(gb_w)
                prt, prtb = ps_a()

                def f(e):
                    ins = None
                    for ti in tiles_moe:
                        for c in range(KC):
                            ins = e.matmul(prt[:, ti * 16:(ti + 1) * 16], hT[:, c, ti * 128:(ti + 1) * 128], wrt[:, c, :], start=(c == 0), stop=(c == KC - 1))
                    return ins
                s.op("pe", f, reads=[wr_b] + h_bufs, writes=[prtb])
                lo, hi = tiles_moe[0], tiles_moe[-1] + 1

                def v3(ap):
                    return ap[:, lo * 16:hi * 16].rearrange("p (t e) -> p t e", e=16)

                def v4(ap):
                    return ap[:, lo * 16:hi * 16].rearrange("p (t e) -> p t e", e=4)

                def v2(ap):
                    return ap[:, lo * 4:hi * 4]
                s.op("act", lambda e: e.activation(out=sc_ap[:, lo * 16:hi * 16], in_=prt[:, lo * 16:hi * 16], func=AF.Sigmoid), reads=[prtb], writes=[sc_b])
                s.op("dve", lambda e: e.tensor_tensor(v3(bi_ap), v3(sc_ap), brt_ap.unsqueeze(1).to_broadcast([128, ntm, 16]), ALU.add), reads=[sc_b, brt_b], writes=[bi_b])
                s.op("dve", lambda e: e.tensor_reduce(v2(m1_ap), v4(bi_ap), AX.X, ALU.max), reads=[bi_b], writes=[m1_b])
                s.op("dve", lambda e: e.tensor_tensor(v4(t_ap), v4(bi_ap), v2(m1_ap).unsqueeze(2).to_broadcast([128, ntm * 4, 4]), ALU.is_ge), reads=[bi_b, m1_b], writes=[t_b])
                s.op("dve", lambda e: e.scalar_tensor_tensor(t_ap[:, lo * 16:hi * 16], t_ap[:, lo * 16:hi * 16], -1e9, bi_ap[:, lo * 16:hi * 16], ALU.mult, ALU.add), reads=[t_b, bi_b], writes=[t_b])
                s.op("dve", lambda e: e.tensor_reduce(v2(m2_ap), v4(t_ap), AX.X, ALU.max), reads=[t_b], writes=[m2_b])
                s.op("dve", lambda e: e.tensor_tensor(v2(gs_ap), v2(m1_ap), v2(m2_ap), ALU.add), reads=[m1_b, m2_b], writes=[gs_b])
                s.op("dve", lambda e: e.tensor_reduce(gm_ap[:, lo:hi], v2(gs_ap).rearrange("p (t g) -> p t g", g=4), AX.X, ALU.max), reads=[gs_b], writes=[gm_b])
                s.op("dve", lambda e: e.tensor_tensor(v2(gs_ap).rearrange("p (t g) -> p t g", g=4), v2(gs_ap).rearrange("p (t g) -> p t g", g=4),
                                                      gm_ap[:, lo:hi].unsqueeze(2).to_broadcast([128, ntm, 4]), ALU.is_ge), reads=[gs_b, gm_b], writes=[gs_b])
                s.op("dve", lambda e: e.tensor_tensor(v4(t_ap), v4(bi_ap), v2(m2_ap).unsqueeze(2).to_broadcast([128, ntm * 4, 4]), ALU.is_ge), reads=[bi_b, m2_b], writes=[t_b])
                s.op("dve", lambda e: e.tensor_tensor(v4(t_ap), v4(t_ap), v2(gs_ap).unsqueeze(2).to_broadcast([128, ntm * 4, 4]), ALU.mult), reads=[t_b, gs_b], writes=[t_b])
                s.op("dve", lambda e: e.tensor_tensor(t_ap[:, lo * 16:hi * 16], t_ap[:, lo * 16:hi * 16], sc_ap[:, lo * 16:hi * 16], ALU.mult), reads=[t_b, sc_b], writes=[t_b])
                s.op("dve", lambda e: e.tensor_reduce(gm_ap[:, lo:hi], v3(t_ap), AX.X, ALU.add), reads=[t_b], writes=[gm_b])
                s.op("dve", lambda e: e.reciprocal(gm_ap[:, lo:hi], gm_ap[:, lo:hi]), reads=[gm_b], writes=[gm_b])
                s.op("dve", lambda e: e.tensor_tensor(v3(g_ap), v3(t_ap), gm_ap[:, lo:hi].unsqueeze(2).to_broadcast([128, ntm, 16]), ALU.mult), reads=[t_b, gm_b], writes=[g_b])
                if b == 0 and l == 0:
                    dbg("gates", g_ap, [g_b])
                for t4 in range(lo, hi, 4):
                    nt4 = min(4, hi - t4)
                    pgt, pgtb = ps_a()

                    def f(e, pgt=pgt, t4=t4, nt4=nt4):
                        ins = None
                        for j in range(nt4):
                            ins = e.transpose(pgt[0:16, j * 128:(j + 1) * 128], g_ap[:, (t4 + j) * 16:(t4 + j + 1) * 16], ident_ap)
                        return ins
                    s.op("pe", f, reads=[g_b, ident_b], writes=[pgtb])
                    s.op("act", lambda e, pgt=pgt, t4=t4, nt4=nt4: e.copy(gT[0:16, t4 * 128:(t4 + nt4) * 128], pgt[0:16, 0:nt4 * 128]), reads=[pgtb], writes=[gT_b])
                chk("L%d_moe_r" % l)
                m_exp = A.mark()
                rr["bn"] = 4
                wu_slots = []
                for i in range(2):
                    g_, (gb_,) = A.alloc(KC * 512 // 2, 1, "wg%d" % i)
                    u_, (ub_,) = A.alloc(KC * 512 // 2, 1, "wu%d" % i)
                    d_, (db_,) = A.alloc(4 * D // 2, 1, "wd%d" % i)
                    wu_slots.append((bf(g_).rearrange("p (c n) -> p c n", c=KC), gb_, s.dma_sem(),
                                     bf(u_).rearrange("p (c n) -> p c n", c=KC), ub_, s.dma_sem(),
                                     bf(d_).rearrange("p (c n) -> p c n", c=4), db_, s.dma_sem()))
                hid_slots = []
                for i in range(2):
                    hid_w, (hid_b,) = A.alloc(4 * 512 // 2, 1, "hid%d" % i)
                    hid_slots.append((bf(hid_w).rearrange("p (c n) -> p c n", c=4), hid_b))
                mcnt = {"sli": 0, "hi": 0}
                sl_slots = []
                for i in range(2):
                    a_, (ab_,) = A.alloc(512, 1, "silu%d" % i)
                    sl_slots.append((a_, ab_))
                sli = 0
                ui = 0
                for ex in range(NEXP):
                    for (gi_, t0, n, isc) in tg_moe:
                        pgb_, pgbb_ = ps_a()
                        s.op("pe", lambda e, pgb_=pgb_, ex=ex, t0=t0, n=n: e.matmul(pgb_[:, 0:n], onehot[0:16, ex * 128:(ex + 1) * 128], gT[0:16, t0:t0 + n], start=True, stop=True),
                             reads=[onehot_b, gT_b], writes=[pgbb_])
                        s.op("act", lambda e, pgb_=pgb_, t0=t0, n=n: e.copy(gbs[:, t0:t0 + n], pgb_[:, 0:n]), reads=[pgbb_], writes=[gb_bufs[gi_]])
                    for fh in range(2):
                        wg_, wgb_, wgs_, wu_, wub_, wus_, wd_, wdb_, wds_ = wu_slots[ui % 2]
                        ui += 1
                        load_w(wg_, wgb_, wgs_, w_gate[l, ex].rearrange("(c p) n -> p c n", p=128)[:, :, fh * 512:(fh + 1) * 512])
                        load_w(wu_, wub_, wus_, w_up[l, ex].rearrange("(c p) n -> p c n", p=128)[:, :, fh * 512:(fh + 1) * 512])
                        load_w(wd_, wdb_, wds_, w_down[l, ex, fh * 512:(fh + 1) * 512, :].rearrange("(c p) n -> p c n", p=128))
                        def emit_gu(gi_, t0, n, hid, hid_b, wg_=wg_, wgb_=wgb_, wu_=wu_, wub_=wub_):
                            for fc in range(4):
                                pg, pgb = ps_a()
                                pu, pub = ps_a()

                                def f(e, pg=pg, fc=fc):
                                    ins = None
                                    for c in range(KC):
                                        ins = e.matmul(pg[:, 0:n], wg_[:, c, fc * 128:(fc + 1) * 128], hT[:, c, t0:t0 + n], start=(c == 0), stop=(c == KC - 1))
                                    return ins
                                s.op("pe", f, reads=[wgb_, h_bufs[gi_]], writes=[pgb])

                                def f(e, pu=pu, fc=fc):
                                    ins = None
                                    for c in range(KC):
                                        ins = e.matmul(pu[:, 0:n], wu_[:, c, fc * 128:(fc + 1) * 128], hT[:, c, t0:t0 + n], start=(c == 0), stop=(c == KC - 1))
                                    return ins
                                s.op("pe", f, reads=[wub_, h_bufs[gi_]], writes=[pub])
                                sl_, slb_ = sl_slots[mcnt["sli"] % 2]
                                mcnt["sli"] += 1
                                s.op("act", lambda e: e.activation(out=sl_[:, 0:n], in_=pg[:, 0:n], func=AF.Silu), reads=[pgb], writes=[slb_])
                                s.op("dve", lambda e: e.tensor_tensor(sl_[:, 0:n], pu[:, 0:n], sl_[:, 0:n], ALU.mult), reads=[pub, slb_], writes=[slb_])
                                s.op("pool", lambda e: e.tensor_tensor(hid[:, fc, 0:n], sl_[:, 0:n], gbs[:, t0:t0 + n], ALU.mult),
                                     reads=[slb_, gb_bufs[gi_]], writes=[hid_b])

                        def emit_down(gi_, t0, n, isc, hid, hid_b, wd_=wd_, wdb_=wdb_):
                            col = NB if isc else b
                            for oc in range(KC):
                                pd, pdb = ps_b()

                                def f(e, pd=pd, oc=oc):
                                    ins = None
                                    for c in range(4):
                                        ins = e.matmul(pd[:, 0:n], wd_[:, c, oc * 128:(oc + 1) * 128], hid[:, c, 0:n], start=(c == 0), stop=(c == 3))
                                    return ins
                                s.op("pe", f, reads=[wdb_, hid_b], writes=[pdb])
                                s.op("dve", lambda e: e.scalar_tensor_tensor(
                                    xres[:, oc, t0:t0 + n], pd[:, 0:n], modv[:, l, 40 + oc, col:col + 1], xres[:, oc, t0:t0 + n], ALU.mult, ALU.add),
                                    reads=[pdb, xr_bufs[gi_], modv_b], writes=[xr_bufs[gi_]])

                        nseq = len(tg_moe)
                        hs = [hid_slots[(mcnt["hi"] + i) % 2] for i in range(nseq)]
                        mcnt["hi"] += nseq
                        g0 = tg_moe[0]
                        emit_gu(g0[0], g0[1], g0[2], hs[0][0], hs[0][1])
                        for i in range(nseq):
                            if i + 1 < nseq:
                                g1 = tg_moe[i + 1]
                                emit_gu(g1[0], g1[1], g1[2], hs[i + 1][0], hs[i + 1][1])
                            gc = tg_moe[i]
                            emit_down(gc[0], gc[1], gc[2], gc[3], hs[i][0], hs[i][1])
                rr["bn"] = 2
                chk("L%d_moe_e" % l)
                if not last:
                    xwsem = [s.dma_sem() for _ in TG]
                    for (gi_, t0, n, isc) in tg_moe:
                        s.dma("sp", xwsem[gi_], xT[b, :, :, t0:t0 + n], xres[:, :, t0:t0 + n], reads=[xr_bufs[gi_]], writes=[xT_b[b][ti] for ti in tiles_of(t0, n)])
                    if b == 0 and l == 0:
                        dscr("x2", xT[0], xT_b[0])
                else:
                    A.reset(m_exp)
                    fin_slots = []
                    for i in range(2):
                        y_, (yb_,) = A.alloc(KC * 512, 1, "yfin%d" % i)
                        fin_slots.append((y_.rearrange("p (c n) -> p c n", c=KC), yb_))
                    rsf_ap, (rsf_b,) = A.alloc(512, 1, "rsf")
                    ot_slots = []
                    for i in range(2):
                        o_, (ob_,) = A.alloc(D, 1, "ot%d" % i)
                        ot_slots.append((o_, ob_, s.dma_sem()))
                    oti = 0
                    for fi, (gi_, t0, n, isc) in enumerate(tg_moe):
                        y_, yb_ = fin_slots[fi % 2]
                        yf = y_[:, :, 0:n]
                        s.op("act", lambda e, yf=yf, t0=t0, n=n: e.activation(out=yf, in_=xres[:, :, t0:t0 + n], func=AF.Square), reads=[xr_bufs[gi_]], writes=[yb_])
                        pss, pssb = ps_a()

                        def f(e, pss=pss, yf=yf, n=n):
                            ins = None
                            for c in range(KC):
                                ins = e.matmul(pss[:, 0:n], ones_ap, yf[:, c, :], start=(c == 0), stop=(c == KC - 1))
                            return ins
                        s.op("pe", f, reads=[yb_, ones_b], writes=[pssb])
                        act_rstd(rsf_ap[:, 0:n], pss[:, 0:n], 1.0 / D, [pssb], [rsf_b])
                        chk("fin_a")
                        for c in range(KC):
                            s.op("dve", lambda e, yf=yf, c=c, t0=t0, n=n: e.scalar_tensor_tensor(yf[:, c, :], xres[:, c, t0:t0 + n], gfv[:, c:c + 1], rsf_ap[:, 0:n], ALU.mult, ALU.mult),
                                 reads=[xr_bufs[gi_], rsf_b, small_b, yb_], writes=[yb_])
                        chk("fin_b")
                        for tj in range(n // 128):
                            o_, ob_, osem_ = ot_slots[oti % 2]
                            oti += 1
                            for half in range(2):
                                pt, ptb = ps_a()

                                def f(e, pt=pt, yf=yf, tj=tj, half=half):
                                    ins = None
                                    for j in range(4):
                                        c = half * 4 + j
                                        ins = e.transpose(pt[:, j * 128:(j + 1) * 128], yf[:, c, tj * 128:(tj + 1) * 128], ident_ap)
                                    return ins
                                s.op("pe", f, reads=[yb_, ident_b], writes=[ptb])
                                if half == 0:
                                    s.op("act", lambda e, o_=o_, pt=pt: e.copy(o_[:, 0:512], pt[:, :]), reads=[ptb], writes=[ob_])
                                else:
                                    s.op("dve", lambda e, o_=o_, pt=pt: e.tensor_copy(o_[:, 512:1024], pt[:, :]), reads=[ptb], writes=[ob_])
                            chk("fin_c")
                            tl = t0 - C + tj * 128
                            out_toks.append(s.dma("sp", osem_, out[b, tl:tl + 128, :], o_, reads=[ob_]))
                chk("L%d_moe" % l)
                A.reset(mk)
                A.reset(mL)

        s.dead = False
        s.wait_only("sp", out_toks + dbg_sem)
        s.final_barrier("sp")
        s.emit()
        print("program: ins=%d waits=%d sems=%d arena_peak=%d words" % (s.n_ins, s.n_wait, len(s.semh), A.peak))
    return nc


_CACHE = {}


def _get_program(NB, S, C, L, debug=()):
    stop = os.environ.get("K_STOP") or None
    key = (NB, S, C, L, tuple(debug), stop)
    if key not in _CACHE:
        _CACHE[key] = build_program(NB, S, C, L, debug, stop)
    return _CACHE[key]


def run(inputs, n_cores=8, debug=()):
    x = np.asarray(inputs["x"], np.float32)
    ctx = np.asarray(inputs["ctx"], np.float32)
    c = np.asarray(inputs["c"], np.float32)
    c_ctx = np.asarray(inputs["c_ctx"], np.float32)
    B, S, _ = x.shape
    C = ctx.shape[1]
    L = inputs["w_mod"].shape[0]
    NB = B // n_cores
    nc = _get_program(NB, S, C, L, debug)
    consts = host_constants(C, S)
    shared = {}
    for k in ("w_mod", "b_mod", "g_norm1", "g_norm2", "w_in", "lambda_q1", "lambda_k1", "lambda_q2", "lambda_k2", "g_subln",
              "lb_logits", "g_rec_norm", "w_br_attn", "w_br_rec", "w_out", "w_router", "w_gate", "w_up", "w_down"):
        shared[k] = np.ascontiguousarray(np.asarray(inputs[k], np.float32))
    shared["b_router"] = np.ascontiguousarray(np.asarray(inputs["b_router"], np.float32).reshape(1, NEXP))
    shared["g_final"] = np.ascontiguousarray(np.asarray(inputs["g_final"], np.float32).reshape(1, D))
    shared.update(consts)
    in_maps = []
    for i in range(n_cores):
        m = dict(shared)
        m["x"] = np.ascontiguousarray(x[i * NB:(i + 1) * NB])
        m["ctx"] = np.ascontiguousarray(ctx[i * NB:(i + 1) * NB])
        cv4 = np.zeros((4, D), np.float32)
        cv4[0:NB] = c[i * NB:(i + 1) * NB]
        cv4[NB] = c_ctx
        m["cvec"] = cv4
        in_maps.append(m)
    res = run_bass_kernel_spmd(nc, in_maps, core_ids=list(range(n_cores)))
    outs = np.concatenate([np.asarray(r["out"]) for r in res.results], axis=0)
    return outs, res


def kernel(**inputs):
    out, _ = run(inputs, n_cores=8)
    return out.astype(np.float32)
```

```python
import contextlib
import math
import os
import sys
import numpy as np
import ml_dtypes
import concourse.bass as bass
import concourse.mybir as mybir
from concourse.bass_utils import run_bass_kernel_spmd

F32 = mybir.dt.float32
BF16 = mybir.dt.bfloat16
I32 = mybir.dt.int32
AF = mybir.ActivationFunctionType
ALU = mybir.AluOpType
AX = mybir.AxisListType

DEBUG_ANNOTATE = bool(os.environ.get('K_ANNOTATE'))
DMA_SEM_POOL = int(os.environ.get('K_DSEMS', '1000'))
POOL_TO_DVE = os.environ.get('K_POOLDVE', '0') == '1'
D = 1024
KC = 8
N_IN = 10240
EPS = 1e-6
NEXP = 16
CHUNK = 32
NPT = 128 // CHUNK
GRID_W = 64
OFF_AQ, OFF_AK, OFF_AV, OFF_RQ, OFF_RFF, OFF_RFB, OFF_RI, OFF_RG, OFF_GA, OFF_GR = [1024 * i for i in range(10)]


class Buf:
    __slots__ = ("name", "w", "r", "psum")

    def __init__(self, name="", psum=False):
        self.name = name
        self.w = None
        self.r = {}
        self.psum = psum


class DmaSem:
    __slots__ = ("idx", "h", "total")

    def __init__(self, idx, h):
        self.idx = idx
        self.h = h
        self.total = 0


class _Rec:
    def __init__(self):
        self.calls = []

    def __getattr__(self, name):
        def m(*a, **k):
            self.calls.append((name, a, k))
            return self
        return m


class Sched:
    ENG = ("pe", "act", "dve", "pool", "sp")

    def __init__(self, nc, stack):
        self.nc = nc
        self.stack = stack
        self.semh = []
        self.semidx = {}
        self.cnt = {}
        self.ops = {}
        self.known = {}
        for e in self.ENG:
            self.semidx[e] = self._new_sem("s_" + e)
            self.cnt[e] = 0
            self.ops[e] = []
            self.known[e] = {}
        self.n_wait = 0
        self.n_ins = 0
        self.dead = False

    def _new_sem(self, name):
        h = self.stack.enter_context(self.nc.semaphore(name))
        self.semh.append(h)
        return len(self.semh) - 1

    def begin_iter(self):
        if not hasattr(self, "_ipool"):
            self._ipool = []
        self._iidx = 0

    def dma_sem(self, name=None):
        if not hasattr(self, "_dpool"):
            self._dpool = []
            self._dnext = 0
        if getattr(self, "_iidx", None) is not None:
            if self._iidx < len(self._ipool):
                ds = self._ipool[self._iidx]
            else:
                idx = self._new_sem("d%d" % len(self.semh))
                ds = DmaSem(idx, self.semh[idx])
                self._ipool.append(ds)
                self._dpool.append(ds)
            self._iidx += 1
            return ds
        if len(self._dpool) < DMA_SEM_POOL:
            idx = self._new_sem("d%d" % len(self.semh))
            ds = DmaSem(idx, self.semh[idx])
            self._dpool.append(ds)
            return ds
        ds = self._dpool[self._dnext % len(self._dpool)]
        self._dnext += 1
        return ds

    @property
    def _dsems(self):
        return getattr(self, "_dpool", [])

    def _collect(self, eng, reads, writes, extra=()):
        deps = {}
        own_ = self.semidx[eng]
        for b in reads:
            t = b.w
            if t is not None and deps.get(t[0], 0) < t[1]:
                deps[t[0]] = t[1]
            if b.psum:
                for si, v in b.r.items():
                    if si != own_ and deps.get(si, 0) < v:
                        deps[si] = v
        for b in writes:
            t = b.w
            if t is not None and deps.get(t[0], 0) < t[1]:
                deps[t[0]] = t[1]
            for si, v in b.r.items():
                if deps.get(si, 0) < v:
                    deps[si] = v
        for t in extra:
            if t is not None and deps.get(t[0], 0) < t[1]:
                deps[t[0]] = t[1]
        own = self.semidx[eng]
        kn = self.known[eng]
        waits = []
        for si, v in deps.items():
            if eng == "pe" and si == own:
                continue
            if kn.get(si, 0) >= v:
                continue
            kn[si] = v
            waits.append((si, v))
        self.n_wait += len(waits)
        return waits

    def _mark(self, tok, reads, writes):
        si, v = tok
        for b in reads:
            if b.r.get(si, 0) < v:
                b.r[si] = v
        for b in writes:
            b.w = tok
            b.r = {}

    def op(self, eng, fn, reads=(), writes=(), extra=()):
        if self.dead:
            return (0, 0)
        if eng == "pool" and POOL_TO_DVE:
            eng = "dve"
        rec = _Rec()
        fn(rec)
        calls = rec.calls
        lineno = sys._getframe(1).f_lineno

        def fn(e, calls=calls, lineno=lineno):
            ins = None
            for name, a, k in calls:
                ins = getattr(e, name)(*a, **k)
                if DEBUG_ANNOTATE:
                    ins.annotate("L%d" % lineno)
            return ins
        waits = self._collect(eng, reads, writes, extra)
        self.cnt[eng] += 1
        tok = (self.semidx[eng], self.cnt[eng])
        self.ops[eng].append((waits, fn, tok[0]))
        self._mark(tok, reads, writes)
        self.n_ins += 1
        return tok

    def dma(self, queue, dsem, out, in_, reads=(), writes=(), extra=()):
        if self.dead:
            return (0, 0)
        if dsem.total > 0:
            extra = tuple(extra) + ((dsem.idx, dsem.total),)
        waits = self._collect(queue, reads, writes, extra)
        dsem.total += 16
        tok = (dsem.idx, dsem.total)
        h = dsem.h

        def fn(e, out=out, in_=in_, h=h):
            e.dma_start(out=out, in_=in_).then_inc(h, 16)
            return None

        self.ops[queue].append((waits, fn, None))
        self._mark(tok, reads, writes)
        self.n_ins += 1
        return tok

    def wait_only(self, eng, toks):
        waits = self._collect(eng, (), (), toks)
        self.ops[eng].append((waits, None, None))

    def final_barrier(self, eng="sp"):
        toks = [(self.semidx[e], self.cnt[e]) for e in self.ENG if self.cnt[e] > 0 and e != eng]
        for ds in self._dsems:
            if ds.total > 0:
                toks.append((ds.idx, ds.total))
        self.wait_only(eng, toks)

    def emit(self):
        nc = self.nc
        semh = self.semh
        if os.environ.get("K_SEMCLR", "1") == "1":
            for h in semh:
                nc.sync.sem_clear(h)
            nc.all_engine_barrier()
        with nc.Block() as block:
            def mk(name):
                ops = self.ops[name]

                def body(e):
                    for waits, fn, si in ops:
                        for (wi, v) in waits:
                            e.wait_ge(semh[wi], v)
                        if fn is None:
                            continue
                        ins = fn(e)
                        if si is not None:
                            ins.then_inc(semh[si], 1)
                return body

            block.tensor(mk("pe"))
            block.scalar(mk("act"))
            block.vector(mk("dve"))
            block.gpsimd(mk("pool"))
            block.sync(mk("sp"))


class Arena:
    def __init__(self, ap, words):
        self.ap = ap
        self.words = words
        self.top = 0
        self.hist = []
        self.peak = 0

    def mark(self):
        return self.top

    def reset(self, m):
        self.top = m

    def alloc(self, words, nbuf=1, name=""):
        words = (words + 1) // 2 * 2
        st, en = self.top, self.top + words
        assert en <= self.words, "SBUF arena overflow %s %d" % (name, en)
        self.top = en
        self.peak = max(self.peak, en)
        bufs = [Buf(name) for _ in range(nbuf)]
        inherit = {}
        keep = []
        for (a, b, bl) in self.hist:
            if a < en and st < b:
                for ob in bl:
                    if ob.w is not None and inherit.get(ob.w[0], 0) < ob.w[1]:
                        inherit[ob.w[0]] = ob.w[1]
                    for si, v in ob.r.items():
                        if inherit.get(si, 0) < v:
                            inherit[si] = v
                if not (st <= a and b <= en):
                    keep.append((a, b, bl))
            else:
                keep.append((a, b, bl))
        self.hist = keep
        for nb in bufs:
            nb.r = dict(inherit)
        self.hist.append((st, en, bufs))
        return self.ap[:, st:en], bufs


def host_constants(C, S):
    T = C + S
    ident = np.eye(128, dtype=np.float32)
    inv = (10000.0 ** (-np.arange(16, dtype=np.float32) / 16.0)).astype(np.float32)
    t = np.arange(S)
    row = (t // GRID_W).astype(np.float32)
    col = (t % GRID_W).astype(np.float32)
    cos = np.ones((128, T), np.float32)
    sin = np.zeros((128, T), np.float32)
    swap = np.zeros((128, 128), np.float32)
    for p in range(128):
        j = p % 64
        base = row if j < 32 else col
        ang = (base * inv[j % 16]).astype(np.float32)
        first = (j % 32) < 16
        cos[p, C:] = np.cos(ang)
        sin[p, C:] = (-np.sin(ang)) if first else np.sin(ang)
        sp = p + 16 if first else p - 16
        swap[sp, p] = 1.0
    s_i = np.arange(128)[:, None]
    t_i = np.arange(128)[None, :]
    same = (s_i // CHUNK) == (t_i // CHUNK)
    maskf = (same & (s_i <= t_i)).astype(np.float32)
    maskb = (same & (s_i >= t_i)).astype(np.float32)
    scanm = np.ones((128, T), np.float32)
    scanm[:, ::CHUNK] = 0.0
    cmask = np.zeros((128, NPT), np.float32)
    for p in range(128):
        cmask[p, p // CHUNK] = 1.0
    onehot = np.zeros((16, 16, 128), np.float32)
    for e in range(16):
        onehot[e, e, :] = 1.0
    return dict(c_ident=ident, c_cos=cos, c_sin=sin, c_swap=swap, c_maskf=maskf, c_maskb=maskb,
                c_scanm=scanm, c_onehot=onehot.reshape(16, 16 * 128), c_cmask=cmask)


def build_program(NB, S, C, L, debug=(), stop=None):
    T = C + S
    NT = T // 128
    NCT = C // 128
    NCH = T // CHUNK
    TG = [(0, C, True)]
    gsz = min(512, S)
    for g in range(S // gsz):
        TG.append((C + g * gsz, gsz, False))
    MGN = 256 if S >= 256 else S
    TGM = []
    for st_ in range(0, C, min(MGN, C)):
        TGM.append((st_, min(MGN, C), True))
    for st_ in range(C, T, MGN):
        TGM.append((st_, MGN, False))

    nc = bass.Bass("TRN2", target_bir_lowering=False)

    def din(name, shape, dt=F32):
        return nc.dram_tensor(name, list(shape), dt, kind="ExternalInput").ap()

    x_in = din("x", [NB, S, D])
    ctx_in = din("ctx", [NB, C, D])
    cvec = din("cvec", [4, D])
    w_mod = din("w_mod", [L, D, 6 * D])
    b_mod = din("b_mod", [L, 6 * D])
    g_norm1 = din("g_norm1", [L, D])
    g_norm2 = din("g_norm2", [L, D])
    w_in = din("w_in", [L, D, N_IN])
    lam_q1 = din("lambda_q1", [L, 64])
    lam_k1 = din("lambda_k1", [L, 64])
    lam_q2 = din("lambda_q2", [L, 64])
    lam_k2 = din("lambda_k2", [L, 64])
    g_subln = din("g_subln", [L, 128])
    lb_logits = din("lb_logits", [L, D])
    g_rec = din("g_rec_norm", [L, 128])
    w_bra = din("w_br_attn", [L, D, D])
    w_brr = din("w_br_rec", [L, D, D])
    w_out = din("w_out", [L, D, D])
    w_router = din("w_router", [D, NEXP])
    b_router = din("b_router", [1, NEXP])
    w_gate = din("w_gate", [L, NEXP, D, D])
    w_up = din("w_up", [L, NEXP, D, D])
    w_down = din("w_down", [L, NEXP, D, D])
    g_final = din("g_final", [1, D])
    c_ident = din("c_ident", [128, 128])
    c_cos = din("c_cos", [128, T])
    c_sin = din("c_sin", [128, T])
    c_swap = din("c_swap", [128, 128])
    c_maskf = din("c_maskf", [128, 128])
    c_maskb = din("c_maskb", [128, 128])
    c_scanm = din("c_scanm", [128, T])
    c_onehot = din("c_onehot", [16, 16 * 128])
    c_cmask = din("c_cmask", [128, NPT])

    out = nc.dram_tensor("out", [NB, S, D], F32, kind="ExternalOutput").ap()
    xT = nc.dram_tensor("xT_scr", [NB, 128, KC, T], F32, kind="Internal").ap()
    aT = nc.dram_tensor("aT_scr", [128, KC, T], BF16, kind="Internal").ap()
    rT = nc.dram_tensor("rT_scr", [128, KC, T], BF16, kind="Internal").ap()
    dbg_out = {}

    st = contextlib.ExitStack()
    with st:
        s = Sched(nc, st)
        AW = int(os.environ.get("K_AW", 52224))
        arena_t = st.enter_context(nc.sbuf_tensor("arena", [128, AW], F32))
        A = Arena(arena_t, AW)
        psum = [st.enter_context(nc.psum_tensor("ps%d" % i, [128, 512], F32)) for i in range(8)]
        psb = [Buf("ps%d" % i, psum=True) for i in range(8)]
        rr = {"a": 0, "b": 0, "bn": 2}

        def ps_a():
            i = rr["a"] % 4
            rr["a"] += 1
            return psum[i], psb[i]

        def ps_b():
            i = 4 + rr["b"] % rr["bn"]
            rr["b"] += 1
            return psum[i], psb[i]

        def bf(ap):
            return ap.bitcast(BF16)

        dbg_sem = []
        dbg_ds = [None]

        def chk(name):
            if stop == name:
                s.dead = True

        def _dbg_ds():
            if dbg_ds[0] is None:
                dbg_ds[0] = s.dma_sem()
            return dbg_ds[0]

        def dbg(name, ap, bufs):
            if name not in debug:
                return
            shp = list(ap.shape)
            o = nc.dram_tensor("dbg_" + name, shp, ap.dtype, kind="ExternalOutput").ap()
            dbg_out[name] = o
            ds = _dbg_ds()
            tok = s.dma("sp", ds, o, ap, reads=bufs)
            dbg_sem.append(tok)

        def dscr(name, dram_ap, dram_bufs):
            if name not in debug:
                return
            o = nc.dram_tensor("dbg_" + name, list(dram_ap.shape), dram_ap.dtype, kind="ExternalOutput").ap()
            ds = _dbg_ds()
            tok = s.dma("sp", ds, o, dram_ap, reads=dram_bufs)
            dbg_sem.append(tok)

        ident_ap, (ident_b,) = A.alloc(128, 1, "ident")
        identb_w, (identb_b,) = A.alloc(64, 1, "identb")
        identb = bf(identb_w)
        ones_ap, (ones_b,) = A.alloc(128, 1, "ones")
        onesb_w, (onesb_b,) = A.alloc(64, 1, "onesb")
        onesb = bf(onesb_w)
        swap_w, (swap_b,) = A.alloc(64, 1, "swap")
        swapm = bf(swap_w)
        maskf_w, (maskf_b,) = A.alloc(64, 1, "maskf")
        maskb_w, (maskb_b,) = A.alloc(64, 1, "maskb")
        maskf = bf(maskf_w)
        maskb = bf(maskb_w)
        NCOL = NB + 1
        modv_ap, (modv_b,) = A.alloc(L * 48 * NCOL, 1, "modv")
        modv = modv_ap.rearrange("p (l c n) -> p l c n", l=L, c=48)
        a1_ap, (a1_b,) = A.alloc(L * KC * NCOL, 1, "a1")
        a1 = a1_ap.rearrange("p (l c n) -> p l c n", l=L, c=KC)
        a2_ap, (a2_b,) = A.alloc(L * KC * NCOL, 1, "a2")
        a2 = a2_ap.rearrange("p (l c n) -> p l c n", l=L, c=KC)
        small_ap, (small_b,) = A.alloc(256, 1, "small")
        lbv_ap, (lbv_b,) = A.alloc(L * 8 * 3, 1, "lbv")
        lbv = lbv_ap.rearrange("p (l h k) -> p l h k", l=L, h=8)
        lamv_ap, (lamv_b,) = A.alloc(L * 4, 1, "lamv")
        lamv = lamv_ap.rearrange("p (l k) -> p l k", l=L)
        brt_ap, (brt_b,) = A.alloc(16, 1, "brt")
        wr_w, (wr_b,) = A.alloc(KC * 16 // 2, 1, "wr")
        wrt = bf(wr_w).rearrange("p (c e) -> p c e", c=KC)
        eps_ap, (eps_b,) = A.alloc(2, 1, "eps")
        cmask_ap, (cmask_b,) = A.alloc(NPT, 1, "cmask")

        cs = [s.dma_sem() for _ in range(4)]
        s.dma("sp", cs[0], ident_ap, c_ident, writes=[ident_b])
        s.dma("sp", cs[0], cmask_ap, c_cmask, writes=[cmask_b])
        hm_ap, (hm_b,) = A.alloc(2, 1, "hmask")
        s.op("dve", lambda e: e.tensor_tensor(hm_ap[:, 0:1], cmask_ap[:, 0:1], cmask_ap[:, 1:2], ALU.add), reads=[cmask_b], writes=[hm_b])
        s.op("dve", lambda e: e.tensor_tensor(hm_ap[:, 1:2], cmask_ap[:, 2:3], cmask_ap[:, 3:4], ALU.add), reads=[cmask_b], writes=[hm_b])
        cs2 = [s.dma_sem() for _ in range(5)]
        s.dma("pool", cs2[0], swapm, c_swap, writes=[swap_b])
        s.dma("pool", cs2[1], maskf, c_maskf, writes=[maskf_b])
        s.dma("pool", cs2[2], maskb, c_maskb, writes=[maskb_b])
        s.dma("pool", cs2[4], wrt, w_router.rearrange("(c p) e -> p c e", p=128), writes=[wr_b])
        s.op("pool", lambda e: e.memset(ones_ap, 1.0), writes=[ones_b])
        s.op("pool", lambda e: e.memset(lamv_ap, 0.0), writes=[lamv_b])
        s.op("pool", lambda e: e.memset(onesb, 1.0), writes=[onesb_b])
        s.op("pool", lambda e: e.memset(eps_ap, EPS), writes=[eps_b])
        s.op("act", lambda e: e.copy(identb, ident_ap), reads=[ident_b], writes=[identb_b])

        chk("c0")
        m0 = A.mark()
        stA_ap, (stA_b,) = A.alloc(128, 1, "stA")
        stB_ap, (stB_b,) = A.alloc(128, 1, "stB")
        cv_ap, (cv_b,) = A.alloc(D, 1, "cv")
        scT_ap, (scT_b,) = A.alloc(KC * NCOL, 1, "scT")
        scT = scT_ap.rearrange("p (c n) -> p c n", c=KC)
        lamst_ap, (lamst_b,) = A.alloc(4 * L * 64 + 2 * L * 128, 1, "lamst")
        p0s = [s.dma_sem() for _ in range(4)]
        s.dma("sp", p0s[0], stA_ap[0:L * 48, :], b_mod.rearrange("l (c p) -> (l c) p", p=128), writes=[stA_b])
        s.dma("sp", p0s[1], stB_ap[0:L * 8, :], g_norm1.rearrange("l (c p) -> (l c) p", p=128), writes=[stB_b])
        s.dma("sp", p0s[1], stB_ap[16:16 + L * 8, :], g_norm2.rearrange("l (c p) -> (l c) p", p=128), writes=[stB_b])
        s.dma("sp", p0s[1], stB_ap[32:40, :], g_final.rearrange("o (c p) -> (o c) p", p=128), writes=[stB_b])
        s.dma("sp", p0s[1], stB_ap[40:40 + L * 8, :], lb_logits.rearrange("l (c p) -> (l c) p", p=128), writes=[stB_b])
        s.dma("sp", p0s[2], cv_ap[0:4, :], cvec, writes=[cv_b])
        lam_srcs = [lam_q1, lam_k1, lam_q2, lam_k2]
        for i, src in enumerate(lam_srcs):
            s.dma("sp", p0s[3], lamst_ap[:, i * L * 64:(i + 1) * L * 64],
                  src.rearrange("l d -> (l d)").partition_broadcast(128), writes=[lamst_b])
        s.dma("sp", p0s[3], brt_ap, b_router.rearrange("o e -> (o e)").partition_broadcast(128), writes=[brt_b])
        pst, pstb = ps_a()
        s.op("pe", lambda e: e.transpose(pst[:, 0:96], stA_ap[0:96, :], ident_ap[0:96, 0:96]),
             reads=[stA_b, ident_b], writes=[pstb])
        s.op("dve", lambda e: e.tensor_copy(small_ap[:, 0:96], pst[:, 0:96]), reads=[pstb], writes=[small_b])
        pst2, pstb2 = ps_a()
        s.op("pe", lambda e: e.transpose(pst2[:, 0:56], stB_ap[0:56, :], ident_ap[0:56, 0:56]),
             reads=[stB_b, ident_b], writes=[pstb2])
        s.op("dve", lambda e: e.tensor_copy(small_ap[:, 96:152], pst2[:, 0:56]), reads=[pstb2], writes=[small_b])
        chk("p0a")
        g1v = small_ap[:, 96:112].rearrange("p (l c) -> p l c", l=2)
        g2v = small_ap[:, 112:128].rearrange("p (l c) -> p l c", l=2)
        gfv = small_ap[:, 128:136]
        lbl = small_ap[:, 136:152].rearrange("p (l c) -> p l c", l=2)
        bmv = small_ap[:, 0:96].rearrange("p (l c) -> p l c", l=2)
        for l in range(L):
            s.dma("sp", p0s[3], lamv[:, l, 1:2], g_subln[l:l + 1, :].rearrange("o p -> p o"), writes=[lamv_b])
            s.dma("sp", p0s[3], lamv[:, l, 2:3], g_rec[l:l + 1, :].rearrange("o p -> p o"), writes=[lamv_b])
        lt_ap, (lt_b,) = A.alloc(L * 64 * 2 + 8 * L, 1, "lamtmp")
        for l in range(L):
            lam_init = 0.8 - 0.6 * math.exp(-0.3 * l)
            for j in range(2):
                qv = lamst_ap[:, (2 * j) * L * 64 + l * 64:(2 * j) * L * 64 + (l + 1) * 64]
                kv = lamst_ap[:, (2 * j + 1) * L * 64 + l * 64:(2 * j + 1) * L * 64 + (l + 1) * 64]
                pr = lt_ap[:, (l * 2 + j) * 64:(l * 2 + j + 1) * 64]
                sm = lt_ap[:, L * 128 + l * 4 + j:L * 128 + l * 4 + j + 1]
                s.op("dve", lambda e, pr=pr, qv=qv, kv=kv: e.tensor_tensor(pr, qv, kv, ALU.mult), reads=[lamst_b], writes=[lt_b])
                s.op("dve", lambda e, pr=pr, sm=sm: e.tensor_reduce(sm, pr, AX.X, ALU.add), reads=[lt_b], writes=[lt_b])
                s.op("act", lambda e, sm=sm: e.activation(out=sm, in_=sm, func=AF.Exp), reads=[lt_b], writes=[lt_b])
            e1 = lt_ap[:, L * 128 + l * 4:L * 128 + l * 4 + 1]
            e2 = lt_ap[:, L * 128 + l * 4 + 1:L * 128 + l * 4 + 2]
            s.op("dve", lambda e, l=l, e1=e1, e2=e2, li=lam_init: e.scalar_tensor_tensor(
                lamv[:, l, 0:1], e2, -li, e1, ALU.add, ALU.subtract), reads=[lt_b], writes=[lamv_b])
            s.op("dve", lambda e, l=l, li=lam_init: e.tensor_scalar(lamv[:, l, 1:2], lamv[:, l, 1:2], 1.0 - li, None, ALU.mult),
                 reads=[lamv_b], writes=[lamv_b])
        chk("p0b")
        lbe_ap, (lbe_b,) = A.alloc(L * 8 + 16, 1, "lbe")
        lbe = lbe_ap[:, 0:L * 8].rearrange("p (l c) -> p l c", l=L)
        lsum = lbe_ap[:, L * 8:L * 8 + 8]
        s.op("act", lambda e: e.activation(out=lbe_ap[:, 0:L * 8], in_=small_ap[:, 136:136 + L * 8], func=AF.Exp), reads=[small_b], writes=[lbe_b])
        s.op("dve", lambda e: e.tensor_copy(lsum, lbe[:, 0, :]), reads=[lbe_b], writes=[lbe_b])
        for l in range(1, L):
            s.op("dve", lambda e, l=l: e.tensor_tensor(lsum, lsum, lbe[:, l, :], ALU.add), reads=[lbe_b], writes=[lbe_b])
        s.op("dve", lambda e: e.reciprocal(lsum, lsum), reads=[lbe_b], writes=[lbe_b])
        s.op("pool", lambda e: e.memset(lbv[:, 0, :, 0], 0.0), writes=[lbv_b])
        for l in range(1, L):
            s.op("dve", lambda e, l=l: e.tensor_tensor(lbe[:, l, :], lbe[:, l, :], lsum, ALU.mult), reads=[lbe_b], writes=[lbe_b])
            s.op("dve", lambda e, l=l: e.tensor_tensor(lbv[:, l, :, 0], lbv[:, l - 1, :, 0], lbe[:, l, :], ALU.add),
                 reads=[lbe_b, lbv_b], writes=[lbv_b])
        for l in range(L):
            s.op("dve", lambda e, l=l: e.tensor_scalar(lbv[:, l, :, 1], lbv[:, l, :, 0], -1.0, 1.0, ALU.mult, ALU.add),
                 reads=[lbv_b], writes=[lbv_b])
            s.op("dve", lambda e, l=l: e.tensor_scalar(lbv[:, l, :, 2], lbv[:, l, :, 0], 1.0, -1.0, ALU.mult, ALU.add),
                 reads=[lbv_b], writes=[lbv_b])
        chk("p0c")
        s.op("act", lambda e: e.activation(out=cv_ap[0:NCOL, :], in_=cv_ap[0:NCOL, :], func=AF.Silu), reads=[cv_b], writes=[cv_b])
        pst3, pstb3 = ps_a()

        def f_ct(e):
            ins = None
            for c in range(KC):
                ins = e.transpose(pst3[:, c * NCOL:(c + 1) * NCOL], cv_ap[0:NCOL, c * 128:(c + 1) * 128], ident_ap[0:NCOL, 0:NCOL])
            return ins
        s.op("pe", f_ct, reads=[cv_b, ident_b], writes=[pstb3])
        s.op("dve", lambda e: e.tensor_copy(scT_ap, pst3[:, 0:KC * NCOL]), reads=[pstb3], writes=[scT_b])
        chk("p0d")
        wm_slots = []
        for i in range(2):
            ap_, (b_,) = A.alloc(KC * 512, 1, "wmod%d" % i)
            wm_slots.append((ap_.rearrange("p (c n) -> p c n", c=KC), b_, s.dma_sem()))
        gi = 0
        for l in range(L):
            pm, pmb = ps_a()
            for g in range(12):
                wt, wb, wsem = wm_slots[gi % 2]
                gi += 1
                s.dma("sp", wsem, wt, w_mod[l].rearrange("(c p) n -> p c n", p=128)[:, :, g * 512:(g + 1) * 512], writes=[wb])

                def f_mod(e, wt=wt, g=g, pm=pm):
                    ins = None
                    for j in range(4):
                        oc = g * 4 + j
                        for c in range(KC):
                            ins = e.matmul(pm[:, oc * NCOL:(oc + 1) * NCOL], wt[:, c, j * 128:(j + 1) * 128], scT[:, c, :],
                                           start=(c == 0), stop=(c == KC - 1))
                    return ins
                s.op("pe", f_mod, reads=[wb, scT_b], writes=[pmb])
            if l == 0:
                chk("p0e")
            s.op("dve", lambda e, l=l, pm=pm: e.tensor_tensor(
                modv[:, l], pm[:, 0:48 * NCOL].rearrange("p (c n) -> p c n", c=48),
                bmv[:, l, :].unsqueeze(2).to_broadcast([128, 48, NCOL]), ALU.add),
                reads=[pmb, small_b], writes=[modv_b])
            if l == 0:
                chk("p0f")
            for (dst, gv, off) in ((a1, g1v, 8), (a2, g2v, 32)):
                for col_ in range(NCOL):
                    s.op("dve", lambda e, l=l, dst=dst, gv=gv, off=off, col_=col_: e.scalar_tensor_tensor(
                        dst[:, l, :, col_], modv[:, l, off:off + 8, col_], 1.0, gv[:, l, :], ALU.add, ALU.mult),
                        reads=[modv_b, small_b], writes=[a1_b if dst is a1 else a2_b])
        dbg("modv", modv_ap, [modv_b])
        dbg("a1", a1_ap, [a1_b])
        dbg("lamv", lamv_ap, [lamv_b])
        dbg("lbv", lbv_ap, [lbv_b])
        A.reset(m0)
        chk("p0")

        xT_b = [[Buf("xT%d_%d" % (b, g)) for g in range(T // 128)] for b in range(NB)]
        aT_b = [Buf("aT%d" % h) for h in range(8)]
        rT_b = [Buf("rT%d" % h) for h in range(8)]

        def tiles_of(t0, n):
            return range(t0 // 128, (t0 + n) // 128)

        def act_rstd(dst, src_ps, inv_n, rd, wr):
            s.op("act", lambda e: e.activation(out=dst, in_=src_ps, func=AF.Ln, bias=eps_ap[:, 0:1], scale=inv_n), reads=rd + [eps_b], writes=wr)
            s.op("act", lambda e: e.activation(out=dst, in_=dst, func=AF.Exp, scale=-0.5), reads=wr, writes=wr)

        def norm_mod(xg, xg_b, n, hdst, hdst_b, av, sv, av_b):
            mk = A.mark()
            sq_ap, (sq_b,) = A.alloc(KC * n, 1, "sq")
            sq = sq_ap.rearrange("p (c n) -> p c n", c=KC)
            rs_ap, (rs_b,) = A.alloc(n, 1, "rstd")
            s.op("act", lambda e: e.activation(out=sq, in_=xg, func=AF.Square), reads=[xg_b], writes=[sq_b])
            pss, pssb = ps_a()

            def f(e):
                ins = None
                for c in range(KC):
                    ins = e.matmul(pss[:, 0:n], ones_ap, sq[:, c, :], start=(c == 0), stop=(c == KC - 1))
                return ins
            s.op("pe", f, reads=[sq_b, ones_b], writes=[pssb])
            act_rstd(rs_ap, pss[:, 0:n], 1.0 / D, [pssb], [rs_b])
            for c in range(KC):
                tmp = sq[:, c, :]
                s.op("dve", lambda e, c=c, tmp=tmp: e.scalar_tensor_tensor(tmp, xg[:, c, :], av[:, c:c + 1], rs_ap, ALU.mult, ALU.mult),
                     reads=[xg_b, rs_b, av_b], writes=[sq_b])
                s.op("act", lambda e, c=c, tmp=tmp: e.activation(out=hdst[:, c, :], in_=tmp, func=AF.Identity, bias=sv[:, c:c + 1], scale=1.0),
                     reads=[sq_b, av_b], writes=[hdst_b])
            A.reset(mk)

        def load_w(dst, dst_b, sem, src):
            return s.dma("pool", sem, dst, src, writes=[dst_b])

        m1 = A.mark()
        xin_slots = []
        for i in range(2):
            ap_, (b_,) = A.alloc(D, 1, "xin%d" % i)
            xo_, (ob_,) = A.alloc(D, 1, "xo%d" % i)
            xin_slots.append((ap_, b_, s.dma_sem(), xo_, ob_, s.dma_sem()))
        k = 0
        for b in range(NB):
            for ti in range(NT):
                ap_, b_, sm_, xo_, ob_, osm_ = xin_slots[k % 2]
                k += 1
                src = ctx_in[b, ti * 128:(ti + 1) * 128, :] if ti < NCT else x_in[b, (ti - NCT) * 128:(ti - NCT + 1) * 128, :]
                s.dma("sp", sm_, ap_, src, writes=[b_])
                for half in range(2):
                    pt, ptb = ps_a()

                    def f(e, ap_=ap_, pt=pt, half=half):
                        ins = None
                        for j in range(4):
                            c = half * 4 + j
                            ins = e.transpose(pt[:, j * 128:(j + 1) * 128], ap_[:, c * 128:(c + 1) * 128], ident_ap)
                        return ins
                    s.op("pe", f, reads=[b_, ident_b], writes=[ptb])
                    eng = "act" if half == 0 else "dve"
                    if eng == "act":
                        s.op("act", lambda e, xo_=xo_, pt=pt, half=half: e.copy(xo_[:, half * 512:(half + 1) * 512], pt[:, :]), reads=[ptb], writes=[ob_])
                    else:
                        s.op("dve", lambda e, xo_=xo_, pt=pt, half=half: e.tensor_copy(xo_[:, half * 512:(half + 1) * 512], pt[:, :]), reads=[ptb], writes=[ob_])
                s.dma("sp", osm_, xT[b, :, :, ti * 128:(ti + 1) * 128], xo_.rearrange("p (c t) -> p c t", c=KC), reads=[ob_], writes=[xT_b[b][ti]])
        A.reset(m1)
        chk("x0")

        out_toks = []
        for b in range(NB):
            for l in range(L):
                last = (l == L - 1)
                s.begin_iter()
                mL = A.mark()
                h_w, h_bufs = A.alloc(KC * T // 2, len(TG), "h")
                hT = bf(h_w).rearrange("p (c t) -> p c t", c=KC)
                hb_of = {}
                for gi_, (t0, n, isc) in enumerate(TG):
                    for ti in tiles_of(t0, n):
                        hb_of[ti] = h_bufs[gi_]

                def hbufs(t0, n):
                    return list({id(hb_of[ti]): hb_of[ti] for ti in tiles_of(t0, n)}.values())

                mk = A.mark()
                xg_slots = []
                for i in range(2):
                    ap_, (b_,) = A.alloc(KC * 512, 1, "xg%d" % i)
                    xg_slots.append((ap_, b_, s.dma_sem()))
                for gi_, (t0, n, isc) in enumerate(TG):
                    ap_, b_, sm_ = xg_slots[gi_ % 2]
                    xg = ap_[:, 0:KC * n].rearrange("p (c n) -> p c n", c=KC)
                    s.dma("sp", sm_, xg, xT[b, :, :, t0:t0 + n], reads=[xT_b[b][ti] for ti in tiles_of(t0, n)], writes=[b_])
                    col = NB if isc else b
                    norm_mod(xg, b_, n, hT[:, :, t0:t0 + n], h_bufs[gi_], a1[:, l, :, col], modv[:, l, 0:8, col], a1_b)
                A.reset(mk)
                if b == 0 and l == 0:
                    dbg("h", h_w, h_bufs)
                chk("n1")

                mk = A.mark()
                cos_ap, (cos_b,) = A.alloc(T, 1, "cos")
                sin_ap, (sin_b,) = A.alloc(T, 1, "sin")
                s.dma("sp", cs[1], cos_ap, c_cos, writes=[cos_b])
                s.dma("sp", cs[2], sin_ap, c_sin, writes=[sin_b])
                wq_w, (wq_b,) = A.alloc(KC * 128 // 2, 1, "wq")
                wk_w, (wk_b,) = A.alloc(KC * 128 // 2, 1, "wk")
                wv_w, (wv_b,) = A.alloc(KC * 128 // 2, 1, "wv")
                wq = bf(wq_w).rearrange("p (c n) -> p c n", c=KC)
                wk = bf(wk_w).rearrange("p (c n) -> p c n", c=KC)
                wv = bf(wv_w).rearrange("p (c n) -> p c n", c=KC)
                wsem = [s.dma_sem() for _ in range(3)]
                qr_w, (qr_b,) = A.alloc(T // 2, 1, "qr")
                qr1_w, (qr1_b,) = A.alloc(T // 2, 1, "qr1")
                kr_w, (kr_b,) = A.alloc(T // 2, 1, "kr")
                qr = bf(qr_w)
                qr1 = bf(qr1_w)
                kr = bf(kr_w)
                v_w, (v_b,) = A.alloc(NT * 128 // 2, 1, "v")
                vv = bf(v_w).rearrange("p (t d) -> p t d", t=NT)
                qb_slots = []
                for i in range(2):
                    ap_, (b_,) = A.alloc(256, 1, "qb%d" % i)
                    t1_, (t1b_,) = A.alloc(512, 1, "t1_%d" % i)
                    t2_, (t2b_,) = A.alloc(512, 1, "t2_%d" % i)
                    qb_slots.append((bf(ap_), b_, t1_, t1b_, t2_, t2b_))
                pT_slots = []
                for i in range(6):
                    ap_, (b_,) = A.alloc(256, 1, "pT%d" % i)
                    pT_slots.append((bf(ap_), b_))
                acc_slots = []
                for i in range(2):
                    ap_, (b_,) = A.alloc(512, 1, "acc%d" % i)
                    acc_slots.append((ap_, b_))
                rc_slots = []
                for i in range(2):
                    ap_, (b_,) = A.alloc(512, 1, "rc%d" % i)
                    rc_slots.append((ap_, b_))
                o_ap, (o_b,) = A.alloc(512, 1, "o")
                osq_ap, (osq_b,) = A.alloc(512, 1, "osq")
                ors_ap, (ors_b,) = A.alloc(512, 1, "ors")
                ao_slots = []
                for i in range(2):
                    ap_, (b_,) = A.alloc(256, 1, "ao%d" % i)
                    ao_slots.append((bf(ap_), b_, s.dma_sem()))
                qk_i = 0
                pt_i = 0
                ao_i = 0
                for hd in range(8):
                    wsrc = w_in[l].rearrange("(c p) n -> p c n", p=128)
                    load_w(wq, wq_b, wsem[0], wsrc[:, :, OFF_AQ + hd * 128:OFF_AQ + (hd + 1) * 128])
                    load_w(wk, wk_b, wsem[1], wsrc[:, :, OFF_AK + hd * 128:OFF_AK + (hd + 1) * 128])
                    load_w(wv, wv_b, wsem[2], wsrc[:, :, OFF_AV + hd * 128:OFF_AV + (hd + 1) * 128])
                    chk("a1")
                    for gi_, (t0, n, isc) in enumerate(TG):
                        for which in range(2):
                            if which == 0 and isc and last:
                                continue
                            wmat, wmb = (wq, wq_b) if which == 0 else (wk, wk_b)
                            dst, dst_b = (qr, qr_b) if which == 0 else (kr, kr_b)
                            qb_, qbb_, t1_, t1b_, t2_, t2b_ = qb_slots[qk_i % 2]
                            qk_i += 1
                            pq, pqb = ps_a()

                            def f(e, pq=pq, wmat=wmat, t0=t0, n=n):
                                ins = None
                                for c in range(KC):
                                    ins = e.matmul(pq[:, 0:n], wmat[:, c, :], hT[:, c, t0:t0 + n], start=(c == 0), stop=(c == KC - 1))
                                return ins
                            s.op("pe", f, reads=[wmb, h_bufs[gi_]], writes=[pqb])
                            s.op("act", lambda e, qb_=qb_, pq=pq, n=n: e.copy(qb_[:, 0:n], pq[:, 0:n]), reads=[pqb], writes=[qbb_])
                            chk("a2")
                            chk("i%d_a2" % qk_i)
                            psw, pswb = ps_a()
                            s.op("pe", lambda e, psw=psw, qb_=qb_, n=n: e.matmul(psw[:, 0:n], swapm, qb_[:, 0:n], start=True, stop=True),
                                 reads=[qbb_, swap_b], writes=[pswb])
                            chk("a3")
                            chk("i%d_a3" % qk_i)
                            s.op("dve", lambda e, t1_=t1_, pq=pq, t0=t0, n=n: e.tensor_tensor(t1_[:, 0:n], pq[:, 0:n], cos_ap[:, t0:t0 + n], ALU.mult),
                                 reads=[pqb, cos_b], writes=[t1b_])
                            s.op("dve", lambda e, t2_=t2_, psw=psw, t0=t0, n=n: e.tensor_tensor(t2_[:, 0:n], psw[:, 0:n], sin_ap[:, t0:t0 + n], ALU.mult),
                                 reads=[pswb, sin_b], writes=[t2b_])
                            chk("a4")
                            chk("i%d_a4" % qk_i)
                            if which == 1:
                                s.op("pool", lambda e, dst=dst, t1_=t1_, t2_=t2_, t0=t0, n=n: e.tensor_tensor(dst[:, t0:t0 + n], t1_[:, 0:n], t2_[:, 0:n], ALU.add),
                                     reads=[t1b_, t2b_], writes=[dst_b])
                            else:
                                s.op("pool", lambda e, t1_=t1_, t2_=t2_, n=n: e.tensor_tensor(t1_[:, 0:n], t1_[:, 0:n], t2_[:, 0:n], ALU.add),
                                     reads=[t1b_, t2b_], writes=[t1b_])
                                s.op("dve", lambda e, t1_=t1_, t0=t0, n=n: e.tensor_scalar(qr[:, t0:t0 + n], t1_[:, 0:n], hm_ap[:, 0:1], None, ALU.mult),
                                     reads=[t1b_, hm_b], writes=[qr_b])
                                s.op("dve", lambda e, t1_=t1_, t0=t0, n=n: e.tensor_scalar(qr1[:, t0:t0 + n], t1_[:, 0:n], hm_ap[:, 1:2], None, ALU.mult),
                                     reads=[t1b_, hm_b], writes=[qr1_b])
                            chk("a5")
                            chk("qk%d" % qk_i)
                    chk("att_a")
                    for t4 in range(0, NT, 4):
                        pv, pvb = ps_a()
                        nt4 = min(4, NT - t4)

                        def f(e, pv=pv, t4=t4, nt4=nt4):
                            ins = None
                            for j in range(nt4):
                                ti = t4 + j
                                for c in range(KC):
                                    ins = e.matmul(pv[:, j * 128:(j + 1) * 128], hT[:, c, ti * 128:(ti + 1) * 128], wv[:, c, :],
                                                   start=(c == 0), stop=(c == KC - 1))
                            return ins
                        s.op("pe", f, reads=[wv_b] + hbufs(t4 * 128, nt4 * 128), writes=[pvb])
                        s.op("act", lambda e, pv=pv, t4=t4, nt4=nt4: e.copy(
                            vv[:, t4:t4 + nt4, :], pv[:, 0:nt4 * 128].rearrange("p (t d) -> p t d", t=nt4)), reads=[pvb], writes=[v_b])
                    if b == 0 and l == 0 and hd == 0:
                        dbg("qr", qr_w, [qr_b])
                        dbg("kr", kr_w, [kr_b])
                        dbg("v", v_w, [v_b])
                    chk("att_b")
                    for gi_, (t0, n, isc) in enumerate(TG):
                        if isc and last:
                            continue
                        kts = range(0, NCT) if isc else range(0, NT)
                        nk = len(kts)
                        po = [psum[6], psum[7]]
                        pob = [psb[6], psb[7]]
                        for ki, kt in enumerate(kts):
                            for m in range(2):
                                pS, pSb = ps_a()
                                qm, qmb = (qr, qr_b) if m == 0 else (qr1, qr1_b)
                                s.op("pe", lambda e, pS=pS, kt=kt, qm=qm, t0=t0, n=n: e.matmul(
                                    pS[:, 0:n], kr[:, kt * 128:(kt + 1) * 128], qm[:, t0:t0 + n],
                                    start=True, stop=True), reads=[kr_b, qmb], writes=[pSb])
                                pT_, pTb_ = pT_slots[pt_i % 6]
                                pt_i += 1
                                s.op("act", lambda e, pT_=pT_, pS=pS, n=n: e.activation(out=pT_[:, 0:n], in_=pS[:, 0:n], func=AF.Exp, scale=0.125),
                                     reads=[pSb], writes=[pTb_])
                                acc_, accb_ = acc_slots[m]
                                if ki == 0:
                                    s.op("dve", lambda e, acc_=acc_, pT_=pT_, n=n: e.tensor_copy(acc_[:, 0:n], pT_[:, 0:n]), reads=[pTb_], writes=[accb_])
                                else:
                                    s.op("dve", lambda e, acc_=acc_, pT_=pT_, n=n: e.tensor_tensor(acc_[:, 0:n], acc_[:, 0:n], pT_[:, 0:n], ALU.add),
                                         reads=[pTb_, accb_], writes=[accb_])
                                s.op("pe", lambda e, m=m, kt=kt, pT_=pT_, n=n, ki=ki, nk=nk: e.matmul(
                                    po[m][:, 0:n], vv[:, kt, :], pT_[:, 0:n], start=(ki == 0), stop=(ki == nk - 1)),
                                    reads=[v_b, pTb_], writes=[pob[m]])
                        chk("att_c")
                        for m in range(2):
                            acc_, accb_ = acc_slots[m]
                            rc_, rcb_ = rc_slots[m]
                            pS, pSb = ps_a()
                            s.op("pe", lambda e, pS=pS, acc_=acc_, n=n: e.matmul(pS[:, 0:n], ones_ap, acc_[:, 0:n], start=True, stop=True),
                                 reads=[accb_, ones_b], writes=[pSb])
                            s.op("act", lambda e, rc_=rc_, pS=pS, n=n: e.activation(out=rc_[:, 0:n], in_=pS[:, 0:n], func=AF.Ln), reads=[pSb], writes=[rcb_])
                            s.op("act", lambda e, rc_=rc_, n=n: e.activation(out=rc_[:, 0:n], in_=rc_[:, 0:n], func=AF.Exp, scale=-1.0), reads=[rcb_], writes=[rcb_])
                            s.op("dve", lambda e, rc_=rc_, m=m, n=n: e.tensor_tensor(rc_[:, 0:n], po[m][:, 0:n], rc_[:, 0:n], ALU.mult),
                                 reads=[pob[m], rcb_], writes=[rcb_])
                        s.op("dve", lambda e, n=n: e.scalar_tensor_tensor(o_ap[:, 0:n], rc_slots[1][0][:, 0:n], lamv[:, l, 0:1], rc_slots[0][0][:, 0:n], ALU.mult, ALU.add),
                             reads=[rc_slots[0][1], rc_slots[1][1], lamv_b], writes=[o_b])
                        s.op("act", lambda e, n=n: e.activation(out=osq_ap[:, 0:n], in_=o_ap[:, 0:n], func=AF.Square), reads=[o_b], writes=[osq_b])
                        pS, pSb = ps_a()
                        s.op("pe", lambda e, pS=pS, n=n: e.matmul(pS[:, 0:n], ones_ap, osq_ap[:, 0:n], start=True, stop=True), reads=[osq_b, ones_b], writes=[pSb])
                        act_rstd(ors_ap[:, 0:n], pS[:, 0:n], 1.0 / 128, [pSb], [ors_b])
                        ao_, aob_, aosem_ = ao_slots[ao_i % 2]
                        ao_i += 1
                        s.op("dve", lambda e, ao_=ao_, n=n: e.scalar_tensor_tensor(ao_[:, 0:n], o_ap[:, 0:n], lamv[:, l, 1:2], ors_ap[:, 0:n], ALU.mult, ALU.mult),
                             reads=[o_b, ors_b, lamv_b], writes=[aob_])
                        chk("att_d")
                        s.dma("sp", aosem_, aT[:, hd, t0:t0 + n], ao_[:, 0:n], reads=[aob_], writes=[aT_b[hd]])
                        chk("att_e")
                A.reset(mk)
                if b == 0 and l == 0:
                    dscr("aT", aT, aT_b)
                chk("att")
                chk("L%d_att" % l)

                mk = A.mark()
                wnames = ["rq", "rff", "rfb", "ri", "rg"]
                woffs = [OFF_RQ, OFF_RFF, OFF_RFB, OFF_RI, OFF_RG]
                wr_ = {}
                for nm in wnames:
                    w_, (wb_,) = A.alloc(KC * 128 // 2, 1, "w" + nm)
                    wr_[nm] = (bf(w_).rearrange("p (c n) -> p c n", c=KC), wb_, s.dma_sem())
                scanm_w, (scanm_b,) = A.alloc(T // 2, 1, "scanm")
                scanm_ap = bf(scanm_w)
                s.dma("pool", cs2[3], scanm_ap, c_scanm, writes=[scanm_b])
                W1, (W1b,) = A.alloc(T, 1, "W1")
                W2, (W2b,) = A.alloc(T, 1, "W2")
                qf_w, (qf_b,) = A.alloc(T // 2, 1, "qf")
                kk_w, (kk_b,) = A.alloc(T // 2, 1, "kk")
                eg_w, (eg_b,) = A.alloc(T // 2, 1, "eg")
                en_w, (en_b,) = A.alloc(T // 2, 1, "en")
                sg_w, (sg_b,) = A.alloc(T // 2, 1, "sg")
                qf, kk, eg, en, sg = bf(qf_w), bf(kk_w), bf(eg_w), bf(en_w), bf(sg_w)
                vr_w, (vr_b,) = A.alloc(NT * 128 // 2, 1, "vr")
                vr = bf(vr_w).rearrange("p (t d) -> p t d", t=NT)
                vblk_w, (vblk_b,) = A.alloc(NT * NPT * 128 // 2, 1, "vblk")
                vblk = bf(vblk_w).rearrange("p (t c d) -> p t c d", t=NT, c=NPT)
                dirs = {}
                for dname in ("f", "b"):
                    QT_w, (QT_b,) = A.alloc(T // 2, 1, "QT" + dname)
                    KT_w, (KT_b,) = A.alloc(T // 2, 1, "KT" + dname)
                    KH_w, (KH_b,) = A.alloc(T // 2, 1, "KH" + dname)
                    egl_ap, (egl_b,) = A.alloc(NCH, 1, "egl" + dname)
                    st_w, st_bufs = A.alloc((NCH + 1) * 128 // 2, NCH + 1, "st" + dname)
                    S32w, S32bufs = A.alloc(256, 2, "S32" + dname)
                    S32 = [S32w[:, 0:128], S32w[:, 128:256]]
                    S32b = S32bufs
                    dirs[dname] = dict(QT=bf(QT_w), QT_b=QT_b, KT=bf(KT_w), KT_b=KT_b, KH=bf(KH_w), KH_b=KH_b, egl=egl_ap, egl_b=egl_b,
                                       st=bf(st_w).rearrange("p (c d) -> p c d", c=NCH + 1), st_bufs=st_bufs, S32=S32, S32b=S32b)
                khT_slots = []
                for i in range(4):
                    ap_, (b_,) = A.alloc(64, 1, "khT%d" % i)
                    khT_slots.append((bf(ap_), b_))
                am_slots = []
                for i in range(4):
                    ap_, (b_,) = A.alloc(64, 1, "am%d" % i)
                    am_slots.append((bf(ap_), b_))
                osq2_ap, (osq2_b,) = A.alloc(512, 1, "osq2")
                ors2_ap, (ors2_b,) = A.alloc(512, 1, "ors2")
                ro_slots = []
                for i in range(2):
                    ap_, (b_,) = A.alloc(256, 1, "ro%d" % i)
                    ro_slots.append((bf(ap_), b_, s.dma_sem()))
                kh_i = 0
                am_i = 0
                ro_i = 0
                ctx_ch = list(range(0, C // CHUNK))
                lat_ch = list(range(C // CHUNK, NCH))
                order = {"f": ctx_ch + lat_ch, "b": ctx_ch[::-1] + lat_ch[::-1]}
                for hd in range(8):
                    wsrc = w_in[l].rearrange("(c p) n -> p c n", p=128)
                    for nm, off in zip(wnames, woffs):
                        load_w(wr_[nm][0], wr_[nm][1], wr_[nm][2], wsrc[:, :, off + hd * 128:off + (hd + 1) * 128])
                    lb_s = lbv[:, l, hd, 0:1]
                    oml_s = lbv[:, l, hd, 1:2]
                    noml_s = lbv[:, l, hd, 2:3]
                    for gi_, (t0, n, isc) in enumerate(TG):
                        pq, pqb = ps_a()

                        def f(e, pq=pq, t0=t0, n=n):
                            ins = None
                            for c in range(KC):
                                ins = e.matmul(pq[:, 0:n], wr_["rq"][0][:, c, :], hT[:, c, t0:t0 + n], start=(c == 0), stop=(c == KC - 1))
                            return ins
                        s.op("pe", f, reads=[wr_["rq"][1], h_bufs[gi_]], writes=[pqb])
                        s.op("act", lambda e, pq=pq, t0=t0, n=n: e.copy(qf[:, t0:t0 + n], pq[:, 0:n]), reads=[pqb], writes=[qf_b])
                        pg, pgb = ps_a()

                        def f(e, pg=pg, t0=t0, n=n):
                            ins = None
                            for c in range(KC):
                                ins = e.matmul(pg[:, 0:n], wr_["rg"][0][:, c, :], hT[:, c, t0:t0 + n], start=(c == 0), stop=(c == KC - 1))
                            return ins
                        s.op("pe", f, reads=[wr_["rg"][1], h_bufs[gi_]], writes=[pgb])
                        s.op("act", lambda e, pg=pg, t0=t0, n=n: e.activation(out=sg[:, t0:t0 + n], in_=pg[:, 0:n], func=AF.Silu), reads=[pgb], writes=[sg_b])
                    for t4 in range(0, NT, 4):
                        pv, pvb = ps_a()
                        nt4 = min(4, NT - t4)

                        def f(e, pv=pv, t4=t4, nt4=nt4):
                            ins = None
                            for j in range(nt4):
                                ti = t4 + j
                                for c in range(KC):
                                    ins = e.matmul(pv[:, j * 128:(j + 1) * 128], hT[:, c, ti * 128:(ti + 1) * 128], wr_["ri"][0][:, c, :],
                                                   start=(c == 0), stop=(c == KC - 1))
                            return ins
                        s.op("pe", f, reads=[wr_["ri"][1]] + hbufs(t4 * 128, nt4 * 128), writes=[pvb])
                        s.op("act", lambda e, pv=pv, t4=t4, nt4=nt4: e.copy(
                            vr[:, t4:t4 + nt4, :], pv[:, 0:nt4 * 128].rearrange("p (t d) -> p t d", t=nt4)), reads=[pvb], writes=[vr_b])
                        for cc in range(NPT):
                            s.op("dve", lambda e, pv=pv, t4=t4, nt4=nt4, cc=cc: e.tensor_scalar(
                                vblk[:, t4:t4 + nt4, cc, :], pv[:, 0:nt4 * 128].rearrange("p (t d) -> p t d", t=nt4), cmask_ap[:, cc:cc + 1], None, ALU.mult),
                                reads=[pvb, cmask_b], writes=[vblk_b])
                    for dname in ("f", "b"):
                        dd = dirs[dname]
                        wz = wr_["rff"] if dname == "f" else wr_["rfb"]
                        for gi_, (t0, n, isc) in enumerate(TG):
                            pz, pzb = ps_a()

                            def f(e, pz=pz, t0=t0, n=n, wz=wz):
                                ins = None
                                for c in range(KC):
                                    ins = e.matmul(pz[:, 0:n], wz[0][:, c, :], hT[:, c, t0:t0 + n], start=(c == 0), stop=(c == KC - 1))
                                return ins
                            s.op("pe", f, reads=[wz[1], h_bufs[gi_]], writes=[pzb])
                            s.op("act", lambda e, pz=pz, t0=t0, n=n: e.activation(out=W1[:, t0:t0 + n], in_=pz[:, 0:n], func=AF.Sigmoid), reads=[pzb], writes=[W1b])
                        s.op("dve", lambda e: e.tensor_scalar(W2, W1, oml_s, lb_s, ALU.mult, ALU.add), reads=[W1b, lbv_b], writes=[W2b])
                        s.op("dve", lambda e: e.tensor_scalar(kk, W1, noml_s, oml_s, ALU.mult, ALU.add), reads=[W1b, lbv_b], writes=[kk_b])
                        s.op("act", lambda e: e.activation(out=W1, in_=W2, func=AF.Ln), reads=[W2b], writes=[W1b])
                        s.op("dve", lambda e: e.tensor_tensor_scan(W2, scanm_ap, W1, 0.0, ALU.mult, ALU.add), reads=[W1b, scanm_b], writes=[W2b])
                        W13 = W1.rearrange("p (c t) -> p c t", t=CHUNK)
                        W23 = W2.rearrange("p (c t) -> p c t", t=CHUNK)
                        if dname == "f":
                            G, Gb, G3 = W2, W2b, W23
                            last_col = CHUNK - 1
                            X, Xb = W1, W1b
                        else:
                            s.op("dve", lambda e: e.tensor_tensor(W1, W1, W2, ALU.subtract), reads=[W1b, W2b], writes=[W1b])
                            s.op("dve", lambda e: e.tensor_tensor(W13, W13, W23[:, :, CHUNK - 1:CHUNK].to_broadcast([128, NCH, CHUNK]), ALU.add),
                                 reads=[W1b, W2b], writes=[W1b])
                            G, Gb, G3 = W1, W1b, W13
                            last_col = 0
                            X, Xb = W2, W2b
                        s.op("act", lambda e, G=G: e.activation(out=eg, in_=G, func=AF.Exp), reads=[Gb], writes=[eg_b])
                        s.op("act", lambda e, G=G: e.activation(out=en, in_=G, func=AF.Exp, scale=-1.0), reads=[Gb], writes=[en_b])
                        s.op("act", lambda e, G3=G3, last_col=last_col, dd=dd: e.activation(out=dd["egl"], in_=G3[:, :, last_col], func=AF.Exp), reads=[Gb], writes=[dd["egl_b"]])
                        s.op("dve", lambda e, dd=dd: e.tensor_tensor(dd["QT"], qf, eg, ALU.mult), reads=[qf_b, eg_b], writes=[dd["QT_b"]])
                        s.op("pool", lambda e, dd=dd: e.tensor_tensor(dd["KT"], kk, en, ALU.mult), reads=[kk_b, en_b], writes=[dd["KT_b"]])
                        X3 = X.rearrange("p (c t) -> p c t", t=CHUNK)
                        s.op("dve", lambda e, dd=dd, X3=X3: e.tensor_tensor(
                            X3, dd["KT"].rearrange("p (c t) -> p c t", t=CHUNK), dd["egl"].unsqueeze(2).to_broadcast([128, NCH, CHUNK]), ALU.mult),
                            reads=[dd["KT_b"], dd["egl_b"], Xb], writes=[Xb])
                        s.op("pool", lambda e, dd=dd, X=X: e.tensor_copy(dd["KH"], X), reads=[Xb], writes=[dd["KH_b"]])
                        if b == 0 and l == 0 and hd == 0:
                            dbg("QT" + dname, dd["QT"].bitcast(F32), [dd["QT_b"]])
                            dbg("KT" + dname, dd["KT"].bitcast(F32), [dd["KT_b"]])
                            dbg("KH" + dname, dd["KH"].bitcast(F32), [dd["KH_b"]])
                            dbg("egl" + dname, dd["egl"], [dd["egl_b"]])
                    for dname in ("f", "b"):
                        dd = dirs[dname]
                        ordr = order[dname]
                        s.op("pool", lambda e, dd=dd: e.memset(dd["S32"][0], 0.0), writes=[dd["S32b"][0]])
                        kstep = 0
                        c0 = ordr[0]
                        s.op("pool", lambda e, dd=dd, c0=c0: e.memset(dd["st"][:, c0, :], 0.0), writes=[dd["st_bufs"][c0]])
                        tile_order = []
                        for cch in ordr:
                            if cch // NPT not in tile_order:
                                tile_order.append(cch // NPT)
                        pos = 0
                        for ti in tile_order:
                            khT_, khTb_ = khT_slots[kh_i % 4]
                            kh_i += 1
                            ptr, ptrb = ps_b()
                            ptr_bf = ptr[:, 0:64].bitcast(BF16)
                            s.op("pe", lambda e, ptr_bf=ptr_bf, dd=dd, ti=ti: e.transpose(ptr_bf, dd["KH"][:, ti * 128:(ti + 1) * 128], identb),
                                 reads=[dd["KH_b"], identb_b], writes=[ptrb])
                            s.op("act", lambda e, khT_=khT_, ptr_bf=ptr_bf: e.copy(khT_, ptr_bf), reads=[ptrb], writes=[khTb_])
                            pu, pub = ps_b()

                            s.op("pe", lambda e, pu=pu, khT_=khT_, ti=ti: e.matmul(
                                pu[:, 0:NPT * 128], khT_, vblk[:, ti].rearrange("p c d -> p (c d)"), start=True, stop=True),
                                reads=[khTb_, vblk_b], writes=[pub])
                            chunks_here = [cch for cch in ordr if cch // NPT == ti]
                            for cch in chunks_here:
                                j = cch % NPT
                                nxt = ordr[pos + 1] if pos + 1 < len(ordr) else NCH
                                pos += 1
                                sa, sab = dd["S32"][kstep % 2], dd["S32b"][kstep % 2]
                                sn, snb = dd["S32"][(kstep + 1) % 2], dd["S32b"][(kstep + 1) % 2]
                                kstep += 1
                                s.op("dve", lambda e, dd=dd, cch=cch, j=j, pu=pu: e.scalar_tensor_tensor(
                                    sn, sa, dd["egl"][:, cch:cch + 1], pu[:, j * 128:(j + 1) * 128], ALU.mult, ALU.add),
                                    reads=[sab, dd["egl_b"], pub], writes=[snb])
                                s.op("pool", lambda e, dd=dd, nxt=nxt: e.tensor_copy(dd["st"][:, nxt, :], sn), reads=[snb], writes=[dd["st_bufs"][nxt]])
                    for t4 in range(0, NT, 4):
                        nt4 = min(4, NT - t4)
                        po_, pob_ = psum[6 + (t4 // 4) % 2], psb[6 + (t4 // 4) % 2]
                        for jt in range(nt4):
                            ti = t4 + jt
                            ams = []
                            for dname in ("f", "b"):
                                dd = dirs[dname]
                                pa_, pab_ = ps_b()
                                s.op("pe", lambda e, pa_=pa_, dd=dd, ti=ti: e.matmul(
                                    pa_[:, 0:128], dd["KT"][:, ti * 128:(ti + 1) * 128], dd["QT"][:, ti * 128:(ti + 1) * 128], start=True, stop=True),
                                    reads=[dd["KT_b"], dd["QT_b"]], writes=[pab_])
                                am_, amb_ = am_slots[am_i % 4]
                                am_i += 1
                                mk_, mkb_ = (maskf, maskf_b) if dname == "f" else (maskb, maskb_b)
                                s.op("dve", lambda e, am_=am_, pa_=pa_, mk_=mk_: e.tensor_tensor(am_, pa_[:, 0:128], mk_, ALU.mult), reads=[pab_, mkb_], writes=[amb_])
                                ams.append((am_, amb_))

                            def f(e, po_=po_, jt=jt, ti=ti, ams=ams):
                                ins = None
                                first = True
                                for di, dname in enumerate(("f", "b")):
                                    dd = dirs[dname]
                                    ins = e.matmul(po_[:, jt * 128:(jt + 1) * 128], vr[:, ti, :], ams[di][0], start=first, stop=False)
                                    first = False
                                    for j in range(NPT):
                                        cch = ti * NPT + j
                                        ins = e.matmul(po_[:, jt * 128 + j * CHUNK:jt * 128 + (j + 1) * CHUNK], dd["st"][:, cch, :],
                                                       dd["QT"][:, cch * CHUNK:(cch + 1) * CHUNK], start=False, stop=(di == 1 and j == NPT - 1))
                                return ins
                            rds = [vr_b, ams[0][1], ams[1][1]]
                            for dname in ("f", "b"):
                                dd = dirs[dname]
                                rds += [dd["QT_b"]] + [dd["st_bufs"][ti * NPT + j] for j in range(NPT)]
                            s.op("pe", f, reads=rds, writes=[pob_])
                        n = nt4 * 128
                        t0 = t4 * 128
                        s.op("act", lambda e, po_=po_, n=n: e.activation(out=osq2_ap[:, 0:n], in_=po_[:, 0:n], func=AF.Square), reads=[pob_], writes=[osq2_b])
                        pS, pSb = ps_a()
                        s.op("pe", lambda e, pS=pS, n=n: e.matmul(pS[:, 0:n], ones_ap, osq2_ap[:, 0:n], start=True, stop=True), reads=[osq2_b, ones_b], writes=[pSb])
                        act_rstd(ors2_ap[:, 0:n], pS[:, 0:n], 1.0 / 128, [pSb], [ors2_b])
                        s.op("dve", lambda e, po_=po_, n=n: e.scalar_tensor_tensor(osq2_ap[:, 0:n], po_[:, 0:n], lamv[:, l, 2:3], ors2_ap[:, 0:n], ALU.mult, ALU.mult),
                             reads=[pob_, ors2_b, lamv_b], writes=[osq2_b])
                        ro_, rob_, rosem_ = ro_slots[ro_i % 2]
                        ro_i += 1
                        s.op("pool", lambda e, ro_=ro_, n=n, t0=t0: e.tensor_tensor(ro_[:, 0:n], osq2_ap[:, 0:n], sg[:, t0:t0 + n], ALU.mult),
                             reads=[osq2_b, sg_b], writes=[rob_])
                        s.dma("sp", rosem_, rT[:, hd, t0:t0 + n], ro_[:, 0:n], reads=[rob_], writes=[rT_b[hd]])
                A.reset(mk)
                if b == 0 and l == 0:
                    dscr("rT", rT, rT_b)
                chk("rec")
                chk("L%d_rec" % l)

                mk = A.mark()
                wm = {}
                for nm in ("ga", "gr", "ba", "br", "wo"):
                    w_, (wb_,) = A.alloc(KC * D // 2, 1, "wm" + nm)
                    wm[nm] = (bf(w_).rearrange("p (c n) -> p c n", c=KC), wb_, s.dma_sem())
                wsrc = w_in[l].rearrange("(c p) n -> p c n", p=128)
                load_w(wm["ga"][0], wm["ga"][1], wm["ga"][2], wsrc[:, :, OFF_GA:OFF_GA + D])
                load_w(wm["gr"][0], wm["gr"][1], wm["gr"][2], wsrc[:, :, OFF_GR:OFF_GR + D])
                load_w(wm["ba"][0], wm["ba"][1], wm["ba"][2], w_bra[l].rearrange("(c p) n -> p c n", p=128))
                load_w(wm["br"][0], wm["br"][1], wm["br"][2], w_brr[l].rearrange("(c p) n -> p c n", p=128))
                load_w(wm["wo"][0], wm["wo"][1], wm["wo"][2], w_out[l].rearrange("(c p) n -> p c n", p=128))
                ar_slots = []
                for i in range(2):
                    a_, (ab_,) = A.alloc(KC * MGN // 2, 1, "ag%d" % i)
                    r_, (rb_,) = A.alloc(KC * MGN // 2, 1, "rg%d" % i)
                    x_, (xb_,) = A.alloc(KC * MGN, 1, "xm%d" % i)
                    ar_slots.append((bf(a_).rearrange("p (c n) -> p c n", c=KC), ab_, s.dma_sem(),
                                     bf(r_).rearrange("p (c n) -> p c n", c=KC), rb_, s.dma_sem(),
                                     x_.rearrange("p (c n) -> p c n", c=KC), xb_, s.dma_sem(), s.dma_sem()))
                mx_w, (mx_b,) = A.alloc(KC * MGN // 2, 1, "mixed")
                mixed = bf(mx_w).rearrange("p (c n) -> p c n", c=KC)
                sg_slots = []
                for i in range(2):
                    s1_, (s1b_,) = A.alloc(MGN, 1, "sga%d" % i)
                    s2_, (s2b_,) = A.alloc(MGN, 1, "sgr%d" % i)
                    sg_slots.append((s1_, s1b_, s2_, s2b_))
                sgi = 0
                for gi_, (t0, n, isc) in enumerate(TGM):
                    if isc and last:
                        continue
                    col = NB if isc else b
                    a_, ab_, asem_, r_, rb_, rsem_, x_, xb_, xsem_, xosem_ = ar_slots[gi_ % 2]
                    s.dma("sp", asem_, a_[:, :, 0:n], aT[:, :, t0:t0 + n], reads=aT_b, writes=[ab_])
                    s.dma("sp", rsem_, r_[:, :, 0:n], rT[:, :, t0:t0 + n], reads=rT_b, writes=[rb_])
                    s.dma("sp", xsem_, x_[:, :, 0:n], xT[b, :, :, t0:t0 + n], reads=[xT_b[b][ti] for ti in tiles_of(t0, n)], writes=[xb_])
                    hbs = hbufs(t0, n)
                    for oc in range(KC):
                        s1_, s1b_, s2_, s2b_ = sg_slots[sgi % 2]
                        sgi += 1
                        pgs = []
                        for (wnm, src, srcb) in (("ga", hT[:, :, t0:t0 + n], hbs), ("gr", hT[:, :, t0:t0 + n], hbs), ("ba", a_[:, :, 0:n], [ab_]), ("br", r_[:, :, 0:n], [rb_])):
                            pg, pgb = ps_a()

                            def f(e, pg=pg, wnm=wnm, src=src, oc=oc, n=n):
                                ins = None
                                for c in range(KC):
                                    ins = e.matmul(pg[:, 0:n], wm[wnm][0][:, c, oc * 128:(oc + 1) * 128], src[:, c, :], start=(c == 0), stop=(c == KC - 1))
                                return ins
                            s.op("pe", f, reads=[wm[wnm][1]] + list(srcb), writes=[pgb])
                            pgs.append((pg, pgb))
                        s.op("act", lambda e, s1_=s1_, pg=pgs[0][0], n=n: e.activation(out=s1_[:, 0:n], in_=pg[:, 0:n], func=AF.Sigmoid), reads=[pgs[0][1]], writes=[s1b_])
                        s.op("act", lambda e, s2_=s2_, pg=pgs[1][0], n=n: e.activation(out=s2_[:, 0:n], in_=pg[:, 0:n], func=AF.Sigmoid), reads=[pgs[1][1]], writes=[s2b_])
                        s.op("dve", lambda e, s1_=s1_, pg=pgs[2][0], n=n: e.tensor_tensor(s1_[:, 0:n], pg[:, 0:n], s1_[:, 0:n], ALU.mult), reads=[pgs[2][1], s1b_], writes=[s1b_])
                        s.op("dve", lambda e, s2_=s2_, pg=pgs[3][0], n=n: e.tensor_tensor(s2_[:, 0:n], pg[:, 0:n], s2_[:, 0:n], ALU.mult), reads=[pgs[3][1], s2b_], writes=[s2b_])
                        s.op("pool", lambda e, s1_=s1_, s2_=s2_, oc=oc, n=n: e.tensor_tensor(mixed[:, oc, 0:n], s1_[:, 0:n], s2_[:, 0:n], ALU.add),
                             reads=[s1b_, s2b_], writes=[mx_b])
                    for oc in range(KC):
                        py, pyb = ps_a()

                        def f(e, py=py, oc=oc, n=n):
                            ins = None
                            for c in range(KC):
                                ins = e.matmul(py[:, 0:n], wm["wo"][0][:, c, oc * 128:(oc + 1) * 128], mixed[:, c, 0:n], start=(c == 0), stop=(c == KC - 1))
                            return ins
                        s.op("pe", f, reads=[wm["wo"][1], mx_b], writes=[pyb])
                        s.op("dve", lambda e, py=py, oc=oc, n=n, x_=x_, col=col: e.scalar_tensor_tensor(
                            x_[:, oc, 0:n], py[:, 0:n], modv[:, l, 16 + oc, col:col + 1], x_[:, oc, 0:n], ALU.mult, ALU.add),
                            reads=[pyb, xb_, modv_b], writes=[xb_])
                    s.dma("sp", xosem_, xT[b, :, :, t0:t0 + n], x_[:, :, 0:n], reads=[xb_], writes=[xT_b[b][ti] for ti in tiles_of(t0, n)])
                    norm_mod(x_[:, :, 0:n], xb_, n, hT[:, :, t0:t0 + n], hbs[0], a2[:, l, :, col], modv[:, l, 24:32, col], a2_b)
                    for ob in hbs[1:]:
                        ob.w = hbs[0].w
                        ob.r = {}
                A.reset(mk)
                if b == 0 and l == 0:
                    dbg("h2", h_w, h_bufs)
                    dscr("x1", xT[0], xT_b[0])
                chk("merge")
                chk("L%d_merge" % l)

                mk = A.mark()
                onehot_w, (onehot_b,) = A.alloc(16 * 128 // 2, 1, "onehot")
                onehot = bf(onehot_w)
                s.dma("pool", cs2[3], onehot[0:16, :], c_onehot, writes=[onehot_b])
                xr_ap, xr_bufs = A.alloc(KC * T, len(TG), "xres")
                xres = xr_ap.rearrange("p (c t) -> p c t", c=KC)
                xrsem = [s.dma_sem() for _ in TG]
                tg_moe = [(gi_, t0, n, isc) for gi_, (t0, n, isc) in enumerate(TG) if not (isc and last)]
                for (gi_, t0, n, isc) in tg_moe:
                    s.dma("sp", xrsem[gi_], xres[:, :, t0:t0 + n], xT[b, :, :, t0:t0 + n], reads=[xT_b[b][ti] for ti in tiles_of(t0, n)], writes=[xr_bufs[gi_]])
                tiles_moe = [ti for ti in range(NT) if not (last and ti < NCT)]
                ntm = len(tiles_moe)
                sc_ap, (sc_b,) = A.alloc(NT * 16, 1, "scores")
                bi_ap, (bi_b,) = A.alloc(NT * 16, 1, "biased")
                t_ap, (t_b,) = A.alloc(NT * 16, 1, "rt_tmp")
                g_ap, (g_b,) = A.alloc(NT * 16, 1, "gates")
                m1_ap, (m1_b,) = A.alloc(NT * 4, 1, "max1")
                m2_ap, (m2_b,) = A.alloc(NT * 4, 1, "max2")
                gs_ap, (gs_b,) = A.alloc(NT * 4, 1, "gsel")
                gm_ap, (gm_b,) = A.alloc(NT, 1, "gmax")
                gT_w, (gT_b,) = A.alloc(T // 2, 1, "gT")
                gT = bf(gT_w)
                gb_w, gb_bufs = A.alloc(T // 2, len(TG), "gbs")
                gbs = bf(gb_w)
                prt, prtb = ps_a()

                def f(e):
                    ins = None
                    for ti in tiles_moe:
                        for c in range(KC):
                            ins = e.matmul(prt[:, ti * 16:(ti + 1) * 16], hT[:, c, ti * 128:(ti + 1) * 128], wrt[:, c, :], start=(c == 0), stop=(c == KC - 1))
                    return ins
                s.op("pe", f, reads=[wr_b] + h_bufs, writes=[prtb])
                lo, hi = tiles_moe[0], tiles_moe[-1] + 1

                def v3(ap):
                    return ap[:, lo * 16:hi * 16].rearrange("p (t e) -> p t e", e=16)

                def v4(ap):
                    return ap[:, lo * 16:hi * 16].rearrange("p (t e) -> p t e", e=4)

                def v2(ap):
                    return ap[:, lo * 4:hi * 4]
                s.op("act", lambda e: e.activation(out=sc_ap[:, lo * 16:hi * 16], in_=prt[:, lo * 16:hi * 16], func=AF.Sigmoid), reads=[prtb], writes=[sc_b])
                s.op("dve", lambda e: e.tensor_tensor(v3(bi_ap), v3(sc_ap), brt_ap.unsqueeze(1).to_broadcast([128, ntm, 16]), ALU.add), reads=[sc_b, brt_b], writes=[bi_b])
                s.op("dve", lambda e: e.tensor_reduce(v2(m1_ap), v4(bi_ap), AX.X, ALU.max), reads=[bi_b], writes=[m1_b])
                s.op("dve", lambda e: e.tensor_tensor(v4(t_ap), v4(bi_ap), v2(m1_ap).unsqueeze(2).to_broadcast([128, ntm * 4, 4]), ALU.is_ge), reads=[bi_b, m1_b], writes=[t_b])
                s.op("dve", lambda e: e.scalar_tensor_tensor(t_ap[:, lo * 16:hi * 16], t_ap[:, lo * 16:hi * 16], -1e9, bi_ap[:, lo * 16:hi * 16], ALU.mult, ALU.add), reads=[t_b, bi_b], writes=[t_b])
                s.op("dve", lambda e: e.tensor_reduce(v2(m2_ap), v4(t_ap), AX.X, ALU.max), reads=[t_b], writes=[m2_b])
                s.op("dve", lambda e: e.tensor_tensor(v2(gs_ap), v2(m1_ap), v2(m2_ap), ALU.add), reads=[m1_b, m2_b], writes=[gs_b])
                s.op("dve", lambda e: e.tensor_reduce(gm_ap[:, lo:hi], v2(gs_ap).rearrange("p (t g) -> p t g", g=4), AX.X, ALU.max), reads=[gs_b], writes=[gm_b])
                s.op("dve", lambda e: e.tensor_tensor(v2(gs_ap).rearrange("p (t g) -> p t g", g=4), v2(gs_ap).rearrange("p (t g) -> p t g", g=4),
                                                      gm_ap[:, lo:hi].unsqueeze(2).to_broadcast([128, ntm, 4]), ALU.is_ge), reads=[gs_b, gm_b], writes=[gs_b])
                s.op("dve", lambda e: e.tensor_tensor(v4(t_ap), v4(bi_ap), v2(m2_ap).unsqueeze(2).to_broadcast([128, ntm * 4, 4]), ALU.is_ge), reads=[bi_b, m2_b], writes=[t_b])
                s.op("dve", lambda e: e.tensor_tensor(v4(t_ap), v4(t_ap), v2(gs_ap).unsqueeze(2).to_broadcast([128, ntm * 4, 4]), ALU.mult), reads=[t_b, gs_b], writes=[t_b])
                s.op("dve", lambda e: e.tensor_tensor(t_ap[:, lo * 16:hi * 16], t_ap[:, lo * 16:hi * 16], sc_ap[:, lo * 16:hi * 16], ALU.mult), reads=[t_b, sc_b], writes=[t_b])
                s.op("dve", lambda e: e.tensor_reduce(gm_ap[:, lo:hi], v3(t_ap), AX.X, ALU.add), reads=[t_b], writes=[gm_b])
                s.op("dve", lambda e: e.reciprocal(gm_ap[:, lo:hi], gm_ap[:, lo:hi]), reads=[gm_b], writes=[gm_b])
                s.op("dve", lambda e: e.tensor_tensor(v3(g_ap), v3(t_ap), gm_ap[:, lo:hi].unsqueeze(2).to_broadcast([128, ntm, 16]), ALU.mult), reads=[t_b, gm_b], writes=[g_b])
                if b == 0 and l == 0:
                    dbg("gates", g_ap, [g_b])
                for t4 in range(lo, hi, 4):
                    nt4 = min(4, hi - t4)
                    pgt, pgtb = ps_a()

                    def f(e, pgt=pgt, t4=t4, nt4=nt4):
                        ins = None
                        for j in range(nt4):
                            ins = e.transpose(pgt[0:16, j * 128:(j + 1) * 128], g_ap[:, (t4 + j) * 16:(t4 + j + 1) * 16], ident_ap)
                        return ins
                    s.op("pe", f, reads=[g_b, ident_b], writes=[pgtb])
                    s.op("act", lambda e, pgt=pgt, t4=t4, nt4=nt4: e.copy(gT[0:16, t4 * 128:(t4 + nt4) * 128], pgt[0:16, 0:nt4 * 128]), reads=[pgtb], writes=[gT_b])
                chk("L%d_moe_r" % l)
                m_exp = A.mark()
                rr["bn"] = 4
                wu_slots = []
                for i in range(2):
                    g_, (gb_,) = A.alloc(KC * 512 // 2, 1, "wg%d" % i)
                    u_, (ub_,) = A.alloc(KC * 512 // 2, 1, "wu%d" % i)
                    d_, (db_,) = A.alloc(4 * D // 2, 1, "wd%d" % i)
                    wu_slots.append((bf(g_).rearrange("p (c n) -> p c n", c=KC), gb_, s.dma_sem(),
                                     bf(u_).rearrange("p (c n) -> p c n", c=KC), ub_, s.dma_sem(),
                                     bf(d_).rearrange("p (c n) -> p c n", c=4), db_, s.dma_sem()))
                hid_slots = []
                for i in range(2):
                    hid_w, (hid_b,) = A.alloc(4 * 512 // 2, 1, "hid%d" % i)
                    hid_slots.append((bf(hid_w).rearrange("p (c n) -> p c n", c=4), hid_b))
                mcnt = {"sli": 0, "hi": 0}
                sl_slots = []
                for i in range(2):
                    a_, (ab_,) = A.alloc(512, 1, "silu%d" % i)
                    sl_slots.append((a_, ab_))
                sli = 0
                ui = 0
                units = [(ex_, fh_) for ex_ in range(NEXP) for fh_ in range(2)]

                def issue_load(u):
                    ex_, fh_ = units[u]
                    wg_, wgb_, wgs_, wu_, wub_, wus_, wd_, wdb_, wds_ = wu_slots[u % 2]
                    load_w(wg_, wgb_, wgs_, w_gate[l, ex_].rearrange("(c p) n -> p c n", p=128)[:, :, fh_ * 512:(fh_ + 1) * 512])
                    load_w(wu_, wub_, wus_, w_up[l, ex_].rearrange("(c p) n -> p c n", p=128)[:, :, fh_ * 512:(fh_ + 1) * 512])
                    load_w(wd_, wdb_, wds_, w_down[l, ex_, fh_ * 512:(fh_ + 1) * 512, :].rearrange("(c p) n -> p c n", p=128))
                issue_load(0)
                for ex in range(NEXP):
                    for (gi_, t0, n, isc) in tg_moe:
                        pgb_, pgbb_ = ps_a()
                        s.op("pe", lambda e, pgb_=pgb_, ex=ex, t0=t0, n=n: e.matmul(pgb_[:, 0:n], onehot[0:16, ex * 128:(ex + 1) * 128], gT[0:16, t0:t0 + n], start=True, stop=True),
                             reads=[onehot_b, gT_b], writes=[pgbb_])
                        s.op("act", lambda e, pgb_=pgb_, t0=t0, n=n: e.copy(gbs[:, t0:t0 + n], pgb_[:, 0:n]), reads=[pgbb_], writes=[gb_bufs[gi_]])
                    for fh in range(2):
                        wg_, wgb_, wgs_, wu_, wub_, wus_, wd_, wdb_, wds_ = wu_slots[ui % 2]
                        if ui + 1 < len(units):
                            issue_load(ui + 1)
                        ui += 1
                        def emit_gu(gi_, t0, n, hid, hid_b, wg_=wg_, wgb_=wgb_, wu_=wu_, wub_=wub_):
                            for fc in range(4):
                                pg, pgb = ps_a()
                                pu, pub = ps_a()

                                def f(e, pg=pg, fc=fc):
                                    ins = None
                                    for c in range(KC):
                                        ins = e.matmul(pg[:, 0:n], wg_[:, c, fc * 128:(fc + 1) * 128], hT[:, c, t0:t0 + n], start=(c == 0), stop=(c == KC - 1))
                                    return ins
                                s.op("pe", f, reads=[wgb_, h_bufs[gi_]], writes=[pgb])

                                def f(e, pu=pu, fc=fc):
                                    ins = None
                                    for c in range(KC):
                                        ins = e.matmul(pu[:, 0:n], wu_[:, c, fc * 128:(fc + 1) * 128], hT[:, c, t0:t0 + n], start=(c == 0), stop=(c == KC - 1))
                                    return ins
                                s.op("pe", f, reads=[wub_, h_bufs[gi_]], writes=[pub])
                                sl_, slb_ = sl_slots[mcnt["sli"] % 2]
                                mcnt["sli"] += 1
                                s.op("act", lambda e: e.activation(out=sl_[:, 0:n], in_=pg[:, 0:n], func=AF.Silu), reads=[pgb], writes=[slb_])
                                s.op("dve", lambda e: e.tensor_tensor(sl_[:, 0:n], pu[:, 0:n], sl_[:, 0:n], ALU.mult), reads=[pub, slb_], writes=[slb_])
                                s.op("pool", lambda e: e.tensor_tensor(hid[:, fc, 0:n], sl_[:, 0:n], gbs[:, t0:t0 + n], ALU.mult),
                                     reads=[slb_, gb_bufs[gi_]], writes=[hid_b])

                        def emit_down(gi_, t0, n, isc, hid, hid_b, wd_=wd_, wdb_=wdb_):
                            col = NB if isc else b
                            for oc in range(KC):
                                pd, pdb = ps_b()

                                def f(e, pd=pd, oc=oc):
                                    ins = None
                                    for c in range(4):
                                        ins = e.matmul(pd[:, 0:n], wd_[:, c, oc * 128:(oc + 1) * 128], hid[:, c, 0:n], start=(c == 0), stop=(c == 3))
                                    return ins
                                s.op("pe", f, reads=[wdb_, hid_b], writes=[pdb])
                                s.op("dve", lambda e: e.scalar_tensor_tensor(
                                    xres[:, oc, t0:t0 + n], pd[:, 0:n], modv[:, l, 40 + oc, col:col + 1], xres[:, oc, t0:t0 + n], ALU.mult, ALU.add),
                                    reads=[pdb, xr_bufs[gi_], modv_b], writes=[xr_bufs[gi_]])

                        nseq = len(tg_moe)
                        hs = [hid_slots[(mcnt["hi"] + i) % 2] for i in range(nseq)]
                        mcnt["hi"] += nseq
                        g0 = tg_moe[0]
                        emit_gu(g0[0], g0[1], g0[2], hs[0][0], hs[0][1])
                        for i in range(nseq):
                            if i + 1 < nseq:
                                g1 = tg_moe[i + 1]
                                emit_gu(g1[0], g1[1], g1[2], hs[i + 1][0], hs[i + 1][1])
                            gc = tg_moe[i]
                            emit_down(gc[0], gc[1], gc[2], gc[3], hs[i][0], hs[i][1])
                rr["bn"] = 2
                chk("L%d_moe_e" % l)
                if not last:
                    xwsem = [s.dma_sem() for _ in TG]
                    for (gi_, t0, n, isc) in tg_moe:
                        s.dma("sp", xwsem[gi_], xT[b, :, :, t0:t0 + n], xres[:, :, t0:t0 + n], reads=[xr_bufs[gi_]], writes=[xT_b[b][ti] for ti in tiles_of(t0, n)])
                    if b == 0 and l == 0:
                        dscr("x2", xT[0], xT_b[0])
                else:
                    A.reset(m_exp)
                    fin_slots = []
                    for i in range(2):
                        y_, (yb_,) = A.alloc(KC * 512, 1, "yfin%d" % i)
                        fin_slots.append((y_.rearrange("p (c n) -> p c n", c=KC), yb_))
                    rsf_ap, (rsf_b,) = A.alloc(512, 1, "rsf")
                    ot_slots = []
                    for i in range(2):
                        o_, (ob_,) = A.alloc(D, 1, "ot%d" % i)
                        ot_slots.append((o_, ob_, s.dma_sem()))
                    oti = 0
                    for fi, (gi_, t0, n, isc) in enumerate(tg_moe):
                        y_, yb_ = fin_slots[fi % 2]
                        yf = y_[:, :, 0:n]
                        s.op("act", lambda e, yf=yf, t0=t0, n=n: e.activation(out=yf, in_=xres[:, :, t0:t0 + n], func=AF.Square), reads=[xr_bufs[gi_]], writes=[yb_])
                        pss, pssb = ps_a()

                        def f(e, pss=pss, yf=yf, n=n):
                            ins = None
                            for c in range(KC):
                                ins = e.matmul(pss[:, 0:n], ones_ap, yf[:, c, :], start=(c == 0), stop=(c == KC - 1))
                            return ins
                        s.op("pe", f, reads=[yb_, ones_b], writes=[pssb])
                        act_rstd(rsf_ap[:, 0:n], pss[:, 0:n], 1.0 / D, [pssb], [rsf_b])
                        chk("fin_a")
                        for c in range(KC):
                            s.op("dve", lambda e, yf=yf, c=c, t0=t0, n=n: e.scalar_tensor_tensor(yf[:, c, :], xres[:, c, t0:t0 + n], gfv[:, c:c + 1], rsf_ap[:, 0:n], ALU.mult, ALU.mult),
                                 reads=[xr_bufs[gi_], rsf_b, small_b, yb_], writes=[yb_])
                        chk("fin_b")
                        for tj in range(n // 128):
                            o_, ob_, osem_ = ot_slots[oti % 2]
                            oti += 1
                            for half in range(2):
                                pt, ptb = ps_a()

                                def f(e, pt=pt, yf=yf, tj=tj, half=half):
                                    ins = None
                                    for j in range(4):
                                        c = half * 4 + j
                                        ins = e.transpose(pt[:, j * 128:(j + 1) * 128], yf[:, c, tj * 128:(tj + 1) * 128], ident_ap)
                                    return ins
                                s.op("pe", f, reads=[yb_, ident_b], writes=[ptb])
                                if half == 0:
                                    s.op("act", lambda e, o_=o_, pt=pt: e.copy(o_[:, 0:512], pt[:, :]), reads=[ptb], writes=[ob_])
                                else:
                                    s.op("dve", lambda e, o_=o_, pt=pt: e.tensor_copy(o_[:, 512:1024], pt[:, :]), reads=[ptb], writes=[ob_])
                            chk("fin_c")
                            tl = t0 - C + tj * 128
                            out_toks.append(s.dma("sp", osem_, out[b, tl:tl + 128, :], o_, reads=[ob_]))
                chk("L%d_moe" % l)
                A.reset(mk)
                A.reset(mL)

        s.dead = False
        s.wait_only("sp", out_toks + dbg_sem)
        s.final_barrier("sp")
        s.emit()
        print("program: ins=%d waits=%d sems=%d arena_peak=%d words" % (s.n_ins, s.n_wait, len(s.semh), A.peak))
    return nc


_CACHE = {}


def _get_program(NB, S, C, L, debug=()):
    stop = os.environ.get("K_STOP") or None
    key = (NB, S, C, L, tuple(debug), stop)
    if key not in _CACHE:
        _CACHE[key] = build_program(NB, S, C, L, debug, stop)
    return _CACHE[key]


def run(inputs, n_cores=8, debug=()):
    x = np.asarray(inputs["x"], np.float32)
    ctx = np.asarray(inputs["ctx"], np.float32)
    c = np.asarray(inputs["c"], np.float32)
    c_ctx = np.asarray(inputs["c_ctx"], np.float32)
    B, S, _ = x.shape
    C = ctx.shape[1]
    L = inputs["w_mod"].shape[0]
    NB = B // n_cores
    nc = _get_program(NB, S, C, L, debug)
    consts = host_constants(C, S)
    shared = {}
    for k in ("w_mod", "b_mod", "g_norm1", "g_norm2", "w_in", "lambda_q1", "lambda_k1", "lambda_q2", "lambda_k2", "g_subln",
              "lb_logits", "g_rec_norm", "w_br_attn", "w_br_rec", "w_out", "w_router", "w_gate", "w_up", "w_down"):
        shared[k] = np.ascontiguousarray(np.asarray(inputs[k], np.float32))
    shared["b_router"] = np.ascontiguousarray(np.asarray(inputs["b_router"], np.float32).reshape(1, NEXP))
    shared["g_final"] = np.ascontiguousarray(np.asarray(inputs["g_final"], np.float32).reshape(1, D))
    shared.update(consts)
    in_maps = []
    for i in range(n_cores):
        m = dict(shared)
        m["x"] = np.ascontiguousarray(x[i * NB:(i + 1) * NB])
        m["ctx"] = np.ascontiguousarray(ctx[i * NB:(i + 1) * NB])
        cv4 = np.zeros((4, D), np.float32)
        cv4[0:NB] = c[i * NB:(i + 1) * NB]
        cv4[NB] = c_ctx
        m["cvec"] = cv4
        in_maps.append(m)
    res = run_bass_kernel_spmd(nc, in_maps, core_ids=list(range(n_cores)))
    outs = np.concatenate([np.asarray(r["out"]) for r in res.results], axis=0)
    return outs, res


def kernel(**inputs):
    out, _ = run(inputs, n_cores=8)
    return out.astype(np.float32)
```

```python
import contextlib
import math
import os
import sys
import numpy as np
import ml_dtypes
import concourse.bass as bass
import concourse.mybir as mybir
from concourse.bass_utils import run_bass_kernel_spmd

F32 = mybir.dt.float32
BF16 = mybir.dt.bfloat16
I32 = mybir.dt.int32
AF = mybir.ActivationFunctionType
ALU = mybir.AluOpType
AX = mybir.AxisListType

DEBUG_ANNOTATE = bool(os.environ.get('K_ANNOTATE'))
DMA_SEM_POOL = int(os.environ.get('K_DSEMS', '1000'))
POOL_TO_DVE = os.environ.get('K_POOLDVE', '0') == '1'
D = 1024
KC = 8
N_IN = 10240
EPS = 1e-6
NEXP = 16
CHUNK = 32
NPT = 128 // CHUNK
GRID_W = 64
OFF_AQ, OFF_AK, OFF_AV, OFF_RQ, OFF_RFF, OFF_RFB, OFF_RI, OFF_RG, OFF_GA, OFF_GR = [1024 * i for i in range(10)]


class Buf:
    __slots__ = ("name", "w", "r", "psum")

    def __init__(self, name="", psum=False):
        self.name = name
        self.w = None
        self.r = {}
        self.psum = psum


class DmaSem:
    __slots__ = ("idx", "h", "total")

    def __init__(self, idx, h):
        self.idx = idx
        self.h = h
        self.total = 0


class _Rec:
    def __init__(self):
        self.calls = []

    def __getattr__(self, name):
        def m(*a, **k):
            self.calls.append((name, a, k))
            return self
        return m


class Sched:
    ENG = ("pe", "act", "dve", "pool", "sp")

    def __init__(self, nc, stack):
        self.nc = nc
        self.stack = stack
        self.semh = []
        self.semidx = {}
        self.cnt = {}
        self.ops = {}
        self.known = {}
        for e in self.ENG:
            self.semidx[e] = self._new_sem("s_" + e)
            self.cnt[e] = 0
            self.ops[e] = []
            self.known[e] = {}
        self.n_wait = 0
        self.n_ins = 0
        self.dead = False

    def _new_sem(self, name):
        h = self.stack.enter_context(self.nc.semaphore(name))
        self.semh.append(h)
        return len(self.semh) - 1

    def begin_iter(self):
        if not hasattr(self, "_ipool"):
            self._ipool = []
        self._iidx = 0

    def dma_sem(self, name=None):
        if not hasattr(self, "_dpool"):
            self._dpool = []
            self._dnext = 0
        if getattr(self, "_iidx", None) is not None:
            if self._iidx < len(self._ipool):
                ds = self._ipool[self._iidx]
            else:
                idx = self._new_sem("d%d" % len(self.semh))
                ds = DmaSem(idx, self.semh[idx])
                self._ipool.append(ds)
                self._dpool.append(ds)
            self._iidx += 1
            return ds
        if len(self._dpool) < DMA_SEM_POOL:
            idx = self._new_sem("d%d" % len(self.semh))
            ds = DmaSem(idx, self.semh[idx])
            self._dpool.append(ds)
            return ds
        ds = self._dpool[self._dnext % len(self._dpool)]
        self._dnext += 1
        return ds

    @property
    def _dsems(self):
        return getattr(self, "_dpool", [])

    def _collect(self, eng, reads, writes, extra=()):
        deps = {}
        own_ = self.semidx[eng]
        for b in reads:
            t = b.w
            if t is not None and deps.get(t[0], 0) < t[1]:
                deps[t[0]] = t[1]
            if b.psum:
                for si, v in b.r.items():
                    if si != own_ and deps.get(si, 0) < v:
                        deps[si] = v
        for b in writes:
            t = b.w
            if t is not None and deps.get(t[0], 0) < t[1]:
                deps[t[0]] = t[1]
            for si, v in b.r.items():
                if deps.get(si, 0) < v:
                    deps[si] = v
        for t in extra:
            if t is not None and deps.get(t[0], 0) < t[1]:
                deps[t[0]] = t[1]
        own = self.semidx[eng]
        kn = self.known[eng]
        waits = []
        for si, v in deps.items():
            if eng == "pe" and si == own:
                continue
            if kn.get(si, 0) >= v:
                continue
            kn[si] = v
            waits.append((si, v))
        self.n_wait += len(waits)
        return waits

    def _mark(self, tok, reads, writes):
        si, v = tok
        for b in reads:
            if b.r.get(si, 0) < v:
                b.r[si] = v
        for b in writes:
            b.w = tok
            b.r = {}

    def op(self, eng, fn, reads=(), writes=(), extra=()):
        if self.dead:
            return (0, 0)
        if eng == "pool" and POOL_TO_DVE:
            eng = "dve"
        rec = _Rec()
        fn(rec)
        calls = rec.calls
        lineno = sys._getframe(1).f_lineno

        def fn(e, calls=calls, lineno=lineno):
            ins = None
            for name, a, k in calls:
                ins = getattr(e, name)(*a, **k)
                if DEBUG_ANNOTATE:
                    ins.annotate("L%d" % lineno)
            return ins
        waits = self._collect(eng, reads, writes, extra)
        self.cnt[eng] += 1
        tok = (self.semidx[eng], self.cnt[eng])
        self.ops[eng].append((waits, fn, tok[0]))
        self._mark(tok, reads, writes)
        self.n_ins += 1
        return tok

    def dma(self, queue, dsem, out, in_, reads=(), writes=(), extra=()):
        if self.dead:
            return (0, 0)
        if dsem.total > 0:
            extra = tuple(extra) + ((dsem.idx, dsem.total),)
        waits = self._collect(queue, reads, writes, extra)
        dsem.total += 16
        tok = (dsem.idx, dsem.total)
        h = dsem.h

        def fn(e, out=out, in_=in_, h=h):
            e.dma_start(out=out, in_=in_).then_inc(h, 16)
            return None

        self.ops[queue].append((waits, fn, None))
        self._mark(tok, reads, writes)
        self.n_ins += 1
        return tok

    def wait_only(self, eng, toks):
        waits = self._collect(eng, (), (), toks)
        self.ops[eng].append((waits, None, None))

    def final_barrier(self, eng="sp"):
        toks = [(self.semidx[e], self.cnt[e]) for e in self.ENG if self.cnt[e] > 0 and e != eng]
        for ds in self._dsems:
            if ds.total > 0:
                toks.append((ds.idx, ds.total))
        self.wait_only(eng, toks)

    def emit(self):
        nc = self.nc
        semh = self.semh
        if os.environ.get("K_SEMCLR", "1") == "1":
            for h in semh:
                nc.sync.sem_clear(h)
            nc.all_engine_barrier()
        with nc.Block() as block:
            def mk(name):
                ops = self.ops[name]

                def body(e):
                    for waits, fn, si in ops:
                        for (wi, v) in waits:
                            e.wait_ge(semh[wi], v)
                        if fn is None:
                            continue
                        ins = fn(e)
                        if si is not None:
                            ins.then_inc(semh[si], 1)
                return body

            block.tensor(mk("pe"))
            block.scalar(mk("act"))
            block.vector(mk("dve"))
            block.gpsimd(mk("pool"))
            block.sync(mk("sp"))


class Arena:
    def __init__(self, ap, words):
        self.ap = ap
        self.words = words
        self.top = 0
        self.hist = []
        self.peak = 0

    def mark(self):
        return self.top

    def reset(self, m):
        self.top = m

    def alloc(self, words, nbuf=1, name=""):
        words = (words + 1) // 2 * 2
        st, en = self.top, self.top + words
        assert en <= self.words, "SBUF arena overflow %s %d" % (name, en)
        self.top = en
        self.peak = max(self.peak, en)
        bufs = [Buf(name) for _ in range(nbuf)]
        inherit = {}
        keep = []
        for (a, b, bl) in self.hist:
            if a < en and st < b:
                for ob in bl:
                    if ob.w is not None and inherit.get(ob.w[0], 0) < ob.w[1]:
                        inherit[ob.w[0]] = ob.w[1]
                    for si, v in ob.r.items():
                        if inherit.get(si, 0) < v:
                            inherit[si] = v
                if not (st <= a and b <= en):
                    keep.append((a, b, bl))
            else:
                keep.append((a, b, bl))
        self.hist = keep
        for nb in bufs:
            nb.r = dict(inherit)
        self.hist.append((st, en, bufs))
        return self.ap[:, st:en], bufs


def host_constants(C, S):
    T = C + S
    ident = np.eye(128, dtype=np.float32)
    inv = (10000.0 ** (-np.arange(16, dtype=np.float32) / 16.0)).astype(np.float32)
    t = np.arange(S)
    row = (t // GRID_W).astype(np.float32)
    col = (t % GRID_W).astype(np.float32)
    cos = np.ones((128, T), np.float32)
    sin = np.zeros((128, T), np.float32)
    swap = np.zeros((128, 128), np.float32)
    for p in range(128):
        j = p % 64
        base = row if j < 32 else col
        ang = (base * inv[j % 16]).astype(np.float32)
        first = (j % 32) < 16
        cos[p, C:] = np.cos(ang)
        sin[p, C:] = (-np.sin(ang)) if first else np.sin(ang)
        sp = p + 16 if first else p - 16
        swap[sp, p] = 1.0
    s_i = np.arange(128)[:, None]
    t_i = np.arange(128)[None, :]
    same = (s_i // CHUNK) == (t_i // CHUNK)
    maskf = (same & (s_i <= t_i)).astype(np.float32)
    maskb = (same & (s_i >= t_i)).astype(np.float32)
    scanm = np.ones((128, T), np.float32)
    scanm[:, ::CHUNK] = 0.0
    cmask = np.zeros((128, NPT), np.float32)
    for p in range(128):
        cmask[p, p // CHUNK] = 1.0
    onehot = np.zeros((16, 16, 128), np.float32)
    for e in range(16):
        onehot[e, e, :] = 1.0
    return dict(c_ident=ident, c_cos=cos, c_sin=sin, c_swap=swap, c_maskf=maskf, c_maskb=maskb,
                c_scanm=scanm, c_onehot=onehot.reshape(16, 16 * 128), c_cmask=cmask)


def build_program(NB, S, C, L, debug=(), stop=None):
    T = C + S
    NT = T // 128
    NCT = C // 128
    NCH = T // CHUNK
    TG = [(0, C, True)]
    gsz = min(512, S)
    for g in range(S // gsz):
        TG.append((C + g * gsz, gsz, False))
    MGN = 256 if S >= 256 else S
    TGM = []
    for st_ in range(0, C, min(MGN, C)):
        TGM.append((st_, min(MGN, C), True))
    for st_ in range(C, T, MGN):
        TGM.append((st_, MGN, False))

    nc = bass.Bass("TRN2", target_bir_lowering=False)

    def din(name, shape, dt=F32):
        return nc.dram_tensor(name, list(shape), dt, kind="ExternalInput").ap()

    x_in = din("x", [NB, S, D])
    ctx_in = din("ctx", [NB, C, D])
    cvec = din("cvec", [4, D])
    w_mod = din("w_mod", [L, D, 6 * D])
    b_mod = din("b_mod", [L, 6 * D])
    g_norm1 = din("g_norm1", [L, D])
    g_norm2 = din("g_norm2", [L, D])
    w_in = din("w_in", [L, D, N_IN])
    lam_q1 = din("lambda_q1", [L, 64])
    lam_k1 = din("lambda_k1", [L, 64])
    lam_q2 = din("lambda_q2", [L, 64])
    lam_k2 = din("lambda_k2", [L, 64])
    g_subln = din("g_subln", [L, 128])
    lb_logits = din("lb_logits", [L, D])
    g_rec = din("g_rec_norm", [L, 128])
    w_bra = din("w_br_attn", [L, D, D])
    w_brr = din("w_br_rec", [L, D, D])
    w_out = din("w_out", [L, D, D])
    w_router = din("w_router", [D, NEXP])
    b_router = din("b_router", [1, NEXP])
    w_gate = din("w_gate", [L, NEXP, D, D])
    w_up = din("w_up", [L, NEXP, D, D])
    w_down = din("w_down", [L, NEXP, D, D])
    g_final = din("g_final", [1, D])
    c_ident = din("c_ident", [128, 128])
    c_cos = din("c_cos", [128, T])
    c_sin = din("c_sin", [128, T])
    c_swap = din("c_swap", [128, 128])
    c_maskf = din("c_maskf", [128, 128])
    c_maskb = din("c_maskb", [128, 128])
    c_scanm = din("c_scanm", [128, T])
    c_onehot = din("c_onehot", [16, 16 * 128])
    c_cmask = din("c_cmask", [128, NPT])

    out = nc.dram_tensor("out", [NB, S, D], F32, kind="ExternalOutput").ap()
    xT = nc.dram_tensor("xT_scr", [NB, 128, KC, T], F32, kind="Internal").ap()
    aT = nc.dram_tensor("aT_scr", [128, KC, T], BF16, kind="Internal").ap()
    rT = nc.dram_tensor("rT_scr", [128, KC, T], BF16, kind="Internal").ap()
    dbg_out = {}

    st = contextlib.ExitStack()
    with st:
        s = Sched(nc, st)
        AW = int(os.environ.get("K_AW", 52224))
        arena_t = st.enter_context(nc.sbuf_tensor("arena", [128, AW], F32))
        A = Arena(arena_t, AW)
        psum = [st.enter_context(nc.psum_tensor("ps%d" % i, [128, 512], F32)) for i in range(8)]
        psb = [Buf("ps%d" % i, psum=True) for i in range(8)]
        rr = {"a": 0, "b": 0, "bn": 2}

        def ps_a():
            i = rr["a"] % 4
            rr["a"] += 1
            return psum[i], psb[i]

        def ps_b():
            i = 4 + rr["b"] % rr["bn"]
            rr["b"] += 1
            return psum[i], psb[i]

        def bf(ap):
            return ap.bitcast(BF16)

        dbg_sem = []
        dbg_ds = [None]

        def chk(name):
            if stop == name:
                s.dead = True

        def _dbg_ds():
            if dbg_ds[0] is None:
                dbg_ds[0] = s.dma_sem()
            return dbg_ds[0]

        def dbg(name, ap, bufs):
            if name not in debug:
                return
            shp = list(ap.shape)
            o = nc.dram_tensor("dbg_" + name, shp, ap.dtype, kind="ExternalOutput").ap()
            dbg_out[name] = o
            ds = _dbg_ds()
            tok = s.dma("sp", ds, o, ap, reads=bufs)
            dbg_sem.append(tok)

        def dscr(name, dram_ap, dram_bufs):
            if name not in debug:
                return
            o = nc.dram_tensor("dbg_" + name, list(dram_ap.shape), dram_ap.dtype, kind="ExternalOutput").ap()
            ds = _dbg_ds()
            tok = s.dma("sp", ds, o, dram_ap, reads=dram_bufs)
            dbg_sem.append(tok)

        ident_ap, (ident_b,) = A.alloc(128, 1, "ident")
        identb_w, (identb_b,) = A.alloc(64, 1, "identb")
        identb = bf(identb_w)
        ones_ap, (ones_b,) = A.alloc(128, 1, "ones")
        onesb_w, (onesb_b,) = A.alloc(64, 1, "onesb")
        onesb = bf(onesb_w)
        swap_w, (swap_b,) = A.alloc(64, 1, "swap")
        swapm = bf(swap_w)
        maskf_w, (maskf_b,) = A.alloc(64, 1, "maskf")
        maskb_w, (maskb_b,) = A.alloc(64, 1, "maskb")
        maskf = bf(maskf_w)
        maskb = bf(maskb_w)
        NCOL = NB + 1
        modv_ap, (modv_b,) = A.alloc(L * 48 * NCOL, 1, "modv")
        modv = modv_ap.rearrange("p (l c n) -> p l c n", l=L, c=48)
        a1_ap, (a1_b,) = A.alloc(L * KC * NCOL, 1, "a1")
        a1 = a1_ap.rearrange("p (l c n) -> p l c n", l=L, c=KC)
        a2_ap, (a2_b,) = A.alloc(L * KC * NCOL, 1, "a2")
        a2 = a2_ap.rearrange("p (l c n) -> p l c n", l=L, c=KC)
        small_ap, (small_b,) = A.alloc(256, 1, "small")
        lbv_ap, (lbv_b,) = A.alloc(L * 8 * 3, 1, "lbv")
        lbv = lbv_ap.rearrange("p (l h k) -> p l h k", l=L, h=8)
        lamv_ap, (lamv_b,) = A.alloc(L * 4, 1, "lamv")
        lamv = lamv_ap.rearrange("p (l k) -> p l k", l=L)
        brt_ap, (brt_b,) = A.alloc(16, 1, "brt")
        wr_w, (wr_b,) = A.alloc(KC * 16 // 2, 1, "wr")
        wrt = bf(wr_w).rearrange("p (c e) -> p c e", c=KC)
        eps_ap, (eps_b,) = A.alloc(2, 1, "eps")
        cmask_ap, (cmask_b,) = A.alloc(NPT, 1, "cmask")

        cs = [s.dma_sem() for _ in range(4)]
        s.dma("sp", cs[0], ident_ap, c_ident, writes=[ident_b])
        s.dma("sp", cs[0], cmask_ap, c_cmask, writes=[cmask_b])
        hm_ap, (hm_b,) = A.alloc(2, 1, "hmask")
        s.op("dve", lambda e: e.tensor_tensor(hm_ap[:, 0:1], cmask_ap[:, 0:1], cmask_ap[:, 1:2], ALU.add), reads=[cmask_b], writes=[hm_b])
        s.op("dve", lambda e: e.tensor_tensor(hm_ap[:, 1:2], cmask_ap[:, 2:3], cmask_ap[:, 3:4], ALU.add), reads=[cmask_b], writes=[hm_b])
        cs2 = [s.dma_sem() for _ in range(5)]
        s.dma("pool", cs2[0], swapm, c_swap, writes=[swap_b])
        s.dma("pool", cs2[1], maskf, c_maskf, writes=[maskf_b])
        s.dma("pool", cs2[2], maskb, c_maskb, writes=[maskb_b])
        s.dma("pool", cs2[4], wrt, w_router.rearrange("(c p) e -> p c e", p=128), writes=[wr_b])
        s.op("pool", lambda e: e.memset(ones_ap, 1.0), writes=[ones_b])
        s.op("pool", lambda e: e.memset(lamv_ap, 0.0), writes=[lamv_b])
        s.op("pool", lambda e: e.memset(onesb, 1.0), writes=[onesb_b])
        s.op("pool", lambda e: e.memset(eps_ap, EPS), writes=[eps_b])
        s.op("act", lambda e: e.copy(identb, ident_ap), reads=[ident_b], writes=[identb_b])

        chk("c0")
        m0 = A.mark()
        stA_ap, (stA_b,) = A.alloc(128, 1, "stA")
        stB_ap, (stB_b,) = A.alloc(128, 1, "stB")
        cv_ap, (cv_b,) = A.alloc(D, 1, "cv")
        scT_ap, (scT_b,) = A.alloc(KC * NCOL, 1, "scT")
        scT = scT_ap.rearrange("p (c n) -> p c n", c=KC)
        lamst_ap, (lamst_b,) = A.alloc(4 * L * 64 + 2 * L * 128, 1, "lamst")
        p0s = [s.dma_sem() for _ in range(4)]
        s.dma("sp", p0s[0], stA_ap[0:L * 48, :], b_mod.rearrange("l (c p) -> (l c) p", p=128), writes=[stA_b])
        s.dma("sp", p0s[1], stB_ap[0:L * 8, :], g_norm1.rearrange("l (c p) -> (l c) p", p=128), writes=[stB_b])
        s.dma("sp", p0s[1], stB_ap[16:16 + L * 8, :], g_norm2.rearrange("l (c p) -> (l c) p", p=128), writes=[stB_b])
        s.dma("sp", p0s[1], stB_ap[32:40, :], g_final.rearrange("o (c p) -> (o c) p", p=128), writes=[stB_b])
        s.dma("sp", p0s[1], stB_ap[40:40 + L * 8, :], lb_logits.rearrange("l (c p) -> (l c) p", p=128), writes=[stB_b])
        s.dma("sp", p0s[2], cv_ap[0:4, :], cvec, writes=[cv_b])
        lam_srcs = [lam_q1, lam_k1, lam_q2, lam_k2]
        for i, src in enumerate(lam_srcs):
            s.dma("sp", p0s[3], lamst_ap[:, i * L * 64:(i + 1) * L * 64],
                  src.rearrange("l d -> (l d)").partition_broadcast(128), writes=[lamst_b])
        s.dma("sp", p0s[3], brt_ap, b_router.rearrange("o e -> (o e)").partition_broadcast(128), writes=[brt_b])
        pst, pstb = ps_a()
        s.op("pe", lambda e: e.transpose(pst[:, 0:96], stA_ap[0:96, :], ident_ap[0:96, 0:96]),
             reads=[stA_b, ident_b], writes=[pstb])
        s.op("dve", lambda e: e.tensor_copy(small_ap[:, 0:96], pst[:, 0:96]), reads=[pstb], writes=[small_b])
        pst2, pstb2 = ps_a()
        s.op("pe", lambda e: e.transpose(pst2[:, 0:56], stB_ap[0:56, :], ident_ap[0:56, 0:56]),
             reads=[stB_b, ident_b], writes=[pstb2])
        s.op("dve", lambda e: e.tensor_copy(small_ap[:, 96:152], pst2[:, 0:56]), reads=[pstb2], writes=[small_b])
        chk("p0a")
        g1v = small_ap[:, 96:112].rearrange("p (l c) -> p l c", l=2)
        g2v = small_ap[:, 112:128].rearrange("p (l c) -> p l c", l=2)
        gfv = small_ap[:, 128:136]
        lbl = small_ap[:, 136:152].rearrange("p (l c) -> p l c", l=2)
        bmv = small_ap[:, 0:96].rearrange("p (l c) -> p l c", l=2)
        for l in range(L):
            s.dma("sp", p0s[3], lamv[:, l, 1:2], g_subln[l:l + 1, :].rearrange("o p -> p o"), writes=[lamv_b])
            s.dma("sp", p0s[3], lamv[:, l, 2:3], g_rec[l:l + 1, :].rearrange("o p -> p o"), writes=[lamv_b])
        lt_ap, (lt_b,) = A.alloc(L * 64 * 2 + 8 * L, 1, "lamtmp")
        for l in range(L):
            lam_init = 0.8 - 0.6 * math.exp(-0.3 * l)
            for j in range(2):
                qv = lamst_ap[:, (2 * j) * L * 64 + l * 64:(2 * j) * L * 64 + (l + 1) * 64]
                kv = lamst_ap[:, (2 * j + 1) * L * 64 + l * 64:(2 * j + 1) * L * 64 + (l + 1) * 64]
                pr = lt_ap[:, (l * 2 + j) * 64:(l * 2 + j + 1) * 64]
                sm = lt_ap[:, L * 128 + l * 4 + j:L * 128 + l * 4 + j + 1]
                s.op("dve", lambda e, pr=pr, qv=qv, kv=kv: e.tensor_tensor(pr, qv, kv, ALU.mult), reads=[lamst_b], writes=[lt_b])
                s.op("dve", lambda e, pr=pr, sm=sm: e.tensor_reduce(sm, pr, AX.X, ALU.add), reads=[lt_b], writes=[lt_b])
                s.op("act", lambda e, sm=sm: e.activation(out=sm, in_=sm, func=AF.Exp), reads=[lt_b], writes=[lt_b])
            e1 = lt_ap[:, L * 128 + l * 4:L * 128 + l * 4 + 1]
            e2 = lt_ap[:, L * 128 + l * 4 + 1:L * 128 + l * 4 + 2]
            s.op("dve", lambda e, l=l, e1=e1, e2=e2, li=lam_init: e.scalar_tensor_tensor(
                lamv[:, l, 0:1], e2, -li, e1, ALU.add, ALU.subtract), reads=[lt_b], writes=[lamv_b])
            s.op("dve", lambda e, l=l, li=lam_init: e.tensor_scalar(lamv[:, l, 1:2], lamv[:, l, 1:2], 1.0 - li, None, ALU.mult),
                 reads=[lamv_b], writes=[lamv_b])
        chk("p0b")
        lbe_ap, (lbe_b,) = A.alloc(L * 8 + 16, 1, "lbe")
        lbe = lbe_ap[:, 0:L * 8].rearrange("p (l c) -> p l c", l=L)
        lsum = lbe_ap[:, L * 8:L * 8 + 8]
        s.op("act", lambda e: e.activation(out=lbe_ap[:, 0:L * 8], in_=small_ap[:, 136:136 + L * 8], func=AF.Exp), reads=[small_b], writes=[lbe_b])
        s.op("dve", lambda e: e.tensor_copy(lsum, lbe[:, 0, :]), reads=[lbe_b], writes=[lbe_b])
        for l in range(1, L):
            s.op("dve", lambda e, l=l: e.tensor_tensor(lsum, lsum, lbe[:, l, :], ALU.add), reads=[lbe_b], writes=[lbe_b])
        s.op("dve", lambda e: e.reciprocal(lsum, lsum), reads=[lbe_b], writes=[lbe_b])
        s.op("pool", lambda e: e.memset(lbv[:, 0, :, 0], 0.0), writes=[lbv_b])
        for l in range(1, L):
            s.op("dve", lambda e, l=l: e.tensor_tensor(lbe[:, l, :], lbe[:, l, :], lsum, ALU.mult), reads=[lbe_b], writes=[lbe_b])
            s.op("dve", lambda e, l=l: e.tensor_tensor(lbv[:, l, :, 0], lbv[:, l - 1, :, 0], lbe[:, l, :], ALU.add),
                 reads=[lbe_b, lbv_b], writes=[lbv_b])
        for l in range(L):
            s.op("dve", lambda e, l=l: e.tensor_scalar(lbv[:, l, :, 1], lbv[:, l, :, 0], -1.0, 1.0, ALU.mult, ALU.add),
                 reads=[lbv_b], writes=[lbv_b])
            s.op("dve", lambda e, l=l: e.tensor_scalar(lbv[:, l, :, 2], lbv[:, l, :, 0], 1.0, -1.0, ALU.mult, ALU.add),
                 reads=[lbv_b], writes=[lbv_b])
        chk("p0c")
        s.op("act", lambda e: e.activation(out=cv_ap[0:NCOL, :], in_=cv_ap[0:NCOL, :], func=AF.Silu), reads=[cv_b], writes=[cv_b])
        pst3, pstb3 = ps_a()

        def f_ct(e):
            ins = None
            for c in range(KC):
                ins = e.transpose(pst3[:, c * NCOL:(c + 1) * NCOL], cv_ap[0:NCOL, c * 128:(c + 1) * 128], ident_ap[0:NCOL, 0:NCOL])
            return ins
        s.op("pe", f_ct, reads=[cv_b, ident_b], writes=[pstb3])
        s.op("dve", lambda e: e.tensor_copy(scT_ap, pst3[:, 0:KC * NCOL]), reads=[pstb3], writes=[scT_b])
        chk("p0d")
        wm_slots = []
        for i in range(2):
            ap_, (b_,) = A.alloc(KC * 512, 1, "wmod%d" % i)
            wm_slots.append((ap_.rearrange("p (c n) -> p c n", c=KC), b_, s.dma_sem()))
        gi = 0
        for l in range(L):
            pm, pmb = ps_a()
            for g in range(12):
                wt, wb, wsem = wm_slots[gi % 2]
                gi += 1
                s.dma("sp", wsem, wt, w_mod[l].rearrange("(c p) n -> p c n", p=128)[:, :, g * 512:(g + 1) * 512], writes=[wb])

                def f_mod(e, wt=wt, g=g, pm=pm):
                    ins = None
                    for j in range(4):
                        oc = g * 4 + j
                        for c in range(KC):
                            ins = e.matmul(pm[:, oc * NCOL:(oc + 1) * NCOL], wt[:, c, j * 128:(j + 1) * 128], scT[:, c, :],
                                           start=(c == 0), stop=(c == KC - 1))
                    return ins
                s.op("pe", f_mod, reads=[wb, scT_b], writes=[pmb])
            if l == 0:
                chk("p0e")
            s.op("dve", lambda e, l=l, pm=pm: e.tensor_tensor(
                modv[:, l], pm[:, 0:48 * NCOL].rearrange("p (c n) -> p c n", c=48),
                bmv[:, l, :].unsqueeze(2).to_broadcast([128, 48, NCOL]), ALU.add),
                reads=[pmb, small_b], writes=[modv_b])
            if l == 0:
                chk("p0f")
            for (dst, gv, off) in ((a1, g1v, 8), (a2, g2v, 32)):
                for col_ in range(NCOL):
                    s.op("dve", lambda e, l=l, dst=dst, gv=gv, off=off, col_=col_: e.scalar_tensor_tensor(
                        dst[:, l, :, col_], modv[:, l, off:off + 8, col_], 1.0, gv[:, l, :], ALU.add, ALU.mult),
                        reads=[modv_b, small_b], writes=[a1_b if dst is a1 else a2_b])
        dbg("modv", modv_ap, [modv_b])
        dbg("a1", a1_ap, [a1_b])
        dbg("lamv", lamv_ap, [lamv_b])
        dbg("lbv", lbv_ap, [lbv_b])
        A.reset(m0)
        chk("p0")

        xT_b = [[Buf("xT%d_%d" % (b, g)) for g in range(T // 128)] for b in range(NB)]
        aT_b = [Buf("aT%d" % h) for h in range(8)]
        rT_b = [Buf("rT%d" % h) for h in range(8)]

        def tiles_of(t0, n):
            return range(t0 // 128, (t0 + n) // 128)

        def act_rstd(dst, src_ps, inv_n, rd, wr):
            s.op("act", lambda e: e.activation(out=dst, in_=src_ps, func=AF.Ln, bias=eps_ap[:, 0:1], scale=inv_n), reads=rd + [eps_b], writes=wr)
            s.op("act", lambda e: e.activation(out=dst, in_=dst, func=AF.Exp, scale=-0.5), reads=wr, writes=wr)

        def norm_mod(xg, xg_b, n, hdst, hdst_b, av, sv, av_b):
            mk = A.mark()
            sq_ap, (sq_b,) = A.alloc(KC * n, 1, "sq")
            sq = sq_ap.rearrange("p (c n) -> p c n", c=KC)
            rs_ap, (rs_b,) = A.alloc(n, 1, "rstd")
            s.op("act", lambda e: e.activation(out=sq, in_=xg, func=AF.Square), reads=[xg_b], writes=[sq_b])
            pss, pssb = ps_a()

            def f(e):
                ins = None
                for c in range(KC):
                    ins = e.matmul(pss[:, 0:n], ones_ap, sq[:, c, :], start=(c == 0), stop=(c == KC - 1))
                return ins
            s.op("pe", f, reads=[sq_b, ones_b], writes=[pssb])
            act_rstd(rs_ap, pss[:, 0:n], 1.0 / D, [pssb], [rs_b])
            for c in range(KC):
                tmp = sq[:, c, :]
                s.op("dve", lambda e, c=c, tmp=tmp: e.scalar_tensor_tensor(tmp, xg[:, c, :], av[:, c:c + 1], rs_ap, ALU.mult, ALU.mult),
                     reads=[xg_b, rs_b, av_b], writes=[sq_b])
                s.op("act", lambda e, c=c, tmp=tmp: e.activation(out=hdst[:, c, :], in_=tmp, func=AF.Identity, bias=sv[:, c:c + 1], scale=1.0),
                     reads=[sq_b, av_b], writes=[hdst_b])
            A.reset(mk)

        def load_w(dst, dst_b, sem, src):
            return s.dma("pool", sem, dst, src, writes=[dst_b])

        m1 = A.mark()
        xin_slots = []
        for i in range(2):
            ap_, (b_,) = A.alloc(D, 1, "xin%d" % i)
            xo_, (ob_,) = A.alloc(D, 1, "xo%d" % i)
            xin_slots.append((ap_, b_, s.dma_sem(), xo_, ob_, s.dma_sem()))
        k = 0
        for b in range(NB):
            for ti in range(NT):
                ap_, b_, sm_, xo_, ob_, osm_ = xin_slots[k % 2]
                k += 1
                src = ctx_in[b, ti * 128:(ti + 1) * 128, :] if ti < NCT else x_in[b, (ti - NCT) * 128:(ti - NCT + 1) * 128, :]
                s.dma("sp", sm_, ap_, src, writes=[b_])
                for half in range(2):
                    pt, ptb = ps_a()

                    def f(e, ap_=ap_, pt=pt, half=half):
                        ins = None
                        for j in range(4):
                            c = half * 4 + j
                            ins = e.transpose(pt[:, j * 128:(j + 1) * 128], ap_[:, c * 128:(c + 1) * 128], ident_ap)
                        return ins
                    s.op("pe", f, reads=[b_, ident_b], writes=[ptb])
                    eng = "act" if half == 0 else "dve"
                    if eng == "act":
                        s.op("act", lambda e, xo_=xo_, pt=pt, half=half: e.copy(xo_[:, half * 512:(half + 1) * 512], pt[:, :]), reads=[ptb], writes=[ob_])
                    else:
                        s.op("dve", lambda e, xo_=xo_, pt=pt, half=half: e.tensor_copy(xo_[:, half * 512:(half + 1) * 512], pt[:, :]), reads=[ptb], writes=[ob_])
                s.dma("sp", osm_, xT[b, :, :, ti * 128:(ti + 1) * 128], xo_.rearrange("p (c t) -> p c t", c=KC), reads=[ob_], writes=[xT_b[b][ti]])
        A.reset(m1)
        chk("x0")

        out_toks = []
        for b in range(NB):
            for l in range(L):
                last = (l == L - 1)
                s.begin_iter()
                mL = A.mark()
                h_w, h_bufs = A.alloc(KC * T // 2, len(TG), "h")
                hT = bf(h_w).rearrange("p (c t) -> p c t", c=KC)
                hb_of = {}
                for gi_, (t0, n, isc) in enumerate(TG):
                    for ti in tiles_of(t0, n):
                        hb_of[ti] = h_bufs[gi_]

                def hbufs(t0, n):
                    return list({id(hb_of[ti]): hb_of[ti] for ti in tiles_of(t0, n)}.values())

                mk = A.mark()
                xg_slots = []
                for i in range(2):
                    ap_, (b_,) = A.alloc(KC * 512, 1, "xg%d" % i)
                    xg_slots.append((ap_, b_, s.dma_sem()))
                for gi_, (t0, n, isc) in enumerate(TG):
                    ap_, b_, sm_ = xg_slots[gi_ % 2]
                    xg = ap_[:, 0:KC * n].rearrange("p (c n) -> p c n", c=KC)
                    s.dma("sp", sm_, xg, xT[b, :, :, t0:t0 + n], reads=[xT_b[b][ti] for ti in tiles_of(t0, n)], writes=[b_])
                    col = NB if isc else b
                    norm_mod(xg, b_, n, hT[:, :, t0:t0 + n], h_bufs[gi_], a1[:, l, :, col], modv[:, l, 0:8, col], a1_b)
                A.reset(mk)
                if b == 0 and l == 0:
                    dbg("h", h_w, h_bufs)
                chk("n1")

                mk = A.mark()
                cos_ap, (cos_b,) = A.alloc(T, 1, "cos")
                sin_ap, (sin_b,) = A.alloc(T, 1, "sin")
                s.dma("sp", cs[1], cos_ap, c_cos, writes=[cos_b])
                s.dma("sp", cs[2], sin_ap, c_sin, writes=[sin_b])
                att_w = []
                for i in range(2):
                    ent = []
                    for nm_ in ("wq", "wk", "wv"):
                        w_, (wb_,) = A.alloc(KC * 128 // 2, 1, nm_ + str(i))
                        ent.append((bf(w_).rearrange("p (c n) -> p c n", c=KC), wb_, s.dma_sem()))
                    att_w.append(ent)
                wsrc_att = w_in[l].rearrange("(c p) n -> p c n", p=128)

                def att_load(hd_):
                    for (w_, wb_, ws_), off_ in zip(att_w[hd_ % 2], (OFF_AQ, OFF_AK, OFF_AV)):
                        load_w(w_, wb_, ws_, wsrc_att[:, :, off_ + hd_ * 128:off_ + (hd_ + 1) * 128])
                att_load(0)
                qr_w, (qr_b,) = A.alloc(T // 2, 1, "qr")
                qr1_w, (qr1_b,) = A.alloc(T // 2, 1, "qr1")
                kr_w, (kr_b,) = A.alloc(T // 2, 1, "kr")
                qr = bf(qr_w)
                qr1 = bf(qr1_w)
                kr = bf(kr_w)
                v_w, (v_b,) = A.alloc(NT * 128 // 2, 1, "v")
                vv = bf(v_w).rearrange("p (t d) -> p t d", t=NT)
                qb_slots = []
                for i in range(2):
                    ap_, (b_,) = A.alloc(256, 1, "qb%d" % i)
                    t1_, (t1b_,) = A.alloc(512, 1, "t1_%d" % i)
                    t2_, (t2b_,) = A.alloc(512, 1, "t2_%d" % i)
                    qb_slots.append((bf(ap_), b_, t1_, t1b_, t2_, t2b_))
                pT_slots = []
                for i in range(6):
                    ap_, (b_,) = A.alloc(256, 1, "pT%d" % i)
                    pT_slots.append((bf(ap_), b_))
                acc_slots = []
                for i in range(2):
                    ap_, (b_,) = A.alloc(512, 1, "acc%d" % i)
                    acc_slots.append((ap_, b_))
                rc_slots = []
                for i in range(2):
                    ap_, (b_,) = A.alloc(512, 1, "rc%d" % i)
                    rc_slots.append((ap_, b_))
                o_ap, (o_b,) = A.alloc(512, 1, "o")
                osq_ap, (osq_b,) = A.alloc(512, 1, "osq")
                ors_ap, (ors_b,) = A.alloc(512, 1, "ors")
                ao_slots = []
                for i in range(2):
                    ap_, (b_,) = A.alloc(256, 1, "ao%d" % i)
                    ao_slots.append((bf(ap_), b_, s.dma_sem()))
                qk_i = 0
                pt_i = 0
                ao_i = 0
                for hd in range(8):
                    if hd + 1 < 8:
                        att_load(hd + 1)
                    (wq, wq_b, _), (wk, wk_b, _), (wv, wv_b, _) = att_w[hd % 2]
                    chk("a1")
                    for gi_, (t0, n, isc) in enumerate(TG):
                        for which in range(2):
                            if which == 0 and isc and last:
                                continue
                            wmat, wmb = (wq, wq_b) if which == 0 else (wk, wk_b)
                            dst, dst_b = (qr, qr_b) if which == 0 else (kr, kr_b)
                            qb_, qbb_, t1_, t1b_, t2_, t2b_ = qb_slots[qk_i % 2]
                            qk_i += 1
                            pq, pqb = ps_a()

                            def f(e, pq=pq, wmat=wmat, t0=t0, n=n):
                                ins = None
                                for c in range(KC):
                                    ins = e.matmul(pq[:, 0:n], wmat[:, c, :], hT[:, c, t0:t0 + n], start=(c == 0), stop=(c == KC - 1))
                                return ins
                            s.op("pe", f, reads=[wmb, h_bufs[gi_]], writes=[pqb])
                            s.op("act", lambda e, qb_=qb_, pq=pq, n=n: e.copy(qb_[:, 0:n], pq[:, 0:n]), reads=[pqb], writes=[qbb_])
                            chk("a2")
                            chk("i%d_a2" % qk_i)
                            psw, pswb = ps_a()
                            s.op("pe", lambda e, psw=psw, qb_=qb_, n=n: e.matmul(psw[:, 0:n], swapm, qb_[:, 0:n], start=True, stop=True),
                                 reads=[qbb_, swap_b], writes=[pswb])
                            chk("a3")
                            chk("i%d_a3" % qk_i)
                            s.op("dve", lambda e, t1_=t1_, pq=pq, t0=t0, n=n: e.tensor_tensor(t1_[:, 0:n], pq[:, 0:n], cos_ap[:, t0:t0 + n], ALU.mult),
                                 reads=[pqb, cos_b], writes=[t1b_])
                            s.op("dve", lambda e, t2_=t2_, psw=psw, t0=t0, n=n: e.tensor_tensor(t2_[:, 0:n], psw[:, 0:n], sin_ap[:, t0:t0 + n], ALU.mult),
                                 reads=[pswb, sin_b], writes=[t2b_])
                            chk("a4")
                            chk("i%d_a4" % qk_i)
                            if which == 1:
                                s.op("pool", lambda e, dst=dst, t1_=t1_, t2_=t2_, t0=t0, n=n: e.tensor_tensor(dst[:, t0:t0 + n], t1_[:, 0:n], t2_[:, 0:n], ALU.add),
                                     reads=[t1b_, t2b_], writes=[dst_b])
                            else:
                                s.op("pool", lambda e, t1_=t1_, t2_=t2_, n=n: e.tensor_tensor(t1_[:, 0:n], t1_[:, 0:n], t2_[:, 0:n], ALU.add),
                                     reads=[t1b_, t2b_], writes=[t1b_])
                                s.op("dve", lambda e, t1_=t1_, t0=t0, n=n: e.tensor_scalar(qr[:, t0:t0 + n], t1_[:, 0:n], hm_ap[:, 0:1], None, ALU.mult),
                                     reads=[t1b_, hm_b], writes=[qr_b])
                                s.op("dve", lambda e, t1_=t1_, t0=t0, n=n: e.tensor_scalar(qr1[:, t0:t0 + n], t1_[:, 0:n], hm_ap[:, 1:2], None, ALU.mult),
                                     reads=[t1b_, hm_b], writes=[qr1_b])
                            chk("a5")
                            chk("qk%d" % qk_i)
                    chk("att_a")
                    for t4 in range(0, NT, 4):
                        pv, pvb = ps_a()
                        nt4 = min(4, NT - t4)

                        def f(e, pv=pv, t4=t4, nt4=nt4):
                            ins = None
                            for j in range(nt4):
                                ti = t4 + j
                                for c in range(KC):
                                    ins = e.matmul(pv[:, j * 128:(j + 1) * 128], hT[:, c, ti * 128:(ti + 1) * 128], wv[:, c, :],
                                                   start=(c == 0), stop=(c == KC - 1))
                            return ins
                        s.op("pe", f, reads=[wv_b] + hbufs(t4 * 128, nt4 * 128), writes=[pvb])
                        s.op("act", lambda e, pv=pv, t4=t4, nt4=nt4: e.copy(
                            vv[:, t4:t4 + nt4, :], pv[:, 0:nt4 * 128].rearrange("p (t d) -> p t d", t=nt4)), reads=[pvb], writes=[v_b])
                    if b == 0 and l == 0 and hd == 0:
                        dbg("qr", qr_w, [qr_b])
                        dbg("kr", kr_w, [kr_b])
                        dbg("v", v_w, [v_b])
                    chk("att_b")
                    for gi_, (t0, n, isc) in enumerate(TG):
                        if isc and last:
                            continue
                        kts = range(0, NCT) if isc else range(0, NT)
                        nk = len(kts)
                        po = [psum[6], psum[7]]
                        pob = [psb[6], psb[7]]
                        for ki, kt in enumerate(kts):
                            for m in range(2):
                                pS, pSb = ps_a()
                                qm, qmb = (qr, qr_b) if m == 0 else (qr1, qr1_b)
                                s.op("pe", lambda e, pS=pS, kt=kt, qm=qm, t0=t0, n=n: e.matmul(
                                    pS[:, 0:n], kr[:, kt * 128:(kt + 1) * 128], qm[:, t0:t0 + n],
                                    start=True, stop=True), reads=[kr_b, qmb], writes=[pSb])
                                pT_, pTb_ = pT_slots[pt_i % 6]
                                pt_i += 1
                                s.op("act", lambda e, pT_=pT_, pS=pS, n=n: e.activation(out=pT_[:, 0:n], in_=pS[:, 0:n], func=AF.Exp, scale=0.125),
                                     reads=[pSb], writes=[pTb_])
                                acc_, accb_ = acc_slots[m]
                                if ki == 0:
                                    s.op("dve", lambda e, acc_=acc_, pT_=pT_, n=n: e.tensor_copy(acc_[:, 0:n], pT_[:, 0:n]), reads=[pTb_], writes=[accb_])
                                else:
                                    s.op("dve", lambda e, acc_=acc_, pT_=pT_, n=n: e.tensor_tensor(acc_[:, 0:n], acc_[:, 0:n], pT_[:, 0:n], ALU.add),
                                         reads=[pTb_, accb_], writes=[accb_])
                                s.op("pe", lambda e, m=m, kt=kt, pT_=pT_, n=n, ki=ki, nk=nk: e.matmul(
                                    po[m][:, 0:n], vv[:, kt, :], pT_[:, 0:n], start=(ki == 0), stop=(ki == nk - 1)),
                                    reads=[v_b, pTb_], writes=[pob[m]])
                        chk("att_c")
                        for m in range(2):
                            acc_, accb_ = acc_slots[m]
                            rc_, rcb_ = rc_slots[m]
                            pS, pSb = ps_a()
                            s.op("pe", lambda e, pS=pS, acc_=acc_, n=n: e.matmul(pS[:, 0:n], ones_ap, acc_[:, 0:n], start=True, stop=True),
                                 reads=[accb_, ones_b], writes=[pSb])
                            s.op("act", lambda e, rc_=rc_, pS=pS, n=n: e.activation(out=rc_[:, 0:n], in_=pS[:, 0:n], func=AF.Ln), reads=[pSb], writes=[rcb_])
                            s.op("act", lambda e, rc_=rc_, n=n: e.activation(out=rc_[:, 0:n], in_=rc_[:, 0:n], func=AF.Exp, scale=-1.0), reads=[rcb_], writes=[rcb_])
                            s.op("dve", lambda e, rc_=rc_, m=m, n=n: e.tensor_tensor(rc_[:, 0:n], po[m][:, 0:n], rc_[:, 0:n], ALU.mult),
                                 reads=[pob[m], rcb_], writes=[rcb_])
                        s.op("dve", lambda e, n=n: e.scalar_tensor_tensor(o_ap[:, 0:n], rc_slots[1][0][:, 0:n], lamv[:, l, 0:1], rc_slots[0][0][:, 0:n], ALU.mult, ALU.add),
                             reads=[rc_slots[0][1], rc_slots[1][1], lamv_b], writes=[o_b])
                        s.op("act", lambda e, n=n: e.activation(out=osq_ap[:, 0:n], in_=o_ap[:, 0:n], func=AF.Square), reads=[o_b], writes=[osq_b])
                        pS, pSb = ps_a()
                        s.op("pe", lambda e, pS=pS, n=n: e.matmul(pS[:, 0:n], ones_ap, osq_ap[:, 0:n], start=True, stop=True), reads=[osq_b, ones_b], writes=[pSb])
                        act_rstd(ors_ap[:, 0:n], pS[:, 0:n], 1.0 / 128, [pSb], [ors_b])
                        ao_, aob_, aosem_ = ao_slots[ao_i % 2]
                        ao_i += 1
                        s.op("dve", lambda e, ao_=ao_, n=n: e.scalar_tensor_tensor(ao_[:, 0:n], o_ap[:, 0:n], lamv[:, l, 1:2], ors_ap[:, 0:n], ALU.mult, ALU.mult),
                             reads=[o_b, ors_b, lamv_b], writes=[aob_])
                        chk("att_d")
                        s.dma("sp", aosem_, aT[:, hd, t0:t0 + n], ao_[:, 0:n], reads=[aob_], writes=[aT_b[hd]])
                        chk("att_e")
                A.reset(mk)
                if b == 0 and l == 0:
                    dscr("aT", aT, aT_b)
                chk("att")
                chk("L%d_att" % l)

                mk = A.mark()
                wnames = ["rq", "rff", "rfb", "ri", "rg"]
                woffs = [OFF_RQ, OFF_RFF, OFF_RFB, OFF_RI, OFF_RG]
                rec_w = []
                for i in range(2):
                    d_ = {}
                    for nm in wnames:
                        w_, (wb_,) = A.alloc(KC * 128 // 2, 1, "w" + nm + str(i))
                        d_[nm] = (bf(w_).rearrange("p (c n) -> p c n", c=KC), wb_, s.dma_sem())
                    rec_w.append(d_)
                wsrc_rec = w_in[l].rearrange("(c p) n -> p c n", p=128)

                def rec_load(hd_):
                    for nm, off in zip(wnames, woffs):
                        ent = rec_w[hd_ % 2][nm]
                        load_w(ent[0], ent[1], ent[2], wsrc_rec[:, :, off + hd_ * 128:off + (hd_ + 1) * 128])
                rec_load(0)
                scanm_w, (scanm_b,) = A.alloc(T // 2, 1, "scanm")
                scanm_ap = bf(scanm_w)
                s.dma("pool", cs2[3], scanm_ap, c_scanm, writes=[scanm_b])
                W1, (W1b,) = A.alloc(T, 1, "W1")
                W2, (W2b,) = A.alloc(T, 1, "W2")
                qf_w, (qf_b,) = A.alloc(T // 2, 1, "qf")
                kk_w, (kk_b,) = A.alloc(T // 2, 1, "kk")
                eg_w, (eg_b,) = A.alloc(T // 2, 1, "eg")
                en_w, (en_b,) = A.alloc(T // 2, 1, "en")
                sg_w, (sg_b,) = A.alloc(T // 2, 1, "sg")
                qf, kk, eg, en, sg = bf(qf_w), bf(kk_w), bf(eg_w), bf(en_w), bf(sg_w)
                vr_w, (vr_b,) = A.alloc(NT * 128 // 2, 1, "vr")
                vr = bf(vr_w).rearrange("p (t d) -> p t d", t=NT)
                vblk_w, (vblk_b,) = A.alloc(NT * NPT * 128 // 2, 1, "vblk")
                vblk = bf(vblk_w).rearrange("p (t c d) -> p t c d", t=NT, c=NPT)
                dirs = {}
                for dname in ("f", "b"):
                    QT_w, (QT_b,) = A.alloc(T // 2, 1, "QT" + dname)
                    KT_w, (KT_b,) = A.alloc(T // 2, 1, "KT" + dname)
                    KH_w, (KH_b,) = A.alloc(T // 2, 1, "KH" + dname)
                    egl_ap, (egl_b,) = A.alloc(NCH, 1, "egl" + dname)
                    st_w, st_bufs = A.alloc((NCH + 1) * 128 // 2, NCH + 1, "st" + dname)
                    S32w, S32bufs = A.alloc(256, 2, "S32" + dname)
                    S32 = [S32w[:, 0:128], S32w[:, 128:256]]
                    S32b = S32bufs
                    dirs[dname] = dict(QT=bf(QT_w), QT_b=QT_b, KT=bf(KT_w), KT_b=KT_b, KH=bf(KH_w), KH_b=KH_b, egl=egl_ap, egl_b=egl_b,
                                       st=bf(st_w).rearrange("p (c d) -> p c d", c=NCH + 1), st_bufs=st_bufs, S32=S32, S32b=S32b)
                khT_slots = []
                for i in range(4):
                    ap_, (b_,) = A.alloc(64, 1, "khT%d" % i)
                    khT_slots.append((bf(ap_), b_))
                am_slots = []
                for i in range(4):
                    ap_, (b_,) = A.alloc(64, 1, "am%d" % i)
                    am_slots.append((bf(ap_), b_))
                osq2_ap, (osq2_b,) = A.alloc(512, 1, "osq2")
                ors2_ap, (ors2_b,) = A.alloc(512, 1, "ors2")
                ro_slots = []
                for i in range(2):
                    ap_, (b_,) = A.alloc(256, 1, "ro%d" % i)
                    ro_slots.append((bf(ap_), b_, s.dma_sem()))
                kh_i = 0
                am_i = 0
                ro_i = 0
                ctx_ch = list(range(0, C // CHUNK))
                lat_ch = list(range(C // CHUNK, NCH))
                order = {"f": ctx_ch + lat_ch, "b": ctx_ch[::-1] + lat_ch[::-1]}
                for hd in range(8):
                    if hd + 1 < 8:
                        rec_load(hd + 1)
                    wr_ = rec_w[hd % 2]
                    lb_s = lbv[:, l, hd, 0:1]
                    oml_s = lbv[:, l, hd, 1:2]
                    noml_s = lbv[:, l, hd, 2:3]
                    for gi_, (t0, n, isc) in enumerate(TG):
                        pq, pqb = ps_a()

                        def f(e, pq=pq, t0=t0, n=n):
                            ins = None
                            for c in range(KC):
                                ins = e.matmul(pq[:, 0:n], wr_["rq"][0][:, c, :], hT[:, c, t0:t0 + n], start=(c == 0), stop=(c == KC - 1))
                            return ins
                        s.op("pe", f, reads=[wr_["rq"][1], h_bufs[gi_]], writes=[pqb])
                        s.op("act", lambda e, pq=pq, t0=t0, n=n: e.copy(qf[:, t0:t0 + n], pq[:, 0:n]), reads=[pqb], writes=[qf_b])
                        pg, pgb = ps_a()

                        def f(e, pg=pg, t0=t0, n=n):
                            ins = None
                            for c in range(KC):
                                ins = e.matmul(pg[:, 0:n], wr_["rg"][0][:, c, :], hT[:, c, t0:t0 + n], start=(c == 0), stop=(c == KC - 1))
                            return ins
                        s.op("pe", f, reads=[wr_["rg"][1], h_bufs[gi_]], writes=[pgb])
                        s.op("act", lambda e, pg=pg, t0=t0, n=n: e.activation(out=sg[:, t0:t0 + n], in_=pg[:, 0:n], func=AF.Silu), reads=[pgb], writes=[sg_b])
                    for t4 in range(0, NT, 4):
                        pv, pvb = ps_a()
                        nt4 = min(4, NT - t4)

                        def f(e, pv=pv, t4=t4, nt4=nt4):
                            ins = None
                            for j in range(nt4):
                                ti = t4 + j
                                for c in range(KC):
                                    ins = e.matmul(pv[:, j * 128:(j + 1) * 128], hT[:, c, ti * 128:(ti + 1) * 128], wr_["ri"][0][:, c, :],
                                                   start=(c == 0), stop=(c == KC - 1))
                            return ins
                        s.op("pe", f, reads=[wr_["ri"][1]] + hbufs(t4 * 128, nt4 * 128), writes=[pvb])
                        s.op("act", lambda e, pv=pv, t4=t4, nt4=nt4: e.copy(
                            vr[:, t4:t4 + nt4, :], pv[:, 0:nt4 * 128].rearrange("p (t d) -> p t d", t=nt4)), reads=[pvb], writes=[vr_b])
                        for cc in range(NPT):
                            s.op("dve", lambda e, pv=pv, t4=t4, nt4=nt4, cc=cc: e.tensor_scalar(
                                vblk[:, t4:t4 + nt4, cc, :], pv[:, 0:nt4 * 128].rearrange("p (t d) -> p t d", t=nt4), cmask_ap[:, cc:cc + 1], None, ALU.mult),
                                reads=[pvb, cmask_b], writes=[vblk_b])
                    for dname in ("f", "b"):
                        dd = dirs[dname]
                        wz = wr_["rff"] if dname == "f" else wr_["rfb"]
                        for gi_, (t0, n, isc) in enumerate(TG):
                            pz, pzb = ps_a()

                            def f(e, pz=pz, t0=t0, n=n, wz=wz):
                                ins = None
                                for c in range(KC):
                                    ins = e.matmul(pz[:, 0:n], wz[0][:, c, :], hT[:, c, t0:t0 + n], start=(c == 0), stop=(c == KC - 1))
                                return ins
                            s.op("pe", f, reads=[wz[1], h_bufs[gi_]], writes=[pzb])
                            s.op("act", lambda e, pz=pz, t0=t0, n=n: e.activation(out=W1[:, t0:t0 + n], in_=pz[:, 0:n], func=AF.Sigmoid), reads=[pzb], writes=[W1b])
                        s.op("dve", lambda e: e.tensor_scalar(W2, W1, oml_s, lb_s, ALU.mult, ALU.add), reads=[W1b, lbv_b], writes=[W2b])
                        s.op("dve", lambda e: e.tensor_scalar(kk, W1, noml_s, oml_s, ALU.mult, ALU.add), reads=[W1b, lbv_b], writes=[kk_b])
                        s.op("act", lambda e: e.activation(out=W1, in_=W2, func=AF.Ln), reads=[W2b], writes=[W1b])
                        s.op("dve", lambda e: e.tensor_tensor_scan(W2, scanm_ap, W1, 0.0, ALU.mult, ALU.add), reads=[W1b, scanm_b], writes=[W2b])
                        W13 = W1.rearrange("p (c t) -> p c t", t=CHUNK)
                        W23 = W2.rearrange("p (c t) -> p c t", t=CHUNK)
                        if dname == "f":
                            G, Gb, G3 = W2, W2b, W23
                            last_col = CHUNK - 1
                            X, Xb = W1, W1b
                        else:
                            s.op("dve", lambda e: e.tensor_tensor(W1, W1, W2, ALU.subtract), reads=[W1b, W2b], writes=[W1b])
                            s.op("dve", lambda e: e.tensor_tensor(W13, W13, W23[:, :, CHUNK - 1:CHUNK].to_broadcast([128, NCH, CHUNK]), ALU.add),
                                 reads=[W1b, W2b], writes=[W1b])
                            G, Gb, G3 = W1, W1b, W13
                            last_col = 0
                            X, Xb = W2, W2b
                        s.op("act", lambda e, G=G: e.activation(out=eg, in_=G, func=AF.Exp), reads=[Gb], writes=[eg_b])
                        s.op("act", lambda e, G=G: e.activation(out=en, in_=G, func=AF.Exp, scale=-1.0), reads=[Gb], writes=[en_b])
                        s.op("act", lambda e, G3=G3, last_col=last_col, dd=dd: e.activation(out=dd["egl"], in_=G3[:, :, last_col], func=AF.Exp), reads=[Gb], writes=[dd["egl_b"]])
                        s.op("dve", lambda e, dd=dd: e.tensor_tensor(dd["QT"], qf, eg, ALU.mult), reads=[qf_b, eg_b], writes=[dd["QT_b"]])
                        s.op("pool", lambda e, dd=dd: e.tensor_tensor(dd["KT"], kk, en, ALU.mult), reads=[kk_b, en_b], writes=[dd["KT_b"]])
                        X3 = X.rearrange("p (c t) -> p c t", t=CHUNK)
                        s.op("dve", lambda e, dd=dd, X3=X3: e.tensor_tensor(
                            X3, dd["KT"].rearrange("p (c t) -> p c t", t=CHUNK), dd["egl"].unsqueeze(2).to_broadcast([128, NCH, CHUNK]), ALU.mult),
                            reads=[dd["KT_b"], dd["egl_b"], Xb], writes=[Xb])
                        s.op("pool", lambda e, dd=dd, X=X: e.tensor_copy(dd["KH"], X), reads=[Xb], writes=[dd["KH_b"]])
                        if b == 0 and l == 0 and hd == 0:
                            dbg("QT" + dname, dd["QT"].bitcast(F32), [dd["QT_b"]])
                            dbg("KT" + dname, dd["KT"].bitcast(F32), [dd["KT_b"]])
                            dbg("KH" + dname, dd["KH"].bitcast(F32), [dd["KH_b"]])
                            dbg("egl" + dname, dd["egl"], [dd["egl_b"]])
                    for dname in ("f", "b"):
                        dd = dirs[dname]
                        ordr = order[dname]
                        s.op("pool", lambda e, dd=dd: e.memset(dd["S32"][0], 0.0), writes=[dd["S32b"][0]])
                        kstep = 0
                        c0 = ordr[0]
                        s.op("pool", lambda e, dd=dd, c0=c0: e.memset(dd["st"][:, c0, :], 0.0), writes=[dd["st_bufs"][c0]])
                        tile_order = []
                        for cch in ordr:
                            if cch // NPT not in tile_order:
                                tile_order.append(cch // NPT)
                        pos = 0
                        for ti in tile_order:
                            khT_, khTb_ = khT_slots[kh_i % 4]
                            kh_i += 1
                            ptr, ptrb = ps_b()
                            ptr_bf = ptr[:, 0:64].bitcast(BF16)
                            s.op("pe", lambda e, ptr_bf=ptr_bf, dd=dd, ti=ti: e.transpose(ptr_bf, dd["KH"][:, ti * 128:(ti + 1) * 128], identb),
                                 reads=[dd["KH_b"], identb_b], writes=[ptrb])
                            s.op("act", lambda e, khT_=khT_, ptr_bf=ptr_bf: e.copy(khT_, ptr_bf), reads=[ptrb], writes=[khTb_])
                            pu, pub = ps_b()

                            s.op("pe", lambda e, pu=pu, khT_=khT_, ti=ti: e.matmul(
                                pu[:, 0:NPT * 128], khT_, vblk[:, ti].rearrange("p c d -> p (c d)"), start=True, stop=True),
                                reads=[khTb_, vblk_b], writes=[pub])
                            chunks_here = [cch for cch in ordr if cch // NPT == ti]
                            for cch in chunks_here:
                                j = cch % NPT
                                nxt = ordr[pos + 1] if pos + 1 < len(ordr) else NCH
                                pos += 1
                                sa, sab = dd["S32"][kstep % 2], dd["S32b"][kstep % 2]
                                sn, snb = dd["S32"][(kstep + 1) % 2], dd["S32b"][(kstep + 1) % 2]
                                kstep += 1
                                s.op("dve", lambda e, dd=dd, cch=cch, j=j, pu=pu: e.scalar_tensor_tensor(
                                    sn, sa, dd["egl"][:, cch:cch + 1], pu[:, j * 128:(j + 1) * 128], ALU.mult, ALU.add),
                                    reads=[sab, dd["egl_b"], pub], writes=[snb])
                                s.op("pool", lambda e, dd=dd, nxt=nxt: e.tensor_copy(dd["st"][:, nxt, :], sn), reads=[snb], writes=[dd["st_bufs"][nxt]])
                    for t4 in range(0, NT, 4):
                        nt4 = min(4, NT - t4)
                        po_, pob_ = psum[6 + (t4 // 4) % 2], psb[6 + (t4 // 4) % 2]
                        for jt in range(nt4):
                            ti = t4 + jt
                            ams = []
                            for dname in ("f", "b"):
                                dd = dirs[dname]
                                pa_, pab_ = ps_b()
                                s.op("pe", lambda e, pa_=pa_, dd=dd, ti=ti: e.matmul(
                                    pa_[:, 0:128], dd["KT"][:, ti * 128:(ti + 1) * 128], dd["QT"][:, ti * 128:(ti + 1) * 128], start=True, stop=True),
                                    reads=[dd["KT_b"], dd["QT_b"]], writes=[pab_])
                                am_, amb_ = am_slots[am_i % 4]
                                am_i += 1
                                mk_, mkb_ = (maskf, maskf_b) if dname == "f" else (maskb, maskb_b)
                                s.op("dve", lambda e, am_=am_, pa_=pa_, mk_=mk_: e.tensor_tensor(am_, pa_[:, 0:128], mk_, ALU.mult), reads=[pab_, mkb_], writes=[amb_])
                                ams.append((am_, amb_))

                            def f(e, po_=po_, jt=jt, ti=ti, ams=ams):
                                ins = None
                                first = True
                                for di, dname in enumerate(("f", "b")):
                                    dd = dirs[dname]
                                    ins = e.matmul(po_[:, jt * 128:(jt + 1) * 128], vr[:, ti, :], ams[di][0], start=first, stop=False)
                                    first = False
                                    for j in range(NPT):
                                        cch = ti * NPT + j
                                        ins = e.matmul(po_[:, jt * 128 + j * CHUNK:jt * 128 + (j + 1) * CHUNK], dd["st"][:, cch, :],
                                                       dd["QT"][:, cch * CHUNK:(cch + 1) * CHUNK], start=False, stop=(di == 1 and j == NPT - 1))
                                return ins
                            rds = [vr_b, ams[0][1], ams[1][1]]
                            for dname in ("f", "b"):
                                dd = dirs[dname]
                                rds += [dd["QT_b"]] + [dd["st_bufs"][ti * NPT + j] for j in range(NPT)]
                            s.op("pe", f, reads=rds, writes=[pob_])
                        n = nt4 * 128
                        t0 = t4 * 128
                        s.op("act", lambda e, po_=po_, n=n: e.activation(out=osq2_ap[:, 0:n], in_=po_[:, 0:n], func=AF.Square), reads=[pob_], writes=[osq2_b])
                        pS, pSb = ps_a()
                        s.op("pe", lambda e, pS=pS, n=n: e.matmul(pS[:, 0:n], ones_ap, osq2_ap[:, 0:n], start=True, stop=True), reads=[osq2_b, ones_b], writes=[pSb])
                        act_rstd(ors2_ap[:, 0:n], pS[:, 0:n], 1.0 / 128, [pSb], [ors2_b])
                        s.op("dve", lambda e, po_=po_, n=n: e.scalar_tensor_tensor(osq2_ap[:, 0:n], po_[:, 0:n], lamv[:, l, 2:3], ors2_ap[:, 0:n], ALU.mult, ALU.mult),
                             reads=[pob_, ors2_b, lamv_b], writes=[osq2_b])
                        ro_, rob_, rosem_ = ro_slots[ro_i % 2]
                        ro_i += 1
                        s.op("pool", lambda e, ro_=ro_, n=n, t0=t0: e.tensor_tensor(ro_[:, 0:n], osq2_ap[:, 0:n], sg[:, t0:t0 + n], ALU.mult),
                             reads=[osq2_b, sg_b], writes=[rob_])
                        s.dma("sp", rosem_, rT[:, hd, t0:t0 + n], ro_[:, 0:n], reads=[rob_], writes=[rT_b[hd]])
                A.reset(mk)
                if b == 0 and l == 0:
                    dscr("rT", rT, rT_b)
                chk("rec")
                chk("L%d_rec" % l)

                mk = A.mark()
                wm = {}
                for nm in ("ga", "gr", "ba", "br", "wo"):
                    w_, (wb_,) = A.alloc(KC * D // 2, 1, "wm" + nm)
                    wm[nm] = (bf(w_).rearrange("p (c n) -> p c n", c=KC), wb_, s.dma_sem())
                wsrc = w_in[l].rearrange("(c p) n -> p c n", p=128)
                load_w(wm["ga"][0], wm["ga"][1], wm["ga"][2], wsrc[:, :, OFF_GA:OFF_GA + D])
                load_w(wm["gr"][0], wm["gr"][1], wm["gr"][2], wsrc[:, :, OFF_GR:OFF_GR + D])
                load_w(wm["ba"][0], wm["ba"][1], wm["ba"][2], w_bra[l].rearrange("(c p) n -> p c n", p=128))
                load_w(wm["br"][0], wm["br"][1], wm["br"][2], w_brr[l].rearrange("(c p) n -> p c n", p=128))
                load_w(wm["wo"][0], wm["wo"][1], wm["wo"][2], w_out[l].rearrange("(c p) n -> p c n", p=128))
                ar_slots = []
                for i in range(2):
                    a_, (ab_,) = A.alloc(KC * MGN // 2, 1, "ag%d" % i)
                    r_, (rb_,) = A.alloc(KC * MGN // 2, 1, "rg%d" % i)
                    x_, (xb_,) = A.alloc(KC * MGN, 1, "xm%d" % i)
                    ar_slots.append((bf(a_).rearrange("p (c n) -> p c n", c=KC), ab_, s.dma_sem(),
                                     bf(r_).rearrange("p (c n) -> p c n", c=KC), rb_, s.dma_sem(),
                                     x_.rearrange("p (c n) -> p c n", c=KC), xb_, s.dma_sem(), s.dma_sem()))
                mx_w, (mx_b,) = A.alloc(KC * MGN // 2, 1, "mixed")
                mixed = bf(mx_w).rearrange("p (c n) -> p c n", c=KC)
                sg_slots = []
                for i in range(2):
                    s1_, (s1b_,) = A.alloc(MGN, 1, "sga%d" % i)
                    s2_, (s2b_,) = A.alloc(MGN, 1, "sgr%d" % i)
                    sg_slots.append((s1_, s1b_, s2_, s2b_))
                sgi = 0
                for gi_, (t0, n, isc) in enumerate(TGM):
                    if isc and last:
                        continue
                    col = NB if isc else b
                    a_, ab_, asem_, r_, rb_, rsem_, x_, xb_, xsem_, xosem_ = ar_slots[gi_ % 2]
                    s.dma("sp", asem_, a_[:, :, 0:n], aT[:, :, t0:t0 + n], reads=aT_b, writes=[ab_])
                    s.dma("sp", rsem_, r_[:, :, 0:n], rT[:, :, t0:t0 + n], reads=rT_b, writes=[rb_])
                    s.dma("sp", xsem_, x_[:, :, 0:n], xT[b, :, :, t0:t0 + n], reads=[xT_b[b][ti] for ti in tiles_of(t0, n)], writes=[xb_])
                    hbs = hbufs(t0, n)
                    for oc in range(KC):
                        s1_, s1b_, s2_, s2b_ = sg_slots[sgi % 2]
                        sgi += 1
                        pgs = []
                        for (wnm, src, srcb) in (("ga", hT[:, :, t0:t0 + n], hbs), ("gr", hT[:, :, t0:t0 + n], hbs), ("ba", a_[:, :, 0:n], [ab_]), ("br", r_[:, :, 0:n], [rb_])):
                            pg, pgb = ps_a()

                            def f(e, pg=pg, wnm=wnm, src=src, oc=oc, n=n):
                                ins = None
                                for c in range(KC):
                                    ins = e.matmul(pg[:, 0:n], wm[wnm][0][:, c, oc * 128:(oc + 1) * 128], src[:, c, :], start=(c == 0), stop=(c == KC - 1))
                                return ins
                            s.op("pe", f, reads=[wm[wnm][1]] + list(srcb), writes=[pgb])
                            pgs.append((pg, pgb))
                        s.op("act", lambda e, s1_=s1_, pg=pgs[0][0], n=n: e.activation(out=s1_[:, 0:n], in_=pg[:, 0:n], func=AF.Sigmoid), reads=[pgs[0][1]], writes=[s1b_])
                        s.op("act", lambda e, s2_=s2_, pg=pgs[1][0], n=n: e.activation(out=s2_[:, 0:n], in_=pg[:, 0:n], func=AF.Sigmoid), reads=[pgs[1][1]], writes=[s2b_])
                        s.op("dve", lambda e, s1_=s1_, pg=pgs[2][0], n=n: e.tensor_tensor(s1_[:, 0:n], pg[:, 0:n], s1_[:, 0:n], ALU.mult), reads=[pgs[2][1], s1b_], writes=[s1b_])
                        s.op("dve", lambda e, s2_=s2_, pg=pgs[3][0], n=n: e.tensor_tensor(s2_[:, 0:n], pg[:, 0:n], s2_[:, 0:n], ALU.mult), reads=[pgs[3][1], s2b_], writes=[s2b_])
                        s.op("pool", lambda e, s1_=s1_, s2_=s2_, oc=oc, n=n: e.tensor_tensor(mixed[:, oc, 0:n], s1_[:, 0:n], s2_[:, 0:n], ALU.add),
                             reads=[s1b_, s2b_], writes=[mx_b])
                    for oc in range(KC):
                        py, pyb = ps_a()

                        def f(e, py=py, oc=oc, n=n):
                            ins = None
                            for c in range(KC):
                                ins = e.matmul(py[:, 0:n], wm["wo"][0][:, c, oc * 128:(oc + 1) * 128], mixed[:, c, 0:n], start=(c == 0), stop=(c == KC - 1))
                            return ins
                        s.op("pe", f, reads=[wm["wo"][1], mx_b], writes=[pyb])
                        s.op("dve", lambda e, py=py, oc=oc, n=n, x_=x_, col=col: e.scalar_tensor_tensor(
                            x_[:, oc, 0:n], py[:, 0:n], modv[:, l, 16 + oc, col:col + 1], x_[:, oc, 0:n], ALU.mult, ALU.add),
                            reads=[pyb, xb_, modv_b], writes=[xb_])
                    s.dma("sp", xosem_, xT[b, :, :, t0:t0 + n], x_[:, :, 0:n], reads=[xb_], writes=[xT_b[b][ti] for ti in tiles_of(t0, n)])
                    norm_mod(x_[:, :, 0:n], xb_, n, hT[:, :, t0:t0 + n], hbs[0], a2[:, l, :, col], modv[:, l, 24:32, col], a2_b)
                    for ob in hbs[1:]:
                        ob.w = hbs[0].w
                        ob.r = {}
                A.reset(mk)
                if b == 0 and l == 0:
                    dbg("h2", h_w, h_bufs)
                    dscr("x1", xT[0], xT_b[0])
                chk("merge")
                chk("L%d_merge" % l)

                mk = A.mark()
                onehot_w, (onehot_b,) = A.alloc(16 * 128 // 2, 1, "onehot")
                onehot = bf(onehot_w)
                s.dma("pool", cs2[3], onehot[0:16, :], c_onehot, writes=[onehot_b])
                xr_ap, xr_bufs = A.alloc(KC * T, len(TG), "xres")
                xres = xr_ap.rearrange("p (c t) -> p c t", c=KC)
                xrsem = [s.dma_sem() for _ in TG]
                tg_moe = [(gi_, t0, n, isc) for gi_, (t0, n, isc) in enumerate(TG) if not (isc and last)]
                for (gi_, t0, n, isc) in tg_moe:
                    s.dma("sp", xrsem[gi_], xres[:, :, t0:t0 + n], xT[b, :, :, t0:t0 + n], reads=[xT_b[b][ti] for ti in tiles_of(t0, n)], writes=[xr_bufs[gi_]])
                tiles_moe = [ti for ti in range(NT) if not (last and ti < NCT)]
                ntm = len(tiles_moe)
                sc_ap, (sc_b,) = A.alloc(NT * 16, 1, "scores")
                bi_ap, (bi_b,) = A.alloc(NT * 16, 1, "biased")
                t_ap, (t_b,) = A.alloc(NT * 16, 1, "rt_tmp")
                g_ap, (g_b,) = A.alloc(NT * 16, 1, "gates")
                m1_ap, (m1_b,) = A.alloc(NT * 4, 1, "max1")
                m2_ap, (m2_b,) = A.alloc(NT * 4, 1, "max2")
                gs_ap, (gs_b,) = A.alloc(NT * 4, 1, "gsel")
                gm_ap, (gm_b,) = A.alloc(NT, 1, "gmax")
                gT_w, (gT_b,) = A.alloc(T // 2, 1, "gT")
                gT = bf(gT_w)
                gb_w, gb_bufs = A.alloc(T // 2, len(TG), "gbs")
                gbs = bf(gb_w)
                prt, prtb = ps_a()

                def f(e):
                    ins = None
                    for ti in tiles_moe:
                        for c in range(KC):
                            ins = e.matmul(prt[:, ti * 16:(ti + 1) * 16], hT[:, c, ti * 128:(ti + 1) * 128], wrt[:, c, :], start=(c == 0), stop=(c == KC - 1))
                    return ins
                s.op("pe", f, reads=[wr_b] + h_bufs, writes=[prtb])
                lo, hi = tiles_moe[0], tiles_moe[-1] + 1

                def v3(ap):
                    return ap[:, lo * 16:hi * 16].rearrange("p (t e) -> p t e", e=16)

                def v4(ap):
                    return ap[:, lo * 16:hi * 16].rearrange("p (t e) -> p t e", e=4)

                def v2(ap):
                    return ap[:, lo * 4:hi * 4]
                s.op("act", lambda e: e.activation(out=sc_ap[:, lo * 16:hi * 16], in_=prt[:, lo * 16:hi * 16], func=AF.Sigmoid), reads=[prtb], writes=[sc_b])
                s.op("dve", lambda e: e.tensor_tensor(v3(bi_ap), v3(sc_ap), brt_ap.unsqueeze(1).to_broadcast([128, ntm, 16]), ALU.add), reads=[sc_b, brt_b], writes=[bi_b])
                s.op("dve", lambda e: e.tensor_reduce(v2(m1_ap), v4(bi_ap), AX.X, ALU.max), reads=[bi_b], writes=[m1_b])
                s.op("dve", lambda e: e.tensor_tensor(v4(t_ap), v4(bi_ap), v2(m1_ap).unsqueeze(2).to_broadcast([128, ntm * 4, 4]), ALU.is_ge), reads=[bi_b, m1_b], writes=[t_b])
                s.op("dve", lambda e: e.scalar_tensor_tensor(t_ap[:, lo * 16:hi * 16], t_ap[:, lo * 16:hi * 16], -1e9, bi_ap[:, lo * 16:hi * 16], ALU.mult, ALU.add), reads=[t_b, bi_b], writes=[t_b])
                s.op("dve", lambda e: e.tensor_reduce(v2(m2_ap), v4(t_ap), AX.X, ALU.max), reads=[t_b], writes=[m2_b])
                s.op("dve", lambda e: e.tensor_tensor(v2(gs_ap), v2(m1_ap), v2(m2_ap), ALU.add), reads=[m1_b, m2_b], writes=[gs_b])
                s.op("dve", lambda e: e.tensor_reduce(gm_ap[:, lo:hi], v2(gs_ap).rearrange("p (t g) -> p t g", g=4), AX.X, ALU.max), reads=[gs_b], writes=[gm_b])
                s.op("dve", lambda e: e.tensor_tensor(v2(gs_ap).rearrange("p (t g) -> p t g", g=4), v2(gs_ap).rearrange("p (t g) -> p t g", g=4),
                                                      gm_ap[:, lo:hi].unsqueeze(2).to_broadcast([128, ntm, 4]), ALU.is_ge), reads=[gs_b, gm_b], writes=[gs_b])
                s.op("dve", lambda e: e.tensor_tensor(v4(t_ap), v4(bi_ap), v2(m2_ap).unsqueeze(2).to_broadcast([128, ntm * 4, 4]), ALU.is_ge), reads=[bi_b, m2_b], writes=[t_b])
                s.op("dve", lambda e: e.tensor_tensor(v4(t_ap), v4(t_ap), v2(gs_ap).unsqueeze(2).to_broadcast([128, ntm * 4, 4]), ALU.mult), reads=[t_b, gs_b], writes=[t_b])
                s.op("dve", lambda e: e.tensor_tensor(t_ap[:, lo * 16:hi * 16], t_ap[:, lo * 16:hi * 16], sc_ap[:, lo * 16:hi * 16], ALU.mult), reads=[t_b, sc_b], writes=[t_b])
                s.op("dve", lambda e: e.tensor_reduce(gm_ap[:, lo:hi], v3(t_ap), AX.X, ALU.add), reads=[t_b], writes=[gm_b])
                s.op("dve", lambda e: e.reciprocal(gm_ap[:, lo:hi], gm_ap[:, lo:hi]), reads=[gm_b], writes=[gm_b])
                s.op("dve", lambda e: e.tensor_tensor(v3(g_ap), v3(t_ap), gm_ap[:, lo:hi].unsqueeze(2).to_broadcast([128, ntm, 16]), ALU.mult), reads=[t_b, gm_b], writes=[g_b])
                if b == 0 and l == 0:
                    dbg("gates", g_ap, [g_b])
                for t4 in range(lo, hi, 4):
                    nt4 = min(4, hi - t4)
                    pgt, pgtb = ps_a()

                    def f(e, pgt=pgt, t4=t4, nt4=nt4):
                        ins = None
                        for j in range(nt4):
                            ins = e.transpose(pgt[0:16, j * 128:(j + 1) * 128], g_ap[:, (t4 + j) * 16:(t4 + j + 1) * 16], ident_ap)
                        return ins
                    s.op("pe", f, reads=[g_b, ident_b], writes=[pgtb])
                    s.op("act", lambda e, pgt=pgt, t4=t4, nt4=nt4: e.copy(gT[0:16, t4 * 128:(t4 + nt4) * 128], pgt[0:16, 0:nt4 * 128]), reads=[pgtb], writes=[gT_b])
                chk("L%d_moe_r" % l)
                m_exp = A.mark()
                rr["bn"] = 4
                wu_slots = []
                for i in range(2):
                    g_, (gb_,) = A.alloc(KC * 512 // 2, 1, "wg%d" % i)
                    u_, (ub_,) = A.alloc(KC * 512 // 2, 1, "wu%d" % i)
                    d_, (db_,) = A.alloc(4 * D // 2, 1, "wd%d" % i)
                    wu_slots.append((bf(g_).rearrange("p (c n) -> p c n", c=KC), gb_, s.dma_sem(),
                                     bf(u_).rearrange("p (c n) -> p c n", c=KC), ub_, s.dma_sem(),
                                     bf(d_).rearrange("p (c n) -> p c n", c=4), db_, s.dma_sem()))
                hid_slots = []
                for i in range(2):
                    hid_w, (hid_b,) = A.alloc(4 * 512 // 2, 1, "hid%d" % i)
                    hid_slots.append((bf(hid_w).rearrange("p (c n) -> p c n", c=4), hid_b))
                mcnt = {"sli": 0, "hi": 0}
                sl_slots = []
                for i in range(2):
                    a_, (ab_,) = A.alloc(512, 1, "silu%d" % i)
                    sl_slots.append((a_, ab_))
                sli = 0
                ui = 0
                units = [(ex_, fh_) for ex_ in range(NEXP) for fh_ in range(2)]

                def issue_load(u):
                    ex_, fh_ = units[u]
                    wg_, wgb_, wgs_, wu_, wub_, wus_, wd_, wdb_, wds_ = wu_slots[u % 2]
                    load_w(wg_, wgb_, wgs_, w_gate[l, ex_].rearrange("(c p) n -> p c n", p=128)[:, :, fh_ * 512:(fh_ + 1) * 512])
                    load_w(wu_, wub_, wus_, w_up[l, ex_].rearrange("(c p) n -> p c n", p=128)[:, :, fh_ * 512:(fh_ + 1) * 512])
                    load_w(wd_, wdb_, wds_, w_down[l, ex_, fh_ * 512:(fh_ + 1) * 512, :].rearrange("(c p) n -> p c n", p=128))
                issue_load(0)
                for ex in range(NEXP):
                    for (gi_, t0, n, isc) in tg_moe:
                        pgb_, pgbb_ = ps_a()
                        s.op("pe", lambda e, pgb_=pgb_, ex=ex, t0=t0, n=n: e.matmul(pgb_[:, 0:n], onehot[0:16, ex * 128:(ex + 1) * 128], gT[0:16, t0:t0 + n], start=True, stop=True),
                             reads=[onehot_b, gT_b], writes=[pgbb_])
                        s.op("act", lambda e, pgb_=pgb_, t0=t0, n=n: e.copy(gbs[:, t0:t0 + n], pgb_[:, 0:n]), reads=[pgbb_], writes=[gb_bufs[gi_]])
                    for fh in range(2):
                        wg_, wgb_, wgs_, wu_, wub_, wus_, wd_, wdb_, wds_ = wu_slots[ui % 2]
                        if ui + 1 < len(units):
                            issue_load(ui + 1)
                        ui += 1
                        def emit_gu(gi_, t0, n, hid, hid_b, wg_=wg_, wgb_=wgb_, wu_=wu_, wub_=wub_):
                            for fc in range(4):
                                pg, pgb = ps_a()
                                pu, pub = ps_a()

                                def f(e, pg=pg, fc=fc):
                                    ins = None
                                    for c in range(KC):
                                        ins = e.matmul(pg[:, 0:n], wg_[:, c, fc * 128:(fc + 1) * 128], hT[:, c, t0:t0 + n], start=(c == 0), stop=(c == KC - 1))
                                    return ins
                                s.op("pe", f, reads=[wgb_, h_bufs[gi_]], writes=[pgb])

                                def f(e, pu=pu, fc=fc):
                                    ins = None
                                    for c in range(KC):
                                        ins = e.matmul(pu[:, 0:n], wu_[:, c, fc * 128:(fc + 1) * 128], hT[:, c, t0:t0 + n], start=(c == 0), stop=(c == KC - 1))
                                    return ins
                                s.op("pe", f, reads=[wub_, h_bufs[gi_]], writes=[pub])
                                sl_, slb_ = sl_slots[mcnt["sli"] % 2]
                                mcnt["sli"] += 1
                                s.op("act", lambda e: e.activation(out=sl_[:, 0:n], in_=pg[:, 0:n], func=AF.Silu), reads=[pgb], writes=[slb_])
                                s.op("dve", lambda e: e.tensor_tensor(sl_[:, 0:n], pu[:, 0:n], sl_[:, 0:n], ALU.mult), reads=[pub, slb_], writes=[slb_])
                                s.op("pool", lambda e: e.tensor_tensor(hid[:, fc, 0:n], sl_[:, 0:n], gbs[:, t0:t0 + n], ALU.mult),
                                     reads=[slb_, gb_bufs[gi_]], writes=[hid_b])

                        def emit_down(gi_, t0, n, isc, hid, hid_b, wd_=wd_, wdb_=wdb_):
                            col = NB if isc else b
                            for oc in range(KC):
                                pd, pdb = ps_b()

                                def f(e, pd=pd, oc=oc):
                                    ins = None
                                    for c in range(4):
                                        ins = e.matmul(pd[:, 0:n], wd_[:, c, oc * 128:(oc + 1) * 128], hid[:, c, 0:n], start=(c == 0), stop=(c == 3))
                                    return ins
                                s.op("pe", f, reads=[wdb_, hid_b], writes=[pdb])
                                s.op("dve", lambda e: e.scalar_tensor_tensor(
                                    xres[:, oc, t0:t0 + n], pd[:, 0:n], modv[:, l, 40 + oc, col:col + 1], xres[:, oc, t0:t0 + n], ALU.mult, ALU.add),
                                    reads=[pdb, xr_bufs[gi_], modv_b], writes=[xr_bufs[gi_]])

                        nseq = len(tg_moe)
                        hs = [hid_slots[(mcnt["hi"] + i) % 2] for i in range(nseq)]
                        mcnt["hi"] += nseq
                        g0 = tg_moe[0]
                        emit_gu(g0[0], g0[1], g0[2], hs[0][0], hs[0][1])
                        for i in range(nseq):
                            if i + 1 < nseq:
                                g1 = tg_moe[i + 1]
                                emit_gu(g1[0], g1[1], g1[2], hs[i + 1][0], hs[i + 1][1])
                            gc = tg_moe[i]
                            emit_down(gc[0], gc[1], gc[2], gc[3], hs[i][0], hs[i][1])
                rr["bn"] = 2
                chk("L%d_moe_e" % l)
                if not last:
                    xwsem = [s.dma_sem() for _ in TG]
                    for (gi_, t0, n, isc) in tg_moe:
                        s.dma("sp", xwsem[gi_], xT[b, :, :, t0:t0 + n], xres[:, :, t0:t0 + n], reads=[xr_bufs[gi_]], writes=[xT_b[b][ti] for ti in tiles_of(t0, n)])
                    if b == 0 and l == 0:
                        dscr("x2", xT[0], xT_b[0])
                else:
                    A.reset(m_exp)
                    fin_slots = []
                    for i in range(2):
                        y_, (yb_,) = A.alloc(KC * 512, 1, "yfin%d" % i)
                        fin_slots.append((y_.rearrange("p (c n) -> p c n", c=KC), yb_))
                    rsf_ap, (rsf_b,) = A.alloc(512, 1, "rsf")
                    ot_slots = []
                    for i in range(2):
                        o_, (ob_,) = A.alloc(D, 1, "ot%d" % i)
                        ot_slots.append((o_, ob_, s.dma_sem()))
                    oti = 0
                    for fi, (gi_, t0, n, isc) in enumerate(tg_moe):
                        y_, yb_ = fin_slots[fi % 2]
                        yf = y_[:, :, 0:n]
                        s.op("act", lambda e, yf=yf, t0=t0, n=n: e.activation(out=yf, in_=xres[:, :, t0:t0 + n], func=AF.Square), reads=[xr_bufs[gi_]], writes=[yb_])
                        pss, pssb = ps_a()

                        def f(e, pss=pss, yf=yf, n=n):
                            ins = None
                            for c in range(KC):
                                ins = e.matmul(pss[:, 0:n], ones_ap, yf[:, c, :], start=(c == 0), stop=(c == KC - 1))
                            return ins
                        s.op("pe", f, reads=[yb_, ones_b], writes=[pssb])
                        act_rstd(rsf_ap[:, 0:n], pss[:, 0:n], 1.0 / D, [pssb], [rsf_b])
                        chk("fin_a")
                        for c in range(KC):
                            s.op("dve", lambda e, yf=yf, c=c, t0=t0, n=n: e.scalar_tensor_tensor(yf[:, c, :], xres[:, c, t0:t0 + n], gfv[:, c:c + 1], rsf_ap[:, 0:n], ALU.mult, ALU.mult),
                                 reads=[xr_bufs[gi_], rsf_b, small_b, yb_], writes=[yb_])
                        chk("fin_b")
                        for tj in range(n // 128):
                            o_, ob_, osem_ = ot_slots[oti % 2]
                            oti += 1
                            for half in range(2):
                                pt, ptb = ps_a()

                                def f(e, pt=pt, yf=yf, tj=tj, half=half):
                                    ins = None
                                    for j in range(4):
                                        c = half * 4 + j
                                        ins = e.transpose(pt[:, j * 128:(j + 1) * 128], yf[:, c, tj * 128:(tj + 1) * 128], ident_ap)
                                    return ins
                                s.op("pe", f, reads=[yb_, ident_b], writes=[ptb])
                                if half == 0:
                                    s.op("act", lambda e, o_=o_, pt=pt: e.copy(o_[:, 0:512], pt[:, :]), reads=[ptb], writes=[ob_])
                                else:
                                    s.op("dve", lambda e, o_=o_, pt=pt: e.tensor_copy(o_[:, 512:1024], pt[:, :]), reads=[ptb], writes=[ob_])
                            chk("fin_c")
                            tl = t0 - C + tj * 128
                            out_toks.append(s.dma("sp", osem_, out[b, tl:tl + 128, :], o_, reads=[ob_]))
                chk("L%d_moe" % l)
                A.reset(mk)
                A.reset(mL)

        s.dead = False
        s.wait_only("sp", out_toks + dbg_sem)
        s.final_barrier("sp")
        s.emit()
        print("program: ins=%d waits=%d sems=%d arena_peak=%d words" % (s.n_ins, s.n_wait, len(s.semh), A.peak))
    return nc


_CACHE = {}


def _get_program(NB, S, C, L, debug=()):
    stop = os.environ.get("K_STOP") or None
    key = (NB, S, C, L, tuple(debug), stop)
    if key not in _CACHE:
        _CACHE[key] = build_program(NB, S, C, L, debug, stop)
    return _CACHE[key]


def run(inputs, n_cores=8, debug=()):
    x = np.asarray(inputs["x"], np.float32)
    ctx = np.asarray(inputs["ctx"], np.float32)
    c = np.asarray(inputs["c"], np.float32)
    c_ctx = np.asarray(inputs["c_ctx"], np.float32)
    B, S, _ = x.shape
    C = ctx.shape[1]
    L = inputs["w_mod"].shape[0]
    NB = B // n_cores
    nc = _get_program(NB, S, C, L, debug)
    consts = host_constants(C, S)
    shared = {}
    for k in ("w_mod", "b_mod", "g_norm1", "g_norm2", "w_in", "lambda_q1", "lambda_k1", "lambda_q2", "lambda_k2", "g_subln",
              "lb_logits", "g_rec_norm", "w_br_attn", "w_br_rec", "w_out", "w_router", "w_gate", "w_up", "w_down"):
        shared[k] = np.ascontiguousarray(np.asarray(inputs[k], np.float32))
    shared["b_router"] = np.ascontiguousarray(np.asarray(inputs["b_router"], np.float32).reshape(1, NEXP))
    shared["g_final"] = np.ascontiguousarray(np.asarray(inputs["g_final"], np.float32).reshape(1, D))
    shared.update(consts)
    in_maps = []
    for i in range(n_cores):
        m = dict(shared)
        m["x"] = np.ascontiguousarray(x[i * NB:(i + 1) * NB])
        m["ctx"] = np.ascontiguousarray(ctx[i * NB:(i + 1) * NB])
        cv4 = np.zeros((4, D), np.float32)
        cv4[0:NB] = c[i * NB:(i + 1) * NB]
        cv4[NB] = c_ctx
        m["cvec"] = cv4
        in_maps.append(m)
    res = run_bass_kernel_spmd(nc, in_maps, core_ids=list(range(n_cores)))
    outs = np.concatenate([np.asarray(r["out"]) for r in res.results], axis=0)
    return outs, res


def kernel(**inputs):
    out, _ = run(inputs, n_cores=8)
    return out.astype(np.float32)
```

```python
import contextlib
import math
import os
import sys
import numpy as np
import ml_dtypes
import concourse.bass as bass
import concourse.mybir as mybir
from concourse.bass_utils import run_bass_kernel_spmd

F32 = mybir.dt.float32
BF16 = mybir.dt.bfloat16
I32 = mybir.dt.int32
AF = mybir.ActivationFunctionType
ALU = mybir.AluOpType
AX = mybir.AxisListType

DEBUG_ANNOTATE = bool(os.environ.get('K_ANNOTATE'))
DMA_SEM_POOL = int(os.environ.get('K_DSEMS', '1000'))
POOL_TO_DVE = os.environ.get('K_POOLDVE', '0') == '1'
D = 1024
KC = 8
N_IN = 10240
EPS = 1e-6
NEXP = 16
CHUNK = 32
NPT = 128 // CHUNK
GRID_W = 64
OFF_AQ, OFF_AK, OFF_AV, OFF_RQ, OFF_RFF, OFF_RFB, OFF_RI, OFF_RG, OFF_GA, OFF_GR = [1024 * i for i in range(10)]


class Buf:
    __slots__ = ("name", "w", "r", "psum")

    def __init__(self, name="", psum=False):
        self.name = name
        self.w = None
        self.r = {}
        self.psum = psum


class DmaSem:
    __slots__ = ("idx", "h", "total")

    def __init__(self, idx, h):
        self.idx = idx
        self.h = h
        self.total = 0


class _Rec:
    def __init__(self):
        self.calls = []

    def __getattr__(self, name):
        def m(*a, **k):
            self.calls.append((name, a, k))
            return self
        return m


class Sched:
    ENG = ("pe", "act", "dve", "pool", "sp")

    def __init__(self, nc, stack):
        self.nc = nc
        self.stack = stack
        self.semh = []
        self.semidx = {}
        self.cnt = {}
        self.ops = {}
        self.known = {}
        for e in self.ENG:
            self.semidx[e] = self._new_sem("s_" + e)
            self.cnt[e] = 0
            self.ops[e] = []
            self.known[e] = {}
        self.n_wait = 0
        self.n_ins = 0
        self.dead = False

    def _new_sem(self, name):
        h = self.stack.enter_context(self.nc.semaphore(name))
        self.semh.append(h)
        return len(self.semh) - 1

    def begin_iter(self):
        if not hasattr(self, "_ipool"):
            self._ipool = []
        self._iidx = 0

    def dma_sem(self, name=None):
        if not hasattr(self, "_dpool"):
            self._dpool = []
            self._dnext = 0
        if getattr(self, "_iidx", None) is not None:
            if self._iidx < len(self._ipool):
                ds = self._ipool[self._iidx]
            else:
                idx = self._new_sem("d%d" % len(self.semh))
                ds = DmaSem(idx, self.semh[idx])
                self._ipool.append(ds)
                self._dpool.append(ds)
            self._iidx += 1
            return ds
        if len(self._dpool) < DMA_SEM_POOL:
            idx = self._new_sem("d%d" % len(self.semh))
            ds = DmaSem(idx, self.semh[idx])
            self._dpool.append(ds)
            return ds
        ds = self._dpool[self._dnext % len(self._dpool)]
        self._dnext += 1
        return ds

    @property
    def _dsems(self):
        return getattr(self, "_dpool", [])

    def _collect(self, eng, reads, writes, extra=()):
        deps = {}
        own_ = self.semidx[eng]
        for b in reads:
            t = b.w
            if t is not None and deps.get(t[0], 0) < t[1]:
                deps[t[0]] = t[1]
            if b.psum:
                for si, v in b.r.items():
                    if si != own_ and deps.get(si, 0) < v:
                        deps[si] = v
        for b in writes:
            t = b.w
            if t is not None and deps.get(t[0], 0) < t[1]:
                deps[t[0]] = t[1]
            for si, v in b.r.items():
                if deps.get(si, 0) < v:
                    deps[si] = v
        for t in extra:
            if t is not None and deps.get(t[0], 0) < t[1]:
                deps[t[0]] = t[1]
        own = self.semidx[eng]
        kn = self.known[eng]
        waits = []
        for si, v in deps.items():
            if eng == "pe" and si == own:
                continue
            if kn.get(si, 0) >= v:
                continue
            kn[si] = v
            waits.append((si, v))
        self.n_wait += len(waits)
        return waits

    def _mark(self, tok, reads, writes):
        si, v = tok
        for b in reads:
            if b.r.get(si, 0) < v:
                b.r[si] = v
        for b in writes:
            b.w = tok
            b.r = {}

    def op(self, eng, fn, reads=(), writes=(), extra=()):
        if self.dead:
            return (0, 0)
        if eng == "pool" and POOL_TO_DVE:
            eng = "dve"
        rec = _Rec()
        fn(rec)
        calls = rec.calls
        lineno = sys._getframe(1).f_lineno

        def fn(e, calls=calls, lineno=lineno):
            ins = None
            for name, a, k in calls:
                ins = getattr(e, name)(*a, **k)
                if DEBUG_ANNOTATE:
                    ins.annotate("L%d" % lineno)
            return ins
        waits = self._collect(eng, reads, writes, extra)
        self.cnt[eng] += 1
        tok = (self.semidx[eng], self.cnt[eng])
        self.ops[eng].append((waits, fn, tok[0]))
        self._mark(tok, reads, writes)
        self.n_ins += 1
        return tok

    def dma(self, queue, dsem, out, in_, reads=(), writes=(), extra=()):
        if self.dead:
            return (0, 0)
        if dsem.total > 0:
            extra = tuple(extra) + ((dsem.idx, dsem.total),)
        waits = self._collect(queue, reads, writes, extra)
        dsem.total += 16
        tok = (dsem.idx, dsem.total)
        h = dsem.h

        def fn(e, out=out, in_=in_, h=h):
            e.dma_start(out=out, in_=in_).then_inc(h, 16)
            return None

        self.ops[queue].append((waits, fn, None))
        self._mark(tok, reads, writes)
        self.n_ins += 1
        return tok

    def wait_only(self, eng, toks):
        waits = self._collect(eng, (), (), toks)
        self.ops[eng].append((waits, None, None))

    def final_barrier(self, eng="sp"):
        toks = [(self.semidx[e], self.cnt[e]) for e in self.ENG if self.cnt[e] > 0 and e != eng]
        for ds in self._dsems:
            if ds.total > 0:
                toks.append((ds.idx, ds.total))
        self.wait_only(eng, toks)

    def emit(self):
        nc = self.nc
        semh = self.semh
        if os.environ.get("K_SEMCLR", "1") == "1":
            for h in semh:
                nc.sync.sem_clear(h)
            nc.all_engine_barrier()
        with nc.Block() as block:
            def mk(name):
                ops = self.ops[name]

                def body(e):
                    for waits, fn, si in ops:
                        for (wi, v) in waits:
                            e.wait_ge(semh[wi], v)
                        if fn is None:
                            continue
                        ins = fn(e)
                        if si is not None:
                            ins.then_inc(semh[si], 1)
                return body

            block.tensor(mk("pe"))
            block.scalar(mk("act"))
            block.vector(mk("dve"))
            block.gpsimd(mk("pool"))
            block.sync(mk("sp"))


class Arena:
    def __init__(self, ap, words):
        self.ap = ap
        self.words = words
        self.top = 0
        self.hist = []
        self.peak = 0

    def mark(self):
        return self.top

    def reset(self, m):
        self.top = m

    def alloc(self, words, nbuf=1, name=""):
        words = (words + 1) // 2 * 2
        st, en = self.top, self.top + words
        assert en <= self.words, "SBUF arena overflow %s %d" % (name, en)
        self.top = en
        self.peak = max(self.peak, en)
        bufs = [Buf(name) for _ in range(nbuf)]
        inherit = {}
        keep = []
        for (a, b, bl) in self.hist:
            if a < en and st < b:
                for ob in bl:
                    if ob.w is not None and inherit.get(ob.w[0], 0) < ob.w[1]:
                        inherit[ob.w[0]] = ob.w[1]
                    for si, v in ob.r.items():
                        if inherit.get(si, 0) < v:
                            inherit[si] = v
                if not (st <= a and b <= en):
                    keep.append((a, b, bl))
            else:
                keep.append((a, b, bl))
        self.hist = keep
        for nb in bufs:
            nb.r = dict(inherit)
        self.hist.append((st, en, bufs))
        return self.ap[:, st:en], bufs


def host_constants(C, S):
    T = C + S
    ident = np.eye(128, dtype=np.float32)
    inv = (10000.0 ** (-np.arange(16, dtype=np.float32) / 16.0)).astype(np.float32)
    t = np.arange(S)
    row = (t // GRID_W).astype(np.float32)
    col = (t % GRID_W).astype(np.float32)
    cos = np.ones((128, T), np.float32)
    sin = np.zeros((128, T), np.float32)
    swap = np.zeros((128, 128), np.float32)
    for p in range(128):
        j = p % 64
        base = row if j < 32 else col
        ang = (base * inv[j % 16]).astype(np.float32)
        first = (j % 32) < 16
        cos[p, C:] = np.cos(ang)
        sin[p, C:] = (-np.sin(ang)) if first else np.sin(ang)
        sp = p + 16 if first else p - 16
        swap[sp, p] = 1.0
    s_i = np.arange(128)[:, None]
    t_i = np.arange(128)[None, :]
    same = (s_i // CHUNK) == (t_i // CHUNK)
    maskf = (same & (s_i <= t_i)).astype(np.float32)
    maskb = (same & (s_i >= t_i)).astype(np.float32)
    scanm = np.ones((128, T), np.float32)
    scanm[:, ::CHUNK] = 0.0
    cmask = np.zeros((128, NPT), np.float32)
    for p in range(128):
        cmask[p, p // CHUNK] = 1.0
    onehot = np.zeros((16, 16, 128), np.float32)
    for e in range(16):
        onehot[e, e, :] = 1.0
    return dict(c_ident=ident, c_cos=cos, c_sin=sin, c_swap=swap, c_maskf=maskf, c_maskb=maskb,
                c_scanm=scanm, c_onehot=onehot.reshape(16, 16 * 128), c_cmask=cmask)


def build_program(NB, S, C, L, debug=(), stop=None):
    T = C + S
    NT = T // 128
    NCT = C // 128
    NCH = T // CHUNK
    TG = [(0, C, True)]
    gsz = min(512, S)
    for g in range(S // gsz):
        TG.append((C + g * gsz, gsz, False))
    MGN = 256 if S >= 256 else S
    TGM = []
    for st_ in range(0, C, min(MGN, C)):
        TGM.append((st_, min(MGN, C), True))
    for st_ in range(C, T, MGN):
        TGM.append((st_, MGN, False))

    nc = bass.Bass("TRN2", target_bir_lowering=False)

    def din(name, shape, dt=F32):
        return nc.dram_tensor(name, list(shape), dt, kind="ExternalInput").ap()

    x_in = din("x", [NB, S, D])
    ctx_in = din("ctx", [NB, C, D])
    cvec = din("cvec", [4, D])
    w_mod = din("w_mod", [L, D, 6 * D])
    b_mod = din("b_mod", [L, 6 * D])
    g_norm1 = din("g_norm1", [L, D])
    g_norm2 = din("g_norm2", [L, D])
    w_in = din("w_in", [L, D, N_IN])
    lam_q1 = din("lambda_q1", [L, 64])
    lam_k1 = din("lambda_k1", [L, 64])
    lam_q2 = din("lambda_q2", [L, 64])
    lam_k2 = din("lambda_k2", [L, 64])
    g_subln = din("g_subln", [L, 128])
    lb_logits = din("lb_logits", [L, D])
    g_rec = din("g_rec_norm", [L, 128])
    w_bra = din("w_br_attn", [L, D, D])
    w_brr = din("w_br_rec", [L, D, D])
    w_out = din("w_out", [L, D, D])
    w_router = din("w_router", [D, NEXP])
    b_router = din("b_router", [1, NEXP])
    w_gate = din("w_gate", [L, NEXP, D, D])
    w_up = din("w_up", [L, NEXP, D, D])
    w_down = din("w_down", [L, NEXP, D, D])
    g_final = din("g_final", [1, D])
    c_ident = din("c_ident", [128, 128])
    c_cos = din("c_cos", [128, T])
    c_sin = din("c_sin", [128, T])
    c_swap = din("c_swap", [128, 128])
    c_maskf = din("c_maskf", [128, 128])
    c_maskb = din("c_maskb", [128, 128])
    c_scanm = din("c_scanm", [128, T])
    c_onehot = din("c_onehot", [16, 16 * 128])
    c_cmask = din("c_cmask", [128, NPT])

    out = nc.dram_tensor("out", [NB, S, D], F32, kind="ExternalOutput").ap()
    xT = nc.dram_tensor("xT_scr", [NB, 128, KC, T], F32, kind="Internal").ap()
    aT = nc.dram_tensor("aT_scr", [128, KC, T], BF16, kind="Internal").ap()
    rT = nc.dram_tensor("rT_scr", [128, KC, T], BF16, kind="Internal").ap()
    dbg_out = {}

    st = contextlib.ExitStack()
    with st:
        s = Sched(nc, st)
        AW = int(os.environ.get("K_AW", 52224))
        arena_t = st.enter_context(nc.sbuf_tensor("arena", [128, AW], F32))
        A = Arena(arena_t, AW)
        psum = [st.enter_context(nc.psum_tensor("ps%d" % i, [128, 512], F32)) for i in range(8)]
        psb = [Buf("ps%d" % i, psum=True) for i in range(8)]
        rr = {"a": 0, "b": 0, "bn": 2}

        def ps_a():
            i = rr["a"] % 4
            rr["a"] += 1
            return psum[i], psb[i]

        def ps_b():
            i = 4 + rr["b"] % rr["bn"]
            rr["b"] += 1
            return psum[i], psb[i]

        def bf(ap):
            return ap.bitcast(BF16)

        dbg_sem = []
        dbg_ds = [None]

        def chk(name):
            if stop == name:
                s.dead = True

        def _dbg_ds():
            if dbg_ds[0] is None:
                dbg_ds[0] = s.dma_sem()
            return dbg_ds[0]

        def dbg(name, ap, bufs):
            if name not in debug:
                return
            shp = list(ap.shape)
            o = nc.dram_tensor("dbg_" + name, shp, ap.dtype, kind="ExternalOutput").ap()
            dbg_out[name] = o
            ds = _dbg_ds()
            tok = s.dma("sp", ds, o, ap, reads=bufs)
            dbg_sem.append(tok)

        def dscr(name, dram_ap, dram_bufs):
            if name not in debug:
                return
            o = nc.dram_tensor("dbg_" + name, list(dram_ap.shape), dram_ap.dtype, kind="ExternalOutput").ap()
            ds = _dbg_ds()
            tok = s.dma("sp", ds, o, dram_ap, reads=dram_bufs)
            dbg_sem.append(tok)

        ident_ap, (ident_b,) = A.alloc(128, 1, "ident")
        identb_w, (identb_b,) = A.alloc(64, 1, "identb")
        identb = bf(identb_w)
        ones_ap, (ones_b,) = A.alloc(128, 1, "ones")
        onesb_w, (onesb_b,) = A.alloc(64, 1, "onesb")
        onesb = bf(onesb_w)
        swap_w, (swap_b,) = A.alloc(64, 1, "swap")
        swapm = bf(swap_w)
        maskf_w, (maskf_b,) = A.alloc(64, 1, "maskf")
        maskb_w, (maskb_b,) = A.alloc(64, 1, "maskb")
        maskf = bf(maskf_w)
        maskb = bf(maskb_w)
        NCOL = NB + 1
        modv_ap, (modv_b,) = A.alloc(L * 48 * NCOL, 1, "modv")
        modv = modv_ap.rearrange("p (l c n) -> p l c n", l=L, c=48)
        a1_ap, (a1_b,) = A.alloc(L * KC * NCOL, 1, "a1")
        a1 = a1_ap.rearrange("p (l c n) -> p l c n", l=L, c=KC)
        a2_ap, (a2_b,) = A.alloc(L * KC * NCOL, 1, "a2")
        a2 = a2_ap.rearrange("p (l c n) -> p l c n", l=L, c=KC)
        small_ap, (small_b,) = A.alloc(256, 1, "small")
        lbv_ap, (lbv_b,) = A.alloc(L * 8 * 3, 1, "lbv")
        lbv = lbv_ap.rearrange("p (l h k) -> p l h k", l=L, h=8)
        lamv_ap, (lamv_b,) = A.alloc(L * 4, 1, "lamv")
        lamv = lamv_ap.rearrange("p (l k) -> p l k", l=L)
        brt_ap, (brt_b,) = A.alloc(16, 1, "brt")
        wr_w, (wr_b,) = A.alloc(KC * 16 // 2, 1, "wr")
        wrt = bf(wr_w).rearrange("p (c e) -> p c e", c=KC)
        eps_ap, (eps_b,) = A.alloc(2, 1, "eps")
        cmask_ap, (cmask_b,) = A.alloc(NPT, 1, "cmask")

        cs = [s.dma_sem() for _ in range(4)]
        s.dma("sp", cs[0], ident_ap, c_ident, writes=[ident_b])
        s.dma("sp", cs[0], cmask_ap, c_cmask, writes=[cmask_b])
        hm_ap, (hm_b,) = A.alloc(2, 1, "hmask")
        s.op("dve", lambda e: e.tensor_tensor(hm_ap[:, 0:1], cmask_ap[:, 0:1], cmask_ap[:, 1:2], ALU.add), reads=[cmask_b], writes=[hm_b])
        s.op("dve", lambda e: e.tensor_tensor(hm_ap[:, 1:2], cmask_ap[:, 2:3], cmask_ap[:, 3:4], ALU.add), reads=[cmask_b], writes=[hm_b])
        cs2 = [s.dma_sem() for _ in range(5)]
        s.dma("pool", cs2[0], swapm, c_swap, writes=[swap_b])
        s.dma("pool", cs2[1], maskf, c_maskf, writes=[maskf_b])
        s.dma("pool", cs2[2], maskb, c_maskb, writes=[maskb_b])
        s.dma("pool", cs2[4], wrt, w_router.rearrange("(c p) e -> p c e", p=128), writes=[wr_b])
        s.op("pool", lambda e: e.memset(ones_ap, 1.0), writes=[ones_b])
        s.op("pool", lambda e: e.memset(lamv_ap, 0.0), writes=[lamv_b])
        s.op("pool", lambda e: e.memset(onesb, 1.0), writes=[onesb_b])
        s.op("pool", lambda e: e.memset(eps_ap, EPS), writes=[eps_b])
        s.op("act", lambda e: e.copy(identb, ident_ap), reads=[ident_b], writes=[identb_b])

        chk("c0")
        m0 = A.mark()
        stA_ap, (stA_b,) = A.alloc(128, 1, "stA")
        stB_ap, (stB_b,) = A.alloc(128, 1, "stB")
        cv_ap, (cv_b,) = A.alloc(D, 1, "cv")
        scT_ap, (scT_b,) = A.alloc(KC * NCOL, 1, "scT")
        scT = scT_ap.rearrange("p (c n) -> p c n", c=KC)
        lamst_ap, (lamst_b,) = A.alloc(4 * L * 64 + 2 * L * 128, 1, "lamst")
        p0s = [s.dma_sem() for _ in range(4)]
        s.dma("sp", p0s[0], stA_ap[0:L * 48, :], b_mod.rearrange("l (c p) -> (l c) p", p=128), writes=[stA_b])
        s.dma("sp", p0s[1], stB_ap[0:L * 8, :], g_norm1.rearrange("l (c p) -> (l c) p", p=128), writes=[stB_b])
        s.dma("sp", p0s[1], stB_ap[16:16 + L * 8, :], g_norm2.rearrange("l (c p) -> (l c) p", p=128), writes=[stB_b])
        s.dma("sp", p0s[1], stB_ap[32:40, :], g_final.rearrange("o (c p) -> (o c) p", p=128), writes=[stB_b])
        s.dma("sp", p0s[1], stB_ap[40:40 + L * 8, :], lb_logits.rearrange("l (c p) -> (l c) p", p=128), writes=[stB_b])
        s.dma("sp", p0s[2], cv_ap[0:4, :], cvec, writes=[cv_b])
        lam_srcs = [lam_q1, lam_k1, lam_q2, lam_k2]
        for i, src in enumerate(lam_srcs):
            s.dma("sp", p0s[3], lamst_ap[:, i * L * 64:(i + 1) * L * 64],
                  src.rearrange("l d -> (l d)").partition_broadcast(128), writes=[lamst_b])
        s.dma("sp", p0s[3], brt_ap, b_router.rearrange("o e -> (o e)").partition_broadcast(128), writes=[brt_b])
        pst, pstb = ps_a()
        s.op("pe", lambda e: e.transpose(pst[:, 0:96], stA_ap[0:96, :], ident_ap[0:96, 0:96]),
             reads=[stA_b, ident_b], writes=[pstb])
        s.op("dve", lambda e: e.tensor_copy(small_ap[:, 0:96], pst[:, 0:96]), reads=[pstb], writes=[small_b])
        pst2, pstb2 = ps_a()
        s.op("pe", lambda e: e.transpose(pst2[:, 0:56], stB_ap[0:56, :], ident_ap[0:56, 0:56]),
             reads=[stB_b, ident_b], writes=[pstb2])
        s.op("dve", lambda e: e.tensor_copy(small_ap[:, 96:152], pst2[:, 0:56]), reads=[pstb2], writes=[small_b])
        chk("p0a")
        g1v = small_ap[:, 96:112].rearrange("p (l c) -> p l c", l=2)
        g2v = small_ap[:, 112:128].rearrange("p (l c) -> p l c", l=2)
        gfv = small_ap[:, 128:136]
        lbl = small_ap[:, 136:152].rearrange("p (l c) -> p l c", l=2)
        bmv = small_ap[:, 0:96].rearrange("p (l c) -> p l c", l=2)
        for l in range(L):
            s.dma("sp", p0s[3], lamv[:, l, 1:2], g_subln[l:l + 1, :].rearrange("o p -> p o"), writes=[lamv_b])
            s.dma("sp", p0s[3], lamv[:, l, 2:3], g_rec[l:l + 1, :].rearrange("o p -> p o"), writes=[lamv_b])
        lt_ap, (lt_b,) = A.alloc(L * 64 * 2 + 8 * L, 1, "lamtmp")
        for l in range(L):
            lam_init = 0.8 - 0.6 * math.exp(-0.3 * l)
            for j in range(2):
                qv = lamst_ap[:, (2 * j) * L * 64 + l * 64:(2 * j) * L * 64 + (l + 1) * 64]
                kv = lamst_ap[:, (2 * j + 1) * L * 64 + l * 64:(2 * j + 1) * L * 64 + (l + 1) * 64]
                pr = lt_ap[:, (l * 2 + j) * 64:(l * 2 + j + 1) * 64]
                sm = lt_ap[:, L * 128 + l * 4 + j:L * 128 + l * 4 + j + 1]
                s.op("dve", lambda e, pr=pr, qv=qv, kv=kv: e.tensor_tensor(pr, qv, kv, ALU.mult), reads=[lamst_b], writes=[lt_b])
                s.op("dve", lambda e, pr=pr, sm=sm: e.tensor_reduce(sm, pr, AX.X, ALU.add), reads=[lt_b], writes=[lt_b])
                s.op("act", lambda e, sm=sm: e.activation(out=sm, in_=sm, func=AF.Exp), reads=[lt_b], writes=[lt_b])
            e1 = lt_ap[:, L * 128 + l * 4:L * 128 + l * 4 + 1]
            e2 = lt_ap[:, L * 128 + l * 4 + 1:L * 128 + l * 4 + 2]
            s.op("dve", lambda e, l=l, e1=e1, e2=e2, li=lam_init: e.scalar_tensor_tensor(
                lamv[:, l, 0:1], e2, -li, e1, ALU.add, ALU.subtract), reads=[lt_b], writes=[lamv_b])
            s.op("dve", lambda e, l=l, li=lam_init: e.tensor_scalar(lamv[:, l, 1:2], lamv[:, l, 1:2], 1.0 - li, None, ALU.mult),
                 reads=[lamv_b], writes=[lamv_b])
        chk("p0b")
        lbe_ap, (lbe_b,) = A.alloc(L * 8 + 16, 1, "lbe")
        lbe = lbe_ap[:, 0:L * 8].rearrange("p (l c) -> p l c", l=L)
        lsum = lbe_ap[:, L * 8:L * 8 + 8]
        s.op("act", lambda e: e.activation(out=lbe_ap[:, 0:L * 8], in_=small_ap[:, 136:136 + L * 8], func=AF.Exp), reads=[small_b], writes=[lbe_b])
        s.op("dve", lambda e: e.tensor_copy(lsum, lbe[:, 0, :]), reads=[lbe_b], writes=[lbe_b])
        for l in range(1, L):
            s.op("dve", lambda e, l=l: e.tensor_tensor(lsum, lsum, lbe[:, l, :], ALU.add), reads=[lbe_b], writes=[lbe_b])
        s.op("dve", lambda e: e.reciprocal(lsum, lsum), reads=[lbe_b], writes=[lbe_b])
        s.op("pool", lambda e: e.memset(lbv[:, 0, :, 0], 0.0), writes=[lbv_b])
        for l in range(1, L):
            s.op("dve", lambda e, l=l: e.tensor_tensor(lbe[:, l, :], lbe[:, l, :], lsum, ALU.mult), reads=[lbe_b], writes=[lbe_b])
            s.op("dve", lambda e, l=l: e.tensor_tensor(lbv[:, l, :, 0], lbv[:, l - 1, :, 0], lbe[:, l, :], ALU.add),
                 reads=[lbe_b, lbv_b], writes=[lbv_b])
        for l in range(L):
            s.op("dve", lambda e, l=l: e.tensor_scalar(lbv[:, l, :, 1], lbv[:, l, :, 0], -1.0, 1.0, ALU.mult, ALU.add),
                 reads=[lbv_b], writes=[lbv_b])
            s.op("dve", lambda e, l=l: e.tensor_scalar(lbv[:, l, :, 2], lbv[:, l, :, 0], 1.0, -1.0, ALU.mult, ALU.add),
                 reads=[lbv_b], writes=[lbv_b])
        chk("p0c")
        s.op("act", lambda e: e.activation(out=cv_ap[0:NCOL, :], in_=cv_ap[0:NCOL, :], func=AF.Silu), reads=[cv_b], writes=[cv_b])
        pst3, pstb3 = ps_a()

        def f_ct(e):
            ins = None
            for c in range(KC):
                ins = e.transpose(pst3[:, c * NCOL:(c + 1) * NCOL], cv_ap[0:NCOL, c * 128:(c + 1) * 128], ident_ap[0:NCOL, 0:NCOL])
            return ins
        s.op("pe", f_ct, reads=[cv_b, ident_b], writes=[pstb3])
        s.op("dve", lambda e: e.tensor_copy(scT_ap, pst3[:, 0:KC * NCOL]), reads=[pstb3], writes=[scT_b])
        chk("p0d")
        wm_slots = []
        for i in range(2):
            ap_, (b_,) = A.alloc(KC * 512, 1, "wmod%d" % i)
            wm_slots.append((ap_.rearrange("p (c n) -> p c n", c=KC), b_, s.dma_sem()))
        gi = 0
        for l in range(L):
            pm, pmb = ps_a()
            for g in range(12):
                wt, wb, wsem = wm_slots[gi % 2]
                gi += 1
                s.dma("sp", wsem, wt, w_mod[l].rearrange("(c p) n -> p c n", p=128)[:, :, g * 512:(g + 1) * 512], writes=[wb])

                def f_mod(e, wt=wt, g=g, pm=pm):
                    ins = None
                    for j in range(4):
                        oc = g * 4 + j
                        for c in range(KC):
                            ins = e.matmul(pm[:, oc * NCOL:(oc + 1) * NCOL], wt[:, c, j * 128:(j + 1) * 128], scT[:, c, :],
                                           start=(c == 0), stop=(c == KC - 1))
                    return ins
                s.op("pe", f_mod, reads=[wb, scT_b], writes=[pmb])
            if l == 0:
                chk("p0e")
            s.op("dve", lambda e, l=l, pm=pm: e.tensor_tensor(
                modv[:, l], pm[:, 0:48 * NCOL].rearrange("p (c n) -> p c n", c=48),
                bmv[:, l, :].unsqueeze(2).to_broadcast([128, 48, NCOL]), ALU.add),
                reads=[pmb, small_b], writes=[modv_b])
            if l == 0:
                chk("p0f")
            for (dst, gv, off) in ((a1, g1v, 8), (a2, g2v, 32)):
                for col_ in range(NCOL):
                    s.op("dve", lambda e, l=l, dst=dst, gv=gv, off=off, col_=col_: e.scalar_tensor_tensor(
                        dst[:, l, :, col_], modv[:, l, off:off + 8, col_], 1.0, gv[:, l, :], ALU.add, ALU.mult),
                        reads=[modv_b, small_b], writes=[a1_b if dst is a1 else a2_b])
        dbg("modv", modv_ap, [modv_b])
        dbg("a1", a1_ap, [a1_b])
        dbg("lamv", lamv_ap, [lamv_b])
        dbg("lbv", lbv_ap, [lbv_b])
        A.reset(m0)
        chk("p0")

        xT_b = [[Buf("xT%d_%d" % (b, g)) for g in range(T // 128)] for b in range(NB)]
        aT_b = [Buf("aT%d" % h) for h in range(8)]
        rT_b = [Buf("rT%d" % h) for h in range(8)]

        def tiles_of(t0, n):
            return range(t0 // 128, (t0 + n) // 128)

        def act_rstd(dst, src_ps, inv_n, rd, wr):
            s.op("act", lambda e: e.activation(out=dst, in_=src_ps, func=AF.Ln, bias=eps_ap[:, 0:1], scale=inv_n), reads=rd + [eps_b], writes=wr)
            s.op("act", lambda e: e.activation(out=dst, in_=dst, func=AF.Exp, scale=-0.5), reads=wr, writes=wr)

        def norm_mod(xg, xg_b, n, hdst, hdst_b, av, sv, av_b):
            mk = A.mark()
            sq_ap, (sq_b,) = A.alloc(KC * n, 1, "sq")
            sq = sq_ap.rearrange("p (c n) -> p c n", c=KC)
            rs_ap, (rs_b,) = A.alloc(n, 1, "rstd")
            s.op("act", lambda e: e.activation(out=sq, in_=xg, func=AF.Square), reads=[xg_b], writes=[sq_b])
            pss, pssb = ps_a()

            def f(e):
                ins = None
                for c in range(KC):
                    ins = e.matmul(pss[:, 0:n], ones_ap, sq[:, c, :], start=(c == 0), stop=(c == KC - 1))
                return ins
            s.op("pe", f, reads=[sq_b, ones_b], writes=[pssb])
            act_rstd(rs_ap, pss[:, 0:n], 1.0 / D, [pssb], [rs_b])
            for c in range(KC):
                tmp = sq[:, c, :]
                s.op("dve", lambda e, c=c, tmp=tmp: e.scalar_tensor_tensor(tmp, xg[:, c, :], av[:, c:c + 1], rs_ap, ALU.mult, ALU.mult),
                     reads=[xg_b, rs_b, av_b], writes=[sq_b])
                s.op("act", lambda e, c=c, tmp=tmp: e.activation(out=hdst[:, c, :], in_=tmp, func=AF.Identity, bias=sv[:, c:c + 1], scale=1.0),
                     reads=[sq_b, av_b], writes=[hdst_b])
            A.reset(mk)

        def load_w(dst, dst_b, sem, src):
            return s.dma("pool", sem, dst, src, writes=[dst_b])

        m1 = A.mark()
        xin_slots = []
        for i in range(2):
            ap_, (b_,) = A.alloc(D, 1, "xin%d" % i)
            xo_, (ob_,) = A.alloc(D, 1, "xo%d" % i)
            xin_slots.append((ap_, b_, s.dma_sem(), xo_, ob_, s.dma_sem()))
        k = 0
        for b in range(NB):
            for ti in range(NT):
                ap_, b_, sm_, xo_, ob_, osm_ = xin_slots[k % 2]
                k += 1
                src = ctx_in[b, ti * 128:(ti + 1) * 128, :] if ti < NCT else x_in[b, (ti - NCT) * 128:(ti - NCT + 1) * 128, :]
                s.dma("sp", sm_, ap_, src, writes=[b_])
                for half in range(2):
                    pt, ptb = ps_a()

                    def f(e, ap_=ap_, pt=pt, half=half):
                        ins = None
                        for j in range(4):
                            c = half * 4 + j
                            ins = e.transpose(pt[:, j * 128:(j + 1) * 128], ap_[:, c * 128:(c + 1) * 128], ident_ap)
                        return ins
                    s.op("pe", f, reads=[b_, ident_b], writes=[ptb])
                    eng = "act" if half == 0 else "dve"
                    if eng == "act":
                        s.op("act", lambda e, xo_=xo_, pt=pt, half=half: e.copy(xo_[:, half * 512:(half + 1) * 512], pt[:, :]), reads=[ptb], writes=[ob_])
                    else:
                        s.op("dve", lambda e, xo_=xo_, pt=pt, half=half: e.tensor_copy(xo_[:, half * 512:(half + 1) * 512], pt[:, :]), reads=[ptb], writes=[ob_])
                s.dma("sp", osm_, xT[b, :, :, ti * 128:(ti + 1) * 128], xo_.rearrange("p (c t) -> p c t", c=KC), reads=[ob_], writes=[xT_b[b][ti]])
        A.reset(m1)
        chk("x0")

        out_toks = []
        for b in range(NB):
            for l in range(L):
                last = (l == L - 1)
                s.begin_iter()
                mL = A.mark()
                h_w, h_bufs = A.alloc(KC * T // 2, len(TG), "h")
                hT = bf(h_w).rearrange("p (c t) -> p c t", c=KC)
                hb_of = {}
                for gi_, (t0, n, isc) in enumerate(TG):
                    for ti in tiles_of(t0, n):
                        hb_of[ti] = h_bufs[gi_]

                def hbufs(t0, n):
                    return list({id(hb_of[ti]): hb_of[ti] for ti in tiles_of(t0, n)}.values())

                mk = A.mark()
                xg_slots = []
                for i in range(2):
                    ap_, (b_,) = A.alloc(KC * 512, 1, "xg%d" % i)
                    xg_slots.append((ap_, b_, s.dma_sem()))
                for gi_, (t0, n, isc) in enumerate(TG):
                    ap_, b_, sm_ = xg_slots[gi_ % 2]
                    xg = ap_[:, 0:KC * n].rearrange("p (c n) -> p c n", c=KC)
                    s.dma("sp", sm_, xg, xT[b, :, :, t0:t0 + n], reads=[xT_b[b][ti] for ti in tiles_of(t0, n)], writes=[b_])
                    col = NB if isc else b
                    norm_mod(xg, b_, n, hT[:, :, t0:t0 + n], h_bufs[gi_], a1[:, l, :, col], modv[:, l, 0:8, col], a1_b)
                A.reset(mk)
                if b == 0 and l == 0:
                    dbg("h", h_w, h_bufs)
                chk("n1")

                mk = A.mark()
                cos_ap, (cos_b,) = A.alloc(T, 1, "cos")
                sin_ap, (sin_b,) = A.alloc(T, 1, "sin")
                s.dma("sp", cs[1], cos_ap, c_cos, writes=[cos_b])
                s.dma("sp", cs[2], sin_ap, c_sin, writes=[sin_b])
                att_w = []
                for i in range(2):
                    ent = []
                    for nm_ in ("wq", "wk", "wv"):
                        w_, (wb_,) = A.alloc(KC * 128 // 2, 1, nm_ + str(i))
                        ent.append((bf(w_).rearrange("p (c n) -> p c n", c=KC), wb_, s.dma_sem()))
                    att_w.append(ent)
                wsrc_att = w_in[l].rearrange("(c p) n -> p c n", p=128)

                def att_load(hd_):
                    for (w_, wb_, ws_), off_ in zip(att_w[hd_ % 2], (OFF_AQ, OFF_AK, OFF_AV)):
                        load_w(w_, wb_, ws_, wsrc_att[:, :, off_ + hd_ * 128:off_ + (hd_ + 1) * 128])
                att_load(0)
                qr_w, (qr_b,) = A.alloc(T // 2, 1, "qr")
                qr1_w, (qr1_b,) = A.alloc(T // 2, 1, "qr1")
                kr_w, (kr_b,) = A.alloc(T // 2, 1, "kr")
                qr = bf(qr_w)
                qr1 = bf(qr1_w)
                kr = bf(kr_w)
                v_w, (v_b,) = A.alloc(NT * 128 // 2, 1, "v")
                vv = bf(v_w).rearrange("p (t d) -> p t d", t=NT)
                qb_slots = []
                for i in range(2):
                    ap_, (b_,) = A.alloc(256, 1, "qb%d" % i)
                    t1_, (t1b_,) = A.alloc(512, 1, "t1_%d" % i)
                    t2_, (t2b_,) = A.alloc(512, 1, "t2_%d" % i)
                    qb_slots.append((bf(ap_), b_, t1_, t1b_, t2_, t2b_))
                pT_slots = []
                for i in range(6):
                    ap_, (b_,) = A.alloc(256, 1, "pT%d" % i)
                    pT_slots.append((bf(ap_), b_))
                acc_slots = []
                for i in range(2):
                    ap_, (b_,) = A.alloc(512, 1, "acc%d" % i)
                    acc_slots.append((ap_, b_))
                rc_slots = []
                for i in range(2):
                    ap_, (b_,) = A.alloc(512, 1, "rc%d" % i)
                    rc_slots.append((ap_, b_))
                o_ap, (o_b,) = A.alloc(512, 1, "o")
                osq_ap, (osq_b,) = A.alloc(512, 1, "osq")
                ors_ap, (ors_b,) = A.alloc(512, 1, "ors")
                ao_slots = []
                for i in range(2):
                    ap_, (b_,) = A.alloc(256, 1, "ao%d" % i)
                    ao_slots.append((bf(ap_), b_, s.dma_sem()))
                qk_i = 0
                pt_i = 0
                ao_i = 0
                for hd in range(8):
                    if hd + 1 < 8:
                        att_load(hd + 1)
                    (wq, wq_b, _), (wk, wk_b, _), (wv, wv_b, _) = att_w[hd % 2]
                    chk("a1")
                    for gi_, (t0, n, isc) in enumerate(TG):
                        for which in range(2):
                            if which == 0 and isc and last:
                                continue
                            wmat, wmb = (wq, wq_b) if which == 0 else (wk, wk_b)
                            dst, dst_b = (qr, qr_b) if which == 0 else (kr, kr_b)
                            qb_, qbb_, t1_, t1b_, t2_, t2b_ = qb_slots[qk_i % 2]
                            qk_i += 1
                            pq, pqb = ps_a()

                            def f(e, pq=pq, wmat=wmat, t0=t0, n=n):
                                ins = None
                                for c in range(KC):
                                    ins = e.matmul(pq[:, 0:n], wmat[:, c, :], hT[:, c, t0:t0 + n], start=(c == 0), stop=(c == KC - 1))
                                return ins
                            s.op("pe", f, reads=[wmb, h_bufs[gi_]], writes=[pqb])
                            s.op("act", lambda e, qb_=qb_, pq=pq, n=n: e.copy(qb_[:, 0:n], pq[:, 0:n]), reads=[pqb], writes=[qbb_])
                            chk("a2")
                            chk("i%d_a2" % qk_i)
                            psw, pswb = ps_a()
                            s.op("pe", lambda e, psw=psw, qb_=qb_, n=n: e.matmul(psw[:, 0:n], swapm, qb_[:, 0:n], start=True, stop=True),
                                 reads=[qbb_, swap_b], writes=[pswb])
                            chk("a3")
                            chk("i%d_a3" % qk_i)
                            s.op("dve", lambda e, t1_=t1_, pq=pq, t0=t0, n=n: e.tensor_tensor(t1_[:, 0:n], pq[:, 0:n], cos_ap[:, t0:t0 + n], ALU.mult),
                                 reads=[pqb, cos_b], writes=[t1b_])
                            s.op("dve", lambda e, t2_=t2_, psw=psw, t0=t0, n=n: e.tensor_tensor(t2_[:, 0:n], psw[:, 0:n], sin_ap[:, t0:t0 + n], ALU.mult),
                                 reads=[pswb, sin_b], writes=[t2b_])
                            chk("a4")
                            chk("i%d_a4" % qk_i)
                            if which == 1:
                                s.op("pool", lambda e, dst=dst, t1_=t1_, t2_=t2_, t0=t0, n=n: e.tensor_tensor(dst[:, t0:t0 + n], t1_[:, 0:n], t2_[:, 0:n], ALU.add),
                                     reads=[t1b_, t2b_], writes=[dst_b])
                            else:
                                s.op("pool", lambda e, t1_=t1_, t2_=t2_, n=n: e.tensor_tensor(t1_[:, 0:n], t1_[:, 0:n], t2_[:, 0:n], ALU.add),
                                     reads=[t1b_, t2b_], writes=[t1b_])
                                s.op("dve", lambda e, t1_=t1_, t0=t0, n=n: e.tensor_scalar(qr[:, t0:t0 + n], t1_[:, 0:n], hm_ap[:, 0:1], None, ALU.mult),
                                     reads=[t1b_, hm_b], writes=[qr_b])
                                s.op("dve", lambda e, t1_=t1_, t0=t0, n=n: e.tensor_scalar(qr1[:, t0:t0 + n], t1_[:, 0:n], hm_ap[:, 1:2], None, ALU.mult),
                                     reads=[t1b_, hm_b], writes=[qr1_b])
                            chk("a5")
                            chk("qk%d" % qk_i)
                    chk("att_a")
                    for t4 in range(0, NT, 4):
                        pv, pvb = ps_a()
                        nt4 = min(4, NT - t4)

                        def f(e, pv=pv, t4=t4, nt4=nt4):
                            ins = None
                            for j in range(nt4):
                                ti = t4 + j
                                for c in range(KC):
                                    ins = e.matmul(pv[:, j * 128:(j + 1) * 128], hT[:, c, ti * 128:(ti + 1) * 128], wv[:, c, :],
                                                   start=(c == 0), stop=(c == KC - 1))
                            return ins
                        s.op("pe", f, reads=[wv_b] + hbufs(t4 * 128, nt4 * 128), writes=[pvb])
                        s.op("act", lambda e, pv=pv, t4=t4, nt4=nt4: e.copy(
                            vv[:, t4:t4 + nt4, :], pv[:, 0:nt4 * 128].rearrange("p (t d) -> p t d", t=nt4)), reads=[pvb], writes=[v_b])
                    if b == 0 and l == 0 and hd == 0:
                        dbg("qr", qr_w, [qr_b])
                        dbg("kr", kr_w, [kr_b])
                        dbg("v", v_w, [v_b])
                    chk("att_b")
                    for gi_, (t0, n, isc) in enumerate(TG):
                        if isc and last:
                            continue
                        kts = range(0, NCT) if isc else range(0, NT)
                        nk = len(kts)
                        po = [psum[6], psum[7]]
                        pob = [psb[6], psb[7]]
                        for ki, kt in enumerate(kts):
                            for m in range(2):
                                pS, pSb = ps_a()
                                qm, qmb = (qr, qr_b) if m == 0 else (qr1, qr1_b)
                                s.op("pe", lambda e, pS=pS, kt=kt, qm=qm, t0=t0, n=n: e.matmul(
                                    pS[:, 0:n], kr[:, kt * 128:(kt + 1) * 128], qm[:, t0:t0 + n],
                                    start=True, stop=True), reads=[kr_b, qmb], writes=[pSb])
                                pT_, pTb_ = pT_slots[pt_i % 6]
                                pt_i += 1
                                s.op("act", lambda e, pT_=pT_, pS=pS, n=n: e.activation(out=pT_[:, 0:n], in_=pS[:, 0:n], func=AF.Exp, scale=0.125),
                                     reads=[pSb], writes=[pTb_])
                                acc_, accb_ = acc_slots[m]
                                if ki == 0:
                                    s.op("dve", lambda e, acc_=acc_, pT_=pT_, n=n: e.tensor_copy(acc_[:, 0:n], pT_[:, 0:n]), reads=[pTb_], writes=[accb_])
                                else:
                                    s.op("dve", lambda e, acc_=acc_, pT_=pT_, n=n: e.tensor_tensor(acc_[:, 0:n], acc_[:, 0:n], pT_[:, 0:n], ALU.add),
                                         reads=[pTb_, accb_], writes=[accb_])
                                s.op("pe", lambda e, m=m, kt=kt, pT_=pT_, n=n, ki=ki, nk=nk: e.matmul(
                                    po[m][:, 0:n], vv[:, kt, :], pT_[:, 0:n], start=(ki == 0), stop=(ki == nk - 1)),
                                    reads=[v_b, pTb_], writes=[pob[m]])
                        chk("att_c")
                        for m in range(2):
                            acc_, accb_ = acc_slots[m]
                            rc_, rcb_ = rc_slots[m]
                            pS, pSb = ps_a()
                            s.op("pe", lambda e, pS=pS, acc_=acc_, n=n: e.matmul(pS[:, 0:n], ones_ap, acc_[:, 0:n], start=True, stop=True),
                                 reads=[accb_, ones_b], writes=[pSb])
                            s.op("act", lambda e, rc_=rc_, pS=pS, n=n: e.activation(out=rc_[:, 0:n], in_=pS[:, 0:n], func=AF.Ln), reads=[pSb], writes=[rcb_])
                            s.op("act", lambda e, rc_=rc_, n=n: e.activation(out=rc_[:, 0:n], in_=rc_[:, 0:n], func=AF.Exp, scale=-1.0), reads=[rcb_], writes=[rcb_])
                            s.op("dve", lambda e, rc_=rc_, m=m, n=n: e.tensor_tensor(rc_[:, 0:n], po[m][:, 0:n], rc_[:, 0:n], ALU.mult),
                                 reads=[pob[m], rcb_], writes=[rcb_])
                        s.op("dve", lambda e, n=n: e.scalar_tensor_tensor(o_ap[:, 0:n], rc_slots[1][0][:, 0:n], lamv[:, l, 0:1], rc_slots[0][0][:, 0:n], ALU.mult, ALU.add),
                             reads=[rc_slots[0][1], rc_slots[1][1], lamv_b], writes=[o_b])
                        s.op("act", lambda e, n=n: e.activation(out=osq_ap[:, 0:n], in_=o_ap[:, 0:n], func=AF.Square), reads=[o_b], writes=[osq_b])
                        pS, pSb = ps_a()
                        s.op("pe", lambda e, pS=pS, n=n: e.matmul(pS[:, 0:n], ones_ap, osq_ap[:, 0:n], start=True, stop=True), reads=[osq_b, ones_b], writes=[pSb])
                        act_rstd(ors_ap[:, 0:n], pS[:, 0:n], 1.0 / 128, [pSb], [ors_b])
                        ao_, aob_, aosem_ = ao_slots[ao_i % 2]
                        ao_i += 1
                        s.op("dve", lambda e, ao_=ao_, n=n: e.scalar_tensor_tensor(ao_[:, 0:n], o_ap[:, 0:n], lamv[:, l, 1:2], ors_ap[:, 0:n], ALU.mult, ALU.mult),
                             reads=[o_b, ors_b, lamv_b], writes=[aob_])
                        chk("att_d")
                        s.dma("sp", aosem_, aT[:, hd, t0:t0 + n], ao_[:, 0:n], reads=[aob_], writes=[aT_b[hd]])
                        chk("att_e")
                A.reset(mk)
                if b == 0 and l == 0:
                    dscr("aT", aT, aT_b)
                chk("att")
                chk("L%d_att" % l)

                mk = A.mark()
                wnames = ["rq", "rff", "rfb", "ri", "rg"]
                woffs = [OFF_RQ, OFF_RFF, OFF_RFB, OFF_RI, OFF_RG]
                rec_w = []
                for i in range(2):
                    d_ = {}
                    for nm in wnames:
                        w_, (wb_,) = A.alloc(KC * 128 // 2, 1, "w" + nm + str(i))
                        d_[nm] = (bf(w_).rearrange("p (c n) -> p c n", c=KC), wb_, s.dma_sem())
                    rec_w.append(d_)
                wsrc_rec = w_in[l].rearrange("(c p) n -> p c n", p=128)

                def rec_load(hd_):
                    for nm, off in zip(wnames, woffs):
                        ent = rec_w[hd_ % 2][nm]
                        load_w(ent[0], ent[1], ent[2], wsrc_rec[:, :, off + hd_ * 128:off + (hd_ + 1) * 128])
                rec_load(0)
                scanm_w, (scanm_b,) = A.alloc(T // 2, 1, "scanm")
                scanm_ap = bf(scanm_w)
                s.dma("pool", cs2[3], scanm_ap, c_scanm, writes=[scanm_b])
                W1, (W1b,) = A.alloc(T, 1, "W1")
                W2, (W2b,) = A.alloc(T, 1, "W2")
                qf_w, (qf_b,) = A.alloc(T // 2, 1, "qf")
                kk_w, (kk_b,) = A.alloc(T // 2, 1, "kk")
                eg_w, (eg_b,) = A.alloc(T // 2, 1, "eg")
                en_w, (en_b,) = A.alloc(T // 2, 1, "en")
                sg_w, (sg_b,) = A.alloc(T // 2, 1, "sg")
                qf, kk, eg, en, sg = bf(qf_w), bf(kk_w), bf(eg_w), bf(en_w), bf(sg_w)
                vr_w, (vr_b,) = A.alloc(NT * 128 // 2, 1, "vr")
                vr = bf(vr_w).rearrange("p (t d) -> p t d", t=NT)
                vblk_w, (vblk_b,) = A.alloc(NT * NPT * 128 // 2, 1, "vblk")
                vblk = bf(vblk_w).rearrange("p (t c d) -> p t c d", t=NT, c=NPT)
                dirs = {}
                for dname in ("f", "b"):
                    QT_w, (QT_b,) = A.alloc(T // 2, 1, "QT" + dname)
                    KT_w, (KT_b,) = A.alloc(T // 2, 1, "KT" + dname)
                    KH_w, (KH_b,) = A.alloc(T // 2, 1, "KH" + dname)
                    egl_ap, (egl_b,) = A.alloc(NCH, 1, "egl" + dname)
                    st_w, st_bufs = A.alloc((NCH + 1) * 128 // 2, NCH + 1, "st" + dname)
                    S32w, S32bufs = A.alloc(256, 2, "S32" + dname)
                    S32 = [S32w[:, 0:128], S32w[:, 128:256]]
                    S32b = S32bufs
                    dirs[dname] = dict(QT=bf(QT_w), QT_b=QT_b, KT=bf(KT_w), KT_b=KT_b, KH=bf(KH_w), KH_b=KH_b, egl=egl_ap, egl_b=egl_b,
                                       st=bf(st_w).rearrange("p (c d) -> p c d", c=NCH + 1), st_bufs=st_bufs, S32=S32, S32b=S32b)
                khT_slots = []
                for i in range(4):
                    ap_, (b_,) = A.alloc(64, 1, "khT%d" % i)
                    khT_slots.append((bf(ap_), b_))
                am_slots = []
                for i in range(4):
                    ap_, (b_,) = A.alloc(64, 1, "am%d" % i)
                    am_slots.append((bf(ap_), b_))
                osq2_ap, (osq2_b,) = A.alloc(512, 1, "osq2")
                ors2_ap, (ors2_b,) = A.alloc(512, 1, "ors2")
                ro_slots = []
                for i in range(2):
                    ap_, (b_,) = A.alloc(256, 1, "ro%d" % i)
                    ro_slots.append((bf(ap_), b_, s.dma_sem()))
                kh_i = 0
                am_i = 0
                ro_i = 0
                ctx_ch = list(range(0, C // CHUNK))
                lat_ch = list(range(C // CHUNK, NCH))
                order = {"f": ctx_ch + lat_ch, "b": ctx_ch[::-1] + lat_ch[::-1]}
                for hd in range(8):
                    if hd + 1 < 8:
                        rec_load(hd + 1)
                    wr_ = rec_w[hd % 2]
                    lb_s = lbv[:, l, hd, 0:1]
                    oml_s = lbv[:, l, hd, 1:2]
                    noml_s = lbv[:, l, hd, 2:3]
                    for gi_, (t0, n, isc) in enumerate(TG):
                        pq, pqb = ps_a()

                        def f(e, pq=pq, t0=t0, n=n):
                            ins = None
                            for c in range(KC):
                                ins = e.matmul(pq[:, 0:n], wr_["rq"][0][:, c, :], hT[:, c, t0:t0 + n], start=(c == 0), stop=(c == KC - 1))
                            return ins
                        s.op("pe", f, reads=[wr_["rq"][1], h_bufs[gi_]], writes=[pqb])
                        s.op("act", lambda e, pq=pq, t0=t0, n=n: e.copy(qf[:, t0:t0 + n], pq[:, 0:n]), reads=[pqb], writes=[qf_b])
                        pg, pgb = ps_a()

                        def f(e, pg=pg, t0=t0, n=n):
                            ins = None
                            for c in range(KC):
                                ins = e.matmul(pg[:, 0:n], wr_["rg"][0][:, c, :], hT[:, c, t0:t0 + n], start=(c == 0), stop=(c == KC - 1))
                            return ins
                        s.op("pe", f, reads=[wr_["rg"][1], h_bufs[gi_]], writes=[pgb])
                        s.op("act", lambda e, pg=pg, t0=t0, n=n: e.activation(out=sg[:, t0:t0 + n], in_=pg[:, 0:n], func=AF.Silu), reads=[pgb], writes=[sg_b])
                    for t4 in range(0, NT, 4):
                        pv, pvb = ps_a()
                        nt4 = min(4, NT - t4)

                        def f(e, pv=pv, t4=t4, nt4=nt4):
                            ins = None
                            for j in range(nt4):
                                ti = t4 + j
                                for c in range(KC):
                                    ins = e.matmul(pv[:, j * 128:(j + 1) * 128], hT[:, c, ti * 128:(ti + 1) * 128], wr_["ri"][0][:, c, :],
                                                   start=(c == 0), stop=(c == KC - 1))
                            return ins
                        s.op("pe", f, reads=[wr_["ri"][1]] + hbufs(t4 * 128, nt4 * 128), writes=[pvb])
                        s.op("act", lambda e, pv=pv, t4=t4, nt4=nt4: e.copy(
                            vr[:, t4:t4 + nt4, :], pv[:, 0:nt4 * 128].rearrange("p (t d) -> p t d", t=nt4)), reads=[pvb], writes=[vr_b])
                        for cc in range(NPT):
                            s.op("dve", lambda e, pv=pv, t4=t4, nt4=nt4, cc=cc: e.tensor_scalar(
                                vblk[:, t4:t4 + nt4, cc, :], pv[:, 0:nt4 * 128].rearrange("p (t d) -> p t d", t=nt4), cmask_ap[:, cc:cc + 1], None, ALU.mult),
                                reads=[pvb, cmask_b], writes=[vblk_b])
                    for dname in ("f", "b"):
                        dd = dirs[dname]
                        wz = wr_["rff"] if dname == "f" else wr_["rfb"]
                        for gi_, (t0, n, isc) in enumerate(TG):
                            pz, pzb = ps_a()

                            def f(e, pz=pz, t0=t0, n=n, wz=wz):
                                ins = None
                                for c in range(KC):
                                    ins = e.matmul(pz[:, 0:n], wz[0][:, c, :], hT[:, c, t0:t0 + n], start=(c == 0), stop=(c == KC - 1))
                                return ins
                            s.op("pe", f, reads=[wz[1], h_bufs[gi_]], writes=[pzb])
                            s.op("act", lambda e, pz=pz, t0=t0, n=n: e.activation(out=W1[:, t0:t0 + n], in_=pz[:, 0:n], func=AF.Sigmoid), reads=[pzb], writes=[W1b])
                        s.op("dve", lambda e: e.tensor_scalar(W2, W1, oml_s, lb_s, ALU.mult, ALU.add), reads=[W1b, lbv_b], writes=[W2b])
                        s.op("dve", lambda e: e.tensor_scalar(kk, W1, noml_s, oml_s, ALU.mult, ALU.add), reads=[W1b, lbv_b], writes=[kk_b])
                        s.op("act", lambda e: e.activation(out=W1, in_=W2, func=AF.Ln), reads=[W2b], writes=[W1b])
                        s.op("dve", lambda e: e.tensor_tensor_scan(W2, scanm_ap, W1, 0.0, ALU.mult, ALU.add), reads=[W1b, scanm_b], writes=[W2b])
                        W13 = W1.rearrange("p (c t) -> p c t", t=CHUNK)
                        W23 = W2.rearrange("p (c t) -> p c t", t=CHUNK)
                        if dname == "f":
                            G, Gb, G3 = W2, W2b, W23
                            last_col = CHUNK - 1
                            X, Xb = W1, W1b
                        else:
                            s.op("dve", lambda e: e.tensor_tensor(W1, W1, W2, ALU.subtract), reads=[W1b, W2b], writes=[W1b])
                            s.op("dve", lambda e: e.tensor_tensor(W13, W13, W23[:, :, CHUNK - 1:CHUNK].to_broadcast([128, NCH, CHUNK]), ALU.add),
                                 reads=[W1b, W2b], writes=[W1b])
                            G, Gb, G3 = W1, W1b, W13
                            last_col = 0
                            X, Xb = W2, W2b
                        s.op("act", lambda e, G=G: e.activation(out=eg, in_=G, func=AF.Exp), reads=[Gb], writes=[eg_b])
                        s.op("act", lambda e, G=G: e.activation(out=en, in_=G, func=AF.Exp, scale=-1.0), reads=[Gb], writes=[en_b])
                        s.op("act", lambda e, G3=G3, last_col=last_col, dd=dd: e.activation(out=dd["egl"], in_=G3[:, :, last_col], func=AF.Exp), reads=[Gb], writes=[dd["egl_b"]])
                        s.op("dve", lambda e, dd=dd: e.tensor_tensor(dd["QT"], qf, eg, ALU.mult), reads=[qf_b, eg_b], writes=[dd["QT_b"]])
                        s.op("pool", lambda e, dd=dd: e.tensor_tensor(dd["KT"], kk, en, ALU.mult), reads=[kk_b, en_b], writes=[dd["KT_b"]])
                        X3 = X.rearrange("p (c t) -> p c t", t=CHUNK)
                        s.op("dve", lambda e, dd=dd, X3=X3: e.tensor_tensor(
                            X3, dd["KT"].rearrange("p (c t) -> p c t", t=CHUNK), dd["egl"].unsqueeze(2).to_broadcast([128, NCH, CHUNK]), ALU.mult),
                            reads=[dd["KT_b"], dd["egl_b"], Xb], writes=[Xb])
                        s.op("pool", lambda e, dd=dd, X=X: e.tensor_copy(dd["KH"], X), reads=[Xb], writes=[dd["KH_b"]])
                        if b == 0 and l == 0 and hd == 0:
                            dbg("QT" + dname, dd["QT"].bitcast(F32), [dd["QT_b"]])
                            dbg("KT" + dname, dd["KT"].bitcast(F32), [dd["KT_b"]])
                            dbg("KH" + dname, dd["KH"].bitcast(F32), [dd["KH_b"]])
                            dbg("egl" + dname, dd["egl"], [dd["egl_b"]])
                    rr["bn"] = 4

                    def chain_gen(dname):
                        nonlocal kh_i
                        dd = dirs[dname]
                        ordr = order[dname]
                        s.op("pool", lambda e, dd=dd: e.memset(dd["S32"][0], 0.0), writes=[dd["S32b"][0]])
                        kstep = 0
                        c0 = ordr[0]
                        s.op("pool", lambda e, dd=dd, c0=c0: e.memset(dd["st"][:, c0, :], 0.0), writes=[dd["st_bufs"][c0]])
                        tile_order = []
                        for cch in ordr:
                            if cch // NPT not in tile_order:
                                tile_order.append(cch // NPT)
                        pos = 0
                        for ti in tile_order:
                            khT_, khTb_ = khT_slots[kh_i % 4]
                            kh_i += 1
                            ptr, ptrb = ps_b()
                            ptr_bf = ptr[:, 0:64].bitcast(BF16)
                            s.op("pe", lambda e, ptr_bf=ptr_bf, dd=dd, ti=ti: e.transpose(ptr_bf, dd["KH"][:, ti * 128:(ti + 1) * 128], identb),
                                 reads=[dd["KH_b"], identb_b], writes=[ptrb])
                            s.op("act", lambda e, khT_=khT_, ptr_bf=ptr_bf: e.copy(khT_, ptr_bf), reads=[ptrb], writes=[khTb_])
                            pu, pub = ps_b()

                            s.op("pe", lambda e, pu=pu, khT_=khT_, ti=ti: e.matmul(
                                pu[:, 0:NPT * 128], khT_, vblk[:, ti].rearrange("p c d -> p (c d)"), start=True, stop=True),
                                reads=[khTb_, vblk_b], writes=[pub])
                            chunks_here = [cch for cch in ordr if cch // NPT == ti]
                            for cch in chunks_here:
                                j = cch % NPT
                                nxt = ordr[pos + 1] if pos + 1 < len(ordr) else NCH
                                pos += 1
                                sa, sab = dd["S32"][kstep % 2], dd["S32b"][kstep % 2]
                                sn, snb = dd["S32"][(kstep + 1) % 2], dd["S32b"][(kstep + 1) % 2]
                                kstep += 1
                                s.op("dve", lambda e, dd=dd, cch=cch, j=j, pu=pu: e.scalar_tensor_tensor(
                                    sn, sa, dd["egl"][:, cch:cch + 1], pu[:, j * 128:(j + 1) * 128], ALU.mult, ALU.add),
                                    reads=[sab, dd["egl_b"], pub], writes=[snb])
                                s.op("pool", lambda e, dd=dd, nxt=nxt: e.tensor_copy(dd["st"][:, nxt, :], sn), reads=[snb], writes=[dd["st_bufs"][nxt]])
                            yield
                    gens_ = [chain_gen("f"), chain_gen("b")]
                    while gens_:
                        for g_ in list(gens_):
                            try:
                                next(g_)
                            except StopIteration:
                                gens_.remove(g_)
                    rr["bn"] = 2
                    for t4 in range(0, NT, 4):
                        nt4 = min(4, NT - t4)
                        po_, pob_ = psum[6 + (t4 // 4) % 2], psb[6 + (t4 // 4) % 2]
                        for jt in range(nt4):
                            ti = t4 + jt
                            ams = []
                            for dname in ("f", "b"):
                                dd = dirs[dname]
                                pa_, pab_ = ps_b()
                                s.op("pe", lambda e, pa_=pa_, dd=dd, ti=ti: e.matmul(
                                    pa_[:, 0:128], dd["KT"][:, ti * 128:(ti + 1) * 128], dd["QT"][:, ti * 128:(ti + 1) * 128], start=True, stop=True),
                                    reads=[dd["KT_b"], dd["QT_b"]], writes=[pab_])
                                am_, amb_ = am_slots[am_i % 4]
                                am_i += 1
                                mk_, mkb_ = (maskf, maskf_b) if dname == "f" else (maskb, maskb_b)
                                s.op("dve", lambda e, am_=am_, pa_=pa_, mk_=mk_: e.tensor_tensor(am_, pa_[:, 0:128], mk_, ALU.mult), reads=[pab_, mkb_], writes=[amb_])
                                ams.append((am_, amb_))

                            def f(e, po_=po_, jt=jt, ti=ti, ams=ams):
                                ins = None
                                first = True
                                for di, dname in enumerate(("f", "b")):
                                    dd = dirs[dname]
                                    ins = e.matmul(po_[:, jt * 128:(jt + 1) * 128], vr[:, ti, :], ams[di][0], start=first, stop=False)
                                    first = False
                                    for j in range(NPT):
                                        cch = ti * NPT + j
                                        ins = e.matmul(po_[:, jt * 128 + j * CHUNK:jt * 128 + (j + 1) * CHUNK], dd["st"][:, cch, :],
                                                       dd["QT"][:, cch * CHUNK:(cch + 1) * CHUNK], start=False, stop=(di == 1 and j == NPT - 1))
                                return ins
                            rds = [vr_b, ams[0][1], ams[1][1]]
                            for dname in ("f", "b"):
                                dd = dirs[dname]
                                rds += [dd["QT_b"]] + [dd["st_bufs"][ti * NPT + j] for j in range(NPT)]
                            s.op("pe", f, reads=rds, writes=[pob_])
                        n = nt4 * 128
                        t0 = t4 * 128
                        s.op("act", lambda e, po_=po_, n=n: e.activation(out=osq2_ap[:, 0:n], in_=po_[:, 0:n], func=AF.Square), reads=[pob_], writes=[osq2_b])
                        pS, pSb = ps_a()
                        s.op("pe", lambda e, pS=pS, n=n: e.matmul(pS[:, 0:n], ones_ap, osq2_ap[:, 0:n], start=True, stop=True), reads=[osq2_b, ones_b], writes=[pSb])
                        act_rstd(ors2_ap[:, 0:n], pS[:, 0:n], 1.0 / 128, [pSb], [ors2_b])
                        s.op("dve", lambda e, po_=po_, n=n: e.scalar_tensor_tensor(osq2_ap[:, 0:n], po_[:, 0:n], lamv[:, l, 2:3], ors2_ap[:, 0:n], ALU.mult, ALU.mult),
                             reads=[pob_, ors2_b, lamv_b], writes=[osq2_b])
                        ro_, rob_, rosem_ = ro_slots[ro_i % 2]
                        ro_i += 1
                        s.op("pool", lambda e, ro_=ro_, n=n, t0=t0: e.tensor_tensor(ro_[:, 0:n], osq2_ap[:, 0:n], sg[:, t0:t0 + n], ALU.mult),
                             reads=[osq2_b, sg_b], writes=[rob_])
                        s.dma("sp", rosem_, rT[:, hd, t0:t0 + n], ro_[:, 0:n], reads=[rob_], writes=[rT_b[hd]])
                A.reset(mk)
                if b == 0 and l == 0:
                    dscr("rT", rT, rT_b)
                chk("rec")
                chk("L%d_rec" % l)

                mk = A.mark()
                wm = {}
                for nm in ("ga", "gr", "ba", "br", "wo"):
                    w_, (wb_,) = A.alloc(KC * D // 2, 1, "wm" + nm)
                    wm[nm] = (bf(w_).rearrange("p (c n) -> p c n", c=KC), wb_, s.dma_sem())
                wsrc = w_in[l].rearrange("(c p) n -> p c n", p=128)
                load_w(wm["ga"][0], wm["ga"][1], wm["ga"][2], wsrc[:, :, OFF_GA:OFF_GA + D])
                load_w(wm["gr"][0], wm["gr"][1], wm["gr"][2], wsrc[:, :, OFF_GR:OFF_GR + D])
                load_w(wm["ba"][0], wm["ba"][1], wm["ba"][2], w_bra[l].rearrange("(c p) n -> p c n", p=128))
                load_w(wm["br"][0], wm["br"][1], wm["br"][2], w_brr[l].rearrange("(c p) n -> p c n", p=128))
                load_w(wm["wo"][0], wm["wo"][1], wm["wo"][2], w_out[l].rearrange("(c p) n -> p c n", p=128))
                ar_slots = []
                for i in range(2):
                    a_, (ab_,) = A.alloc(KC * MGN // 2, 1, "ag%d" % i)
                    r_, (rb_,) = A.alloc(KC * MGN // 2, 1, "rg%d" % i)
                    x_, (xb_,) = A.alloc(KC * MGN, 1, "xm%d" % i)
                    ar_slots.append((bf(a_).rearrange("p (c n) -> p c n", c=KC), ab_, s.dma_sem(),
                                     bf(r_).rearrange("p (c n) -> p c n", c=KC), rb_, s.dma_sem(),
                                     x_.rearrange("p (c n) -> p c n", c=KC), xb_, s.dma_sem(), s.dma_sem()))
                mx_w, (mx_b,) = A.alloc(KC * MGN // 2, 1, "mixed")
                mixed = bf(mx_w).rearrange("p (c n) -> p c n", c=KC)
                sg_slots = []
                for i in range(2):
                    s1_, (s1b_,) = A.alloc(MGN, 1, "sga%d" % i)
                    s2_, (s2b_,) = A.alloc(MGN, 1, "sgr%d" % i)
                    sg_slots.append((s1_, s1b_, s2_, s2b_))
                sgi = 0
                for gi_, (t0, n, isc) in enumerate(TGM):
                    if isc and last:
                        continue
                    col = NB if isc else b
                    a_, ab_, asem_, r_, rb_, rsem_, x_, xb_, xsem_, xosem_ = ar_slots[gi_ % 2]
                    s.dma("sp", asem_, a_[:, :, 0:n], aT[:, :, t0:t0 + n], reads=aT_b, writes=[ab_])
                    s.dma("sp", rsem_, r_[:, :, 0:n], rT[:, :, t0:t0 + n], reads=rT_b, writes=[rb_])
                    s.dma("sp", xsem_, x_[:, :, 0:n], xT[b, :, :, t0:t0 + n], reads=[xT_b[b][ti] for ti in tiles_of(t0, n)], writes=[xb_])
                    hbs = hbufs(t0, n)
                    for oc in range(KC):
                        s1_, s1b_, s2_, s2b_ = sg_slots[sgi % 2]
                        sgi += 1
                        pgs = []
                        for (wnm, src, srcb) in (("ga", hT[:, :, t0:t0 + n], hbs), ("gr", hT[:, :, t0:t0 + n], hbs), ("ba", a_[:, :, 0:n], [ab_]), ("br", r_[:, :, 0:n], [rb_])):
                            pg, pgb = ps_a()

                            def f(e, pg=pg, wnm=wnm, src=src, oc=oc, n=n):
                                ins = None
                                for c in range(KC):
                                    ins = e.matmul(pg[:, 0:n], wm[wnm][0][:, c, oc * 128:(oc + 1) * 128], src[:, c, :], start=(c == 0), stop=(c == KC - 1))
                                return ins
                            s.op("pe", f, reads=[wm[wnm][1]] + list(srcb), writes=[pgb])
                            pgs.append((pg, pgb))
                        s.op("act", lambda e, s1_=s1_, pg=pgs[0][0], n=n: e.activation(out=s1_[:, 0:n], in_=pg[:, 0:n], func=AF.Sigmoid), reads=[pgs[0][1]], writes=[s1b_])
                        s.op("act", lambda e, s2_=s2_, pg=pgs[1][0], n=n: e.activation(out=s2_[:, 0:n], in_=pg[:, 0:n], func=AF.Sigmoid), reads=[pgs[1][1]], writes=[s2b_])
                        s.op("dve", lambda e, s1_=s1_, pg=pgs[2][0], n=n: e.tensor_tensor(s1_[:, 0:n], pg[:, 0:n], s1_[:, 0:n], ALU.mult), reads=[pgs[2][1], s1b_], writes=[s1b_])
                        s.op("dve", lambda e, s2_=s2_, pg=pgs[3][0], n=n: e.tensor_tensor(s2_[:, 0:n], pg[:, 0:n], s2_[:, 0:n], ALU.mult), reads=[pgs[3][1], s2b_], writes=[s2b_])
                        s.op("pool", lambda e, s1_=s1_, s2_=s2_, oc=oc, n=n: e.tensor_tensor(mixed[:, oc, 0:n], s1_[:, 0:n], s2_[:, 0:n], ALU.add),
                             reads=[s1b_, s2b_], writes=[mx_b])
                    for oc in range(KC):
                        py, pyb = ps_a()

                        def f(e, py=py, oc=oc, n=n):
                            ins = None
                            for c in range(KC):
                                ins = e.matmul(py[:, 0:n], wm["wo"][0][:, c, oc * 128:(oc + 1) * 128], mixed[:, c, 0:n], start=(c == 0), stop=(c == KC - 1))
                            return ins
                        s.op("pe", f, reads=[wm["wo"][1], mx_b], writes=[pyb])
                        s.op("dve", lambda e, py=py, oc=oc, n=n, x_=x_, col=col: e.scalar_tensor_tensor(
                            x_[:, oc, 0:n], py[:, 0:n], modv[:, l, 16 + oc, col:col + 1], x_[:, oc, 0:n], ALU.mult, ALU.add),
                            reads=[pyb, xb_, modv_b], writes=[xb_])
                    s.dma("sp", xosem_, xT[b, :, :, t0:t0 + n], x_[:, :, 0:n], reads=[xb_], writes=[xT_b[b][ti] for ti in tiles_of(t0, n)])
                    norm_mod(x_[:, :, 0:n], xb_, n, hT[:, :, t0:t0 + n], hbs[0], a2[:, l, :, col], modv[:, l, 24:32, col], a2_b)
                    for ob in hbs[1:]:
                        ob.w = hbs[0].w
                        ob.r = {}
                A.reset(mk)
                if b == 0 and l == 0:
                    dbg("h2", h_w, h_bufs)
                    dscr("x1", xT[0], xT_b[0])
                chk("merge")
                chk("L%d_merge" % l)

                mk = A.mark()
                onehot_w, (onehot_b,) = A.alloc(16 * 128 // 2, 1, "onehot")
                onehot = bf(onehot_w)
                s.dma("pool", cs2[3], onehot[0:16, :], c_onehot, writes=[onehot_b])
                xr_ap, xr_bufs = A.alloc(KC * T, len(TG), "xres")
                xres = xr_ap.rearrange("p (c t) -> p c t", c=KC)
                xrsem = [s.dma_sem() for _ in TG]
                tg_moe = [(gi_, t0, n, isc) for gi_, (t0, n, isc) in enumerate(TG) if not (isc and last)]
                for (gi_, t0, n, isc) in tg_moe:
                    s.dma("sp", xrsem[gi_], xres[:, :, t0:t0 + n], xT[b, :, :, t0:t0 + n], reads=[xT_b[b][ti] for ti in tiles_of(t0, n)], writes=[xr_bufs[gi_]])
                tiles_moe = [ti for ti in range(NT) if not (last and ti < NCT)]
                ntm = len(tiles_moe)
                sc_ap, (sc_b,) = A.alloc(NT * 16, 1, "scores")
                bi_ap, (bi_b,) = A.alloc(NT * 16, 1, "biased")
                t_ap, (t_b,) = A.alloc(NT * 16, 1, "rt_tmp")
                g_ap, (g_b,) = A.alloc(NT * 16, 1, "gates")
                m1_ap, (m1_b,) = A.alloc(NT * 4, 1, "max1")
                m2_ap, (m2_b,) = A.alloc(NT * 4, 1, "max2")
                gs_ap, (gs_b,) = A.alloc(NT * 4, 1, "gsel")
                gm_ap, (gm_b,) = A.alloc(NT, 1, "gmax")
                gT_w, (gT_b,) = A.alloc(T // 2, 1, "gT")
                gT = bf(gT_w)
                gb_w, gb_bufs = A.alloc(T // 2, len(TG), "gbs")
                gbs = bf(gb_w)
                prt, prtb = ps_a()

                def f(e):
                    ins = None
                    for ti in tiles_moe:
                        for c in range(KC):
                            ins = e.matmul(prt[:, ti * 16:(ti + 1) * 16], hT[:, c, ti * 128:(ti + 1) * 128], wrt[:, c, :], start=(c == 0), stop=(c == KC - 1))
                    return ins
                s.op("pe", f, reads=[wr_b] + h_bufs, writes=[prtb])
                lo, hi = tiles_moe[0], tiles_moe[-1] + 1

                def v3(ap):
                    return ap[:, lo * 16:hi * 16].rearrange("p (t e) -> p t e", e=16)

                def v4(ap):
                    return ap[:, lo * 16:hi * 16].rearrange("p (t e) -> p t e", e=4)

                def v2(ap):
                    return ap[:, lo * 4:hi * 4]
                s.op("act", lambda e: e.activation(out=sc_ap[:, lo * 16:hi * 16], in_=prt[:, lo * 16:hi * 16], func=AF.Sigmoid), reads=[prtb], writes=[sc_b])
                s.op("dve", lambda e: e.tensor_tensor(v3(bi_ap), v3(sc_ap), brt_ap.unsqueeze(1).to_broadcast([128, ntm, 16]), ALU.add), reads=[sc_b, brt_b], writes=[bi_b])
                s.op("dve", lambda e: e.tensor_reduce(v2(m1_ap), v4(bi_ap), AX.X, ALU.max), reads=[bi_b], writes=[m1_b])
                s.op("dve", lambda e: e.tensor_tensor(v4(t_ap), v4(bi_ap), v2(m1_ap).unsqueeze(2).to_broadcast([128, ntm * 4, 4]), ALU.is_ge), reads=[bi_b, m1_b], writes=[t_b])
                s.op("dve", lambda e: e.scalar_tensor_tensor(t_ap[:, lo * 16:hi * 16], t_ap[:, lo * 16:hi * 16], -1e9, bi_ap[:, lo * 16:hi * 16], ALU.mult, ALU.add), reads=[t_b, bi_b], writes=[t_b])
                s.op("dve", lambda e: e.tensor_reduce(v2(m2_ap), v4(t_ap), AX.X, ALU.max), reads=[t_b], writes=[m2_b])
                s.op("dve", lambda e: e.tensor_tensor(v2(gs_ap), v2(m1_ap), v2(m2_ap), ALU.add), reads=[m1_b, m2_b], writes=[gs_b])
                s.op("dve", lambda e: e.tensor_reduce(gm_ap[:, lo:hi], v2(gs_ap).rearrange("p (t g) -> p t g", g=4), AX.X, ALU.max), reads=[gs_b], writes=[gm_b])
                s.op("dve", lambda e: e.tensor_tensor(v2(gs_ap).rearrange("p (t g) -> p t g", g=4), v2(gs_ap).rearrange("p (t g) -> p t g", g=4),
                                                      gm_ap[:, lo:hi].unsqueeze(2).to_broadcast([128, ntm, 4]), ALU.is_ge), reads=[gs_b, gm_b], writes=[gs_b])
                s.op("dve", lambda e: e.tensor_tensor(v4(t_ap), v4(bi_ap), v2(m2_ap).unsqueeze(2).to_broadcast([128, ntm * 4, 4]), ALU.is_ge), reads=[bi_b, m2_b], writes=[t_b])
                s.op("dve", lambda e: e.tensor_tensor(v4(t_ap), v4(t_ap), v2(gs_ap).unsqueeze(2).to_broadcast([128, ntm * 4, 4]), ALU.mult), reads=[t_b, gs_b], writes=[t_b])
                s.op("dve", lambda e: e.tensor_tensor(t_ap[:, lo * 16:hi * 16], t_ap[:, lo * 16:hi * 16], sc_ap[:, lo * 16:hi * 16], ALU.mult), reads=[t_b, sc_b], writes=[t_b])
                s.op("dve", lambda e: e.tensor_reduce(gm_ap[:, lo:hi], v3(t_ap), AX.X, ALU.add), reads=[t_b], writes=[gm_b])
                s.op("dve", lambda e: e.reciprocal(gm_ap[:, lo:hi], gm_ap[:, lo:hi]), reads=[gm_b], writes=[gm_b])
                s.op("dve", lambda e: e.tensor_tensor(v3(g_ap), v3(t_ap), gm_ap[:, lo:hi].unsqueeze(2).to_broadcast([128, ntm, 16]), ALU.mult), reads=[t_b, gm_b], writes=[g_b])
                if b == 0 and l == 0:
                    dbg("gates", g_ap, [g_b])
                for t4 in range(lo, hi, 4):
                    nt4 = min(4, hi - t4)
                    pgt, pgtb = ps_a()

                    def f(e, pgt=pgt, t4=t4, nt4=nt4):
                        ins = None
                        for j in range(nt4):
                            ins = e.transpose(pgt[0:16, j * 128:(j + 1) * 128], g_ap[:, (t4 + j) * 16:(t4 + j + 1) * 16], ident_ap)
                        return ins
                    s.op("pe", f, reads=[g_b, ident_b], writes=[pgtb])
                    s.op("act", lambda e, pgt=pgt, t4=t4, nt4=nt4: e.copy(gT[0:16, t4 * 128:(t4 + nt4) * 128], pgt[0:16, 0:nt4 * 128]), reads=[pgtb], writes=[gT_b])
                chk("L%d_moe_r" % l)
                m_exp = A.mark()
                rr["bn"] = 4
                wu_slots = []
                for i in range(2):
                    g_, (gb_,) = A.alloc(KC * 512 // 2, 1, "wg%d" % i)
                    u_, (ub_,) = A.alloc(KC * 512 // 2, 1, "wu%d" % i)
                    d_, (db_,) = A.alloc(4 * D // 2, 1, "wd%d" % i)
                    wu_slots.append((bf(g_).rearrange("p (c n) -> p c n", c=KC), gb_, s.dma_sem(),
                                     bf(u_).rearrange("p (c n) -> p c n", c=KC), ub_, s.dma_sem(),
                                     bf(d_).rearrange("p (c n) -> p c n", c=4), db_, s.dma_sem()))
                hid_slots = []
                for i in range(2):
                    hid_w, (hid_b,) = A.alloc(4 * 512 // 2, 1, "hid%d" % i)
                    hid_slots.append((bf(hid_w).rearrange("p (c n) -> p c n", c=4), hid_b))
                mcnt = {"sli": 0, "hi": 0}
                sl_slots = []
                for i in range(2):
                    a_, (ab_,) = A.alloc(512, 1, "silu%d" % i)
                    sl_slots.append((a_, ab_))
                sli = 0
                ui = 0
                units = [(ex_, fh_) for ex_ in range(NEXP) for fh_ in range(2)]

                def issue_load(u):
                    ex_, fh_ = units[u]
                    wg_, wgb_, wgs_, wu_, wub_, wus_, wd_, wdb_, wds_ = wu_slots[u % 2]
                    load_w(wg_, wgb_, wgs_, w_gate[l, ex_].rearrange("(c p) n -> p c n", p=128)[:, :, fh_ * 512:(fh_ + 1) * 512])
                    load_w(wu_, wub_, wus_, w_up[l, ex_].rearrange("(c p) n -> p c n", p=128)[:, :, fh_ * 512:(fh_ + 1) * 512])
                    load_w(wd_, wdb_, wds_, w_down[l, ex_, fh_ * 512:(fh_ + 1) * 512, :].rearrange("(c p) n -> p c n", p=128))
                issue_load(0)
                for ex in range(NEXP):
                    for (gi_, t0, n, isc) in tg_moe:
                        pgb_, pgbb_ = ps_a()
                        s.op("pe", lambda e, pgb_=pgb_, ex=ex, t0=t0, n=n: e.matmul(pgb_[:, 0:n], onehot[0:16, ex * 128:(ex + 1) * 128], gT[0:16, t0:t0 + n], start=True, stop=True),
                             reads=[onehot_b, gT_b], writes=[pgbb_])
                        s.op("act", lambda e, pgb_=pgb_, t0=t0, n=n: e.copy(gbs[:, t0:t0 + n], pgb_[:, 0:n]), reads=[pgbb_], writes=[gb_bufs[gi_]])
                    for fh in range(2):
                        wg_, wgb_, wgs_, wu_, wub_, wus_, wd_, wdb_, wds_ = wu_slots[ui % 2]
                        if ui + 1 < len(units):
                            issue_load(ui + 1)
                        ui += 1
                        def emit_gu(gi_, t0, n, hid, hid_b, wg_=wg_, wgb_=wgb_, wu_=wu_, wub_=wub_):
                            for fc in range(4):
                                pg, pgb = ps_a()
                                pu, pub = ps_a()

                                def f(e, pg=pg, fc=fc):
                                    ins = None
                                    for c in range(KC):
                                        ins = e.matmul(pg[:, 0:n], wg_[:, c, fc * 128:(fc + 1) * 128], hT[:, c, t0:t0 + n], start=(c == 0), stop=(c == KC - 1))
                                    return ins
                                s.op("pe", f, reads=[wgb_, h_bufs[gi_]], writes=[pgb])

                                def f(e, pu=pu, fc=fc):
                                    ins = None
                                    for c in range(KC):
                                        ins = e.matmul(pu[:, 0:n], wu_[:, c, fc * 128:(fc + 1) * 128], hT[:, c, t0:t0 + n], start=(c == 0), stop=(c == KC - 1))
                                    return ins
                                s.op("pe", f, reads=[wub_, h_bufs[gi_]], writes=[pub])
                                sl_, slb_ = sl_slots[mcnt["sli"] % 2]
                                mcnt["sli"] += 1
                                s.op("act", lambda e: e.activation(out=sl_[:, 0:n], in_=pg[:, 0:n], func=AF.Silu), reads=[pgb], writes=[slb_])
                                s.op("dve", lambda e: e.tensor_tensor(sl_[:, 0:n], pu[:, 0:n], sl_[:, 0:n], ALU.mult), reads=[pub, slb_], writes=[slb_])
                                s.op("pool", lambda e: e.tensor_tensor(hid[:, fc, 0:n], sl_[:, 0:n], gbs[:, t0:t0 + n], ALU.mult),
                                     reads=[slb_, gb_bufs[gi_]], writes=[hid_b])

                        def emit_down(gi_, t0, n, isc, hid, hid_b, wd_=wd_, wdb_=wdb_):
                            col = NB if isc else b
                            for oc in range(KC):
                                pd, pdb = ps_b()

                                def f(e, pd=pd, oc=oc):
                                    ins = None
                                    for c in range(4):
                                        ins = e.matmul(pd[:, 0:n], wd_[:, c, oc * 128:(oc + 1) * 128], hid[:, c, 0:n], start=(c == 0), stop=(c == 3))
                                    return ins
                                s.op("pe", f, reads=[wdb_, hid_b], writes=[pdb])
                                s.op("dve", lambda e: e.scalar_tensor_tensor(
                                    xres[:, oc, t0:t0 + n], pd[:, 0:n], modv[:, l, 40 + oc, col:col + 1], xres[:, oc, t0:t0 + n], ALU.mult, ALU.add),
                                    reads=[pdb, xr_bufs[gi_], modv_b], writes=[xr_bufs[gi_]])

                        nseq = len(tg_moe)
                        hs = [hid_slots[(mcnt["hi"] + i) % 2] for i in range(nseq)]
                        mcnt["hi"] += nseq
                        g0 = tg_moe[0]
                        emit_gu(g0[0], g0[1], g0[2], hs[0][0], hs[0][1])
                        for i in range(nseq):
                            if i + 1 < nseq:
                                g1 = tg_moe[i + 1]
                                emit_gu(g1[0], g1[1], g1[2], hs[i + 1][0], hs[i + 1][1])
                            gc = tg_moe[i]
                            emit_down(gc[0], gc[1], gc[2], gc[3], hs[i][0], hs[i][1])
                rr["bn"] = 2
                chk("L%d_moe_e" % l)
                if not last:
                    xwsem = [s.dma_sem() for _ in TG]
                    for (gi_, t0, n, isc) in tg_moe:
                        s.dma("sp", xwsem[gi_], xT[b, :, :, t0:t0 + n], xres[:, :, t0:t0 + n], reads=[xr_bufs[gi_]], writes=[xT_b[b][ti] for ti in tiles_of(t0, n)])
                    if b == 0 and l == 0:
                        dscr("x2", xT[0], xT_b[0])
                else:
                    A.reset(m_exp)
                    fin_slots = []
                    for i in range(2):
                        y_, (yb_,) = A.alloc(KC * 512, 1, "yfin%d" % i)
                        fin_slots.append((y_.rearrange("p (c n) -> p c n", c=KC), yb_))
                    rsf_ap, (rsf_b,) = A.alloc(512, 1, "rsf")
                    ot_slots = []
                    for i in range(2):
                        o_, (ob_,) = A.alloc(D, 1, "ot%d" % i)
                        ot_slots.append((o_, ob_, s.dma_sem()))
                    oti = 0
                    for fi, (gi_, t0, n, isc) in enumerate(tg_moe):
                        y_, yb_ = fin_slots[fi % 2]
                        yf = y_[:, :, 0:n]
                        s.op("act", lambda e, yf=yf, t0=t0, n=n: e.activation(out=yf, in_=xres[:, :, t0:t0 + n], func=AF.Square), reads=[xr_bufs[gi_]], writes=[yb_])
                        pss, pssb = ps_a()

                        def f(e, pss=pss, yf=yf, n=n):
                            ins = None
                            for c in range(KC):
                                ins = e.matmul(pss[:, 0:n], ones_ap, yf[:, c, :], start=(c == 0), stop=(c == KC - 1))
                            return ins
                        s.op("pe", f, reads=[yb_, ones_b], writes=[pssb])
                        act_rstd(rsf_ap[:, 0:n], pss[:, 0:n], 1.0 / D, [pssb], [rsf_b])
                        chk("fin_a")
                        for c in range(KC):
                            s.op("dve", lambda e, yf=yf, c=c, t0=t0, n=n: e.scalar_tensor_tensor(yf[:, c, :], xres[:, c, t0:t0 + n], gfv[:, c:c + 1], rsf_ap[:, 0:n], ALU.mult, ALU.mult),
                                 reads=[xr_bufs[gi_], rsf_b, small_b, yb_], writes=[yb_])
                        chk("fin_b")
                        for tj in range(n // 128):
                            o_, ob_, osem_ = ot_slots[oti % 2]
                            oti += 1
                            for half in range(2):
                                pt, ptb = ps_a()

                                def f(e, pt=pt, yf=yf, tj=tj, half=half):
                                    ins = None
                                    for j in range(4):
                                        c = half * 4 + j
                                        ins = e.transpose(pt[:, j * 128:(j + 1) * 128], yf[:, c, tj * 128:(tj + 1) * 128], ident_ap)
                                    return ins
                                s.op("pe", f, reads=[yb_, ident_b], writes=[ptb])
                                if half == 0:
                                    s.op("act", lambda e, o_=o_, pt=pt: e.copy(o_[:, 0:512], pt[:, :]), reads=[ptb], writes=[ob_])
                                else:
                                    s.op("dve", lambda e, o_=o_, pt=pt: e.tensor_copy(o_[:, 512:1024], pt[:, :]), reads=[ptb], writes=[ob_])
                            chk("fin_c")
                            tl = t0 - C + tj * 128
                            out_toks.append(s.dma("sp", osem_, out[b, tl:tl + 128, :], o_, reads=[ob_]))
                chk("L%d_moe" % l)
                A.reset(mk)
                A.reset(mL)

        s.dead = False
        s.wait_only("sp", out_toks + dbg_sem)
        s.final_barrier("sp")
        s.emit()
        print("program: ins=%d waits=%d sems=%d arena_peak=%d words" % (s.n_ins, s.n_wait, len(s.semh), A.peak))
    return nc


_CACHE = {}


def _get_program(NB, S, C, L, debug=()):
    stop = os.environ.get("K_STOP") or None
    key = (NB, S, C, L, tuple(debug), stop)
    if key not in _CACHE:
        _CACHE[key] = build_program(NB, S, C, L, debug, stop)
    return _CACHE[key]


def run(inputs, n_cores=8, debug=()):
    x = np.asarray(inputs["x"], np.float32)
    ctx = np.asarray(inputs["ctx"], np.float32)
    c = np.asarray(inputs["c"], np.float32)
    c_ctx = np.asarray(inputs["c_ctx"], np.float32)
    B, S, _ = x.shape
    C = ctx.shape[1]
    L = inputs["w_mod"].shape[0]
    NB = B // n_cores
    nc = _get_program(NB, S, C, L, debug)
    consts = host_constants(C, S)
    shared = {}
    for k in ("w_mod", "b_mod", "g_norm1", "g_norm2", "w_in", "lambda_q1", "lambda_k1", "lambda_q2", "lambda_k2", "g_subln",
              "lb_logits", "g_rec_norm", "w_br_attn", "w_br_rec", "w_out", "w_router", "w_gate", "w_up", "w_down"):
        shared[k] = np.ascontiguousarray(np.asarray(inputs[k], np.float32))
    shared["b_router"] = np.ascontiguousarray(np.asarray(inputs["b_router"], np.float32).reshape(1, NEXP))
    shared["g_final"] = np.ascontiguousarray(np.asarray(inputs["g_final"], np.float32).reshape(1, D))
    shared.update(consts)
    in_maps = []
    for i in range(n_cores):
        m = dict(shared)
        m["x"] = np.ascontiguousarray(x[i * NB:(i + 1) * NB])
        m["ctx"] = np.ascontiguousarray(ctx[i * NB:(i + 1) * NB])
        cv4 = np.zeros((4, D), np.float32)
        cv4[0:NB] = c[i * NB:(i + 1) * NB]
        cv4[NB] = c_ctx
        m["cvec"] = cv4
        in_maps.append(m)
    res = run_bass_kernel_spmd(nc, in_maps, core_ids=list(range(n_cores)))
    outs = np.concatenate([np.asarray(r["out"]) for r in res.results], axis=0)
    return outs, res


def kernel(**inputs):
    out, _ = run(inputs, n_cores=8)
    return out.astype(np.float32)
```
